# Optimizing a Trainium2 kernel written in Bass

```python
import jax, jax.numpy as jnp
from jax import lax
import numpy as np

D_MODEL = 1024
BATCH = 8
SEQ = 8192
DEPTH = 2

GRID_W = 64
CTX_LEN = 256
EPS = 1e-6
CHUNK = 64

A_HEADS = 4
A_DK = 64
A_DV = 128
A_RANK = 16
GATE_TAU = 16.0
B_HEADS = 4
B_DK = 128
B_DV = 128
N_B_LAYERS = (DEPTH + 1) // 2
C_HEADS = 4
C_DK = 128
C_DV = 128
CONV_W = 5
D_HEADS = 8
D_KV_HEADS = 2
D_HD = 64
WINDOW = 128
WBLK = 128
ROPE_BASE = 10000.0
N_EXPERTS = 32
TOP_K = 4
D_FF = 1024
SWIGLU_LIMIT = 7.0
SWIGLU_ALPHA = 1.702

EVEN_SPLITS = (A_HEADS * A_DK, A_HEADS * A_DK, A_HEADS * A_DV, A_RANK, A_RANK, A_HEADS * A_DV,
               B_HEADS * B_DK, B_HEADS * B_DK, B_HEADS * B_DK, B_HEADS * B_DV, B_HEADS * B_DV)
ODD_SPLITS = (C_HEADS * C_DK, C_HEADS * C_DK, C_HEADS * C_DV, C_HEADS, C_HEADS, C_HEADS, C_HEADS,
              C_HEADS * C_DV, D_HEADS * D_HD, D_KV_HEADS * D_HD, D_KV_HEADS * D_HD)
EVEN_IN = sum(EVEN_SPLITS)
ODD_IN = sum(ODD_SPLITS)
EVEN_MIX = A_HEADS * A_DV + B_HEADS * B_DV
ODD_MIX = C_HEADS * C_DV + D_HEADS * D_HD

kernel_name = 'hybrid_gla_hgrn2_deltanet_swa_moe_dit'


def rms_norm(x, w):
    xf = x.astype(jnp.float32)
    y = xf * lax.rsqrt(jnp.mean(xf * xf, axis=-1, keepdims=True) + EPS)
    return (y * w.astype(jnp.float32)).astype(x.dtype)


def modulate(h, shift, scale):
    return h * (1.0 + scale) + shift


def split_cols(h, sizes):
    return jnp.split(h, np.cumsum(sizes)[:-1].tolist(), axis=-1)


def heads(x, n):
    B, T, _ = x.shape
    return x.reshape(B, T, n, -1).transpose(0, 2, 1, 3)


def merge_heads(x):
    B, n, T, d = x.shape
    return x.transpose(0, 2, 1, 3).reshape(B, T, n * d)


def l2_normalize(x):
    return x * lax.rsqrt(jnp.sum(x * x, axis=-1, keepdims=True) + EPS)


def gated_head_norm(o, w, og):
    o = o * lax.rsqrt(jnp.mean(o * o, axis=-1, keepdims=True) + EPS) * w.astype(jnp.float32)
    return merge_heads(o) * jax.nn.silu(og)


def axial_rope(T):
    rows = T // GRID_W
    row = jnp.repeat(jnp.arange(rows, dtype=jnp.float32), GRID_W)
    col = jnp.tile(jnp.arange(GRID_W, dtype=jnp.float32), rows)
    n_freq = D_HD // 4
    inv = ROPE_BASE ** (-jnp.arange(n_freq, dtype=jnp.float32) / n_freq)
    ang = jnp.concatenate([row[:, None] * inv, col[:, None] * inv], axis=-1)
    return jnp.cos(ang), jnp.sin(ang)


def apply_rope(x, cos, sin):
    x1, x2 = jnp.split(x, 2, axis=-1)
    return jnp.concatenate([x1 * cos - x2 * sin, x2 * cos + x1 * sin], axis=-1)


def centred_conv(x, w):
    pad = CONV_W // 2
    return lax.conv_general_dilated(x, w[:, None, :].astype(x.dtype), window_strides=(1,),
                                    padding=[(pad, pad)], dimension_numbers=('NWC', 'WIO', 'NWC'),
                                    feature_group_count=x.shape[-1])


def chunked_gated_scan(q, k, v, log_f, s0, with_output):
    B, H, T, dk = q.shape
    n = T // CHUNK

    def to_chunks(a):
        return jnp.moveaxis(a.reshape(B, H, n, CHUNK, a.shape[-1]), 2, 0)

    incl = jnp.tril(jnp.ones((CHUNK, CHUNK), dtype=bool))[:, :, None]

    def step(S, inp):
        qc, kc, vc, fc = inp
        b = jnp.cumsum(fc, axis=-2)
        b_last = b[..., -1, :]
        o = None
        if with_output:
            diff = b[..., :, None, :] - b[..., None, :, :]
            decay = jnp.where(incl, jnp.exp(jnp.where(incl, diff, 0.0)), 0.0)
            scores = jnp.einsum('bhtk,bhsk,bhtsk->bhts', qc, kc, decay)
            o = (jnp.einsum('bhtk,bhkv->bhtv', qc * jnp.exp(b), S)
                 + jnp.einsum('bhts,bhsv->bhtv', scores, vc))
        S = (jnp.exp(b_last)[..., None] * S
             + jnp.einsum('bhsk,bhsv->bhkv', kc * jnp.exp(b_last[..., None, :] - b), vc))
        return S, o

    S, o = lax.scan(step, s0, (to_chunks(q), to_chunks(k), to_chunks(v), to_chunks(log_f)))
    if with_output:
        o = jnp.moveaxis(o, 0, 2).reshape(B, H, T, -1)
    return S, o


def chunked_delta_scan(q, k, v, g, beta, s0, with_output):
    B, H, T, dk = q.shape
    n = T // CHUNK
    q, k, v = [a.reshape(B, H, n, CHUNK, a.shape[-1]) for a in (q, k, v)]
    g = g.reshape(B, H, n, CHUNK)
    beta = beta.reshape(B, H, n, CHUNK)
    gam = jnp.cumsum(g, axis=-1)
    incl = jnp.tril(jnp.ones((CHUNK, CHUNK), dtype=bool))
    strict = jnp.tril(jnp.ones((CHUNK, CHUNK), dtype=bool), -1)
    diff = gam[..., :, None] - gam[..., None, :]
    L = jnp.where(incl, jnp.exp(jnp.where(incl, diff, 0.0)), 0.0)
    kb = k * beta[..., None]
    a_mat = jnp.where(strict, jnp.einsum('bhntk,bhnsk->bhnts', kb, k) * L, 0.0)
    eye = jnp.eye(CHUNK, dtype=q.dtype)
    t_inv = lax.linalg.triangular_solve(a_mat + eye, jnp.broadcast_to(eye, a_mat.shape),
                                        left_side=True, lower=True, unit_diagonal=True)
    u = jnp.einsum('bhnts,bhnsv->bhntv', t_inv, v * beta[..., None])
    w = jnp.einsum('bhnts,bhnsk->bhntk', t_inv, kb * jnp.exp(gam)[..., None])

    def step(S, inp):
        qc, kc, uc, wc, gc, Lc = inp
        v_new = uc - jnp.einsum('bhtk,bhkv->bhtv', wc, S)
        o = None
        if with_output:
            scores = jnp.einsum('bhtk,bhsk->bhts', qc, kc) * Lc
            o = (jnp.einsum('bhtk,bhkv->bhtv', qc * jnp.exp(gc)[..., None], S)
                 + jnp.einsum('bhts,bhsv->bhtv', scores, v_new))
        g_last = gc[..., -1]
        S = (jnp.exp(g_last)[..., None, None] * S
             + jnp.einsum('bhsk,bhsv->bhkv', kc * jnp.exp(g_last[..., None] - gc)[..., None], v_new))
        return S, o

    xs = tuple(jnp.moveaxis(a, 2, 0) for a in (q, k, u, w, gam, L))
    S, o = lax.scan(step, s0, xs)
    if with_output:
        o = jnp.moveaxis(o, 0, 2).reshape(B, H, T, -1)
    return S, o


def flip_t(a):
    return jnp.flip(a, axis=2)


def bidirectional(scan_fn, ctx_fwd, ctx_bwd, lat_fwd, lat_bwd, s0, ctx_out):
    sc_f, oc_f = scan_fn(*ctx_fwd, s0, ctx_out)
    _, ox_f = scan_fn(*lat_fwd, sc_f, True)
    sc_b, oc_b = scan_fn(*[flip_t(a) for a in ctx_bwd], s0, ctx_out)
    _, ox_b = scan_fn(*[flip_t(a) for a in lat_bwd], sc_b, True)
    ox = ox_f + flip_t(ox_b)
    oc = oc_f + flip_t(oc_b) if ctx_out else None
    return oc, ox


def window_attention(q, k, v, kc, vc, sinks):
    B, KVH, G, T, d = q.shape
    nb = T // WBLK
    qb = q.reshape(B, KVH, G, nb, WBLK, d)

    def band(a):
        ap = jnp.pad(a, ((0, 0), (0, 0), (WBLK, WBLK), (0, 0))).reshape(B, KVH, nb + 2, WBLK, d)
        return jnp.concatenate([ap[:, :, :-2], ap[:, :, 1:-1], ap[:, :, 2:]], axis=3)

    kband, vband = band(k), band(v)
    qpos = jnp.arange(WBLK)
    kpos = jnp.arange(3 * WBLK) - WBLK
    rel = kpos[None, :] - qpos[:, None]
    abs_k = (jnp.arange(nb) * WBLK)[:, None] + kpos[None, :]
    valid = ((jnp.abs(rel) <= WINDOW)[None]
             & ((abs_k >= 0) & (abs_k < T))[:, None, :])
    scale = d ** -0.5
    s_loc = jnp.einsum('bhgnqd,bhnkd->bhgnqk', qb, kband) * scale
    s_loc = jnp.where(valid, s_loc, -jnp.inf)
    s_ctx = jnp.einsum('bhgnqd,bhcd->bhgnqc', qb, kc) * scale
    sink = jnp.broadcast_to(sinks.astype(jnp.float32).reshape(KVH, G)[None, :, :, None, None, None],
                            (B, KVH, G, nb, WBLK, 1))
    p = jax.nn.softmax(jnp.concatenate([sink, s_loc, s_ctx], axis=-1), axis=-1)
    p_loc = p[..., 1:1 + 3 * WBLK]
    p_ctx = p[..., 1 + 3 * WBLK:]
    out = (jnp.einsum('bhgnqk,bhnkd->bhgnqd', p_loc, vband)
           + jnp.einsum('bhgnqc,bhcd->bhgnqd', p_ctx, vc))
    return out.reshape(B, KVH, G, T, d)


def context_attention(q, k, v, sinks):
    B, KVH, G, Tc, d = q.shape
    s = jnp.einsum('bhgqd,bhkd->bhgqk', q, k) * d ** -0.5
    sink = jnp.broadcast_to(sinks.astype(jnp.float32).reshape(KVH, G)[None, :, :, None, None],
                            (B, KVH, G, Tc, 1))
    p = jax.nn.softmax(jnp.concatenate([sink, s], axis=-1), axis=-1)
    return jnp.einsum('bhgqk,bhkd->bhgqd', p[..., 1:], v)


def mixer_gla_hgrn(hc, hx, ctx_out, w_in, w_out, gla_w2_f, gla_b_f, gla_w2_b, gla_b_b,
                   gla_norm_w, hgrn_norm_w, lb):
    f32 = jnp.float32
    lb = lb.astype(f32)

    def prep(h):
        parts = [p_.astype(f32) for p_ in split_cols(h @ w_in, EVEN_SPLITS)]
        aq, ak, av, ar_f, ar_b, aog, bq, bz_f, bz_b, bi, bog = parts
        aq = heads(aq, A_HEADS) * A_DK ** -0.5
        ak = heads(ak, A_HEADS)
        av = heads(av, A_HEADS)

        def a_dir(r, w2, b2):
            log_a = jax.nn.log_sigmoid(r @ w2.astype(f32) + b2.astype(f32)) / GATE_TAU
            return (aq, ak, av, heads(log_a, A_HEADS))

        bq = heads(bq, B_HEADS)
        bi = heads(bi, B_HEADS)

        def b_dir(z):
            log_f = jnp.logaddexp(jnp.log(lb), jnp.log1p(-lb) + jax.nn.log_sigmoid(z))
            key = (1.0 - lb) * jax.nn.sigmoid(-z)
            return (bq, heads(key, B_HEADS), bi, heads(log_f, B_HEADS))

        return (a_dir(ar_f, gla_w2_f, gla_b_f), a_dir(ar_b, gla_w2_b, gla_b_b),
                b_dir(bz_f), b_dir(bz_b), aog, bog)

    ca_f, ca_b, cb_f, cb_b, c_aog, c_bog = prep(hc)
    xa_f, xa_b, xb_f, xb_b, x_aog, x_bog = prep(hx)
    B = hx.shape[0]
    sa0 = jnp.zeros((B, A_HEADS, A_DK, A_DV), f32)
    sb0 = jnp.zeros((B, B_HEADS, B_DK, B_DV), f32)
    oc_a, ox_a = bidirectional(chunked_gated_scan, ca_f, ca_b, xa_f, xa_b, sa0, ctx_out)
    oc_b, ox_b = bidirectional(chunked_gated_scan, cb_f, cb_b, xb_f, xb_b, sb0, ctx_out)

    def readout(oa, ob, aog, bog, dtype):
        y = jnp.concatenate([gated_head_norm(oa, gla_norm_w, aog),
                             gated_head_norm(ob, hgrn_norm_w, bog)], axis=-1)
        return y.astype(dtype) @ w_out

    yx = readout(ox_a, ox_b, x_aog, x_bog, hx.dtype)
    yc = readout(oc_a, oc_b, c_aog, c_bog, hc.dtype) if ctx_out else None
    return yc, yx


def mixer_delta_swa(hc, hx, ctx_out, w_in, w_out, conv_w, a_log_f, dt_bias_f, a_log_b, dt_bias_b,
                    dn_norm_w, sinks, cos, sin):
    f32 = jnp.float32

    def prep(h, rope):
        cq, ck, cv, bt_f, bt_b, a_f, a_b, og, dq, dk, dv = split_cols(h @ w_in, ODD_SPLITS)
        qkv = jax.nn.silu(centred_conv(jnp.concatenate([cq, ck, cv], axis=-1), conv_w)).astype(f32)
        cq, ck, cv = split_cols(qkv, (C_HEADS * C_DK, C_HEADS * C_DK, C_HEADS * C_DV))
        cq = l2_normalize(heads(cq, C_HEADS)) * C_DK ** -0.5
        ck = l2_normalize(heads(ck, C_HEADS))
        cv = heads(cv, C_HEADS)

        def c_dir(bt, a, a_log, dt_bias):
            g = -jnp.exp(a_log.astype(f32)) * jax.nn.softplus(a.astype(f32) + dt_bias.astype(f32))
            beta = jax.nn.sigmoid(bt.astype(f32))
            return (cq, ck, cv, g.transpose(0, 2, 1), beta.transpose(0, 2, 1))

        dq = heads(dq.astype(f32), D_HEADS)
        dk = heads(dk.astype(f32), D_KV_HEADS)
        dv = heads(dv.astype(f32), D_KV_HEADS)
        if rope:
            dq = apply_rope(dq, cos, sin)
            dk = apply_rope(dk, cos, sin)
        B, _, T, _ = dq.shape
        dq = dq.reshape(B, D_KV_HEADS, D_HEADS // D_KV_HEADS, T, D_HD)
        return (c_dir(bt_f, a_f, a_log_f, dt_bias_f), c_dir(bt_b, a_b, a_log_b, dt_bias_b),
                og.astype(f32), dq, dk, dv)

    cc_f, cc_b, c_og, c_dq, c_dk, c_dv = prep(hc, False)
    xc_f, xc_b, x_og, x_dq, x_dk, x_dv = prep(hx, True)
    B = hx.shape[0]
    s0 = jnp.zeros((B, C_HEADS, C_DK, C_DV), f32)
    oc_c, ox_c = bidirectional(chunked_delta_scan, cc_f, cc_b, xc_f, xc_b, s0, ctx_out)
    ox_d = window_attention(x_dq, x_dk, x_dv, c_dk, c_dv, sinks)

    def readout(o_c, o_d, og, dtype):
        B_, _, T, _ = o_c.shape
        y = jnp.concatenate([gated_head_norm(o_c, dn_norm_w, og),
                             merge_heads(o_d.reshape(B_, D_HEADS, T, D_HD))], axis=-1)
        return y.astype(dtype) @ w_out

    yx = readout(ox_c, ox_d, x_og, hx.dtype)
    yc = None
    if ctx_out:
        oc_d = context_attention(c_dq, c_dk, c_dv, sinks)
        yc = readout(oc_c, oc_d, c_og, hc.dtype)
    return yc, yx


def moe_ffn(h, router_w, router_b, w_up, b_up, w_down, b_down):
    f32 = jnp.float32
    logits = (h @ router_w + router_b).astype(f32)
    top_val, top_idx = lax.top_k(logits, TOP_K)
    top_w = jax.nn.softmax(top_val, axis=-1)
    gates = jnp.sum(jax.nn.one_hot(top_idx, N_EXPERTS, dtype=f32) * top_w[..., None], axis=-2)
    out = jnp.zeros(h.shape, f32)
    for e in range(N_EXPERTS):
        u = (h @ w_up[e] + b_up[e]).astype(f32)
        glu, lin = jnp.split(u, 2, axis=-1)
        glu = jnp.minimum(glu, SWIGLU_LIMIT)
        lin = jnp.clip(lin, -SWIGLU_LIMIT, SWIGLU_LIMIT)
        act = glu * jax.nn.sigmoid(SWIGLU_ALPHA * glu) * (lin + 1.0)
        y = act.astype(h.dtype) @ w_down[e] + b_down[e]
        out = out + gates[..., e:e + 1] * y.astype(f32)
    return out.astype(h.dtype)


def setup_inputs(seed: int = 0) -> dict:
    key = jax.random.key(seed)
    ks = iter(jax.random.split(key, 64))
    f32 = jnp.float32
    d = D_MODEL

    def nrm(shape, scale):
        return jax.random.normal(next(ks), shape, f32) * scale

    def gain(n):
        return 1.0 + nrm((n,), 0.02)

    def a_log(n):
        return jnp.log(jax.random.uniform(next(ks), (n,), f32, 1.0, 16.0))

    def dt_bias(n):
        dt = jnp.exp(jax.random.uniform(next(ks), (n,), f32, float(np.log(1e-3)), float(np.log(1e-1))))
        return dt + jnp.log(-jnp.expm1(-dt))

    inp = {}
    inp['x'] = nrm((BATCH, SEQ, d), 1.0)
    inp['c'] = nrm((BATCH, d), 1.0)
    inp['ctx'] = nrm((BATCH, CTX_LEN, d), 1.0)
    inp['c_ctx'] = nrm((d,), 1.0)
    inp['l0_ada_w'] = nrm((d, 6 * d), 0.5 * d ** -0.5)
    inp['l0_ada_b'] = nrm((6 * d,), 0.02)
    inp['l0_norm_mix_w'] = gain(d)
    inp['l0_w_in'] = nrm((d, EVEN_IN), d ** -0.5)
    inp['l0_w_out'] = nrm((EVEN_MIX, d), EVEN_MIX ** -0.5)
    inp['l0_gla_w2_f'] = nrm((A_RANK, A_HEADS * A_DK), A_RANK ** -0.5)
    inp['l0_gla_b_f'] = nrm((A_HEADS * A_DK,), 0.1)
    inp['l0_gla_w2_b'] = nrm((A_RANK, A_HEADS * A_DK), A_RANK ** -0.5)
    inp['l0_gla_b_b'] = nrm((A_HEADS * A_DK,), 0.1)
    inp['l0_gla_norm_w'] = gain(A_DV)
    inp['l0_hgrn_norm_w'] = gain(B_DV)
    inp['hgrn_lb_logits'] = nrm((N_B_LAYERS + 1, B_HEADS * B_DK), 0.5)
    inp['l0_norm_ffn_w'] = gain(d)
    inp['l0_router_w'] = nrm((d, N_EXPERTS), d ** -0.5)
    inp['l0_router_b'] = nrm((N_EXPERTS,), 0.01)
    inp['l0_w_up'] = nrm((N_EXPERTS, d, 2 * D_FF), d ** -0.5)
    inp['l0_b_up'] = nrm((N_EXPERTS, 2 * D_FF), 0.02)
    inp['l0_w_down'] = nrm((N_EXPERTS, D_FF, d), D_FF ** -0.5)
    inp['l0_b_down'] = nrm((N_EXPERTS, d), 0.02)
    inp['l1_ada_w'] = nrm((d, 6 * d), 0.5 * d ** -0.5)
    inp['l1_ada_b'] = nrm((6 * d,), 0.02)
    inp['l1_norm_mix_w'] = gain(d)
    inp['l1_w_in'] = nrm((d, ODD_IN), d ** -0.5)
    inp['l1_w_out'] = nrm((ODD_MIX, d), ODD_MIX ** -0.5)
    inp['l1_conv_w'] = nrm((CONV_W, 2 * C_HEADS * C_DK + C_HEADS * C_DV), CONV_W ** -0.5)
    inp['l1_a_log_f'] = a_log(C_HEADS)
    inp['l1_dt_bias_f'] = dt_bias(C_HEADS)
    inp['l1_a_log_b'] = a_log(C_HEADS)
    inp['l1_dt_bias_b'] = dt_bias(C_HEADS)
    inp['l1_dn_norm_w'] = gain(C_DV)
    inp['l1_sinks'] = nrm((D_HEADS,), 1.0)
    inp['l1_norm_ffn_w'] = gain(d)
    inp['l1_router_w'] = nrm((d, N_EXPERTS), d ** -0.5)
    inp['l1_router_b'] = nrm((N_EXPERTS,), 0.01)
    inp['l1_w_up'] = nrm((N_EXPERTS, d, 2 * D_FF), d ** -0.5)
    inp['l1_b_up'] = nrm((N_EXPERTS, 2 * D_FF), 0.02)
    inp['l1_w_down'] = nrm((N_EXPERTS, D_FF, d), D_FF ** -0.5)
    inp['l1_b_down'] = nrm((N_EXPERTS, d), 0.02)
    inp['final_norm_w'] = gain(d)
    return inp


def reference(x, c, ctx, c_ctx,
              l0_ada_w, l0_ada_b, l0_norm_mix_w, l0_w_in, l0_w_out,
              l0_gla_w2_f, l0_gla_b_f, l0_gla_w2_b, l0_gla_b_b, l0_gla_norm_w, l0_hgrn_norm_w,
              hgrn_lb_logits, l0_norm_ffn_w,
              l0_router_w, l0_router_b, l0_w_up, l0_b_up, l0_w_down, l0_b_down,
              l1_ada_w, l1_ada_b, l1_norm_mix_w, l1_w_in, l1_w_out,
              l1_conv_w, l1_a_log_f, l1_dt_bias_f, l1_a_log_b, l1_dt_bias_b, l1_dn_norm_w, l1_sinks,
              l1_norm_ffn_w,
              l1_router_w, l1_router_b, l1_w_up, l1_b_up, l1_w_down, l1_b_down,
              final_norm_w):
    T = x.shape[1]
    cos, sin = axial_rope(T)
    lb_table = jnp.cumsum(jax.nn.softmax(hgrn_lb_logits.astype(jnp.float32), axis=0), axis=0)
    layers = (
        dict(ada=(l0_ada_w, l0_ada_b), norm_mix_w=l0_norm_mix_w, norm_ffn_w=l0_norm_ffn_w,
             mix=(l0_w_in, l0_w_out, l0_gla_w2_f, l0_gla_b_f, l0_gla_w2_b, l0_gla_b_b,
                  l0_gla_norm_w, l0_hgrn_norm_w),
             moe=(l0_router_w, l0_router_b, l0_w_up, l0_b_up, l0_w_down, l0_b_down)),
        dict(ada=(l1_ada_w, l1_ada_b), norm_mix_w=l1_norm_mix_w, norm_ffn_w=l1_norm_ffn_w,
             mix=(l1_w_in, l1_w_out, l1_conv_w, l1_a_log_f, l1_dt_bias_f, l1_a_log_b, l1_dt_bias_b,
                  l1_dn_norm_w, l1_sinks),
             moe=(l1_router_w, l1_router_b, l1_w_up, l1_b_up, l1_w_down, l1_b_down)),
    )
    sc = jax.nn.silu(c)
    scc = jax.nn.silu(c_ctx)
    for l in range(DEPTH):
        p = layers[l]
        ctx_out = l < DEPTH - 1
        ada_w, ada_b = p['ada']
        sh1, sc1, g1, sh2, sc2, g2 = jnp.split((sc @ ada_w + ada_b)[:, None, :], 6, axis=-1)
        csh1, csc1, cg1, csh2, csc2, cg2 = jnp.split(scc @ ada_w + ada_b, 6, axis=-1)
        hx = modulate(rms_norm(x, p['norm_mix_w']), sh1, sc1)
        hc = modulate(rms_norm(ctx, p['norm_mix_w']), csh1, csc1)
        if l % 2 == 0:
            yc, yx = mixer_gla_hgrn(hc, hx, ctx_out, *p['mix'], lb_table[l // 2])
        else:
            yc, yx = mixer_delta_swa(hc, hx, ctx_out, *p['mix'], cos, sin)
        x = x + g1 * yx
        x = x + g2 * moe_ffn(modulate(rms_norm(x, p['norm_ffn_w']), sh2, sc2), *p['moe'])
        if ctx_out:
            ctx = ctx + cg1 * yc
            ctx = ctx + cg2 * moe_ffn(modulate(rms_norm(ctx, p['norm_ffn_w']), csh2, csc2), *p['moe'])
    return rms_norm(x, final_norm_w)
```

```python
import numpy as np
from contextlib import ExitStack
import concourse.bass as bass
import concourse.mybir as mybir
from concourse.bass_utils import run_bass_kernel_spmd

dt = mybir.dt
F32 = dt.float32
BF16 = dt.bfloat16
AF = mybir.ActivationFunctionType
ALU = mybir.AluOpType
AX = mybir.AxisListType

ENGS = ['pe', 'act', 'dve', 'pool', 'sp']
NDMA = 6
EPS = 1e-6
DM = 1024
NE = 32


class Tok:
    __slots__ = ('w', 'r')

    def __init__(self):
        self.w = None
        self.r = {}


class Prog:
    def __init__(self, nc):
        self.nc = nc
        self.stack = ExitStack()
        self.sems = {}
        self.cnt = {e: 0 for e in ENGS}
        self.dma_cnt = {}
        self.dma_rr = {e: 0 for e in ENGS}
        self.seen = {e: {} for e in ENGS}
        self.q = {e: [] for e in ENGS}
        self.nops = 0
        for e in ENGS:
            self.sems[('eng', e)] = self.stack.enter_context(nc.semaphore('s_' + e))
        for e in ('sp', 'pool', 'act'):
            for k in range(NDMA):
                self.sems[('dma', e, k)] = self.stack.enter_context(nc.semaphore('d_%s%d' % (e, k)))

    def op(self, eng, fn, reads=(), writes=(), dma=False):
        deps = {}

        def add(k, v):
            if deps.get(k, 0) < v:
                deps[k] = v

        for t in reads:
            if t.w is not None:
                add(*t.w)
        for t in writes:
            if t.w is not None:
                add(*t.w)
            for k, v in t.r.items():
                add(k, v)
        if dma:
            k = self.dma_rr[eng]
            self.dma_rr[eng] = (k + 1) % NDMA
            key = ('dma', eng, k)
            prev = self.dma_cnt.get(key, 0)
            if prev:
                add(key, prev)
            val = prev + 16
            self.dma_cnt[key] = val
        else:
            key = ('eng', eng)
            self.cnt[eng] += 1
            val = self.cnt[eng]
        seen = self.seen[eng]
        waits = []
        for k, v in deps.items():
            if eng == 'pe' and k == ('eng', 'pe'):
                continue
            if seen.get(k, 0) >= v:
                continue
            seen[k] = v
            waits.append((k, v))
        self.q[eng].append((waits, fn, key, dma))
        self.nops += 1
        for t in reads:
            if t.r.get(key, 0) < val:
                t.r[key] = val
        for t in writes:
            t.w = (key, val)
            t.r = {}

    def barrier(self):
        evs = [(('eng', e), self.cnt[e]) for e in ENGS if self.cnt[e]]
        evs += list(self.dma_cnt.items())
        for e in ENGS:
            seen = self.seen[e]
            waits = []
            for k, v in evs:
                if seen.get(k, 0) >= v:
                    continue
                seen[k] = v
                waits.append((k, v))
            if waits:
                self.q[e].append((waits, None, None, False))

    def emit(self):
        nc = self.nc
        sems = self.sems
        with nc.Block() as block:
            def mk(name):
                lst = self.q[name]

                def f(e):
                    for waits, fn, key, dma in lst:
                        for k, v in waits:
                            e.wait_ge(sems[k], v)
                        if fn is None:
                            continue
                        ins = fn(e)
                        ins.then_inc(sems[key], 16 if dma else 1)
                return f
            block.tensor(mk('pe'))
            block.scalar(mk('act'))
            block.vector(mk('dve'))
            block.gpsimd(mk('pool'))
            block.sync(mk('sp'))
        self.q = {e: [] for e in ENGS}

    def close(self):
        self.stack.close()


class Builder:
    def __init__(self, T, TC, phases=('ada', 'l0mix', 'l0moe', 'l1mix', 'l1moe'), dbg=()):
        self.T, self.TC = T, TC
        self.NB, self.NCB = T // 128, TC // 128
        self.phases = phases
        self.dbg = dbg
        self.nc = bass.Bass("TRN2", target_bir_lowering=False)
        self.D = {}
        self.P = Prog(self.nc)
        self.in_names = []

    def din(self, name, shape, d=F32):
        self.D[name] = self.nc.dram_tensor(name, list(shape), d, kind="ExternalInput").ap()
        self.in_names.append(name)
        return self.D[name]

    def dscr(self, name, shape, d=F32):
        kind = "ExternalOutput" if name in self.dbg else "Internal"
        self.D[name] = self.nc.dram_tensor(name, list(shape), d, kind=kind).ap()
        return self.D[name]

    def declare(self):
        T, TC = self.T, self.TC
        din = self.din
        din('x', [T, DM]); din('ctx', [max(TC, 128), DM]); din('cvec', [128, 8, 2])
        din('ident', [128, 128]); din('ones', [128, 128])
        for l in range(2):
            p = 'l%d_' % l
            din(p + 'ada_w', [DM, 6 * DM]); din(p + 'ada_bT', [128, 48])
            din(p + 'nmix', [128, 8]); din(p + 'nffn', [128, 8])
            if ('l%dmoe' % l) in self.phases:
                din(p + 'router_w', [DM, NE]); din(p + 'router_b', [128, NE])
                din(p + 'w_up', [NE, DM, 2 * DM]); din(p + 'b_upT', [128, NE, 16])
                din(p + 'w_down', [NE, DM, DM]); din(p + 'b_down', [NE, DM])
        din('final_w', [128, DM])
        self.out = self.nc.dram_tensor('out', [T, DM], F32, kind="ExternalOutput").ap()
        R = TC + T
        self.dscr('X1', [R, DM]); self.dscr('X2', [R, DM]); self.dscr('X3', [R, DM]); self.dscr('OF', [R, DM])
        if 'l1mix' in self.phases:
            din('l1_w_in', [DM, 2832]); din('l1_w_out', [DM, DM]); din('l1_convw', [128, 5, 1536]); din('l1_pvec', [128, 16])
            din('l1_dnw', [128, 512]); din('l1_sinks', [128, 8]); din('rope', [T, 64])
            for nm in ('mask_fs', 'mask_rs', 'same', 'mb_prev', 'mb_next'):
                din(nm, [128, 128])
            din('csel', [128, 2])
            self.dscr('CX', [R + 8, 1536]); self.dscr('KT', [128, R]); self.dscr('KT2', [128, R]); self.dscr('VV', [R, 128])
            if 'l0mix' not in self.phases:
                din('mask_f', [128, 128]); din('mask_r', [128, 128])
        if 'l0mix' not in self.phases:
            return
        din('l0_w_in', [DM, 4128]); din('l0_w_out', [DM, DM]); din('l0_w2', [16, 2, 256]); din('l0_b2', [128, 2, 2])
        din('l0_lb', [128, 2, 4]); din('l0_normw', [128, DM]); din('rmask', [128, 768]); din('mask_f', [128, 128]); din('mask_r', [128, 128])

    def build(self):
        nc, P = self.nc, self.P
        self.declare()
        with ExitStack() as st0:
            self.st0 = st0
            sb = lambda name, shape, d=F32: st0.enter_context(nc.sbuf_tensor(name, shape, d))
            self.ident = sb('ident_t', [128, 128]); self.ones = sb('ones_t', [128, 128])
            self.identb = sb('identb_t', [128, 128], BF16)
            self.modT = [sb('modT%d' % l, [128, 48, 2]) for l in range(2)]
            self.Acol = [[sb('A%d_%d' % (l, i), [128, 8, 2]) for i in range(2)] for l in range(2)]
            self.gbc = [sb('gbc_x', [128, DM]), sb('gbc_c', [128, DM])]
            self.t_const = Tok(); self.t_mod = Tok(); self.t_gbc = Tok()
            P.op('sp', lambda e: e.dma_start(out=self.ident[:], in_=self.D['ident']), writes=[self.t_const], dma=True)
            P.op('sp', lambda e: e.dma_start(out=self.ones[:], in_=self.D['ones']), writes=[self.t_const], dma=True)
            P.op('dve', lambda e: e.tensor_copy(out=self.identb[:], in_=self.ident[:]), reads=[self.t_const], writes=[self.t_const])
            P.barrier()
            self.phase_ada()
            cur = 'x_in'
            if 'l0mix' in self.phases:
                self.phase_l0mix(cur, 'X1'); cur = 'X1'
            if 'l0moe' in self.phases:
                self.phase_moe(0, cur, 'X2', final=False); cur = 'X2'
            if 'l1mix' in self.phases:
                self.phase_l1mix(cur, 'X3'); cur = 'X3'
            if 'l1moe' in self.phases:
                self.phase_moe(1, cur, None, final=True)
            elif 'final' in self.phases:
                self.phase_final(cur)
            P.barrier()
            P.emit()
        P.close()
        return nc

    def rows(self, name, r0, n=128):
        if name == 'x_in':
            if r0 < self.TC:
                return self.D['ctx'][r0:r0 + n, :]
            return self.D['x'][r0 - self.TC:r0 - self.TC + n, :]
        return self.D[name][r0:r0 + n, :]

    def phase_ada(self):
        nc, P, D = self.nc, self.P, self.D
        with ExitStack() as st:
            sb = lambda name, shape, d=F32: st.enter_context(nc.sbuf_tensor(name, shape, d))
            ps = [st.enter_context(nc.psum_tensor('pa%d' % i, [128, 512], F32)) for i in range(2)]
            cv = sb('cv', [128, 8, 2]); scv = sb('scv', [128, 8, 2])
            wb = [sb('adaw%d' % i, [128, 8, 512]) for i in range(2)]
            bT = sb('adab', [128, 48]); nw = sb('nw', [128, 8])
            t_cv, t_ps, t_b = Tok(), Tok(), Tok()
            t_wb = [Tok(), Tok()]
            P.op('sp', lambda e: e.dma_start(out=cv[:], in_=D['cvec']), writes=[t_cv], dma=True)
            P.op('act', lambda e: e.activation(out=scv[:], in_=cv[:], func=AF.Silu), reads=[t_cv], writes=[t_cv])
            for l in range(2):
                p = 'l%d_' % l
                P.op('sp', lambda e, p=p: e.dma_start(out=bT[:], in_=D[p + 'ada_bT']), writes=[t_b], dma=True)
                for cb in range(12):
                    w = wb[cb % 2]; tw = t_wb[cb % 2]
                    src = D[p + 'ada_w'][:, cb * 512:(cb + 1) * 512].rearrange("(k p) n -> p k n", p=128)
                    P.op('sp', lambda e, w=w, src=src: e.dma_start(out=w[:], in_=src), writes=[tw], dma=True)

                    def mm(e, w=w, cb=cb):
                        for jj in range(4):
                            j = cb * 4 + jj
                            for kc in range(8):
                                ins = e.matmul(ps[0][:, j * 2:j * 2 + 2], w[:, kc, jj * 128:(jj + 1) * 128],
                                               scv[:, kc, :], start=(kc == 0), stop=(kc == 7))
                        return ins
                    P.op('pe', mm, reads=[tw, t_cv], writes=[t_ps])
                modT = self.modT[l]
                P.op('dve', lambda e, modT=modT: e.tensor_tensor(
                    out=modT[:], in0=ps[0][:, 0:96].rearrange("p (j v) -> p j v", v=2),
                    in1=bT[:].unsqueeze(2).to_broadcast([128, 48, 2]), op=ALU.add),
                    reads=[t_ps, t_b], writes=[self.t_mod])
                for i, (nm, c0) in enumerate((('nmix', 8), ('nffn', 32))):
                    A = self.Acol[l][i]
                    P.op('sp', lambda e, nm=nm, p=p: e.dma_start(out=nw[:], in_=D[p + nm]), writes=[t_b], dma=True)
                    P.op('dve', lambda e, A=A, modT=modT, c0=c0: e.scalar_tensor_tensor(
                        out=A[:], in0=modT[:, c0:c0 + 8, :], scalar=1.0,
                        in1=nw[:].unsqueeze(2).to_broadcast([128, 8, 2]), op0=ALU.add, op1=ALU.mult),
                        reads=[self.t_mod, t_b], writes=[self.t_mod])
            P.barrier()
            P.emit()

    def gate_bcast(self, dg, ps, l, c0, variants):
        nc, P = self.nc, self.P
        t_dg, t_p = Tok(), Tok()
        for v in variants:
            for k in range(8):
                P.op('dve', lambda e, k=k, v=v: e.tensor_scalar(
                    out=dg[:, k, :], in0=self.ident[:], scalar1=self.modT[l][:, c0 + k, v:v + 1], scalar2=None,
                    op0=ALU.mult), reads=[self.t_mod, self.t_const], writes=[t_dg])
            for h in range(2):
                def mm(e, h=h):
                    for j in range(4):
                        ins = e.matmul(ps[h][:, j * 128:(j + 1) * 128], self.ones[:], dg[:, h * 4 + j, :],
                                       start=True, stop=True)
                    return ins
                P.op('pe', mm, reads=[t_dg, self.t_const], writes=[t_p])
                P.op('act', lambda e, h=h, v=v: e.activation(out=self.gbc[v][:, h * 512:(h + 1) * 512], in_=ps[h][:],
                                                            func=AF.Identity), reads=[t_p], writes=[self.t_gbc])

    def norm_xn(self, src_rows, xt, t_x, xn, t_xn, small, t_small, junk, t_junk):
        P = self.P
        P.op('sp', lambda e: e.dma_start(out=xt[:], in_=src_rows), writes=[t_x], dma=True)
        P.op('act', lambda e: e.activation(out=junk[:], in_=xt[:], func=AF.Square, accum_out=small[:, 0:1]),
             reads=[t_x], writes=[t_junk, t_small])
        P.op('act', lambda e: e.activation(out=small[:, 1:2], in_=small[:, 0:1], func=AF.Sqrt, scale=1.0 / DM, bias=self.epsc[:, 0:1]),
             reads=[t_small], writes=[t_small])
        P.op('dve', lambda e: e.reciprocal(out=small[:, 2:3], in_=small[:, 1:2]), reads=[t_small], writes=[t_small])
        P.op('dve', lambda e: e.tensor_scalar(out=xn[:], in0=xt[:], scalar1=small[:, 2:3], scalar2=None, op0=ALU.mult),
             reads=[t_x, t_small], writes=[t_xn])

    def phase_moe(self, l, src, dst, final):
        nc, P, D = self.nc, self.P, self.D
        p = 'l%d_' % l
        TC = self.TC if l == 0 else 0
        r_begin = 0 if l == 0 else self.TC
        nblk = (TC + self.T) // 128
        NSB = 8
        with ExitStack() as st:
            sb = lambda name, shape, d=F32: st.enter_context(nc.sbuf_tensor(name + '_m%d' % l, shape, d))
            ps = [st.enter_context(nc.psum_tensor('pm%d_%d' % (l, i), [128, 512], F32)) for i in range(8)]
            self.epsc = sb('epsc', [128, 1])
            h2T = sb('h2T', [128, 8, NSB * 128], BF16)
            acc = sb('acc', [128, NSB, DM])
            gates = sb('gates', [128, NSB, NE])
            wu = [sb('wu%d' % i, [128, 8, 2 * DM], BF16) for i in range(2)]
            wd = [sb('wd%d' % i, [128, 8, DM], BF16) for i in range(2)]
            actT = [sb('actT0', [128, 8, 512], BF16)] * 2
            xt = [sb('xt%d' % i, [128, DM]) for i in range(2)]
            junk = sb('junk', [128, DM], BF16)
            small = sb('small', [128, 16]); h32 = sb('h32', [128, 8, 128])
            rw = sb('rw', [128, 8, NE]); rb = sb('rb', [128, NE]); bup = sb('bup', [128, NE, 16])
            bdn = sb('bdn', [NE, DM]); lg = sb('lg', [128, NE]); top8 = sb('top8', [128, 8])
            em = sb('em', [128, NE]); gT = sb('gT', [NE, 128])
            eg = [sb('eg%d' % i, [128, 512]) for i in range(2)]
            es = [sb('es%d' % i, [128, 512]) for i in range(2)]
            el = [sb('el%d' % i, [128, 512]) for i in range(2)]
            fw = sb('fw', [128, DM]) if final else None
            T_ = lambda: Tok()
            t_c, t_h2T, t_acc, t_gates = T_(), T_(), T_(), T_()
            t_wu, t_wd = [T_(), T_()], [T_(), T_()]
            t_act = [T_()] * 2
            t_xt, t_xn, t_junk, t_small, t_h32 = [T_(), T_()], T_(), T_(), T_(), T_()
            t_lg, t_gT = T_(), T_()
            t_ps = [T_() for _ in range(8)]
            t_eg, t_es, t_el = [T_(), T_()], [T_(), T_()], [T_(), T_()]
            t_out = T_()
            P.op('pool', lambda e: e.memset(self.epsc[:], EPS), writes=[t_c])
            P.op('sp', lambda e: e.dma_start(out=rw[:], in_=D[p + 'router_w'].rearrange("(k p) n -> p k n", p=128)), writes=[t_c], dma=True)
            P.op('sp', lambda e: e.dma_start(out=rb[:], in_=D[p + 'router_b']), writes=[t_c], dma=True)
            P.op('sp', lambda e: e.dma_start(out=bup[:], in_=D[p + 'b_upT']), writes=[t_c], dma=True)
            P.op('sp', lambda e: e.dma_start(out=bdn[:], in_=D[p + 'b_down']), writes=[t_c], dma=True)
            if final:
                P.op('sp', lambda e: e.dma_start(out=fw[:], in_=D['final_w']), writes=[t_c], dma=True)
            variants = (0, 1) if TC else (0,)
            self.gate_bcast(h32, ps[0:2], l, 40, variants)
            P.barrier()
            A2 = self.Acol[l][1]; modT = self.modT[l]
            wcount = 0
            for sb0 in range(0, nblk, NSB):
                nb = min(NSB, nblk - sb0)
                for bi in range(nb):
                    r0 = r_begin + (sb0 + bi) * 128
                    v = 1 if (r0 < self.TC) else 0
                    x_t = xt[bi % 2]; tx = t_xt[bi % 2]
                    self.norm_xn(self.rows(src, r0), x_t, tx, x_t, tx, small, t_small, junk, t_junk)
                    xn, t_xn = x_t, tx
                    for h in range(2):
                        def tr(e, h=h, xn=xn):
                            for j in range(4):
                                k = h * 4 + j
                                ins = e.transpose(ps[h][:, j * 128:(j + 1) * 128], xn[:, k * 128:(k + 1) * 128], self.ident[:])
                            return ins
                        P.op('pe', tr, reads=[t_xn, self.t_const], writes=[t_ps[h]])
                        for j in range(4):
                            k = h * 4 + j
                            P.op('act', lambda e, h=h, j=j, k=k, v=v: e.activation(
                                out=h32[:, k, :], in_=ps[h][:, j * 128:(j + 1) * 128], func=AF.Identity,
                                scale=A2[:, k, v:v + 1], bias=modT[:, 24 + k, v:v + 1]),
                                reads=[t_ps[h], self.t_mod], writes=[t_h32])
                    P.op('dve', lambda e, bi=bi: e.tensor_copy(out=h2T[:, :, bi * 128:(bi + 1) * 128], in_=h32[:]),
                         reads=[t_h32], writes=[t_h2T])

                    def rmm(e):
                        for k in range(8):
                            ins = e.matmul(ps[2][:, 0:NE], h32[:, k, :], rw[:, k, :], start=(k == 0), stop=(k == 7))
                        return ins
                    P.op('pe', rmm, reads=[t_h32, t_c], writes=[t_ps[2]])
                    P.op('dve', lambda e: e.tensor_tensor(out=lg[:], in0=ps[2][:, 0:NE], in1=rb[:], op=ALU.add),
                         reads=[t_ps[2], t_c], writes=[t_lg])
                    P.op('dve', lambda e: e.max(out=top8[:], in_=lg[:]), reads=[t_lg], writes=[t_lg])
                    P.op('dve', lambda e: e.tensor_scalar(out=small[:, 4:5], in0=top8[:, 0:1], scalar1=-1.0, scalar2=None, op0=ALU.mult),
                         reads=[t_lg], writes=[t_small])
                    P.op('act', lambda e: e.activation(out=em[:], in_=lg[:], func=AF.Exp, bias=small[:, 4:5], scale=1.0),
                         reads=[t_lg, t_small], writes=[t_lg])
                    P.op('dve', lambda e: e.scalar_tensor_tensor(out=em[:], in0=lg[:], scalar=top8[:, 3:4], in1=em[:],
                                                                  op0=ALU.is_ge, op1=ALU.mult), reads=[t_lg], writes=[t_lg])
                    P.op('dve', lambda e: e.tensor_reduce(out=small[:, 5:6], in_=em[:], axis=AX.X, op=ALU.add),
                         reads=[t_lg], writes=[t_small])
                    P.op('dve', lambda e: e.reciprocal(out=small[:, 6:7], in_=small[:, 5:6]), reads=[t_small], writes=[t_small])
                    P.op('dve', lambda e, bi=bi: e.tensor_scalar(out=gates[:, bi, :], in0=em[:], scalar1=small[:, 6:7], scalar2=None,
                                                                 op0=ALU.mult), reads=[t_lg, t_small], writes=[t_gates])
                    P.op('pe', lambda e, bi=bi: e.transpose(ps[3][0:NE, 0:128], gates[:, bi, :], self.ident[:]),
                         reads=[t_gates, self.t_const], writes=[t_ps[3]])
                    P.op('act', lambda e: e.activation(out=gT[:], in_=ps[3][0:NE, 0:128], func=AF.Identity),
                         reads=[t_ps[3]], writes=[t_gT])
                    for h in range(2):
                        P.op('pe', lambda e, h=h: e.matmul(ps[h][:], gT[:], bdn[:, h * 512:(h + 1) * 512], start=True, stop=True),
                             reads=[t_gT, t_c], writes=[t_ps[h]])
                        P.op('act', lambda e, h=h, bi=bi: e.activation(out=acc[:, bi, h * 512:(h + 1) * 512], in_=ps[h][:], func=AF.Identity),
                             reads=[t_ps[h]], writes=[t_acc])
                ngrp = (nb + 3) // 4
                for ex in range(NE):
                    wi = wcount % 2; wcount += 1
                    P.op('pool', lambda e, wi=wi, ex=ex: e.dma_start(out=wu[wi][:], in_=D[p + 'w_up'][ex].rearrange("(k p) n -> p k n", p=128)),
                         writes=[t_wu[wi]], dma=True)
                    P.op('pool', lambda e, wi=wi, ex=ex: e.dma_start(out=wd[wi][:], in_=D[p + 'w_down'][ex].rearrange("(k p) n -> p k n", p=128)),
                         writes=[t_wd[wi]], dma=True)
                    for g in range(ngrp):
                        gb = min(4, nb - g * 4)
                        N = gb * 128
                        t0 = g * 512
                        ai = g % 2
                        for fc in range(8):
                            ei = fc % 2
                            for part, pb in ((0, 4 + ei), (1, 6 + ei)):
                                def umm(e, part=part, pb=pb, fc=fc, wi=wi, N=N, t0=t0):
                                    c0 = part * DM + fc * 128
                                    for k in range(8):
                                        ins = e.matmul(ps[pb][:, 0:N], wu[wi][:, k, c0:c0 + 128], h2T[:, k, t0:t0 + N],
                                                       start=(k == 0), stop=(k == 7))
                                    return ins
                                P.op('pe', umm, reads=[t_wu[wi], t_h2T], writes=[t_ps[pb]])
                            pg, pl = ps[4 + ei], ps[6 + ei]
                            P.op('dve', lambda e, pg=pg, ei=ei, fc=fc, ex=ex, N=N: e.tensor_scalar(
                                out=eg[ei][:, 0:N], in0=pg[:, 0:N], scalar1=bup[:, ex, fc:fc + 1], scalar2=7.0,
                                op0=ALU.add, op1=ALU.min), reads=[t_ps[4 + ei], t_c], writes=[t_eg[ei]])
                            P.op('act', lambda e, ei=ei, N=N: e.activation(out=es[ei][:, 0:N], in_=eg[ei][:, 0:N], func=AF.Sigmoid, scale=1.702),
                                 reads=[t_eg[ei]], writes=[t_es[ei]])
                            P.op('dve', lambda e, pl=pl, ei=ei, fc=fc, ex=ex, N=N: e.tensor_scalar(
                                out=el[ei][:, 0:N], in0=pl[:, 0:N], scalar1=bup[:, ex, 8 + fc:9 + fc], scalar2=7.0,
                                op0=ALU.add, op1=ALU.min), reads=[t_ps[6 + ei], t_c], writes=[t_el[ei]])
                            P.op('pool', lambda e, ei=ei, N=N: e.tensor_scalar(
                                out=el[ei][:, 0:N], in0=el[ei][:, 0:N], scalar1=-7.0, scalar2=1.0,
                                op0=ALU.max, op1=ALU.add), reads=[t_el[ei]], writes=[t_el[ei]])
                            P.op('pool', lambda e, ei=ei, N=N: e.tensor_tensor(out=es[ei][:, 0:N], in0=es[ei][:, 0:N], in1=eg[ei][:, 0:N], op=ALU.mult),
                                 reads=[t_eg[ei], t_es[ei]], writes=[t_es[ei]])
                            P.op('pool', lambda e, ei=ei, ai=ai, fc=fc, N=N: e.tensor_tensor(
                                out=actT[ai][:, fc, 0:N], in0=es[ei][:, 0:N], in1=el[ei][:, 0:N], op=ALU.mult),
                                reads=[t_es[ei], t_el[ei]], writes=[t_act[ai]])
                        for b4 in range(gb):
                            bi = g * 4 + b4
                            for h in range(2):
                                pb = (bi * 2 + h) % 4

                                def dmm(e, h=h, pb=pb, b4=b4, ai=ai, wi=wi):
                                    for fc in range(8):
                                        ins = e.matmul(ps[pb][:], actT[ai][:, fc, b4 * 128:(b4 + 1) * 128],
                                                       wd[wi][:, fc, h * 512:(h + 1) * 512], start=(fc == 0), stop=(fc == 7))
                                    return ins
                                P.op('pe', dmm, reads=[t_act[ai], t_wd[wi]], writes=[t_ps[pb]])
                                P.op('dve', lambda e, h=h, pb=pb, bi=bi, ex=ex: e.scalar_tensor_tensor(
                                    out=acc[:, bi, h * 512:(h + 1) * 512], in0=ps[pb][:], scalar=gates[:, bi, ex:ex + 1],
                                    in1=acc[:, bi, h * 512:(h + 1) * 512], op0=ALU.mult, op1=ALU.add),
                                    reads=[t_ps[pb], t_gates, t_acc], writes=[t_acc])
                for bi in range(nb):
                    r0 = r_begin + (sb0 + bi) * 128
                    v = 1 if (r0 < self.TC) else 0
                    x_t = xt[bi % 2]; tx = t_xt[bi % 2]
                    P.op('sp', lambda e, x_t=x_t, r0=r0: e.dma_start(out=x_t[:], in_=self.rows(src, r0)), writes=[tx], dma=True)
                    P.op('pool', lambda e, bi=bi, v=v: e.tensor_tensor(out=acc[:, bi, :], in0=acc[:, bi, :], in1=self.gbc[v][:], op=ALU.mult),
                         reads=[t_acc, self.t_gbc], writes=[t_acc])
                    P.op('dve', lambda e, bi=bi, x_t=x_t: e.tensor_tensor(out=x_t[:], in0=x_t[:], in1=acc[:, bi, :], op=ALU.add),
                         reads=[t_acc, tx], writes=[tx])
                    if final:
                        P.op('act', lambda e, x_t=x_t: e.activation(out=junk[:], in_=x_t[:], func=AF.Square, accum_out=small[:, 8:9]),
                             reads=[tx], writes=[t_junk, t_small])
                        P.op('act', lambda e: e.activation(out=small[:, 9:10], in_=small[:, 8:9], func=AF.Sqrt, scale=1.0 / DM, bias=self.epsc[:, 0:1]),
                             reads=[t_small], writes=[t_small])
                        P.op('dve', lambda e: e.reciprocal(out=small[:, 10:11], in_=small[:, 9:10]), reads=[t_small], writes=[t_small])
                        P.op('dve', lambda e, x_t=x_t: e.scalar_tensor_tensor(out=x_t[:], in0=x_t[:], scalar=small[:, 10:11], in1=fw[:],
                                                                              op0=ALU.mult, op1=ALU.mult), reads=[tx, t_small, t_c], writes=[tx])
                        dst_ap = self.out[r0 - self.TC:r0 - self.TC + 128, :]
                    else:
                        dst_ap = self.D[dst][r0:r0 + 128, :]
                    P.op('sp', lambda e, x_t=x_t, dst_ap=dst_ap: e.dma_start(out=dst_ap, in_=x_t[:]), reads=[tx], writes=[t_out], dma=True)
            P.barrier()
            P.emit()


def col(v, n=128):
    return np.ascontiguousarray(np.asarray(v, np.float32).reshape(-1, n).T)


def rep(v, n=128):
    return np.ascontiguousarray(np.broadcast_to(np.asarray(v, np.float32)[None, :], (n, np.asarray(v).shape[0])))


def make_inputs(inp, b, T, TC):
    m = {}
    m['x'] = np.ascontiguousarray(inp['x'][b, :T])
    m['ctx'] = np.ascontiguousarray(inp['ctx'][b, :max(TC, 128)])
    cv = np.stack([col(inp['c'][b]), col(inp['c_ctx'])], axis=-1)
    m['cvec'] = np.ascontiguousarray(cv)
    m['ident'] = np.eye(128, dtype=np.float32)
    m['ones'] = np.ones((128, 128), np.float32)
    for l in range(2):
        p = 'l%d_' % l
        m[p + 'ada_w'] = inp[p + 'ada_w']
        m[p + 'ada_bT'] = col(inp[p + 'ada_b'])
        m[p + 'nmix'] = col(inp[p + 'norm_mix_w'])
        m[p + 'nffn'] = col(inp[p + 'norm_ffn_w'])
        m[p + 'router_w'] = inp[p + 'router_w']
        m[p + 'router_b'] = rep(inp[p + 'router_b'])
        m[p + 'w_up'] = inp[p + 'w_up']
        m[p + 'b_upT'] = np.ascontiguousarray(np.asarray(inp[p + 'b_up']).reshape(NE, 16, 128).transpose(2, 0, 1))
        m[p + 'w_down'] = inp[p + 'w_down']
        m[p + 'b_down'] = inp[p + 'b_down']
    m['final_w'] = rep(inp['final_norm_w'])
    m['l0_w_in'] = inp['l0_w_in']; m['l0_w_out'] = inp['l0_w_out']
    m['l0_w2'] = np.ascontiguousarray(np.stack([inp['l0_gla_w2_f'], inp['l0_gla_w2_b']], axis=1))
    m['l0_b2'] = np.ascontiguousarray(np.stack([col(inp['l0_gla_b_f']), col(inp['l0_gla_b_b'])], axis=1))
    m['l0_lb'] = np.ascontiguousarray(np.asarray(inp['hgrn_lb_logits'], np.float32).reshape(2, 4, 128).transpose(2, 0, 1))
    m['l0_normw'] = rep(np.concatenate([np.tile(inp['l0_gla_norm_w'], 4), np.tile(inp['l0_hgrn_norm_w'], 4)]))
    m['l1_w_in'] = inp['l1_w_in']; m['l1_w_out'] = inp['l1_w_out']
    m['l1_convw'] = np.ascontiguousarray(np.broadcast_to(np.asarray(inp['l1_conv_w'], np.float32)[None], (128, 5, 1536)))
    m['l1_pvec'] = rep(np.concatenate([inp['l1_a_log_f'], inp['l1_dt_bias_f'], inp['l1_a_log_b'], inp['l1_dt_bias_b']]))
    m['l1_dnw'] = rep(np.tile(inp['l1_dn_norm_w'], 4)); m['l1_sinks'] = rep(inp['l1_sinks'])
    m['rope'] = rope_table(T)
    t_ = np.arange(128)
    m['rmask'] = np.ascontiguousarray(np.broadcast_to(np.tile((t_ % 64 != 0).astype(np.float32), 6)[None, :], (128, 768)))
    same = (t_[:, None] // 64) == (t_[None, :] // 64)
    m['mask_f'] = (same & (t_[:, None] <= t_[None, :])).astype(np.float32)
    m['mask_r'] = (same & (t_[:, None] >= t_[None, :])).astype(np.float32)
    m['mask_fs'] = (same & (t_[:, None] < t_[None, :])).astype(np.float32)
    m['mask_rs'] = (same & (t_[:, None] > t_[None, :])).astype(np.float32)
    m['same'] = same.astype(np.float32)
    m['csel'] = np.stack([(t_ < 64), (t_ >= 64)], axis=1).astype(np.float32)
    m['mb_prev'] = np.where(t_[None, :] >= t_[:, None], 0.0, NEG).astype(np.float32)
    m['mb_next'] = np.where(t_[None, :] <= t_[:, None], 0.0, NEG).astype(np.float32)
    return m


def rope_table(T):
    rows = T // 64
    row = np.repeat(np.arange(rows, dtype=np.float32), 64)
    colp = np.tile(np.arange(64, dtype=np.float32), rows)
    inv = (10000.0 ** (-np.arange(16, dtype=np.float32) / 16)).astype(np.float32)
    ang = np.concatenate([row[:, None] * inv, colp[:, None] * inv], axis=-1).astype(np.float32)
    return np.ascontiguousarray(np.concatenate([np.cos(ang), np.sin(ang)], axis=-1).astype(np.float32))


_CACHE = {}


def kernel(**inputs):
    inp = {k: np.asarray(v) for k, v in inputs.items()}
    B, T, _ = inp['x'].shape
    TC = inp['ctx'].shape[1]
    key = (T, TC)
    import os
    ph = os.environ.get('KPHASES')
    bld = Builder(T, TC, phases=tuple(ph.split(','))) if ph else Builder(T, TC)
    nc = bld.build()
    in_maps = []
    for b in range(B):
        m = make_inputs(inp, b, T, TC)
        in_maps.append({k: m[k] for k in bld.in_names})
    res = run_bass_kernel_spmd(nc, in_maps, core_ids=list(range(B)))
    return np.stack([res.results[b]['out'] for b in range(B)], axis=0)


def phase_l0mix(self, src, dst):
    nc, P, D = self.nc, self.P, self.D
    TC, T = self.TC, self.T
    with ExitStack() as st:
        sb = lambda name, shape, d=F32: st.enter_context(nc.sbuf_tensor(name, shape, d))
        ps = [st.enter_context(nc.psum_tensor('pq%d' % i, [128, 512], F32)) for i in range(8)]
        ps4b = ps[4][:].bitcast(BF16)
        self.epsc = sb('epsc0', [128, 1])
        onec = sb('onec', [128, 1])
        win = sb('win', [128, 8, 4128], BF16)
        wout = sb('wout', [128, 8, DM], BF16)
        w2 = sb('w2', [16, 2, 256], BF16)
        b2 = sb('b2', [128, 2, 2]); lbl = sb('lbl', [128, 2, 4]); lbc = sb('lbc', [128, 4]); omlb = sb('omlb', [128, 4])
        normw = sb('normw', [128, DM]); rmask = sb('rmask_t', [128, 768])
        maskf = sb('maskf', [128, 128]); maskr = sb('maskr', [128, 128])
        xt = [sb('mxt%d' % i, [128, DM]) for i in range(2)]
        xn = sb('mxn', [128, DM]); junk = sb('mjunk', [128, DM], BF16); small = sb('msmall', [128, 32])
        hT = sb('hT', [128, 8, 128], BF16)
        arT = sb('arT', [16, 128], BF16)
        LF = sb('LF', [128, 6, 128]); bb = sb('bb', [128, 6, 128]); EA = sb('EA', [128, 6, 128]); EB = sb('EB', [128, 6, 128])
        E = sb('E', [128, 6, 2]); kH = sb('kH', [128, 4, 128]); sg = sb('sg', [128, 4, 128])
        qT = sb('qT', [128, 6, 128], BF16); kT = sb('kT', [128, 6, 128], BF16)
        ktok = sb('ktok', [128, 768], BF16); vtok = sb('vtok', [128, DM], BF16)
        SCm = sb('SCm', [128, 8, 128], BF16)
        S = sb('S', [128, 6, 128]); Tmp = sb('Tmp', [128, 6, 128])
        SB = [sb('SB%d' % i, [128, 6, 128], BF16) for i in range(4)]
        Oc = sb('Oc', [128, DM]); ofl = sb('ofl', [128, DM]); og = sb('og', [128, DM])
        yb = sb('yb', [128, DM], BF16); yT = sb('yT', [128, 8, 128], BF16)
        tk = {n: Tok() for n in ('c', 'x0', 'x1', 'xn', 'junk', 'small', 'hT', 'arT', 'LF', 'bb', 'EA', 'EB', 'E', 'kH', 'sg',
                                 'qT', 'kT', 'ktok', 'vtok', 'SCm', 'S', 'Tmp', 'SB0', 'SB1', 'SB2', 'SB3', 'Oc', 'ofl', 'og', 'yb', 'yT', 'out')}
        tp = [Tok() for _ in range(8)]
        c_ = [tk['c']]
        P.op('pool', lambda e: e.memset(self.epsc[:], EPS), writes=c_)
        P.op('pool', lambda e: e.memset(onec[:], 1.0), writes=c_)
        for (a, b_) in ((0, 2048), (2048, 4096), (4096, 4128)):
            P.op('pool', lambda e, a=a, b_=b_: e.dma_start(out=win[:, :, a:b_], in_=D['l0_w_in'][:, a:b_].rearrange("(k p) n -> p k n", p=128)),
                 writes=c_, dma=True)
        P.op('pool', lambda e: e.dma_start(out=wout[:], in_=D['l0_w_out'].rearrange("(k p) n -> p k n", p=128)), writes=c_, dma=True)
        P.op('pool', lambda e: e.dma_start(out=w2[:], in_=D['l0_w2']), writes=c_, dma=True)
        for nm, t_ in (('l0_b2', b2), ('l0_lb', lbl), ('l0_normw', normw), ('rmask', rmask), ('mask_f', maskf), ('mask_r', maskr)):
            P.op('sp', lambda e, nm=nm, t_=t_: e.dma_start(out=t_[:], in_=D[nm]), writes=c_, dma=True)
        P.op('dve', lambda e: e.tensor_scalar(out=b2[:], in0=b2[:], scalar1=-1.0, scalar2=None, op0=ALU.mult), reads=c_, writes=c_)
        P.op('dve', lambda e: e.tensor_tensor(out=lbc[:], in0=lbl[:, 0, :], in1=lbl[:, 1, :], op=ALU.subtract), reads=c_, writes=c_)
        P.op('act', lambda e: e.activation(out=omlb[:], in_=lbc[:], func=AF.Sigmoid, scale=-1.0), reads=c_, writes=c_)
        P.op('act', lambda e: e.activation(out=lbc[:], in_=lbc[:], func=AF.Sigmoid), reads=c_, writes=c_)
        self.gate_bcast(Oc[:].rearrange("p (k t) -> p k t", k=8), ps[0:2], 0, 16, (0, 1) if TC else (0,))
        P.barrier()
        A1 = self.Acol[0][0]; modT = self.modT[0]

        def mmgroup(pb, outs):
            def f(e):
                for out_ap, pairs in outs:
                    n = len(pairs)
                    for i, (l_, r_) in enumerate(pairs):
                        ins = e.matmul(out_ap, l_, r_, start=(i == 0), stop=(i == n - 1))
                return ins
            return f

        import os
        STOP = int(os.environ.get('L0STOP', '99'))

        def block(r0, is_ctx, d, first, bidx):
            v = 1 if is_ctx else 0
            x_t = xt[bidx % 2]; tx = tk['x%d' % (bidx % 2)]
            self.norm_xn(self.rows(src, r0), x_t, tx, xn, tk['xn'], small, tk['small'], junk, tk['junk'])
            for h in range(2):
                P.op('pe', mmgroup(None, []) if False else (lambda e, h=h: [e.transpose(ps[h][:, j * 128:(j + 1) * 128], xn[:, (h * 4 + j) * 128:(h * 4 + j + 1) * 128], self.ident[:]) for j in range(4)][-1]),
                     reads=[tk['xn'], self.t_const], writes=[tp[h]])
                for j in range(4):
                    k = h * 4 + j
                    P.op('act', lambda e, h=h, j=j, k=k: e.activation(out=hT[:, k, :], in_=ps[h][:, j * 128:(j + 1) * 128], func=AF.Identity,
                                                                      scale=A1[:, k, v:v + 1], bias=modT[:, k, v:v + 1]),
                         reads=[tp[h], self.t_mod], writes=[tk['hT']])
            rh = [tk['hT'], tk['c']]
            wcol = lambda c0, n=128: [(win[:, k, c0:c0 + n], hT[:, k, :]) for k in range(8)]
            c_ar = 1024 + d * 16
            P.op('pe', mmgroup(0, [(ps[0][0:16, 0:128], [(win[:, k, c_ar:c_ar + 16], hT[:, k, :]) for k in range(8)])]), reads=rh, writes=[tp[0]])
            P.op('act', lambda e: e.activation(out=arT[:], in_=ps[0][0:16, 0:128], func=AF.Identity), reads=[tp[0]], writes=[tk['arT']])
            P.op('pe', mmgroup(0, [(ps[0][:, 128 + c * 128:256 + c * 128], [(w2[:, d, c * 128:(c + 1) * 128], arT[:])]) for c in range(2)]),
                 reads=[tk['arT'], tk['c']], writes=[tp[0]])
            if STOP < 3:
                return
            c_bz = 2080 + d * 512
            P.op('pe', mmgroup(1, [(ps[1][:, c * 128:(c + 1) * 128], wcol(c_bz + c * 128)) for c in range(4)]), reads=rh, writes=[tp[1]])
            if STOP < 4:
                return
            for c in range(2):
                P.op('act', lambda e, c=c: e.activation(out=LF[:, c, :], in_=ps[0][:, 128 + c * 128:256 + c * 128], func=AF.Exp,
                                                        scale=-1.0, bias=b2[:, d, c:c + 1]), reads=[tp[0], tk['c']], writes=[tk['LF']])
            P.op('act', lambda e: e.activation(out=LF[:, 0:2, :], in_=LF[:, 0:2, :], func=AF.Ln, bias=onec[:, 0:1], scale=1.0),
                 reads=[tk['c']], writes=[tk['LF']])
            P.op('act', lambda e: e.activation(out=sg[:], in_=ps[1][:].rearrange("p (c t) -> p c t", c=4), func=AF.Sigmoid),
                 reads=[tp[1]], writes=[tk['sg']])
            P.op('dve', lambda e: e.tensor_scalar(out=LF[:, 0:2, :], in0=LF[:, 0:2, :], scalar1=-1.0 / 16.0, scalar2=None, op0=ALU.mult),
                 writes=[tk['LF']])
            P.op('dve', lambda e: e.tensor_tensor(out=sg[:], in0=sg[:], in1=omlb[:].unsqueeze(2).to_broadcast([128, 4, 128]), op=ALU.mult),
                 reads=[tk['c']], writes=[tk['sg']])
            P.op('dve', lambda e: e.tensor_tensor(out=sg[:], in0=sg[:], in1=lbc[:].unsqueeze(2).to_broadcast([128, 4, 128]), op=ALU.add),
                 reads=[tk['c']], writes=[tk['sg']])
            P.op('act', lambda e: e.activation(out=LF[:, 2:6, :], in_=sg[:], func=AF.Ln), reads=[tk['sg']], writes=[tk['LF']])
            P.op('dve', lambda e: e.tensor_scalar(out=kH[:], in0=sg[:], scalar1=-1.0, scalar2=1.0, op0=ALU.mult, op1=ALU.add),
                 reads=[tk['sg']], writes=[tk['kH']])
            if STOP < 5:
                return
            LF2 = LF[:].rearrange("p c t -> p (c t)"); bb2 = bb[:].rearrange("p c t -> p (c t)")
            P.op('dve', lambda e: e.tensor_tensor_scan(out=bb2, data0=rmask[:], data1=LF2, initial=0.0, op0=ALU.mult, op1=ALU.add),
                 reads=[tk['LF'], tk['c']], writes=[tk['bb']])
            tot = bb[:].rearrange("p c (a t) -> p c a t", a=2)[:, :, :, 63]
            P.op('act', lambda e: e.activation(out=E[:], in_=tot, func=AF.Exp), reads=[tk['bb']], writes=[tk['E']])
            if d == 1:
                P.op('dve', lambda e: e.tensor_tensor(out=bb[:], in0=bb[:], in1=LF[:], op=ALU.subtract), reads=[tk['LF']], writes=[tk['bb']])
            sa, sb_ = (1.0, -1.0) if d == 0 else (-1.0, 1.0)
            P.op('act', lambda e: e.activation(out=EA[:], in_=bb[:], func=AF.Exp, scale=sa), reads=[tk['bb']], writes=[tk['EA']])
            P.op('act', lambda e: e.activation(out=EB[:], in_=bb[:], func=AF.Exp, scale=sb_), reads=[tk['bb']], writes=[tk['EB']])
            if STOP < 6:
                return
            P.op('pe', mmgroup(2, [(ps[2][:, 0:128], wcol(0)), (ps[2][:, 128:256], wcol(128)),
                                   (ps[2][:, 256:384], wcol(1568)), (ps[2][:, 384:512], wcol(1696))]), reads=rh, writes=[tp[2]])
            P.op('pe', mmgroup(3, [(ps[3][:, 0:128], wcol(1824)), (ps[3][:, 128:256], wcol(1952)),
                                   (ps[3][:, 256:384], wcol(256)), (ps[3][:, 384:512], wcol(384))]), reads=rh, writes=[tp[3]])
            v3 = lambda ap, n: ap.rearrange("p (c t) -> p c t", c=n)
            P.op('dve', lambda e: e.scalar_tensor_tensor(out=qT[:, 0:2, :], in0=v3(ps[2][:, 0:256], 2), scalar=0.125, in1=EA[:, 0:2, :],
                                                         op0=ALU.mult, op1=ALU.mult), reads=[tp[2], tk['EA']], writes=[tk['qT']])
            P.op('dve', lambda e: e.tensor_tensor(out=qT[:, 2:4, :], in0=v3(ps[2][:, 256:512], 2), in1=EA[:, 2:4, :], op=ALU.mult),
                 reads=[tp[2], tk['EA']], writes=[tk['qT']])
            P.op('dve', lambda e: e.tensor_tensor(out=qT[:, 4:6, :], in0=v3(ps[3][:, 0:256], 2), in1=EA[:, 4:6, :], op=ALU.mult),
                 reads=[tp[3], tk['EA']], writes=[tk['qT']])
            P.op('dve', lambda e: e.tensor_tensor(out=kT[:, 0:2, :], in0=v3(ps[3][:, 256:512], 2), in1=EB[:, 0:2, :], op=ALU.mult),
                 reads=[tp[3], tk['EB']], writes=[tk['kT']])
            P.op('pool', lambda e: e.tensor_tensor(out=kT[:, 2:6, :], in0=kH[:], in1=EB[:, 2:6, :], op=ALU.mult),
                 reads=[tk['kH'], tk['EB']], writes=[tk['kT']])
            if STOP < 7:
                return
            P.op('pe', lambda e: [e.transpose(ps4b[:, c * 128:(c + 1) * 128], kT[:, c, :], self.identb[:]) for c in range(6)][-1],
                 reads=[tk['kT'], self.t_const], writes=[tp[4]])
            P.op('act', lambda e: e.activation(out=ktok[:], in_=ps4b[:, 0:768], func=AF.Identity), reads=[tp[4]], writes=[tk['ktok']])
            if STOP < 8:
                return
            for i, c0 in enumerate((512, 3104)):
                P.op('pe', mmgroup(5, [(ps[5][:], [(hT[:, k, :], win[:, k, c0:c0 + 512]) for k in range(8)])]), reads=rh, writes=[tp[5]])
                P.op('act', lambda e, i=i: e.activation(out=vtok[:, i * 512:(i + 1) * 512], in_=ps[5][:], func=AF.Identity),
                     reads=[tp[5]], writes=[tk['vtok']])
            if STOP < 9:
                return
            def rows_of(h):
                if h < 4:
                    return slice((h % 2) * 64, (h % 2) * 64 + 64), h // 2
                return slice(0, 128), h - 2
            bankheads = ((0, 2, 4, 5), (1, 3, 6, 7))
            scidx = {h: half * 4 + hh for half in range(2) for hh, h in enumerate(bankheads[half])}
            for half in range(2):
                outs = []
                for hh in range(4):
                    h = bankheads[half][hh]
                    rs, bk = rows_of(h)
                    outs.append((ps[6 + half][:, hh * 128:(hh + 1) * 128], [(kT[rs, bk, :], qT[rs, bk, :])]))
                P.op('pe', mmgroup(6 + half, outs), reads=[tk['kT'], tk['qT']], writes=[tp[6 + half]])
                mk = maskf if d == 0 else maskr
                P.op('dve', lambda e, half=half, mk=mk: e.tensor_tensor(
                    out=SCm[:, half * 4:(half + 1) * 4, :], in0=v3(ps[6 + half][:], 4), in1=mk[:].unsqueeze(1).to_broadcast([128, 4, 128]),
                    op=ALU.mult), reads=[tp[6 + half], tk['c']], writes=[tk['SCm']])
            if STOP < 10:
                return
            order = (0, 1) if d == 0 else (1, 0)
            pbank = {order[0]: (0, 1), order[1]: (2, 3)}
            for c in order:
                pa, pb_ = pbank[c]
                cs = slice(c * 64, c * 64 + 64)
                outsA, outsB = [], []
                for h in range(8):
                    rs, bk = rows_of(h)
                    if h < 4:
                        l_ = ktok[cs, bk * 128 + (h % 2) * 64: bk * 128 + (h % 2) * 64 + 64]
                    else:
                        l_ = ktok[cs, bk * 128:(bk + 1) * 128]
                    r_ = vtok[cs, h * 128:(h + 1) * 128]
                    if bk < 4:
                        outsA.append((ps[pa][rs, bk * 128:(bk + 1) * 128], [(l_, r_)]))
                    else:
                        outsB.append((ps[pb_][rs, (bk - 4) * 128:(bk - 3) * 128], [(l_, r_)]))
                P.op('pe', mmgroup(pa, outsA), reads=[tk['ktok'], tk['vtok']], writes=[tp[pa]])
                P.op('pe', mmgroup(pb_, outsB), reads=[tk['ktok'], tk['vtok']], writes=[tp[pb_]])
            if STOP < 11:
                return
            st_i = 2 * (bidx % 2); mid_i = st_i + 1; end_i = 2 * ((bidx + 1) % 2)
            if first:
                P.op('pool', lambda e: e.memset(S[:], 0.0), writes=[tk['S']])
                P.op('pool', lambda e: e.memset(SB[st_i][:], 0.0), writes=[tk['SB%d' % st_i]])
            for i, c in enumerate(order):
                pa, pb_ = pbank[c]
                Ebc = E[:, :, c:c + 1].to_broadcast([128, 6, 128])
                PA = v3(ps[pa][:], 4); PB = v3(ps[pb_][:, 0:256], 2)
                if d == 0:
                    wi_ = mid_i if i == 0 else end_i
                    P.op('dve', lambda e, PA=PA: e.tensor_tensor(out=Tmp[:, 0:4, :], in0=PA, in1=S[:, 0:4, :], op=ALU.add),
                         reads=[tp[pa], tk['S']], writes=[tk['Tmp']])
                    P.op('dve', lambda e, PB=PB: e.tensor_tensor(out=Tmp[:, 4:6, :], in0=PB, in1=S[:, 4:6, :], op=ALU.add),
                         reads=[tp[pb_], tk['S']], writes=[tk['Tmp']])
                    P.op('dve', lambda e, Ebc=Ebc: e.tensor_tensor(out=S[:], in0=Tmp[:], in1=Ebc, op=ALU.mult),
                         reads=[tk['Tmp'], tk['E']], writes=[tk['S']])
                    P.op('act', lambda e, wi_=wi_: e.activation(out=SB[wi_][:], in_=S[:], func=AF.Identity),
                         reads=[tk['S']], writes=[tk['SB%d' % wi_]])
                else:
                    wi_ = st_i if i == 0 else mid_i
                    P.op('dve', lambda e, Ebc=Ebc: e.tensor_tensor(out=S[:], in0=S[:], in1=Ebc, op=ALU.mult),
                         reads=[tk['E']], writes=[tk['S']])
                    P.op('act', lambda e, wi_=wi_: e.activation(out=SB[wi_][:], in_=S[:], func=AF.Identity),
                         reads=[tk['S']], writes=[tk['SB%d' % wi_]])
                    P.op('dve', lambda e, PA=PA: e.tensor_tensor(out=S[:, 0:4, :], in0=PA, in1=S[:, 0:4, :], op=ALU.add),
                         reads=[tp[pa]], writes=[tk['S']])
                    P.op('dve', lambda e, PB=PB: e.tensor_tensor(out=S[:, 4:6, :], in0=PB, in1=S[:, 4:6, :], op=ALU.add),
                         reads=[tp[pb_]], writes=[tk['S']])
            if STOP < 12:
                return
            for half in range(2):
                def omm(e, half=half):
                    for hh in range(4):
                        h = half * 4 + hh
                        rs, bk = rows_of(h)
                        ob = ps[4 + half]
                        e.matmul(ob[:, hh * 128:(hh + 1) * 128], SCm[:, scidx[h], :], vtok[:, h * 128:(h + 1) * 128], start=True, stop=False)
                        for i, c in enumerate(order):
                            snap = SB[st_i] if i == 0 else SB[mid_i]
                            ins = e.matmul(ob[c * 64:(c + 1) * 64, hh * 128:(hh + 1) * 128], qT[rs, bk, c * 64:(c + 1) * 64],
                                           snap[rs, bk, :], start=False, stop=True)
                    return ins
                P.op('pe', omm, reads=[tk['SCm'], tk['vtok'], tk['qT'], tk['SB%d' % st_i], tk['SB%d' % mid_i]], writes=[tp[4 + half]])
            if STOP < 13:
                return
            if d == 0:
                for half in range(2):
                    P.op('act', lambda e, half=half: e.activation(out=Oc[:, half * 512:(half + 1) * 512], in_=ps[4 + half][:], func=AF.Identity),
                         reads=[tp[4 + half]], writes=[tk['Oc']])
                P.op('sp', lambda e: e.dma_start(out=D['OF'][r0:r0 + 128, :], in_=Oc[:]), reads=[tk['Oc']], writes=[tk['out']], dma=True)
                return
            P.op('sp', lambda e: e.dma_start(out=ofl[:], in_=D['OF'][r0:r0 + 128, :]), writes=[tk['ofl']], dma=True)
            for half in range(2):
                P.op('dve', lambda e, half=half: e.tensor_tensor(out=Oc[:, half * 512:(half + 1) * 512], in0=ps[4 + half][:],
                                                                 in1=ofl[:, half * 512:(half + 1) * 512], op=ALU.add),
                     reads=[tp[4 + half], tk['ofl']], writes=[tk['Oc']])
            for half, c0 in enumerate((1056, 3616)):
                P.op('pe', mmgroup(6 + half, [(ps[6 + half][:], [(hT[:, k, :], win[:, k, c0:c0 + 512]) for k in range(8)])]),
                     reads=rh, writes=[tp[6 + half]])
                P.op('act', lambda e, half=half: e.activation(out=og[:, half * 512:(half + 1) * 512], in_=ps[6 + half][:], func=AF.Silu),
                     reads=[tp[6 + half]], writes=[tk['og']])
            O3 = Oc[:].rearrange("p (h v) -> p h v", h=8)
            P.op('pool', lambda e: e.tensor_tensor(out=ofl[:], in0=Oc[:], in1=Oc[:], op=ALU.mult), reads=[tk['Oc']], writes=[tk['ofl']])
            P.op('dve', lambda e: e.tensor_reduce(out=small[:, 8:16], in_=ofl[:].rearrange("p (h v) -> p h v", h=8), axis=AX.X, op=ALU.add),
                 reads=[tk['ofl']], writes=[tk['small']])
            P.op('act', lambda e: e.activation(out=small[:, 16:24], in_=small[:, 8:16], func=AF.Sqrt, scale=1.0 / 128, bias=self.epsc[:, 0:1]),
                 reads=[tk['small']], writes=[tk['small']])
            P.op('dve', lambda e: e.reciprocal(out=small[:, 24:32], in_=small[:, 16:24]), reads=[tk['small']], writes=[tk['small']])
            P.op('dve', lambda e: e.tensor_tensor(out=O3, in0=O3, in1=small[:, 24:32].unsqueeze(2).to_broadcast([128, 8, 128]), op=ALU.mult),
                 reads=[tk['small']], writes=[tk['Oc']])
            P.op('pool', lambda e: e.tensor_tensor(out=Oc[:], in0=Oc[:], in1=normw[:], op=ALU.mult), reads=[tk['c']], writes=[tk['Oc']])
            P.op('dve', lambda e: e.tensor_tensor(out=yb[:], in0=Oc[:], in1=og[:], op=ALU.mult), reads=[tk['Oc'], tk['og']], writes=[tk['yb']])
            P.op('pe', lambda e: [e.transpose(ps4b[:, k * 128:(k + 1) * 128], yb[:, k * 128:(k + 1) * 128], self.identb[:]) for k in range(8)][-1],
                 reads=[tk['yb'], self.t_const], writes=[tp[4]])
            P.op('act', lambda e: e.activation(out=yT[:].rearrange("p k t -> p (k t)"), in_=ps4b[:, 0:1024], func=AF.Identity),
                 reads=[tp[4]], writes=[tk['yT']])
            for half in range(2):
                P.op('pe', mmgroup(6 + half, [(ps[6 + half][:], [(yT[:, k, :], wout[:, k, half * 512:(half + 1) * 512]) for k in range(8)])]),
                     reads=[tk['yT'], tk['c']], writes=[tp[6 + half]])
                P.op('dve', lambda e, half=half: e.tensor_tensor(out=Oc[:, half * 512:(half + 1) * 512], in0=ps[6 + half][:],
                                                                 in1=self.gbc[v][:, half * 512:(half + 1) * 512], op=ALU.mult),
                     reads=[tp[6 + half], self.t_gbc], writes=[tk['Oc']])
            P.op('pool', lambda e: e.tensor_tensor(out=x_t[:], in0=x_t[:], in1=Oc[:], op=ALU.add), reads=[tk['Oc']], writes=[tx])
            P.op('sp', lambda e: e.dma_start(out=D[dst][r0:r0 + 128, :], in_=x_t[:]), reads=[tx], writes=[tk['out']], dma=True)

        NCB, NB = self.NCB, self.NB
        for d in range(2):
            seq = [(i * 128, True) for i in range(NCB)] + [(TC + i * 128, False) for i in range(NB)]
            if d == 1:
                seq = [(i * 128, True) for i in reversed(range(NCB))] + [(TC + i * 128, False) for i in reversed(range(NB))]
            for n, (r0, is_ctx) in enumerate(seq):
                block(r0, is_ctx, d, n == 0, n)
            P.barrier()
        P.emit()


Builder.phase_l0mix = phase_l0mix


def phase_final(self, src):
    nc, P, D = self.nc, self.P, self.D
    with ExitStack() as st:
        sb = lambda name, shape, d=F32: st.enter_context(nc.sbuf_tensor(name, shape, d))
        self.epsc = sb('epscf', [128, 1])
        xt = [sb('fxt%d' % i, [128, DM]) for i in range(2)]
        junk = sb('fjunk', [128, DM], BF16); small = sb('fsmall', [128, 8]); fw = sb('ffw', [128, DM])
        tc_, tj, ts, to = Tok(), Tok(), Tok(), Tok()
        txs = [Tok(), Tok()]
        P.op('pool', lambda e: e.memset(self.epsc[:], EPS), writes=[tc_])
        P.op('sp', lambda e: e.dma_start(out=fw[:], in_=D['final_w']), writes=[tc_], dma=True)
        for bi in range(self.NB):
            r0 = self.TC + bi * 128
            x_t = xt[bi % 2]; tx = txs[bi % 2]
            self.norm_xn(self.rows(src, r0), x_t, tx, x_t, tx, small, ts, junk, tj)
            P.op('dve', lambda e, x_t=x_t: e.tensor_tensor(out=x_t[:], in0=x_t[:], in1=fw[:], op=ALU.mult), reads=[tc_], writes=[tx])
            P.op('sp', lambda e, x_t=x_t, r0=r0: e.dma_start(out=self.out[r0 - self.TC:r0 - self.TC + 128, :], in_=x_t[:]),
                 reads=[tx], writes=[to], dma=True)
        P.barrier()
        P.emit()


Builder.phase_final = phase_final


NEG = -30000.0


def phase_l1mix(self, src, dst):
    nc, P, D = self.nc, self.P, self.D
    TC, T, NCB, NB = self.TC, self.T, self.NCB, self.NB
    R = TC + T
    cxrow = lambda r: (2 + r) if r < TC else (r + 6)
    A1 = self.Acol[1][0]; modT = self.modT[1]

    def mmgroup(outs):
        def f(e):
            for out_ap, pairs in outs:
                n = len(pairs)
                for i, (l_, r_) in enumerate(pairs):
                    ins = e.matmul(out_ap, l_, r_, start=(i == 0), stop=(i == n - 1))
            return ins
        return f

    def trs(dsts_srcs, idt):
        def f(e):
            for o_, i_ in dsts_srcs:
                ins = e.transpose(o_, i_, idt)
            return ins
        return f

    with ExitStack() as st:
        sb = lambda name, shape, d=F32: st.enter_context(nc.sbuf_tensor(name, shape, d))
        ps = [st.enter_context(nc.psum_tensor('pr%d' % i, [128, 512], F32)) for i in range(8)]
        psb = [p_[:].bitcast(BF16) for p_ in ps]
        self.epsc = sb('epsc1', [128, 1]); onec = sb('onec1', [128, 1])
        win = sb('win1', [128, 8, 2832], BF16); wout = sb('wout1', [128, 8, DM], BF16)
        convw = sb('convw', [128, 5, 1536], BF16); pvec = sb('pvec', [128, 16]); dnw = sb('dnw', [128, 512]); sinks = sb('sinks', [128, 8])
        maskf = sb('maskf1', [128, 128]); maskr = sb('maskr1', [128, 128]); maskfs = sb('maskfs', [128, 128]); maskrs = sb('maskrs', [128, 128])
        same = sb('same_t', [128, 128]); csel = sb('csel_t', [128, 2]); mbp = sb('mbp', [128, 128]); mbn = sb('mbn', [128, 128])
        xt = [sb('lxt%d' % i, [128, DM]) for i in range(2)]
        xn = sb('lxn', [128, DM]); junk = sb('ljunk', [128, DM], BF16); small = sb('lsmall', [128, 64])
        hT = sb('lhT', [128, 8, 128], BF16)
        cxs = sb('cxs', [128, 1536]); cxl = [sb('cxl%d' % i, [128, 1536]) for i in range(2)]
        kv = sb('kv', [128, 256]); rope = sb('rope_t', [128, 64]); rt = sb('rt', [128, 8, 64]); kTs = sb('kTs', [128, 128])
        tk = {n: Tok() for n in ('c', 'x0', 'x1', 'xn', 'junk', 'small', 'hT', 'cxs', 'cxl0', 'cxl1', 'kv', 'rope', 'rt', 'kTs', 'out',
                                 'qkv', 'qkb', 'fT', 'g', 'gs', 'GB', 'Lts', 'Lst', 'A', 'AT', 'Q', 'QT', 'Rm', 'Rb', 'kbg', 'kd', 'bv',
                                 'u', 'wT', 'S', 'Sb0', 'Sb1', 'vn', 'Pst', 'egr', 'qg', 'Oc', 'ofl', 'og', 'yb', 'yT', 'q8', 'qT8',
                                 'kTl', 'vl', 'kcT', 'vc', 'ssb', 'pb', 'pT', 'att')}
        tp = [Tok() for _ in range(8)]
        c_ = [tk['c']]
        P.op('pool', lambda e: e.memset(self.epsc[:], EPS), writes=c_)
        P.op('pool', lambda e: e.memset(onec[:], 1.0), writes=c_)
        for (a, b_) in ((0, 2048), (2048, 2832)):
            P.op('pool', lambda e, a=a, b_=b_: e.dma_start(out=win[:, :, a:b_], in_=D['l1_w_in'][:, a:b_].rearrange("(k p) n -> p k n", p=128)),
                 writes=c_, dma=True)
        P.op('pool', lambda e: e.dma_start(out=wout[:], in_=D['l1_w_out'].rearrange("(k p) n -> p k n", p=128)), writes=c_, dma=True)
        P.op('pool', lambda e: e.dma_start(out=convw[:], in_=D['l1_convw']), writes=c_, dma=True)
        for nm, t_ in (('l1_pvec', pvec), ('l1_dnw', dnw), ('l1_sinks', sinks), ('mask_f', maskf), ('mask_r', maskr),
                       ('mask_fs', maskfs), ('mask_rs', maskrs), ('same', same), ('csel', csel), ('mb_prev', mbp), ('mb_next', mbn)):
            P.op('sp', lambda e, nm=nm, t_=t_: e.dma_start(out=t_[:], in_=D[nm]), writes=c_, dma=True)
        for c0 in (0, 8):
            P.op('act', lambda e, c0=c0: e.activation(out=pvec[:, c0:c0 + 4], in_=pvec[:, c0:c0 + 4], func=AF.Exp), reads=c_, writes=c_)
            P.op('dve', lambda e, c0=c0: e.tensor_scalar(out=pvec[:, c0:c0 + 4], in0=pvec[:, c0:c0 + 4], scalar1=-1.0, scalar2=None, op0=ALU.mult),
                 reads=c_, writes=c_)
        P.op('pool', lambda e: e.memset(cxs[:], 0.0), writes=[tk['cxs']])
        for r in (0, TC + 2, TC + 4, TC + T + 6):
            P.op('sp', lambda e, r=r: e.dma_start(out=D['CX'][r:r + 2, :], in_=cxs[0:2, :]), reads=[tk['cxs']], writes=[tk['out']], dma=True)
        self.gate_bcast(xn[:].rearrange("p (k t) -> p k t", k=8), ps[0:2], 1, 16, (0,))
        P.barrier()

        def norm_hT(r0, v, bidx):
            x_t = xt[bidx % 2]; tx = tk['x%d' % (bidx % 2)]
            self.norm_xn(self.rows(src, r0), x_t, tx, xn, tk['xn'], small, tk['small'], junk, tk['junk'])
            for h in range(2):
                P.op('pe', trs([(ps[h][:, j * 128:(j + 1) * 128], xn[:, (h * 4 + j) * 128:(h * 4 + j + 1) * 128]) for j in range(4)], self.ident[:]),
                     reads=[tk['xn'], self.t_const], writes=[tp[h]])
                for j in range(4):
                    k = h * 4 + j
                    P.op('act', lambda e, h=h, j=j, k=k: e.activation(out=hT[:, k, :], in_=ps[h][:, j * 128:(j + 1) * 128], func=AF.Identity,
                                                                      scale=A1[:, k, v:v + 1], bias=modT[:, k, v:v + 1]),
                         reads=[tp[h], self.t_mod], writes=[tk['hT']])
            return x_t, tx

        rh = [tk['hT'], tk['c']]
        tokmm = lambda c0, n: [(hT[:, k, :], win[:, k, c0:c0 + n]) for k in range(8)]

        def pre_block(r0, is_ctx, bidx):
            norm_hT(r0, 1 if is_ctx else 0, bidx)
            for j in range(3):
                P.op('pe', mmgroup([(ps[2 + j][:], tokmm(j * 512, 512))]), reads=rh, writes=[tp[2 + j]])
                P.op('act', lambda e, j=j: e.activation(out=cxs[:, j * 512:(j + 1) * 512], in_=ps[2 + j][:], func=AF.Identity),
                     reads=[tp[2 + j]], writes=[tk['cxs']])
            cr = cxrow(r0)
            P.op('sp', lambda e: e.dma_start(out=D['CX'][cr:cr + 128, :], in_=cxs[:]), reads=[tk['cxs']], writes=[tk['out']], dma=True)
            P.op('pe', mmgroup([(ps[5][:, 0:256], tokmm(2576, 256))]), reads=rh, writes=[tp[5]])
            P.op('act', lambda e: e.activation(out=kv[:], in_=ps[5][:, 0:256], func=AF.Identity), reads=[tp[5]], writes=[tk['kv']])
            if not is_ctx:
                t0 = r0 - TC
                P.op('sp', lambda e: e.dma_start(out=rope[:], in_=D['rope'][t0:t0 + 128, :]), writes=[tk['rope']], dma=True)
                self.apply_rope(kv[:, 0:128].rearrange("p (h f) -> p h f", h=2), 2, rope, rt, [tk['kv']], tk['rope'], tk['rt'])
            P.op('sp', lambda e: e.dma_start(out=D['VV'][r0:r0 + 128, :], in_=kv[:, 128:256]), reads=[tk['kv']], writes=[tk['out']], dma=True)
            P.op('pe', trs([(ps[6][:, 0:128], kv[:, 0:128])], self.ident[:]), reads=[tk['kv'], self.t_const], writes=[tp[6]])
            P.op('act', lambda e: e.activation(out=kTs[:], in_=ps[6][:, 0:128], func=AF.Identity), reads=[tp[6]], writes=[tk['kTs']])
            P.op('sp', lambda e: e.dma_start(out=D['KT'][:, r0:r0 + 128], in_=kTs[:]), reads=[tk['kTs']], writes=[tk['out']], dma=True)
            P.op('sp', lambda e: e.dma_start(out=D['KT2'][0:64, r0:r0 + 128], in_=kTs[64:128, :]), reads=[tk['kTs']], writes=[tk['out']], dma=True)
            P.op('sp', lambda e: e.dma_start(out=D['KT2'][64:128, r0:r0 + 128], in_=kTs[0:64, :]), reads=[tk['kTs']], writes=[tk['out']], dma=True)

        seq_all = [(i * 128, True) for i in range(NCB)] + [(TC + i * 128, False) for i in range(NB)]
        for n, (r0, is_ctx) in enumerate(seq_all):
            pre_block(r0, is_ctx, n)
        P.barrier()

        qkv = cxs; qkb = sb('qkb', [128, 8, 128], BF16); fT = sb('fT', [128, 8, 128], BF16)
        sm16 = small
        gs = sb('gs', [128, 8]); GB = sb('GB', [128, 4, 128]); totrow = sb('totrow', [128, 8])
        Lts = sb('Lts', [128, 4, 128]); Lst = sb('Lst', [128, 4, 128]); LstS = sb('LstS', [128, 4, 128])
        Am = sb('Am', [128, 4, 128]); AT = sb('AT', [128, 4, 128]); Qm = sb('Qm', [128, 4, 128]); QT = sb('QT', [128, 4, 128])
        Rm = sb('Rm', [128, 4, 128]); Rb = sb('Rb', [128, 4, 128], BF16)
        kbg = sb('kbg', [128, 4, 128], BF16); kd = sb('kd', [128, 4, 128], BF16); bv = sb('bv', [128, 4, 128], BF16)
        u = sb('u', [128, 4, 128]); wT = sb('wT', [128, 4, 128], BF16)
        S = sb('S1', [128, 4, 128]); Sb = [sb('Sb%d' % i, [128, 4, 128], BF16) for i in range(2)]
        vn = sb('vn', [128, 4, 128], BF16); Pst = sb('Pst', [128, 4, 128], BF16); egr = sb('egr', [128, 4, 128]); qg = sb('qg', [128, 4, 128], BF16)
        Oc = sb('Oc1', [128, DM]); ofl = sb('ofl1', [128, 512]); og = sb('og1', [128, 512]); yb = sb('yb1', [128, DM], BF16)
        yT = sb('yT1', [128, 8, 128], BF16)
        q8 = sb('q8', [128, 512]); q8b = sb('q8b', [128, 512], BF16); qT8 = sb('qT8', [128, 4, 128], BF16)
        kTl2 = [sb('kTl%d' % i, [128, 384]) for i in range(2)]; kTlb2 = [sb('kTlb%d' % i, [128, 384], BF16) for i in range(2)]; vl = sb('vl', [128, 3, 128]); vlb = sb('vlb', [128, 3, 128], BF16)
        kcT = sb('kcT', [128, max(TC, 128)]); kcTb2 = [sb('kcTb%d' % i, [128, max(TC, 128)], BF16) for i in range(2)]
        vc = sb('vc', [128, max(NCB, 1), 128]); vcb = sb('vcb', [128, max(NCB, 1), 128], BF16)
        NK = TC + 384
        ssb = sb('ssb', [128, NK]); pbf = sb('pbf', [128, NK], BF16); pT = sb('pT', [128, NK // 128, 128], BF16)
        v4 = lambda ap: ap.rearrange("p (h t) -> p h t", h=4)
        snap_ctr = [0]

        def scan_block(r0, is_ctx, d, first, bidx):
            v = 1 if is_ctx else 0
            x_t, tx = norm_hT(r0, v, bidx)
            c16 = 1536
            P.op('pe', mmgroup([(ps[2][:, 0:16], tokmm(c16, 16))]), reads=rh, writes=[tp[2]])
            P.op('act', lambda e: e.activation(out=small[:, 8:24], in_=ps[2][:, 0:16], func=AF.Identity), reads=[tp[2]], writes=[tk['small']])
            cb = 8 + d * 4; ca = 16 + d * 4; pA = d * 8; pB = d * 8 + 4
            sm = [tk['small']]
            P.op('act', lambda e: e.activation(out=small[:, 24:28], in_=small[:, cb:cb + 4], func=AF.Sigmoid), reads=sm, writes=sm)
            P.op('dve', lambda e: e.tensor_tensor(out=small[:, 28:32], in0=small[:, ca:ca + 4], in1=pvec[:, pB:pB + 4], op=ALU.add), reads=sm + c_, writes=sm)
            P.op('act', lambda e: e.activation(out=small[:, 28:32], in_=small[:, 28:32], func=AF.Exp), reads=sm, writes=sm)
            P.op('act', lambda e: e.activation(out=small[:, 28:32], in_=small[:, 28:32], func=AF.Ln, bias=onec[:, 0:1], scale=1.0), reads=sm, writes=sm)
            P.op('dve', lambda e: e.tensor_tensor(out=small[:, 28:32], in0=small[:, 28:32], in1=pvec[:, pA:pA + 4], op=ALU.mult), reads=sm + c_, writes=sm)
            tri = maskf if d == 0 else maskr
            P.op('pe', mmgroup([(ps[2][:, 32:36], [(tri[:], small[:, 28:32])]), (ps[2][:, 36:40], [(same[:], small[:, 28:32])])]),
                 reads=sm + c_, writes=[tp[2]])
            P.op('dve', lambda e: e.tensor_copy(out=small[:, 32:40], in_=ps[2][:, 32:40]), reads=[tp[2]], writes=sm)
            P.op('act', lambda e: e.activation(out=small[:, 40:44], in_=small[:, 32:36], func=AF.Exp), reads=sm, writes=sm)
            P.op('dve', lambda e: e.tensor_tensor(out=small[:, 40:44], in0=small[:, 40:44], in1=small[:, 24:28], op=ALU.mult), reads=sm, writes=sm)
            P.op('dve', lambda e: e.tensor_tensor(out=small[:, 44:48], in0=small[:, 36:40], in1=small[:, 32:36], op=ALU.subtract), reads=sm, writes=sm)
            P.op('act', lambda e: e.activation(out=small[:, 44:48], in_=small[:, 44:48], func=AF.Exp), reads=sm, writes=sm)
            P.op('dve', lambda e: e.tensor_tensor(out=GB[:], in0=self.ones[:].unsqueeze(1).to_broadcast([128, 4, 128]),
                                                  in1=small[:, 28:32].unsqueeze(2).to_broadcast([128, 4, 128]), op=ALU.mult),
                 reads=sm + [self.t_const], writes=[tk['GB']])
            P.op('dve', lambda e: e.tensor_tensor(out=gs[:].rearrange("p (h c) -> p h c", c=2), in0=small[:, 28:32].unsqueeze(2).to_broadcast([128, 4, 2]),
                                                  in1=csel[:].unsqueeze(1).to_broadcast([128, 4, 2]), op=ALU.mult), reads=sm + c_, writes=[tk['gs']])
            P.op('pe', mmgroup([(ps[3][:, h * 128:(h + 1) * 128], [(GB[:, h, :], tri[:])]) for h in range(4)]), reads=[tk['GB']] + c_, writes=[tp[3]])
            P.op('pe', mmgroup([(ps[2][:, 48:56], [(self.ones[:], gs[:])])]), reads=[tk['gs'], self.t_const], writes=[tp[2]])
            P.op('act', lambda e: e.activation(out=totrow[:], in_=ps[2][:, 48:56], func=AF.Exp), reads=[tp[2]], writes=[tk['gs']])
            gamrow = v4(ps[3][:])
            gcol = small[:, 32:36].unsqueeze(2).to_broadcast([128, 4, 128])
            mts, mst = (maskr, maskf) if d == 0 else (maskf, maskr)
            mtsS, mstS = (maskrs, maskfs) if d == 0 else (maskfs, maskrs)
            P.op('dve', lambda e: e.tensor_tensor(out=Lts[:], in0=gamrow, in1=gcol, op=ALU.subtract), reads=[tp[3]] + sm, writes=[tk['Lts']])
            P.op('pool', lambda e: e.tensor_scalar(out=Lst[:], in0=Lts[:], scalar1=0.0, scalar2=None, op0=ALU.min), reads=[tk['Lts']], writes=[tk['Lst']])
            P.op('dve', lambda e: e.tensor_scalar(out=Lts[:], in0=Lts[:], scalar1=0.0, scalar2=None, op0=ALU.max), reads=[tk['Lst']], writes=[tk['Lts']])
            P.op('act', lambda e: e.activation(out=egr[:], in_=gamrow, func=AF.Exp), reads=[tp[3], tk['Lts']], writes=[tk['egr']])
            P.op('act', lambda e: e.activation(out=Lts[:], in_=Lts[:], func=AF.Exp, scale=-1.0), writes=[tk['Lts']])
            P.op('act', lambda e: e.activation(out=Lst[:], in_=Lst[:], func=AF.Exp), writes=[tk['Lst']])
            P.op('dve', lambda e: e.tensor_tensor(out=Lts[:], in0=Lts[:], in1=mtsS[:].unsqueeze(1).to_broadcast([128, 4, 128]), op=ALU.mult),
                 reads=c_, writes=[tk['Lts']])
            P.op('pool', lambda e: e.tensor_tensor(out=LstS[:], in0=Lst[:], in1=mst[:].unsqueeze(1).to_broadcast([128, 4, 128]), op=ALU.mult),
                 reads=[tk['Lst']] + c_, writes=[tk['A']])
            cr = cxrow(r0)
            for j in range(5):
                cl = cxl[j % 2]; tcl = tk['cxl%d' % (j % 2)]
                P.op('sp', lambda e, j=j, cl=cl: e.dma_start(out=cl[:], in_=D['CX'][cr + j - 2:cr + j - 2 + 128, :]), writes=[tcl], dma=True)
                if j == 0:
                    P.op('dve', lambda e, cl=cl: e.tensor_tensor(out=qkv[:], in0=cl[:], in1=convw[:, 0, :], op=ALU.mult), reads=[tcl] + c_, writes=[tk['qkv']])
                else:
                    P.op('pool', lambda e, j=j, cl=cl: e.tensor_tensor(out=cl[:], in0=cl[:], in1=convw[:, j, :], op=ALU.mult), reads=c_, writes=[tcl])
                    P.op('dve', lambda e, cl=cl: e.tensor_tensor(out=qkv[:], in0=qkv[:], in1=cl[:], op=ALU.add), reads=[tcl], writes=[tk['qkv']])
            P.op('act', lambda e: e.activation(out=qkv[:], in_=qkv[:], func=AF.Silu), writes=[tk['qkv']])
            P.op('pool', lambda e: e.tensor_tensor(out=Oc[:], in0=qkv[:, 0:1024], in1=qkv[:, 0:1024], op=ALU.mult), reads=[tk['qkv']], writes=[tk['Oc']])
            P.op('dve', lambda e: e.tensor_reduce(out=small[:, 48:56], in_=Oc[:].rearrange("p (h v) -> p h v", h=8), axis=AX.X, op=ALU.add),
                 reads=[tk['Oc']], writes=sm)
            P.op('act', lambda e: e.activation(out=small[:, 48:56], in_=small[:, 48:56], func=AF.Sqrt, bias=self.epsc[:, 0:1], scale=1.0), reads=sm, writes=sm)
            P.op('dve', lambda e: e.reciprocal(out=small[:, 56:64], in_=small[:, 48:56]), reads=sm, writes=sm)
            P.op('dve', lambda e: e.tensor_scalar(out=small[:, 56:60], in0=small[:, 56:60], scalar1=128.0 ** -0.5, scalar2=None, op0=ALU.mult), reads=sm, writes=sm)
            P.op('dve', lambda e: e.tensor_tensor(out=qkb[:], in0=qkv[:, 0:1024].rearrange("p (h v) -> p h v", h=8),
                                                  in1=small[:, 56:64].unsqueeze(2).to_broadcast([128, 8, 128]), op=ALU.mult),
                 reads=[tk['qkv']] + sm, writes=[tk['qkb']])
            for half in range(2):
                P.op('pe', trs([(psb[4 + half][:, j * 128:(j + 1) * 128], qkb[:, half * 4 + j, :]) for j in range(4)], self.identb[:]),
                     reads=[tk['qkb'], self.t_const], writes=[tp[4 + half]])
                P.op('act', lambda e, half=half: e.activation(out=fT[:, half * 4:(half + 1) * 4, :].rearrange("p h t -> p (h t)"),
                                                              in_=psb[4 + half][:, 0:512], func=AF.Identity), reads=[tp[4 + half]], writes=[tk['fT']])
            kn = qkb[:, 4:8, :]
            P.op('dve', lambda e: e.tensor_tensor(out=kbg[:], in0=kn, in1=small[:, 40:44].unsqueeze(2).to_broadcast([128, 4, 128]), op=ALU.mult),
                 reads=[tk['qkb']] + sm, writes=[tk['kbg']])
            P.op('pool', lambda e: e.tensor_tensor(out=kd[:], in0=kn, in1=small[:, 44:48].unsqueeze(2).to_broadcast([128, 4, 128]), op=ALU.mult),
                 reads=[tk['qkb']] + sm, writes=[tk['kd']])
            P.op('dve', lambda e: e.tensor_tensor(out=bv[:], in0=qkv[:, 1024:1536].rearrange("p (h v) -> p h v", h=4),
                                                  in1=small[:, 24:28].unsqueeze(2).to_broadcast([128, 4, 128]), op=ALU.mult),
                 reads=[tk['qkv']] + sm, writes=[tk['bv']])
            P.op('dve', lambda e: e.tensor_tensor(out=qg[:], in0=fT[:, 0:4, :], in1=egr[:], op=ALU.mult), reads=[tk['fT'], tk['egr']], writes=[tk['qg']])
            P.op('pe', mmgroup([(ps[6][:, h * 128:(h + 1) * 128], [(fT[:, 4 + h, :], fT[:, 4 + h, :])]) for h in range(4)]), reads=[tk['fT']], writes=[tp[6]])
            P.op('pe', mmgroup([(ps[7][:, h * 128:(h + 1) * 128], [(fT[:, 4 + h, :], fT[:, h, :])]) for h in range(4)]), reads=[tk['fT']], writes=[tp[7]])
            P.op('dve', lambda e: e.tensor_tensor(out=Am[:], in0=v4(ps[6][:]), in1=Lts[:], op=ALU.mult), reads=[tp[6], tk['Lts']], writes=[tk['A']])
            P.op('dve', lambda e: e.tensor_tensor(out=Am[:], in0=Am[:], in1=small[:, 24:28].unsqueeze(2).to_broadcast([128, 4, 128]), op=ALU.mult),
                 reads=sm, writes=[tk['A']])
            P.op('dve', lambda e: e.tensor_tensor(out=Pst[:], in0=v4(ps[7][:]), in1=LstS[:], op=ALU.mult), reads=[tp[7], tk['A']], writes=[tk['Pst']])
            P.op('pe', trs([(ps[6][:, h * 128:(h + 1) * 128], Am[:, h, :]) for h in range(4)], self.ident[:]), reads=[tk['A'], self.t_const], writes=[tp[6]])
            P.op('act', lambda e: e.activation(out=AT[:], in_=v4(ps[6][:]), func=AF.Identity), reads=[tp[6]], writes=[tk['AT']])
            P.op('dve', lambda e: e.scalar_tensor_tensor(out=Rm[:], in0=AT[:], scalar=-1.0, in1=self.ident[:].unsqueeze(1).to_broadcast([128, 4, 128]),
                                                         op0=ALU.mult, op1=ALU.add), reads=[tk['AT'], self.t_const], writes=[tk['Rm']])
            curQ, curQT = AT, Am
            tQ, tQT = tk['AT'], tk['A']
            for step in range(5):
                last = step == 4
                if not last:
                    P.op('pe', mmgroup([(ps[6][:, h * 128:(h + 1) * 128], [(curQT[:, h, :], curQ[:, h, :])]) for h in range(4)]), reads=[tQ, tQT], writes=[tp[6]])
                P.op('pe', mmgroup([(ps[7][:, h * 128:(h + 1) * 128], [(curQ[:, h, :], curQT[:, h, :])]) for h in range(4)]), reads=[tQ, tQT], writes=[tp[7]])
                if not last:
                    P.op('act', lambda e: e.activation(out=Qm[:], in_=v4(ps[6][:]), func=AF.Identity), reads=[tp[6]], writes=[tk['Q']])
                P.op('act', lambda e: e.activation(out=QT[:], in_=v4(ps[7][:]), func=AF.Identity), reads=[tp[7]], writes=[tk['QT']])
                curQ, curQT, tQ, tQT = Qm, QT, tk['Q'], tk['QT']
                P.op('pe', mmgroup([(ps[3][:, h * 128:(h + 1) * 128], [(QT[:, h, :], Rm[:, h, :])]) for h in range(4)]), reads=[tk['QT'], tk['Rm']], writes=[tp[3]])
                P.op('dve', lambda e: e.tensor_tensor(out=Rm[:], in0=v4(ps[3][:]), in1=Rm[:], op=ALU.add), reads=[tp[3]], writes=[tk['Rm']])
            P.op('act', lambda e: e.activation(out=Rb[:], in_=Rm[:], func=AF.Identity), reads=[tk['Rm']], writes=[tk['Rb']])
            P.op('pe', mmgroup([(ps[6][:, h * 128:(h + 1) * 128], [(Rb[:, h, :], bv[:, h, :])]) for h in range(4)]), reads=[tk['Rb'], tk['bv']], writes=[tp[6]])
            P.op('pe', mmgroup([(ps[7][:, h * 128:(h + 1) * 128], [(kbg[:, h, :], Rb[:, h, :])]) for h in range(4)]), reads=[tk['Rb'], tk['kbg']], writes=[tp[7]])
            P.op('act', lambda e: e.activation(out=u[:], in_=v4(ps[6][:]), func=AF.Identity), reads=[tp[6]], writes=[tk['u']])
            P.op('act', lambda e: e.activation(out=wT[:], in_=v4(ps[7][:]), func=AF.Identity), reads=[tp[7]], writes=[tk['wT']])
            if first:
                P.op('pool', lambda e: e.memset(S[:], 0.0), writes=[tk['S']])
                P.op('pool', lambda e: e.memset(Sb[snap_ctr[0] % 2][:], 0.0), writes=[tk['Sb%d' % (snap_ctr[0] % 2)]])
            order = (0, 1) if d == 0 else (1, 0)
            for c in order:
                cs = slice(c * 64, c * 64 + 64)
                si = snap_ctr[0] % 2; snap_ctr[0] += 1; sn = (si + 1) % 2
                Sbi = Sb[si]; tSb = tk['Sb%d' % si]
                P.op('pe', mmgroup([(ps[6][cs, h * 128:(h + 1) * 128], [(wT[:, h, cs], Sbi[:, h, :])]) for h in range(4)]), reads=[tk['wT'], tSb], writes=[tp[6]])
                P.op('dve', lambda e, cs=cs: e.tensor_tensor(out=vn[cs], in0=u[cs], in1=v4(ps[6][cs, :]), op=ALU.subtract),
                     reads=[tp[6], tk['u']], writes=[tk['vn']])
                def omm(e, cs=cs, Sbi=Sbi):
                    for h in range(4):
                        e.matmul(ps[5][cs, h * 128:(h + 1) * 128], qg[:, h, cs], Sbi[:, h, :], start=True, stop=False)
                        ins = e.matmul(ps[5][cs, h * 128:(h + 1) * 128], Pst[cs, h, cs], vn[cs, h, :], start=False, stop=True)
                    return ins
                P.op('pe', omm, reads=[tk['qg'], tSb, tk['Pst'], tk['vn']], writes=[tp[5]])
                P.op('pe', mmgroup([(ps[7][:, h * 128:(h + 1) * 128], [(kd[cs, h, :], vn[cs, h, :])]) for h in range(4)]), reads=[tk['kd'], tk['vn']], writes=[tp[7]])
                P.op('dve', lambda e, c=c: e.tensor_tensor(out=S[:], in0=S[:], in1=totrow[:].rearrange("p (h c) -> p h c", c=2)[:, :, c:c + 1].to_broadcast([128, 4, 128]),
                                                           op=ALU.mult), reads=[tk['gs']], writes=[tk['S']])
                P.op('dve', lambda e: e.tensor_tensor(out=S[:], in0=S[:], in1=v4(ps[7][:]), op=ALU.add), reads=[tp[7]], writes=[tk['S']])
                P.op('act', lambda e, sn=sn: e.activation(out=Sb[sn][:], in_=S[:], func=AF.Identity), reads=[tk['S']], writes=[tk['Sb%d' % sn]])
            if d == 0:
                P.op('act', lambda e: e.activation(out=ofl[:], in_=ps[5][:], func=AF.Identity), reads=[tp[5]], writes=[tk['ofl']])
                P.op('sp', lambda e: e.dma_start(out=D['OF'][r0:r0 + 128, 0:512], in_=ofl[:]), reads=[tk['ofl']], writes=[tk['out']], dma=True)
                return
            if is_ctx:
                return
            P.op('sp', lambda e: e.dma_start(out=ofl[:], in_=D['OF'][r0:r0 + 128, 0:512]), writes=[tk['ofl']], dma=True)
            P.op('dve', lambda e: e.tensor_tensor(out=Oc[:, 0:512], in0=ps[5][:], in1=ofl[:], op=ALU.add), reads=[tp[5], tk['ofl']], writes=[tk['Oc']])
            P.op('pe', mmgroup([(ps[6][:], tokmm(1552, 512))]), reads=rh, writes=[tp[6]])
            P.op('act', lambda e: e.activation(out=og[:], in_=ps[6][:], func=AF.Silu), reads=[tp[6]], writes=[tk['og']])
            P.op('pool', lambda e: e.tensor_tensor(out=ofl[:], in0=Oc[:, 0:512], in1=Oc[:, 0:512], op=ALU.mult), reads=[tk['Oc']], writes=[tk['ofl']])
            P.op('dve', lambda e: e.tensor_reduce(out=small[:, 48:52], in_=ofl[:].rearrange("p (h v) -> p h v", h=4), axis=AX.X, op=ALU.add),
                 reads=[tk['ofl']], writes=sm)
            P.op('act', lambda e: e.activation(out=small[:, 48:52], in_=small[:, 48:52], func=AF.Sqrt, scale=1.0 / 128, bias=self.epsc[:, 0:1]), reads=sm, writes=sm)
            P.op('dve', lambda e: e.reciprocal(out=small[:, 52:56], in_=small[:, 48:52]), reads=sm, writes=sm)
            O4 = Oc[:, 0:512].rearrange("p (h v) -> p h v", h=4)
            P.op('dve', lambda e: e.tensor_tensor(out=O4, in0=O4, in1=small[:, 52:56].unsqueeze(2).to_broadcast([128, 4, 128]), op=ALU.mult), reads=sm, writes=[tk['Oc']])
            P.op('pool', lambda e: e.tensor_tensor(out=Oc[:, 0:512], in0=Oc[:, 0:512], in1=dnw[:], op=ALU.mult), reads=c_, writes=[tk['Oc']])
            P.op('dve', lambda e: e.tensor_tensor(out=yb[:, 0:512], in0=Oc[:, 0:512], in1=og[:], op=ALU.mult), reads=[tk['Oc'], tk['og']], writes=[tk['yb']])
            self.l1_attention(self._l1, r0)
            P.op('pe', trs([(psb[4][:, k * 128:(k + 1) * 128], yb[:, k * 128:(k + 1) * 128]) for k in range(8)], self.identb[:]),
                 reads=[tk['yb'], self.t_const], writes=[tp[4]])
            P.op('act', lambda e: e.activation(out=yT[:].rearrange("p k t -> p (k t)"), in_=psb[4][:, 0:1024], func=AF.Identity), reads=[tp[4]], writes=[tk['yT']])
            for half in range(2):
                P.op('pe', mmgroup([(ps[6 + half][:], [(yT[:, k, :], wout[:, k, half * 512:(half + 1) * 512]) for k in range(8)])]),
                     reads=[tk['yT'], tk['c']], writes=[tp[6 + half]])
                P.op('dve', lambda e, half=half: e.tensor_tensor(out=Oc[:, half * 512:(half + 1) * 512], in0=ps[6 + half][:],
                                                                 in1=self.gbc[0][:, half * 512:(half + 1) * 512], op=ALU.mult),
                     reads=[tp[6 + half], self.t_gbc], writes=[tk['Oc']])
            P.op('pool', lambda e: e.tensor_tensor(out=x_t[:], in0=x_t[:], in1=Oc[:], op=ALU.add), reads=[tk['Oc']], writes=[tx])
            P.op('sp', lambda e: e.dma_start(out=D[dst][r0:r0 + 128, :], in_=x_t[:]), reads=[tx], writes=[tk['out']], dma=True)

        self._l1 = locals()
        if TC:
            for s_, nm in enumerate(('KT', 'KT2')):
                P.op('sp', lambda e, nm=nm: e.dma_start(out=kcT[:, 0:TC], in_=D[nm][:, 0:TC]), writes=[tk['kcT']], dma=True)
                P.op('dve', lambda e, s_=s_: e.tensor_copy(out=kcTb2[s_][:, 0:TC], in_=kcT[:, 0:TC]), reads=[tk['kcT']], writes=[tk['kcT']])
            P.op('sp', lambda e: e.dma_start(out=vc[:], in_=D['VV'][0:TC, :].rearrange("(b p) n -> p b n", p=128)), writes=[tk['vc']], dma=True)
            P.op('dve', lambda e: e.tensor_copy(out=vcb[:], in_=vc[:]), reads=[tk['vc']], writes=[tk['vc']])
        for d in range(2):
            seq = seq_all
            if d == 1:
                seq = [(i * 128, True) for i in reversed(range(NCB))] + [(TC + i * 128, False) for i in reversed(range(NB))]
            for n, (r0, is_ctx) in enumerate(seq):
                scan_block(r0, is_ctx, d, n == 0, n)
            P.barrier()
        P.emit()


def apply_rope(self, x3, nh, rope, rt, t_x, t_rope, t_rt):
    P = self.P
    cosb = rope[:, 0:32].unsqueeze(1).to_broadcast([128, nh, 32]); sinb = rope[:, 32:64].unsqueeze(1).to_broadcast([128, nh, 32])
    x1 = x3[:, :, 0:32]; x2 = x3[:, :, 32:64]
    r = rt[:, 0:nh, :]
    rd = list(t_x) + [t_rope]
    P.op('dve', lambda e: e.tensor_tensor(out=r[:, :, 0:32], in0=x2, in1=sinb, op=ALU.mult), reads=rd, writes=[t_rt])
    P.op('dve', lambda e: e.tensor_tensor(out=r[:, :, 32:64], in0=x1, in1=sinb, op=ALU.mult), reads=rd, writes=[t_rt])
    P.op('dve', lambda e: e.tensor_tensor(out=x1, in0=x1, in1=cosb, op=ALU.mult), reads=[t_rope], writes=list(t_x))
    P.op('dve', lambda e: e.tensor_tensor(out=x2, in0=x2, in1=cosb, op=ALU.mult), reads=[t_rope], writes=list(t_x))
    P.op('dve', lambda e: e.tensor_tensor(out=x1, in0=x1, in1=r[:, :, 0:32], op=ALU.subtract), reads=[t_rt], writes=list(t_x))
    P.op('dve', lambda e: e.tensor_tensor(out=x2, in0=x2, in1=r[:, :, 32:64], op=ALU.add), reads=[t_rt], writes=list(t_x))


Builder.phase_l1mix = phase_l1mix
Builder.apply_rope = apply_rope


def l1_attention(self, L, r0):
    P, D = self.P, self.D
    TC, NB, NCB = self.TC, self.NB, self.NCB
    ps, psb, tp, tk = L['ps'], L['psb'], L['tp'], L['tk']
    mmgroup, trs, tokmm, rh = L['mmgroup'], L['trs'], L['tokmm'], L['rh']
    q8, q8b, qT8, small, yb = L['q8'], L['q8b'], L['qT8'], L['small'], L['yb']
    kTl, kTlb, vl, vlb, kcTb, vcb = L['kTl2'], L['kTlb2'], L['vl'], L['vlb'], L['kcTb2'], L['vcb']
    ssb, pbf, pT, rope, rt, sinks, mbp, mbn = L['ssb'], L['pbf'], L['pT'], L['rope'], L['rt'], L['sinks'], L['mbp'], L['mbn']
    blk = (r0 - TC) // 128
    has_prev, has_next = blk > 0, blk < NB - 1
    NK = TC + 384
    sm = [tk['small']]
    P.op('pe', mmgroup([(ps[0][:], tokmm(2064, 512))]), reads=rh, writes=[tp[0]])
    P.op('act', lambda e: e.activation(out=q8[:], in_=ps[0][:], func=AF.Identity), reads=[tp[0]], writes=[tk['q8']])
    t0 = r0 - TC
    P.op('sp', lambda e: e.dma_start(out=rope[:], in_=D['rope'][t0:t0 + 128, :]), writes=[tk['rope']], dma=True)
    self.apply_rope(q8[:].rearrange("p (h f) -> p h f", h=8), 8, rope, rt, [tk['q8']], tk['rope'], tk['rt'])
    P.op('dve', lambda e: e.tensor_scalar(out=q8b[:], in0=q8[:], scalar1=0.125, scalar2=None, op0=ALU.mult), reads=[tk['q8']], writes=[tk['q8']])
    P.op('pe', trs([(psb[1][:, j * 128:(j + 1) * 128], q8b[:, j * 128:(j + 1) * 128]) for j in range(4)], self.identb[:]),
         reads=[tk['q8'], self.t_const], writes=[tp[1]])
    P.op('act', lambda e: e.activation(out=qT8[:].rearrange("p j t -> p (j t)"), in_=psb[1][:, 0:512], func=AF.Identity), reads=[tp[1]], writes=[tk['qT8']])
    c0 = r0 - 128 if has_prev else r0
    c1 = r0 + 256 if has_next else r0 + 128
    o0 = 0 if has_prev else 128
    for s_, nm in enumerate(('KT', 'KT2')):
        P.op('sp', lambda e, s_=s_, nm=nm: e.dma_start(out=kTl[s_][:, o0:o0 + (c1 - c0)], in_=D[nm][:, c0:c1]), writes=[tk['kTl']], dma=True)
        P.op('pool', lambda e, s_=s_: e.tensor_copy(out=kTlb[s_][:, o0:o0 + (c1 - c0)], in_=kTl[s_][:, o0:o0 + (c1 - c0)]), reads=[tk['kTl']], writes=[tk['kTl']])
    nbk = (c1 - c0) // 128
    b0 = o0 // 128
    P.op('sp', lambda e: e.dma_start(out=vl[:, b0:b0 + nbk, :], in_=D['VV'][c0:c1, :].rearrange("(b p) n -> p b n", p=128)), writes=[tk['vl']], dma=True)
    P.op('pool', lambda e: e.tensor_copy(out=vlb[:, b0:b0 + nbk, :], in_=vl[:, b0:b0 + nbk, :]), reads=[tk['vl']], writes=[tk['vl']])
    for h in range(8):
        pbse = (h % 2) * 64; g = h // 4
        s_ = 0 if g == (h % 2) else 1
        rs = slice(pbse, pbse + 64)
        bA, bB = 2 + 2 * (h % 2), 3 + 2 * (h % 2)
        ql = qT8[rs, h // 2, :]
        outsA = []
        if TC:
            outsA.append((ps[bA][:, 0:TC], [(ql, kcTb[s_][rs, 0:TC])]))
        if has_prev:
            outsA.append((ps[bA][:, TC:TC + 128], [(ql, kTlb[s_][rs, 0:128])]))
        outsA.append((ps[bA][:, TC + 128:TC + 256], [(ql, kTlb[s_][rs, 128:256])]))
        P.op('pe', mmgroup(outsA), reads=[tk['qT8'], tk['kTl'], tk['kcT']], writes=[tp[bA]])
        if has_next:
            P.op('pe', mmgroup([(ps[bB][:, 0:128], [(ql, kTlb[s_][rs, 256:384])])]), reads=[tk['qT8'], tk['kTl']], writes=[tp[bB]])
        w_ = [tk['ssb']]
        if TC:
            P.op('act', lambda e, bA=bA: e.activation(out=ssb[:, 0:TC], in_=ps[bA][:, 0:TC], func=AF.Identity), reads=[tp[bA]], writes=w_)
        if has_prev:
            P.op('dve', lambda e, bA=bA: e.tensor_tensor(out=ssb[:, TC:TC + 128], in0=ps[bA][:, TC:TC + 128], in1=mbp[:], op=ALU.add), reads=[tp[bA], tk['c']], writes=w_)
        else:
            P.op('pool', lambda e: e.memset(ssb[:, TC:TC + 128], NEG), writes=w_)
        P.op('act', lambda e, bA=bA: e.activation(out=ssb[:, TC + 128:TC + 256], in_=ps[bA][:, TC + 128:TC + 256], func=AF.Identity), reads=[tp[bA]], writes=w_)
        if has_next:
            P.op('dve', lambda e, bB=bB: e.tensor_tensor(out=ssb[:, TC + 256:TC + 384], in0=ps[bB][:, 0:128], in1=mbn[:], op=ALU.add), reads=[tp[bB], tk['c']], writes=w_)
        else:
            P.op('pool', lambda e: e.memset(ssb[:, TC + 256:TC + 384], NEG), writes=w_)
        P.op('dve', lambda e: e.tensor_reduce(out=small[:, 16:17], in_=ssb[:], axis=AX.X, op=ALU.max), reads=w_, writes=sm)
        P.op('dve', lambda e, h=h: e.tensor_tensor(out=small[:, 16:17], in0=small[:, 16:17], in1=sinks[:, h:h + 1], op=ALU.max), reads=sm + [tk['c']], writes=sm)
        P.op('dve', lambda e: e.tensor_scalar(out=small[:, 17:18], in0=small[:, 16:17], scalar1=-1.0, scalar2=None, op0=ALU.mult), reads=sm, writes=sm)
        P.op('act', lambda e: e.activation(out=pbf[:], in_=ssb[:], func=AF.Exp, bias=small[:, 17:18], scale=1.0, accum_out=small[:, 18:19]),
             reads=w_ + sm, writes=[tk['pb']] + sm)
        P.op('act', lambda e, h=h: e.activation(out=small[:, 19:20], in_=sinks[:, h:h + 1], func=AF.Exp, bias=small[:, 17:18], scale=1.0), reads=sm, writes=sm)
        P.op('dve', lambda e: e.tensor_tensor(out=small[:, 19:20], in0=small[:, 19:20], in1=small[:, 18:19], op=ALU.add), reads=sm, writes=sm)
        P.op('dve', lambda e, h=h: e.reciprocal(out=small[:, 8 + h:9 + h], in_=small[:, 19:20]), reads=sm, writes=sm)
        nkb = NK // 128
        P.op('pe', trs([(psb[6][:, kb * 128:(kb + 1) * 128], pbf[:, kb * 128:(kb + 1) * 128]) for kb in range(nkb)], self.identb[:]),
             reads=[tk['pb'], self.t_const], writes=[tp[6]])
        P.op('act', lambda e: e.activation(out=pT[:].rearrange("p k t -> p (k t)"), in_=psb[6][:, 0:NK], func=AF.Identity), reads=[tp[6]], writes=[tk['pT']])
        pairs = [(pT[:, b, :], vcb[:, b, g * 64:(g + 1) * 64]) for b in range(NCB)]
        for j in range(3):
            if (j == 0 and not has_prev) or (j == 2 and not has_next):
                continue
            pairs.append((pT[:, NCB + j, :], vlb[:, j, g * 64:(g + 1) * 64]))
        P.op('pe', mmgroup([(ps[7][:, h * 64:(h + 1) * 64], pairs)]), reads=[tk['pT'], tk['vl'], tk['vc']], writes=[tp[7]])
    P.op('dve', lambda e: e.tensor_tensor(out=yb[:, 512:1024].rearrange("p (h f) -> p h f", h=8), in0=ps[7][:].rearrange("p (h f) -> p h f", h=8),
                                          in1=small[:, 8:16].unsqueeze(2).to_broadcast([128, 8, 64]), op=ALU.mult), reads=[tp[7]] + sm, writes=[tk['yb']])


Builder.l1_attention = l1_attention
```

```python
import numpy as np
from contextlib import ExitStack
import concourse.bass as bass
import concourse.mybir as mybir
from concourse.bass_utils import run_bass_kernel_spmd

dt = mybir.dt
F32 = dt.float32
BF16 = dt.bfloat16
AF = mybir.ActivationFunctionType
ALU = mybir.AluOpType
AX = mybir.AxisListType

ENGS = ['pe', 'act', 'dve', 'pool', 'sp']
NDMA = 6
EPS = 1e-6
DM = 1024
NE = 32


class Tok:
    __slots__ = ('w', 'r')

    def __init__(self):
        self.w = None
        self.r = {}


class Prog:
    def __init__(self, nc):
        self.nc = nc
        self.stack = ExitStack()
        self.sems = {}
        self.cnt = {e: 0 for e in ENGS}
        self.dma_cnt = {}
        self.dma_rr = {e: 0 for e in ENGS}
        self.seen = {e: {} for e in ENGS}
        self.q = {e: [] for e in ENGS}
        self.nops = 0
        for e in ENGS:
            self.sems[('eng', e)] = self.stack.enter_context(nc.semaphore('s_' + e))
        for e in ('sp', 'pool', 'act'):
            for k in range(NDMA):
                self.sems[('dma', e, k)] = self.stack.enter_context(nc.semaphore('d_%s%d' % (e, k)))

    def op(self, eng, fn, reads=(), writes=(), dma=False):
        deps = {}

        def add(k, v):
            if deps.get(k, 0) < v:
                deps[k] = v

        for t in reads:
            if t.w is not None:
                add(*t.w)
        for t in writes:
            if t.w is not None:
                add(*t.w)
            for k, v in t.r.items():
                add(k, v)
        if dma:
            k = self.dma_rr[eng]
            self.dma_rr[eng] = (k + 1) % NDMA
            key = ('dma', eng, k)
            prev = self.dma_cnt.get(key, 0)
            if prev:
                add(key, prev)
            val = prev + 16
            self.dma_cnt[key] = val
        else:
            key = ('eng', eng)
            self.cnt[eng] += 1
            val = self.cnt[eng]
        seen = self.seen[eng]
        waits = []
        for k, v in deps.items():
            if eng == 'pe' and k == ('eng', 'pe'):
                continue
            if seen.get(k, 0) >= v:
                continue
            seen[k] = v
            waits.append((k, v))
        self.q[eng].append((waits, fn, key, dma))
        self.nops += 1
        for t in reads:
            if t.r.get(key, 0) < val:
                t.r[key] = val
        for t in writes:
            t.w = (key, val)
            t.r = {}

    def barrier(self):
        evs = [(('eng', e), self.cnt[e]) for e in ENGS if self.cnt[e]]
        evs += list(self.dma_cnt.items())
        for e in ENGS:
            seen = self.seen[e]
            waits = []
            for k, v in evs:
                if seen.get(k, 0) >= v:
                    continue
                seen[k] = v
                waits.append((k, v))
            if waits:
                self.q[e].append((waits, None, None, False))

    def emit(self):
        nc = self.nc
        sems = self.sems
        with nc.Block() as block:
            def mk(name):
                lst = self.q[name]

                def f(e):
                    for waits, fn, key, dma in lst:
                        for k, v in waits:
                            e.wait_ge(sems[k], v)
                        if fn is None:
                            continue
                        ins = fn(e)
                        ins.then_inc(sems[key], 16 if dma else 1)
                return f
            block.tensor(mk('pe'))
            block.scalar(mk('act'))
            block.vector(mk('dve'))
            block.gpsimd(mk('pool'))
            block.sync(mk('sp'))
        self.q = {e: [] for e in ENGS}

    def close(self):
        self.stack.close()


class Builder:
    def __init__(self, T, TC, phases=('ada', 'l0mix', 'l0moe', 'l1mix', 'l1moe'), dbg=()):
        self.T, self.TC = T, TC
        self.NB, self.NCB = T // 128, TC // 128
        self.phases = phases
        self.dbg = dbg
        self.nc = bass.Bass("TRN2", target_bir_lowering=False)
        self.D = {}
        self.P = Prog(self.nc)
        self.in_names = []

    def din(self, name, shape, d=F32):
        self.D[name] = self.nc.dram_tensor(name, list(shape), d, kind="ExternalInput").ap()
        self.in_names.append(name)
        return self.D[name]

    def dscr(self, name, shape, d=F32):
        kind = "ExternalOutput" if name in self.dbg else "Internal"
        self.D[name] = self.nc.dram_tensor(name, list(shape), d, kind=kind).ap()
        return self.D[name]

    def declare(self):
        T, TC = self.T, self.TC
        din = self.din
        din('x', [T, DM]); din('ctx', [max(TC, 128), DM]); din('cvec', [128, 8, 2])
        din('ident', [128, 128]); din('ones', [128, 128])
        for l in range(2):
            p = 'l%d_' % l
            din(p + 'ada_w', [DM, 6 * DM]); din(p + 'ada_bT', [128, 48])
            din(p + 'nmix', [128, 8]); din(p + 'nffn', [128, 8])
            if ('l%dmoe' % l) in self.phases:
                din(p + 'router_w', [DM, NE]); din(p + 'router_b', [128, NE])
                din(p + 'w_up', [NE, DM, 2 * DM]); din(p + 'b_upT', [128, NE, 16])
                din(p + 'w_down', [NE, DM, DM]); din(p + 'b_down', [NE, DM])
        din('final_w', [128, DM])
        self.out = self.nc.dram_tensor('out', [T, DM], F32, kind="ExternalOutput").ap()
        R = TC + T
        self.dscr('X1', [R, DM]); self.dscr('X2', [R, DM]); self.dscr('X3', [R, DM]); self.dscr('OF', [R, DM])
        if 'l1mix' in self.phases:
            din('l1_w_in', [DM, 2832]); din('l1_w_out', [DM, DM]); din('l1_convw', [128, 5, 1536]); din('l1_pvec', [128, 16])
            din('l1_dnw', [128, 512]); din('l1_sinks', [128, 8]); din('rope', [T, 64])
            for nm in ('mask_fs', 'mask_rs', 'same', 'mb_prev', 'mb_next'):
                din(nm, [128, 128])
            din('csel', [128, 2])
            self.dscr('CX', [R + 8, 1536]); self.dscr('KT', [128, R]); self.dscr('KT2', [128, R]); self.dscr('VV', [R, 128])
            if 'l0mix' not in self.phases:
                din('mask_f', [128, 128]); din('mask_r', [128, 128])
        if 'l0mix' not in self.phases:
            return
        din('l0_w_in', [DM, 4128]); din('l0_w_out', [DM, DM]); din('l0_w2', [16, 2, 256]); din('l0_b2', [128, 2, 2])
        din('l0_lb', [128, 2, 4]); din('l0_normw', [128, DM]); din('rmask', [128, 768]); din('mask_f', [128, 128]); din('mask_r', [128, 128])

    def build(self):
        nc, P = self.nc, self.P
        self.declare()
        with ExitStack() as st0:
            self.st0 = st0
            sb = lambda name, shape, d=F32: st0.enter_context(nc.sbuf_tensor(name, shape, d))
            self.ident = sb('ident_t', [128, 128]); self.ones = sb('ones_t', [128, 128])
            self.identb = sb('identb_t', [128, 128], BF16)
            self.modT = [sb('modT%d' % l, [128, 48, 2]) for l in range(2)]
            self.Acol = [[sb('A%d_%d' % (l, i), [128, 8, 2]) for i in range(2)] for l in range(2)]
            self.gbc = [sb('gbc_x', [128, DM]), sb('gbc_c', [128, DM])]
            self.t_const = Tok(); self.t_mod = Tok(); self.t_gbc = Tok()
            P.op('sp', lambda e: e.dma_start(out=self.ident[:], in_=self.D['ident']), writes=[self.t_const], dma=True)
            P.op('sp', lambda e: e.dma_start(out=self.ones[:], in_=self.D['ones']), writes=[self.t_const], dma=True)
            P.op('dve', lambda e: e.tensor_copy(out=self.identb[:], in_=self.ident[:]), reads=[self.t_const], writes=[self.t_const])
            P.barrier()
            self.phase_ada()
            cur = 'x_in'
            if 'l0mix' in self.phases:
                self.phase_l0mix(cur, 'X1'); cur = 'X1'
            if 'l0moe' in self.phases:
                self.phase_moe(0, cur, 'X2', final=False); cur = 'X2'
            if 'l1mix' in self.phases:
                self.phase_l1mix(cur, 'X3'); cur = 'X3'
            if 'l1moe' in self.phases:
                self.phase_moe(1, cur, None, final=True)
            elif 'final' in self.phases:
                self.phase_final(cur)
            P.barrier()
            P.emit()
        P.close()
        return nc

    def rows(self, name, r0, n=128):
        if name == 'x_in':
            if r0 < self.TC:
                return self.D['ctx'][r0:r0 + n, :]
            return self.D['x'][r0 - self.TC:r0 - self.TC + n, :]
        return self.D[name][r0:r0 + n, :]

    def phase_ada(self):
        nc, P, D = self.nc, self.P, self.D
        with ExitStack() as st:
            sb = lambda name, shape, d=F32: st.enter_context(nc.sbuf_tensor(name, shape, d))
            ps = [st.enter_context(nc.psum_tensor('pa%d' % i, [128, 512], F32)) for i in range(2)]
            cv = sb('cv', [128, 8, 2]); scv = sb('scv', [128, 8, 2])
            wb = [sb('adaw%d' % i, [128, 8, 512]) for i in range(2)]
            bT = sb('adab', [128, 48]); nw = sb('nw', [128, 8])
            t_cv, t_ps, t_b = Tok(), Tok(), Tok()
            t_wb = [Tok(), Tok()]
            P.op('sp', lambda e: e.dma_start(out=cv[:], in_=D['cvec']), writes=[t_cv], dma=True)
            P.op('act', lambda e: e.activation(out=scv[:], in_=cv[:], func=AF.Silu), reads=[t_cv], writes=[t_cv])
            for l in range(2):
                p = 'l%d_' % l
                P.op('sp', lambda e, p=p: e.dma_start(out=bT[:], in_=D[p + 'ada_bT']), writes=[t_b], dma=True)
                for cb in range(12):
                    w = wb[cb % 2]; tw = t_wb[cb % 2]
                    src = D[p + 'ada_w'][:, cb * 512:(cb + 1) * 512].rearrange("(k p) n -> p k n", p=128)
                    P.op('sp', lambda e, w=w, src=src: e.dma_start(out=w[:], in_=src), writes=[tw], dma=True)

                    def mm(e, w=w, cb=cb):
                        for jj in range(4):
                            j = cb * 4 + jj
                            for kc in range(8):
                                ins = e.matmul(ps[0][:, j * 2:j * 2 + 2], w[:, kc, jj * 128:(jj + 1) * 128],
                                               scv[:, kc, :], start=(kc == 0), stop=(kc == 7))
                        return ins
                    P.op('pe', mm, reads=[tw, t_cv], writes=[t_ps])
                modT = self.modT[l]
                P.op('dve', lambda e, modT=modT: e.tensor_tensor(
                    out=modT[:], in0=ps[0][:, 0:96].rearrange("p (j v) -> p j v", v=2),
                    in1=bT[:].unsqueeze(2).to_broadcast([128, 48, 2]), op=ALU.add),
                    reads=[t_ps, t_b], writes=[self.t_mod])
                for i, (nm, c0) in enumerate((('nmix', 8), ('nffn', 32))):
                    A = self.Acol[l][i]
                    P.op('sp', lambda e, nm=nm, p=p: e.dma_start(out=nw[:], in_=D[p + nm]), writes=[t_b], dma=True)
                    P.op('dve', lambda e, A=A, modT=modT, c0=c0: e.scalar_tensor_tensor(
                        out=A[:], in0=modT[:, c0:c0 + 8, :], scalar=1.0,
                        in1=nw[:].unsqueeze(2).to_broadcast([128, 8, 2]), op0=ALU.add, op1=ALU.mult),
                        reads=[self.t_mod, t_b], writes=[self.t_mod])
            P.barrier()
            P.emit()

    def gate_bcast(self, dg, ps, l, c0, variants):
        nc, P = self.nc, self.P
        t_dg, t_p = Tok(), Tok()
        for v in variants:
            for k in range(8):
                P.op('dve', lambda e, k=k, v=v: e.tensor_scalar(
                    out=dg[:, k, :], in0=self.ident[:], scalar1=self.modT[l][:, c0 + k, v:v + 1], scalar2=None,
                    op0=ALU.mult), reads=[self.t_mod, self.t_const], writes=[t_dg])
            for h in range(2):
                def mm(e, h=h):
                    for j in range(4):
                        ins = e.matmul(ps[h][:, j * 128:(j + 1) * 128], self.ones[:], dg[:, h * 4 + j, :],
                                       start=True, stop=True)
                    return ins
                P.op('pe', mm, reads=[t_dg, self.t_const], writes=[t_p])
                P.op('act', lambda e, h=h, v=v: e.activation(out=self.gbc[v][:, h * 512:(h + 1) * 512], in_=ps[h][:],
                                                            func=AF.Identity), reads=[t_p], writes=[self.t_gbc])

    def norm_xn(self, src_rows, xt, t_x, xn, t_xn, small, t_small, junk, t_junk):
        P = self.P
        P.op('sp', lambda e: e.dma_start(out=xt[:], in_=src_rows), writes=[t_x], dma=True)
        P.op('act', lambda e: e.activation(out=junk[:], in_=xt[:], func=AF.Square, accum_out=small[:, 0:1]),
             reads=[t_x], writes=[t_junk, t_small])
        P.op('act', lambda e: e.activation(out=small[:, 1:2], in_=small[:, 0:1], func=AF.Sqrt, scale=1.0 / DM, bias=self.epsc[:, 0:1]),
             reads=[t_small], writes=[t_small])
        P.op('dve', lambda e: e.reciprocal(out=small[:, 2:3], in_=small[:, 1:2]), reads=[t_small], writes=[t_small])
        P.op('dve', lambda e: e.tensor_scalar(out=xn[:], in0=xt[:], scalar1=small[:, 2:3], scalar2=None, op0=ALU.mult),
             reads=[t_x, t_small], writes=[t_xn])

    def phase_moe(self, l, src, dst, final):
        nc, P, D = self.nc, self.P, self.D
        p = 'l%d_' % l
        TC = self.TC if l == 0 else 0
        r_begin = 0 if l == 0 else self.TC
        nblk = (TC + self.T) // 128
        NSB = 8
        with ExitStack() as st:
            sb = lambda name, shape, d=F32: st.enter_context(nc.sbuf_tensor(name + '_m%d' % l, shape, d))
            ps = [st.enter_context(nc.psum_tensor('pm%d_%d' % (l, i), [128, 512], F32)) for i in range(8)]
            self.epsc = sb('epsc', [128, 1])
            h2T = sb('h2T', [128, 8, NSB * 128], BF16)
            acc = sb('acc', [128, NSB, DM])
            gates = sb('gates', [128, NSB, NE])
            wu = [sb('wu%d' % i, [128, 8, 2 * DM], BF16) for i in range(2)]
            wd = [sb('wd%d' % i, [128, 8, DM], BF16) for i in range(2)]
            actT = [sb('actT0', [128, 8, 512], BF16)] * 2
            xt = [sb('xt%d' % i, [128, DM]) for i in range(2)]
            junk = sb('junk', [128, DM], BF16)
            small = sb('small', [128, 16]); h32 = sb('h32', [128, 8, 128])
            rw = sb('rw', [128, 8, NE]); rb = sb('rb', [128, NE]); bup = sb('bup', [128, NE, 16])
            bdn = sb('bdn', [NE, DM]); lg = sb('lg', [128, NE]); top8 = sb('top8', [128, 8])
            em = sb('em', [128, NE]); gT = sb('gT', [NE, 128])
            eg = [sb('eg%d' % i, [128, 512]) for i in range(2)]
            es = [sb('es%d' % i, [128, 512]) for i in range(2)]
            el = [sb('el%d' % i, [128, 512]) for i in range(2)]
            fw = sb('fw', [128, DM]) if final else None
            T_ = lambda: Tok()
            t_c, t_h2T, t_acc, t_gates = T_(), T_(), T_(), T_()
            t_wu, t_wd = [T_(), T_()], [T_(), T_()]
            t_act = [T_()] * 2
            t_xt, t_xn, t_junk, t_small, t_h32 = [T_(), T_()], T_(), T_(), T_(), T_()
            t_lg, t_gT = T_(), T_()
            t_ps = [T_() for _ in range(8)]
            t_eg, t_es, t_el = [T_(), T_()], [T_(), T_()], [T_(), T_()]
            t_out = T_()
            P.op('pool', lambda e: e.memset(self.epsc[:], EPS), writes=[t_c])
            P.op('sp', lambda e: e.dma_start(out=rw[:], in_=D[p + 'router_w'].rearrange("(k p) n -> p k n", p=128)), writes=[t_c], dma=True)
            P.op('sp', lambda e: e.dma_start(out=rb[:], in_=D[p + 'router_b']), writes=[t_c], dma=True)
            P.op('sp', lambda e: e.dma_start(out=bup[:], in_=D[p + 'b_upT']), writes=[t_c], dma=True)
            P.op('sp', lambda e: e.dma_start(out=bdn[:], in_=D[p + 'b_down']), writes=[t_c], dma=True)
            if final:
                P.op('sp', lambda e: e.dma_start(out=fw[:], in_=D['final_w']), writes=[t_c], dma=True)
            variants = (0, 1) if TC else (0,)
            self.gate_bcast(h32, ps[0:2], l, 40, variants)
            P.barrier()
            A2 = self.Acol[l][1]; modT = self.modT[l]
            wcount = 0
            for sb0 in range(0, nblk, NSB):
                nb = min(NSB, nblk - sb0)
                for bi in range(nb):
                    r0 = r_begin + (sb0 + bi) * 128
                    v = 1 if (r0 < self.TC) else 0
                    x_t = xt[bi % 2]; tx = t_xt[bi % 2]
                    self.norm_xn(self.rows(src, r0), x_t, tx, x_t, tx, small, t_small, junk, t_junk)
                    xn, t_xn = x_t, tx
                    for h in range(2):
                        def tr(e, h=h, xn=xn):
                            for j in range(4):
                                k = h * 4 + j
                                ins = e.transpose(ps[h][:, j * 128:(j + 1) * 128], xn[:, k * 128:(k + 1) * 128], self.ident[:])
                            return ins
                        P.op('pe', tr, reads=[t_xn, self.t_const], writes=[t_ps[h]])
                        for j in range(4):
                            k = h * 4 + j
                            P.op('act', lambda e, h=h, j=j, k=k, v=v: e.activation(
                                out=h32[:, k, :], in_=ps[h][:, j * 128:(j + 1) * 128], func=AF.Identity,
                                scale=A2[:, k, v:v + 1], bias=modT[:, 24 + k, v:v + 1]),
                                reads=[t_ps[h], self.t_mod], writes=[t_h32])
                    P.op('dve', lambda e, bi=bi: e.tensor_copy(out=h2T[:, :, bi * 128:(bi + 1) * 128], in_=h32[:]),
                         reads=[t_h32], writes=[t_h2T])

                    def rmm(e):
                        for k in range(8):
                            ins = e.matmul(ps[2][:, 0:NE], h32[:, k, :], rw[:, k, :], start=(k == 0), stop=(k == 7))
                        return ins
                    P.op('pe', rmm, reads=[t_h32, t_c], writes=[t_ps[2]])
                    P.op('dve', lambda e: e.tensor_tensor(out=lg[:], in0=ps[2][:, 0:NE], in1=rb[:], op=ALU.add),
                         reads=[t_ps[2], t_c], writes=[t_lg])
                    P.op('dve', lambda e: e.max(out=top8[:], in_=lg[:]), reads=[t_lg], writes=[t_lg])
                    P.op('dve', lambda e: e.tensor_scalar(out=small[:, 4:5], in0=top8[:, 0:1], scalar1=-1.0, scalar2=None, op0=ALU.mult),
                         reads=[t_lg], writes=[t_small])
                    P.op('act', lambda e: e.activation(out=em[:], in_=lg[:], func=AF.Exp, bias=small[:, 4:5], scale=1.0),
                         reads=[t_lg, t_small], writes=[t_lg])
                    P.op('dve', lambda e: e.scalar_tensor_tensor(out=em[:], in0=lg[:], scalar=top8[:, 3:4], in1=em[:],
                                                                  op0=ALU.is_ge, op1=ALU.mult), reads=[t_lg], writes=[t_lg])
                    P.op('dve', lambda e: e.tensor_reduce(out=small[:, 5:6], in_=em[:], axis=AX.X, op=ALU.add),
                         reads=[t_lg], writes=[t_small])
                    P.op('dve', lambda e: e.reciprocal(out=small[:, 6:7], in_=small[:, 5:6]), reads=[t_small], writes=[t_small])
                    P.op('dve', lambda e, bi=bi: e.tensor_scalar(out=gates[:, bi, :], in0=em[:], scalar1=small[:, 6:7], scalar2=None,
                                                                 op0=ALU.mult), reads=[t_lg, t_small], writes=[t_gates])
                    P.op('pe', lambda e, bi=bi: e.transpose(ps[3][0:NE, 0:128], gates[:, bi, :], self.ident[:]),
                         reads=[t_gates, self.t_const], writes=[t_ps[3]])
                    P.op('act', lambda e: e.activation(out=gT[:], in_=ps[3][0:NE, 0:128], func=AF.Identity),
                         reads=[t_ps[3]], writes=[t_gT])
                    for h in range(2):
                        P.op('pe', lambda e, h=h: e.matmul(ps[h][:], gT[:], bdn[:, h * 512:(h + 1) * 512], start=True, stop=True),
                             reads=[t_gT, t_c], writes=[t_ps[h]])
                        P.op('act', lambda e, h=h, bi=bi: e.activation(out=acc[:, bi, h * 512:(h + 1) * 512], in_=ps[h][:], func=AF.Identity),
                             reads=[t_ps[h]], writes=[t_acc])
                ngrp = (nb + 3) // 4
                for ex in range(NE):
                    wi = wcount % 2; wcount += 1
                    P.op('pool', lambda e, wi=wi, ex=ex: e.dma_start(out=wu[wi][:], in_=D[p + 'w_up'][ex].rearrange("(k p) n -> p k n", p=128)),
                         writes=[t_wu[wi]], dma=True)
                    P.op('pool', lambda e, wi=wi, ex=ex: e.dma_start(out=wd[wi][:], in_=D[p + 'w_down'][ex].rearrange("(k p) n -> p k n", p=128)),
                         writes=[t_wd[wi]], dma=True)
                    for g in range(ngrp):
                        gb = min(4, nb - g * 4)
                        N = gb * 128
                        t0 = g * 512
                        ai = g % 2
                        for fc in range(8):
                            ei = fc % 2
                            for part, pb in ((0, 4 + ei), (1, 6 + ei)):
                                def umm(e, part=part, pb=pb, fc=fc, wi=wi, N=N, t0=t0):
                                    c0 = part * DM + fc * 128
                                    for k in range(8):
                                        ins = e.matmul(ps[pb][:, 0:N], wu[wi][:, k, c0:c0 + 128], h2T[:, k, t0:t0 + N],
                                                       start=(k == 0), stop=(k == 7))
                                    return ins
                                P.op('pe', umm, reads=[t_wu[wi], t_h2T], writes=[t_ps[pb]])
                            pg, pl = ps[4 + ei], ps[6 + ei]
                            P.op('dve', lambda e, pg=pg, ei=ei, fc=fc, ex=ex, N=N: e.tensor_scalar(
                                out=eg[ei][:, 0:N], in0=pg[:, 0:N], scalar1=bup[:, ex, fc:fc + 1], scalar2=7.0,
                                op0=ALU.add, op1=ALU.min), reads=[t_ps[4 + ei], t_c], writes=[t_eg[ei]])
                            P.op('act', lambda e, pl=pl, ei=ei, fc=fc, ex=ex, N=N: e.activation(
                                out=el[ei][:, 0:N], in_=pl[:, 0:N], func=AF.Identity, bias=bup[:, ex, 8 + fc:9 + fc], scale=1.0),
                                reads=[t_ps[6 + ei], t_c], writes=[t_el[ei]])
                            P.op('act', lambda e, ei=ei, N=N: e.activation(out=es[ei][:, 0:N], in_=eg[ei][:, 0:N], func=AF.Sigmoid, scale=1.702),
                                 reads=[t_eg[ei]], writes=[t_es[ei]])
                            P.op('dve', lambda e, ei=ei, N=N: e.tensor_scalar(
                                out=el[ei][:, 0:N], in0=el[ei][:, 0:N], scalar1=7.0, scalar2=-7.0,
                                op0=ALU.min, op1=ALU.max), reads=[t_el[ei]], writes=[t_el[ei]])
                            P.op('pool' if fc % 2 else 'dve', lambda e, ei=ei, N=N: e.tensor_tensor(out=es[ei][:, 0:N], in0=es[ei][:, 0:N], in1=eg[ei][:, 0:N], op=ALU.mult),
                                 reads=[t_eg[ei], t_es[ei]], writes=[t_es[ei]])
                            P.op('dve', lambda e, ei=ei, ai=ai, fc=fc, N=N: e.scalar_tensor_tensor(
                                out=actT[ai][:, fc, 0:N], in0=el[ei][:, 0:N], scalar=1.0, in1=es[ei][:, 0:N], op0=ALU.add, op1=ALU.mult),
                                reads=[t_es[ei], t_el[ei]], writes=[t_act[ai]])
                        for b4 in range(gb):
                            bi = g * 4 + b4
                            for h in range(2):
                                pb = (bi * 2 + h) % 4

                                def dmm(e, h=h, pb=pb, b4=b4, ai=ai, wi=wi):
                                    for fc in range(8):
                                        ins = e.matmul(ps[pb][:], actT[ai][:, fc, b4 * 128:(b4 + 1) * 128],
                                                       wd[wi][:, fc, h * 512:(h + 1) * 512], start=(fc == 0), stop=(fc == 7))
                                    return ins
                                P.op('pe', dmm, reads=[t_act[ai], t_wd[wi]], writes=[t_ps[pb]])
                                P.op('dve', lambda e, h=h, pb=pb, bi=bi, ex=ex: e.scalar_tensor_tensor(
                                    out=acc[:, bi, h * 512:(h + 1) * 512], in0=ps[pb][:], scalar=gates[:, bi, ex:ex + 1],
                                    in1=acc[:, bi, h * 512:(h + 1) * 512], op0=ALU.mult, op1=ALU.add),
                                    reads=[t_ps[pb], t_gates, t_acc], writes=[t_acc])
                for bi in range(nb):
                    r0 = r_begin + (sb0 + bi) * 128
                    v = 1 if (r0 < self.TC) else 0
                    x_t = xt[bi % 2]; tx = t_xt[bi % 2]
                    P.op('sp', lambda e, x_t=x_t, r0=r0: e.dma_start(out=x_t[:], in_=self.rows(src, r0)), writes=[tx], dma=True)
                    P.op('pool', lambda e, bi=bi, v=v: e.tensor_tensor(out=acc[:, bi, :], in0=acc[:, bi, :], in1=self.gbc[v][:], op=ALU.mult),
                         reads=[t_acc, self.t_gbc], writes=[t_acc])
                    P.op('dve', lambda e, bi=bi, x_t=x_t: e.tensor_tensor(out=x_t[:], in0=x_t[:], in1=acc[:, bi, :], op=ALU.add),
                         reads=[t_acc, tx], writes=[tx])
                    if final:
                        P.op('act', lambda e, x_t=x_t: e.activation(out=junk[:], in_=x_t[:], func=AF.Square, accum_out=small[:, 8:9]),
                             reads=[tx], writes=[t_junk, t_small])
                        P.op('act', lambda e: e.activation(out=small[:, 9:10], in_=small[:, 8:9], func=AF.Sqrt, scale=1.0 / DM, bias=self.epsc[:, 0:1]),
                             reads=[t_small], writes=[t_small])
                        P.op('dve', lambda e: e.reciprocal(out=small[:, 10:11], in_=small[:, 9:10]), reads=[t_small], writes=[t_small])
                        P.op('dve', lambda e, x_t=x_t: e.scalar_tensor_tensor(out=x_t[:], in0=x_t[:], scalar=small[:, 10:11], in1=fw[:],
                                                                              op0=ALU.mult, op1=ALU.mult), reads=[tx, t_small, t_c], writes=[tx])
                        dst_ap = self.out[r0 - self.TC:r0 - self.TC + 128, :]
                    else:
                        dst_ap = self.D[dst][r0:r0 + 128, :]
                    P.op('sp', lambda e, x_t=x_t, dst_ap=dst_ap: e.dma_start(out=dst_ap, in_=x_t[:]), reads=[tx], writes=[t_out], dma=True)
            P.barrier()
            P.emit()


def col(v, n=128):
    return np.ascontiguousarray(np.asarray(v, np.float32).reshape(-1, n).T)


def rep(v, n=128):
    return np.ascontiguousarray(np.broadcast_to(np.asarray(v, np.float32)[None, :], (n, np.asarray(v).shape[0])))


def make_inputs(inp, b, T, TC):
    m = {}
    m['x'] = np.ascontiguousarray(inp['x'][b, :T])
    m['ctx'] = np.ascontiguousarray(inp['ctx'][b, :max(TC, 128)])
    cv = np.stack([col(inp['c'][b]), col(inp['c_ctx'])], axis=-1)
    m['cvec'] = np.ascontiguousarray(cv)
    m['ident'] = np.eye(128, dtype=np.float32)
    m['ones'] = np.ones((128, 128), np.float32)
    for l in range(2):
        p = 'l%d_' % l
        m[p + 'ada_w'] = inp[p + 'ada_w']
        m[p + 'ada_bT'] = col(inp[p + 'ada_b'])
        m[p + 'nmix'] = col(inp[p + 'norm_mix_w'])
        m[p + 'nffn'] = col(inp[p + 'norm_ffn_w'])
        m[p + 'router_w'] = inp[p + 'router_w']
        m[p + 'router_b'] = rep(inp[p + 'router_b'])
        m[p + 'w_up'] = inp[p + 'w_up']
        m[p + 'b_upT'] = np.ascontiguousarray(np.asarray(inp[p + 'b_up']).reshape(NE, 16, 128).transpose(2, 0, 1))
        m[p + 'w_down'] = inp[p + 'w_down']
        m[p + 'b_down'] = inp[p + 'b_down']
    m['final_w'] = rep(inp['final_norm_w'])
    m['l0_w_in'] = inp['l0_w_in']; m['l0_w_out'] = inp['l0_w_out']
    m['l0_w2'] = np.ascontiguousarray(np.stack([inp['l0_gla_w2_f'], inp['l0_gla_w2_b']], axis=1))
    m['l0_b2'] = np.ascontiguousarray(np.stack([col(inp['l0_gla_b_f']), col(inp['l0_gla_b_b'])], axis=1))
    m['l0_lb'] = np.ascontiguousarray(np.asarray(inp['hgrn_lb_logits'], np.float32).reshape(2, 4, 128).transpose(2, 0, 1))
    m['l0_normw'] = rep(np.concatenate([np.tile(inp['l0_gla_norm_w'], 4), np.tile(inp['l0_hgrn_norm_w'], 4)]))
    m['l1_w_in'] = inp['l1_w_in']; m['l1_w_out'] = inp['l1_w_out']
    m['l1_convw'] = np.ascontiguousarray(np.broadcast_to(np.asarray(inp['l1_conv_w'], np.float32)[None], (128, 5, 1536)))
    m['l1_pvec'] = rep(np.concatenate([inp['l1_a_log_f'], inp['l1_dt_bias_f'], inp['l1_a_log_b'], inp['l1_dt_bias_b']]))
    m['l1_dnw'] = rep(np.tile(inp['l1_dn_norm_w'], 4)); m['l1_sinks'] = rep(inp['l1_sinks'])
    m['rope'] = rope_table(T)
    t_ = np.arange(128)
    m['rmask'] = np.ascontiguousarray(np.broadcast_to(np.tile((t_ % 64 != 0).astype(np.float32), 6)[None, :], (128, 768)))
    same = (t_[:, None] // 64) == (t_[None, :] // 64)
    m['mask_f'] = (same & (t_[:, None] <= t_[None, :])).astype(np.float32)
    m['mask_r'] = (same & (t_[:, None] >= t_[None, :])).astype(np.float32)
    m['mask_fs'] = (same & (t_[:, None] < t_[None, :])).astype(np.float32)
    m['mask_rs'] = (same & (t_[:, None] > t_[None, :])).astype(np.float32)
    m['same'] = same.astype(np.float32)
    m['csel'] = np.stack([(t_ < 64), (t_ >= 64)], axis=1).astype(np.float32)
    m['mb_prev'] = np.where(t_[None, :] >= t_[:, None], 0.0, NEG).astype(np.float32)
    m['mb_next'] = np.where(t_[None, :] <= t_[:, None], 0.0, NEG).astype(np.float32)
    return m


def rope_table(T):
    rows = T // 64
    row = np.repeat(np.arange(rows, dtype=np.float32), 64)
    colp = np.tile(np.arange(64, dtype=np.float32), rows)
    inv = (10000.0 ** (-np.arange(16, dtype=np.float32) / 16)).astype(np.float32)
    ang = np.concatenate([row[:, None] * inv, colp[:, None] * inv], axis=-1).astype(np.float32)
    return np.ascontiguousarray(np.concatenate([np.cos(ang), np.sin(ang)], axis=-1).astype(np.float32))


_CACHE = {}


def kernel(**inputs):
    inp = {k: np.asarray(v) for k, v in inputs.items()}
    B, T, _ = inp['x'].shape
    TC = inp['ctx'].shape[1]
    key = (T, TC)
    import os
    ph = os.environ.get('KPHASES')
    bld = Builder(T, TC, phases=tuple(ph.split(','))) if ph else Builder(T, TC)
    nc = bld.build()
    in_maps = []
    for b in range(B):
        m = make_inputs(inp, b, T, TC)
        in_maps.append({k: m[k] for k in bld.in_names})
    res = run_bass_kernel_spmd(nc, in_maps, core_ids=list(range(B)))
    return np.stack([res.results[b]['out'] for b in range(B)], axis=0)


def phase_l0mix(self, src, dst):
    nc, P, D = self.nc, self.P, self.D
    TC, T = self.TC, self.T
    with ExitStack() as st:
        sb = lambda name, shape, d=F32: st.enter_context(nc.sbuf_tensor(name, shape, d))
        ps = [st.enter_context(nc.psum_tensor('pq%d' % i, [128, 512], F32)) for i in range(8)]
        ps4b = ps[4][:].bitcast(BF16)
        self.epsc = sb('epsc0', [128, 1])
        onec = sb('onec', [128, 1])
        win = sb('win', [128, 8, 4128], BF16)
        wout = sb('wout', [128, 8, DM], BF16)
        w2 = sb('w2', [16, 2, 256], BF16)
        b2 = sb('b2', [128, 2, 2]); lbl = sb('lbl', [128, 2, 4]); lbc = sb('lbc', [128, 4]); omlb = sb('omlb', [128, 4])
        normw = sb('normw', [128, DM]); rmask = sb('rmask_t', [128, 768])
        maskf = sb('maskf', [128, 128]); maskr = sb('maskr', [128, 128])
        xt = [sb('mxt%d' % i, [128, DM]) for i in range(2)]
        xn = sb('mxn', [128, DM]); junk = sb('mjunk', [128, DM], BF16); small = sb('msmall', [128, 32])
        hT = sb('hT', [128, 8, 128], BF16)
        arT = sb('arT', [16, 128], BF16)
        LF = sb('LF', [128, 6, 128]); bb = sb('bb', [128, 6, 128]); EA = sb('EA', [128, 6, 128]); EB = sb('EB', [128, 6, 128])
        E = sb('E', [128, 6, 2]); kH = sb('kH', [128, 4, 128]); sg = sb('sg', [128, 4, 128])
        qT = sb('qT', [128, 6, 128], BF16); kT = sb('kT', [128, 6, 128], BF16)
        ktok = sb('ktok', [128, 768], BF16); vtok = sb('vtok', [128, DM], BF16)
        SCm = sb('SCm', [128, 8, 128], BF16)
        S = sb('S', [128, 6, 128]); Tmp = sb('Tmp', [128, 6, 128])
        SB = [sb('SB%d' % i, [128, 6, 128], BF16) for i in range(4)]
        Oc = sb('Oc', [128, DM]); ofl = sb('ofl', [128, DM]); og = sb('og', [128, DM])
        yb = sb('yb', [128, DM], BF16); yT = sb('yT', [128, 8, 128], BF16)
        tk = {n: Tok() for n in ('c', 'x0', 'x1', 'xn', 'junk', 'small', 'hT', 'arT', 'LF', 'bb', 'EA', 'EB', 'E', 'kH', 'sg',
                                 'qT', 'kT', 'ktok', 'vtok', 'SCm', 'S', 'Tmp', 'SB0', 'SB1', 'SB2', 'SB3', 'Oc', 'ofl', 'og', 'yb', 'yT', 'out')}
        tp = [Tok() for _ in range(8)]
        c_ = [tk['c']]
        P.op('pool', lambda e: e.memset(self.epsc[:], EPS), writes=c_)
        P.op('pool', lambda e: e.memset(onec[:], 1.0), writes=c_)
        for (a, b_) in ((0, 2048), (2048, 4096), (4096, 4128)):
            P.op('pool', lambda e, a=a, b_=b_: e.dma_start(out=win[:, :, a:b_], in_=D['l0_w_in'][:, a:b_].rearrange("(k p) n -> p k n", p=128)),
                 writes=c_, dma=True)
        P.op('pool', lambda e: e.dma_start(out=wout[:], in_=D['l0_w_out'].rearrange("(k p) n -> p k n", p=128)), writes=c_, dma=True)
        P.op('pool', lambda e: e.dma_start(out=w2[:], in_=D['l0_w2']), writes=c_, dma=True)
        for nm, t_ in (('l0_b2', b2), ('l0_lb', lbl), ('l0_normw', normw), ('rmask', rmask), ('mask_f', maskf), ('mask_r', maskr)):
            P.op('sp', lambda e, nm=nm, t_=t_: e.dma_start(out=t_[:], in_=D[nm]), writes=c_, dma=True)
        P.op('dve', lambda e: e.tensor_scalar(out=b2[:], in0=b2[:], scalar1=-1.0, scalar2=None, op0=ALU.mult), reads=c_, writes=c_)
        P.op('dve', lambda e: e.tensor_tensor(out=lbc[:], in0=lbl[:, 0, :], in1=lbl[:, 1, :], op=ALU.subtract), reads=c_, writes=c_)
        P.op('act', lambda e: e.activation(out=omlb[:], in_=lbc[:], func=AF.Sigmoid, scale=-1.0), reads=c_, writes=c_)
        P.op('act', lambda e: e.activation(out=lbc[:], in_=lbc[:], func=AF.Sigmoid), reads=c_, writes=c_)
        self.gate_bcast(Oc[:].rearrange("p (k t) -> p k t", k=8), ps[0:2], 0, 16, (0, 1) if TC else (0,))
        P.barrier()
        A1 = self.Acol[0][0]; modT = self.modT[0]

        def mmgroup(pb, outs):
            def f(e):
                for out_ap, pairs in outs:
                    n = len(pairs)
                    for i, (l_, r_) in enumerate(pairs):
                        ins = e.matmul(out_ap, l_, r_, start=(i == 0), stop=(i == n - 1))
                return ins
            return f

        import os
        STOP = int(os.environ.get('L0STOP', '99'))

        def block(r0, is_ctx, d, first, bidx):
            v = 1 if is_ctx else 0
            x_t = xt[bidx % 2]; tx = tk['x%d' % (bidx % 2)]
            self.norm_xn(self.rows(src, r0), x_t, tx, xn, tk['xn'], small, tk['small'], junk, tk['junk'])
            for h in range(2):
                P.op('pe', mmgroup(None, []) if False else (lambda e, h=h: [e.transpose(ps[h][:, j * 128:(j + 1) * 128], xn[:, (h * 4 + j) * 128:(h * 4 + j + 1) * 128], self.ident[:]) for j in range(4)][-1]),
                     reads=[tk['xn'], self.t_const], writes=[tp[h]])
                for j in range(4):
                    k = h * 4 + j
                    P.op('act', lambda e, h=h, j=j, k=k: e.activation(out=hT[:, k, :], in_=ps[h][:, j * 128:(j + 1) * 128], func=AF.Identity,
                                                                      scale=A1[:, k, v:v + 1], bias=modT[:, k, v:v + 1]),
                         reads=[tp[h], self.t_mod], writes=[tk['hT']])
            rh = [tk['hT'], tk['c']]
            wcol = lambda c0, n=128: [(win[:, k, c0:c0 + n], hT[:, k, :]) for k in range(8)]
            c_ar = 1024 + d * 16
            P.op('pe', mmgroup(0, [(ps[0][0:16, 0:128], [(win[:, k, c_ar:c_ar + 16], hT[:, k, :]) for k in range(8)])]), reads=rh, writes=[tp[0]])
            P.op('act', lambda e: e.activation(out=arT[:], in_=ps[0][0:16, 0:128], func=AF.Identity), reads=[tp[0]], writes=[tk['arT']])
            P.op('pe', mmgroup(0, [(ps[0][:, 128 + c * 128:256 + c * 128], [(w2[:, d, c * 128:(c + 1) * 128], arT[:])]) for c in range(2)]),
                 reads=[tk['arT'], tk['c']], writes=[tp[0]])
            if STOP < 3:
                return
            c_bz = 2080 + d * 512
            P.op('pe', mmgroup(1, [(ps[1][:, c * 128:(c + 1) * 128], wcol(c_bz + c * 128)) for c in range(4)]), reads=rh, writes=[tp[1]])
            if STOP < 4:
                return
            for c in range(2):
                P.op('act', lambda e, c=c: e.activation(out=LF[:, c, :], in_=ps[0][:, 128 + c * 128:256 + c * 128], func=AF.Exp,
                                                        scale=-1.0, bias=b2[:, d, c:c + 1]), reads=[tp[0], tk['c']], writes=[tk['LF']])
            P.op('act', lambda e: e.activation(out=LF[:, 0:2, :], in_=LF[:, 0:2, :], func=AF.Ln, bias=onec[:, 0:1], scale=1.0),
                 reads=[tk['c']], writes=[tk['LF']])
            P.op('act', lambda e: e.activation(out=sg[:], in_=ps[1][:].rearrange("p (c t) -> p c t", c=4), func=AF.Sigmoid),
                 reads=[tp[1]], writes=[tk['sg']])
            P.op('dve', lambda e: e.tensor_scalar(out=LF[:, 0:2, :], in0=LF[:, 0:2, :], scalar1=-1.0 / 16.0, scalar2=None, op0=ALU.mult),
                 writes=[tk['LF']])
            P.op('dve', lambda e: e.tensor_tensor(out=sg[:], in0=sg[:], in1=omlb[:].unsqueeze(2).to_broadcast([128, 4, 128]), op=ALU.mult),
                 reads=[tk['c']], writes=[tk['sg']])
            P.op('dve', lambda e: e.tensor_tensor(out=sg[:], in0=sg[:], in1=lbc[:].unsqueeze(2).to_broadcast([128, 4, 128]), op=ALU.add),
                 reads=[tk['c']], writes=[tk['sg']])
            P.op('act', lambda e: e.activation(out=LF[:, 2:6, :], in_=sg[:], func=AF.Ln), reads=[tk['sg']], writes=[tk['LF']])
            P.op('dve', lambda e: e.tensor_scalar(out=kH[:], in0=sg[:], scalar1=-1.0, scalar2=1.0, op0=ALU.mult, op1=ALU.add),
                 reads=[tk['sg']], writes=[tk['kH']])
            if STOP < 5:
                return
            LF2 = LF[:].rearrange("p c t -> p (c t)"); bb2 = bb[:].rearrange("p c t -> p (c t)")
            P.op('dve', lambda e: e.tensor_tensor_scan(out=bb2, data0=rmask[:], data1=LF2, initial=0.0, op0=ALU.mult, op1=ALU.add),
                 reads=[tk['LF'], tk['c']], writes=[tk['bb']])
            tot = bb[:].rearrange("p c (a t) -> p c a t", a=2)[:, :, :, 63]
            P.op('act', lambda e: e.activation(out=E[:], in_=tot, func=AF.Exp), reads=[tk['bb']], writes=[tk['E']])
            if d == 1:
                P.op('dve', lambda e: e.tensor_tensor(out=bb[:], in0=bb[:], in1=LF[:], op=ALU.subtract), reads=[tk['LF']], writes=[tk['bb']])
            sa, sb_ = (1.0, -1.0) if d == 0 else (-1.0, 1.0)
            P.op('act', lambda e: e.activation(out=EA[:], in_=bb[:], func=AF.Exp, scale=sa), reads=[tk['bb']], writes=[tk['EA']])
            P.op('act', lambda e: e.activation(out=EB[:], in_=bb[:], func=AF.Exp, scale=sb_), reads=[tk['bb']], writes=[tk['EB']])
            if STOP < 6:
                return
            P.op('pe', mmgroup(2, [(ps[2][:, 0:128], wcol(0)), (ps[2][:, 128:256], wcol(128)),
                                   (ps[2][:, 256:384], wcol(1568)), (ps[2][:, 384:512], wcol(1696))]), reads=rh, writes=[tp[2]])
            P.op('pe', mmgroup(3, [(ps[3][:, 0:128], wcol(1824)), (ps[3][:, 128:256], wcol(1952)),
                                   (ps[3][:, 256:384], wcol(256)), (ps[3][:, 384:512], wcol(384))]), reads=rh, writes=[tp[3]])
            v3 = lambda ap, n: ap.rearrange("p (c t) -> p c t", c=n)
            P.op('dve', lambda e: e.scalar_tensor_tensor(out=qT[:, 0:2, :], in0=v3(ps[2][:, 0:256], 2), scalar=0.125, in1=EA[:, 0:2, :],
                                                         op0=ALU.mult, op1=ALU.mult), reads=[tp[2], tk['EA']], writes=[tk['qT']])
            P.op('dve', lambda e: e.tensor_tensor(out=qT[:, 2:4, :], in0=v3(ps[2][:, 256:512], 2), in1=EA[:, 2:4, :], op=ALU.mult),
                 reads=[tp[2], tk['EA']], writes=[tk['qT']])
            P.op('dve', lambda e: e.tensor_tensor(out=qT[:, 4:6, :], in0=v3(ps[3][:, 0:256], 2), in1=EA[:, 4:6, :], op=ALU.mult),
                 reads=[tp[3], tk['EA']], writes=[tk['qT']])
            P.op('dve', lambda e: e.tensor_tensor(out=kT[:, 0:2, :], in0=v3(ps[3][:, 256:512], 2), in1=EB[:, 0:2, :], op=ALU.mult),
                 reads=[tp[3], tk['EB']], writes=[tk['kT']])
            P.op('pool', lambda e: e.tensor_tensor(out=kT[:, 2:6, :], in0=kH[:], in1=EB[:, 2:6, :], op=ALU.mult),
                 reads=[tk['kH'], tk['EB']], writes=[tk['kT']])
            if STOP < 7:
                return
            P.op('pe', lambda e: [e.transpose(ps4b[:, c * 128:(c + 1) * 128], kT[:, c, :], self.identb[:]) for c in range(6)][-1],
                 reads=[tk['kT'], self.t_const], writes=[tp[4]])
            P.op('act', lambda e: e.activation(out=ktok[:], in_=ps4b[:, 0:768], func=AF.Identity), reads=[tp[4]], writes=[tk['ktok']])
            if STOP < 8:
                return
            for i, c0 in enumerate((512, 3104)):
                P.op('pe', mmgroup(5, [(ps[5][:], [(hT[:, k, :], win[:, k, c0:c0 + 512]) for k in range(8)])]), reads=rh, writes=[tp[5]])
                P.op('act', lambda e, i=i: e.activation(out=vtok[:, i * 512:(i + 1) * 512], in_=ps[5][:], func=AF.Identity),
                     reads=[tp[5]], writes=[tk['vtok']])
            if STOP < 9:
                return
            def rows_of(h):
                if h < 4:
                    return slice((h % 2) * 64, (h % 2) * 64 + 64), h // 2
                return slice(0, 128), h - 2
            bankheads = ((0, 2, 4, 5), (1, 3, 6, 7))
            scidx = {h: half * 4 + hh for half in range(2) for hh, h in enumerate(bankheads[half])}
            for half in range(2):
                outs = []
                for hh in range(4):
                    h = bankheads[half][hh]
                    rs, bk = rows_of(h)
                    outs.append((ps[6 + half][:, hh * 128:(hh + 1) * 128], [(kT[rs, bk, :], qT[rs, bk, :])]))
                P.op('pe', mmgroup(6 + half, outs), reads=[tk['kT'], tk['qT']], writes=[tp[6 + half]])
                mk = maskf if d == 0 else maskr
                P.op('dve', lambda e, half=half, mk=mk: e.tensor_tensor(
                    out=SCm[:, half * 4:(half + 1) * 4, :], in0=v3(ps[6 + half][:], 4), in1=mk[:].unsqueeze(1).to_broadcast([128, 4, 128]),
                    op=ALU.mult), reads=[tp[6 + half], tk['c']], writes=[tk['SCm']])
            if STOP < 10:
                return
            order = (0, 1) if d == 0 else (1, 0)
            pbank = {order[0]: (0, 1), order[1]: (2, 3)}
            for c in order:
                pa, pb_ = pbank[c]
                cs = slice(c * 64, c * 64 + 64)
                outsA, outsB = [], []
                for h in range(8):
                    rs, bk = rows_of(h)
                    if h < 4:
                        l_ = ktok[cs, bk * 128 + (h % 2) * 64: bk * 128 + (h % 2) * 64 + 64]
                    else:
                        l_ = ktok[cs, bk * 128:(bk + 1) * 128]
                    r_ = vtok[cs, h * 128:(h + 1) * 128]
                    if bk < 4:
                        outsA.append((ps[pa][rs, bk * 128:(bk + 1) * 128], [(l_, r_)]))
                    else:
                        outsB.append((ps[pb_][rs, (bk - 4) * 128:(bk - 3) * 128], [(l_, r_)]))
                P.op('pe', mmgroup(pa, outsA), reads=[tk['ktok'], tk['vtok']], writes=[tp[pa]])
                P.op('pe', mmgroup(pb_, outsB), reads=[tk['ktok'], tk['vtok']], writes=[tp[pb_]])
            if STOP < 11:
                return
            st_i = 2 * (bidx % 2); mid_i = st_i + 1; end_i = 2 * ((bidx + 1) % 2)
            if first:
                P.op('pool', lambda e: e.memset(S[:], 0.0), writes=[tk['S']])
                P.op('pool', lambda e: e.memset(SB[st_i][:], 0.0), writes=[tk['SB%d' % st_i]])
            for i, c in enumerate(order):
                pa, pb_ = pbank[c]
                Ebc = E[:, :, c:c + 1].to_broadcast([128, 6, 128])
                PA = v3(ps[pa][:], 4); PB = v3(ps[pb_][:, 0:256], 2)
                if d == 0:
                    wi_ = mid_i if i == 0 else end_i
                    P.op('dve', lambda e, PA=PA: e.tensor_tensor(out=Tmp[:, 0:4, :], in0=PA, in1=S[:, 0:4, :], op=ALU.add),
                         reads=[tp[pa], tk['S']], writes=[tk['Tmp']])
                    P.op('dve', lambda e, PB=PB: e.tensor_tensor(out=Tmp[:, 4:6, :], in0=PB, in1=S[:, 4:6, :], op=ALU.add),
                         reads=[tp[pb_], tk['S']], writes=[tk['Tmp']])
                    P.op('dve', lambda e, Ebc=Ebc: e.tensor_tensor(out=S[:], in0=Tmp[:], in1=Ebc, op=ALU.mult),
                         reads=[tk['Tmp'], tk['E']], writes=[tk['S']])
                    P.op('act', lambda e, wi_=wi_: e.activation(out=SB[wi_][:], in_=S[:], func=AF.Identity),
                         reads=[tk['S']], writes=[tk['SB%d' % wi_]])
                else:
                    wi_ = st_i if i == 0 else mid_i
                    P.op('dve', lambda e, Ebc=Ebc: e.tensor_tensor(out=S[:], in0=S[:], in1=Ebc, op=ALU.mult),
                         reads=[tk['E']], writes=[tk['S']])
                    P.op('act', lambda e, wi_=wi_: e.activation(out=SB[wi_][:], in_=S[:], func=AF.Identity),
                         reads=[tk['S']], writes=[tk['SB%d' % wi_]])
                    P.op('dve', lambda e, PA=PA: e.tensor_tensor(out=S[:, 0:4, :], in0=PA, in1=S[:, 0:4, :], op=ALU.add),
                         reads=[tp[pa]], writes=[tk['S']])
                    P.op('dve', lambda e, PB=PB: e.tensor_tensor(out=S[:, 4:6, :], in0=PB, in1=S[:, 4:6, :], op=ALU.add),
                         reads=[tp[pb_]], writes=[tk['S']])
            if STOP < 12:
                return
            for half in range(2):
                def omm(e, half=half):
                    for hh in range(4):
                        h = half * 4 + hh
                        rs, bk = rows_of(h)
                        ob = ps[4 + half]
                        e.matmul(ob[:, hh * 128:(hh + 1) * 128], SCm[:, scidx[h], :], vtok[:, h * 128:(h + 1) * 128], start=True, stop=False)
                        for i, c in enumerate(order):
                            snap = SB[st_i] if i == 0 else SB[mid_i]
                            ins = e.matmul(ob[c * 64:(c + 1) * 64, hh * 128:(hh + 1) * 128], qT[rs, bk, c * 64:(c + 1) * 64],
                                           snap[rs, bk, :], start=False, stop=True)
                    return ins
                P.op('pe', omm, reads=[tk['SCm'], tk['vtok'], tk['qT'], tk['SB%d' % st_i], tk['SB%d' % mid_i]], writes=[tp[4 + half]])
            if STOP < 13:
                return
            if d == 0:
                for half in range(2):
                    P.op('act', lambda e, half=half: e.activation(out=Oc[:, half * 512:(half + 1) * 512], in_=ps[4 + half][:], func=AF.Identity),
                         reads=[tp[4 + half]], writes=[tk['Oc']])
                P.op('sp', lambda e: e.dma_start(out=D['OF'][r0:r0 + 128, :], in_=Oc[:]), reads=[tk['Oc']], writes=[tk['out']], dma=True)
                return
            P.op('sp', lambda e: e.dma_start(out=ofl[:], in_=D['OF'][r0:r0 + 128, :]), writes=[tk['ofl']], dma=True)
            for half in range(2):
                P.op('dve', lambda e, half=half: e.tensor_tensor(out=Oc[:, half * 512:(half + 1) * 512], in0=ps[4 + half][:],
                                                                 in1=ofl[:, half * 512:(half + 1) * 512], op=ALU.add),
                     reads=[tp[4 + half], tk['ofl']], writes=[tk['Oc']])
            for half, c0 in enumerate((1056, 3616)):
                P.op('pe', mmgroup(6 + half, [(ps[6 + half][:], [(hT[:, k, :], win[:, k, c0:c0 + 512]) for k in range(8)])]),
                     reads=rh, writes=[tp[6 + half]])
                P.op('act', lambda e, half=half: e.activation(out=og[:, half * 512:(half + 1) * 512], in_=ps[6 + half][:], func=AF.Silu),
                     reads=[tp[6 + half]], writes=[tk['og']])
            O3 = Oc[:].rearrange("p (h v) -> p h v", h=8)
            P.op('pool', lambda e: e.tensor_tensor(out=ofl[:], in0=Oc[:], in1=Oc[:], op=ALU.mult), reads=[tk['Oc']], writes=[tk['ofl']])
            P.op('dve', lambda e: e.tensor_reduce(out=small[:, 8:16], in_=ofl[:].rearrange("p (h v) -> p h v", h=8), axis=AX.X, op=ALU.add),
                 reads=[tk['ofl']], writes=[tk['small']])
            P.op('act', lambda e: e.activation(out=small[:, 16:24], in_=small[:, 8:16], func=AF.Sqrt, scale=1.0 / 128, bias=self.epsc[:, 0:1]),
                 reads=[tk['small']], writes=[tk['small']])
            P.op('dve', lambda e: e.reciprocal(out=small[:, 24:32], in_=small[:, 16:24]), reads=[tk['small']], writes=[tk['small']])
            P.op('dve', lambda e: e.tensor_tensor(out=O3, in0=O3, in1=small[:, 24:32].unsqueeze(2).to_broadcast([128, 8, 128]), op=ALU.mult),
                 reads=[tk['small']], writes=[tk['Oc']])
            P.op('pool', lambda e: e.tensor_tensor(out=Oc[:], in0=Oc[:], in1=normw[:], op=ALU.mult), reads=[tk['c']], writes=[tk['Oc']])
            P.op('dve', lambda e: e.tensor_tensor(out=yb[:], in0=Oc[:], in1=og[:], op=ALU.mult), reads=[tk['Oc'], tk['og']], writes=[tk['yb']])
            P.op('pe', lambda e: [e.transpose(ps4b[:, k * 128:(k + 1) * 128], yb[:, k * 128:(k + 1) * 128], self.identb[:]) for k in range(8)][-1],
                 reads=[tk['yb'], self.t_const], writes=[tp[4]])
            P.op('act', lambda e: e.activation(out=yT[:].rearrange("p k t -> p (k t)"), in_=ps4b[:, 0:1024], func=AF.Identity),
                 reads=[tp[4]], writes=[tk['yT']])
            for half in range(2):
                P.op('pe', mmgroup(6 + half, [(ps[6 + half][:], [(yT[:, k, :], wout[:, k, half * 512:(half + 1) * 512]) for k in range(8)])]),
                     reads=[tk['yT'], tk['c']], writes=[tp[6 + half]])
                P.op('dve', lambda e, half=half: e.tensor_tensor(out=Oc[:, half * 512:(half + 1) * 512], in0=ps[6 + half][:],
                                                                 in1=self.gbc[v][:, half * 512:(half + 1) * 512], op=ALU.mult),
                     reads=[tp[6 + half], self.t_gbc], writes=[tk['Oc']])
            P.op('pool', lambda e: e.tensor_tensor(out=x_t[:], in0=x_t[:], in1=Oc[:], op=ALU.add), reads=[tk['Oc']], writes=[tx])
            P.op('sp', lambda e: e.dma_start(out=D[dst][r0:r0 + 128, :], in_=x_t[:]), reads=[tx], writes=[tk['out']], dma=True)

        NCB, NB = self.NCB, self.NB
        for d in range(2):
            seq = [(i * 128, True) for i in range(NCB)] + [(TC + i * 128, False) for i in range(NB)]
            if d == 1:
                seq = [(i * 128, True) for i in reversed(range(NCB))] + [(TC + i * 128, False) for i in reversed(range(NB))]
            for n, (r0, is_ctx) in enumerate(seq):
                block(r0, is_ctx, d, n == 0, n)
            P.barrier()
        P.emit()


Builder.phase_l0mix = phase_l0mix


def phase_final(self, src):
    nc, P, D = self.nc, self.P, self.D
    with ExitStack() as st:
        sb = lambda name, shape, d=F32: st.enter_context(nc.sbuf_tensor(name, shape, d))
        self.epsc = sb('epscf', [128, 1])
        xt = [sb('fxt%d' % i, [128, DM]) for i in range(2)]
        junk = sb('fjunk', [128, DM], BF16); small = sb('fsmall', [128, 8]); fw = sb('ffw', [128, DM])
        tc_, tj, ts, to = Tok(), Tok(), Tok(), Tok()
        txs = [Tok(), Tok()]
        P.op('pool', lambda e: e.memset(self.epsc[:], EPS), writes=[tc_])
        P.op('sp', lambda e: e.dma_start(out=fw[:], in_=D['final_w']), writes=[tc_], dma=True)
        for bi in range(self.NB):
            r0 = self.TC + bi * 128
            x_t = xt[bi % 2]; tx = txs[bi % 2]
            self.norm_xn(self.rows(src, r0), x_t, tx, x_t, tx, small, ts, junk, tj)
            P.op('dve', lambda e, x_t=x_t: e.tensor_tensor(out=x_t[:], in0=x_t[:], in1=fw[:], op=ALU.mult), reads=[tc_], writes=[tx])
            P.op('sp', lambda e, x_t=x_t, r0=r0: e.dma_start(out=self.out[r0 - self.TC:r0 - self.TC + 128, :], in_=x_t[:]),
                 reads=[tx], writes=[to], dma=True)
        P.barrier()
        P.emit()


Builder.phase_final = phase_final


NEG = -30000.0


def phase_l1mix(self, src, dst):
    nc, P, D = self.nc, self.P, self.D
    TC, T, NCB, NB = self.TC, self.T, self.NCB, self.NB
    R = TC + T
    cxrow = lambda r: (2 + r) if r < TC else (r + 6)
    A1 = self.Acol[1][0]; modT = self.modT[1]

    def mmgroup(outs):
        def f(e):
            for out_ap, pairs in outs:
                n = len(pairs)
                for i, (l_, r_) in enumerate(pairs):
                    ins = e.matmul(out_ap, l_, r_, start=(i == 0), stop=(i == n - 1))
            return ins
        return f

    def trs(dsts_srcs, idt):
        def f(e):
            for o_, i_ in dsts_srcs:
                ins = e.transpose(o_, i_, idt)
            return ins
        return f

    with ExitStack() as st:
        sb = lambda name, shape, d=F32: st.enter_context(nc.sbuf_tensor(name, shape, d))
        ps = [st.enter_context(nc.psum_tensor('pr%d' % i, [128, 512], F32)) for i in range(8)]
        psb = [p_[:].bitcast(BF16) for p_ in ps]
        self.epsc = sb('epsc1', [128, 1]); onec = sb('onec1', [128, 1])
        win = sb('win1', [128, 8, 2832], BF16); wout = sb('wout1', [128, 8, DM], BF16)
        convw = sb('convw', [128, 5, 1536], BF16); pvec = sb('pvec', [128, 16]); dnw = sb('dnw', [128, 512]); sinks = sb('sinks', [128, 8])
        maskf = sb('maskf1', [128, 128]); maskr = sb('maskr1', [128, 128]); maskfs = sb('maskfs', [128, 128]); maskrs = sb('maskrs', [128, 128])
        same = sb('same_t', [128, 128]); csel = sb('csel_t', [128, 2]); mbp = sb('mbp', [128, 128]); mbn = sb('mbn', [128, 128])
        xt = [sb('lxt%d' % i, [128, DM]) for i in range(2)]
        xn = sb('lxn', [128, DM]); junk = sb('ljunk', [128, DM], BF16); small = sb('lsmall', [128, 64])
        hT = sb('lhT', [128, 8, 128], BF16)
        cxs = sb('cxs', [128, 1536]); cxl = [sb('cxl%d' % i, [128, 1536]) for i in range(2)]
        kv = sb('kv', [128, 256]); rope = sb('rope_t', [128, 64]); rt = sb('rt', [128, 8, 64]); kTs = sb('kTs', [128, 128])
        tk = {n: Tok() for n in ('c', 'x0', 'x1', 'xn', 'junk', 'small', 'hT', 'cxs', 'cxl0', 'cxl1', 'kv', 'rope', 'rt', 'kTs', 'out',
                                 'qkv', 'qkb', 'fT', 'g', 'gs', 'GB', 'Lts', 'Lst', 'A', 'AT', 'Q', 'QT', 'Rm', 'Rb', 'kbg', 'kd', 'bv',
                                 'u', 'wT', 'S', 'Sb0', 'Sb1', 'vn', 'Pst', 'egr', 'qg', 'Oc', 'ofl', 'og', 'yb', 'yT', 'q8', 'qT8',
                                 'kTl', 'vl', 'kcT', 'vc', 'ssb', 'pb', 'pT', 'att')}
        tp = [Tok() for _ in range(8)]
        c_ = [tk['c']]
        P.op('pool', lambda e: e.memset(self.epsc[:], EPS), writes=c_)
        P.op('pool', lambda e: e.memset(onec[:], 1.0), writes=c_)
        for (a, b_) in ((0, 2048), (2048, 2832)):
            P.op('pool', lambda e, a=a, b_=b_: e.dma_start(out=win[:, :, a:b_], in_=D['l1_w_in'][:, a:b_].rearrange("(k p) n -> p k n", p=128)),
                 writes=c_, dma=True)
        P.op('pool', lambda e: e.dma_start(out=wout[:], in_=D['l1_w_out'].rearrange("(k p) n -> p k n", p=128)), writes=c_, dma=True)
        P.op('pool', lambda e: e.dma_start(out=convw[:], in_=D['l1_convw']), writes=c_, dma=True)
        for nm, t_ in (('l1_pvec', pvec), ('l1_dnw', dnw), ('l1_sinks', sinks), ('mask_f', maskf), ('mask_r', maskr),
                       ('mask_fs', maskfs), ('mask_rs', maskrs), ('same', same), ('csel', csel), ('mb_prev', mbp), ('mb_next', mbn)):
            P.op('sp', lambda e, nm=nm, t_=t_: e.dma_start(out=t_[:], in_=D[nm]), writes=c_, dma=True)
        for c0 in (0, 8):
            P.op('act', lambda e, c0=c0: e.activation(out=pvec[:, c0:c0 + 4], in_=pvec[:, c0:c0 + 4], func=AF.Exp), reads=c_, writes=c_)
            P.op('dve', lambda e, c0=c0: e.tensor_scalar(out=pvec[:, c0:c0 + 4], in0=pvec[:, c0:c0 + 4], scalar1=-1.0, scalar2=None, op0=ALU.mult),
                 reads=c_, writes=c_)
        P.op('pool', lambda e: e.memset(cxs[:], 0.0), writes=[tk['cxs']])
        for r in (0, TC + 2, TC + 4, TC + T + 6):
            P.op('sp', lambda e, r=r: e.dma_start(out=D['CX'][r:r + 2, :], in_=cxs[0:2, :]), reads=[tk['cxs']], writes=[tk['out']], dma=True)
        self.gate_bcast(xn[:].rearrange("p (k t) -> p k t", k=8), ps[0:2], 1, 16, (0,))
        P.barrier()

        def norm_hT(r0, v, bidx):
            x_t = xt[bidx % 2]; tx = tk['x%d' % (bidx % 2)]
            self.norm_xn(self.rows(src, r0), x_t, tx, xn, tk['xn'], small, tk['small'], junk, tk['junk'])
            for h in range(2):
                P.op('pe', trs([(ps[h][:, j * 128:(j + 1) * 128], xn[:, (h * 4 + j) * 128:(h * 4 + j + 1) * 128]) for j in range(4)], self.ident[:]),
                     reads=[tk['xn'], self.t_const], writes=[tp[h]])
                for j in range(4):
                    k = h * 4 + j
                    P.op('act', lambda e, h=h, j=j, k=k: e.activation(out=hT[:, k, :], in_=ps[h][:, j * 128:(j + 1) * 128], func=AF.Identity,
                                                                      scale=A1[:, k, v:v + 1], bias=modT[:, k, v:v + 1]),
                         reads=[tp[h], self.t_mod], writes=[tk['hT']])
            return x_t, tx

        rh = [tk['hT'], tk['c']]
        tokmm = lambda c0, n: [(hT[:, k, :], win[:, k, c0:c0 + n]) for k in range(8)]

        def pre_block(r0, is_ctx, bidx):
            norm_hT(r0, 1 if is_ctx else 0, bidx)
            for j in range(3):
                P.op('pe', mmgroup([(ps[2 + j][:], tokmm(j * 512, 512))]), reads=rh, writes=[tp[2 + j]])
                P.op('act', lambda e, j=j: e.activation(out=cxs[:, j * 512:(j + 1) * 512], in_=ps[2 + j][:], func=AF.Identity),
                     reads=[tp[2 + j]], writes=[tk['cxs']])
            cr = cxrow(r0)
            P.op('sp', lambda e: e.dma_start(out=D['CX'][cr:cr + 128, :], in_=cxs[:]), reads=[tk['cxs']], writes=[tk['out']], dma=True)
            P.op('pe', mmgroup([(ps[5][:, 0:256], tokmm(2576, 256))]), reads=rh, writes=[tp[5]])
            P.op('act', lambda e: e.activation(out=kv[:], in_=ps[5][:, 0:256], func=AF.Identity), reads=[tp[5]], writes=[tk['kv']])
            if not is_ctx:
                t0 = r0 - TC
                P.op('sp', lambda e: e.dma_start(out=rope[:], in_=D['rope'][t0:t0 + 128, :]), writes=[tk['rope']], dma=True)
                self.apply_rope(kv[:, 0:128].rearrange("p (h f) -> p h f", h=2), 2, rope, rt, [tk['kv']], tk['rope'], tk['rt'])
            P.op('sp', lambda e: e.dma_start(out=D['VV'][r0:r0 + 128, :], in_=kv[:, 128:256]), reads=[tk['kv']], writes=[tk['out']], dma=True)
            P.op('pe', trs([(ps[6][:, 0:128], kv[:, 0:128])], self.ident[:]), reads=[tk['kv'], self.t_const], writes=[tp[6]])
            P.op('act', lambda e: e.activation(out=kTs[:], in_=ps[6][:, 0:128], func=AF.Identity), reads=[tp[6]], writes=[tk['kTs']])
            P.op('sp', lambda e: e.dma_start(out=D['KT'][:, r0:r0 + 128], in_=kTs[:]), reads=[tk['kTs']], writes=[tk['out']], dma=True)
            P.op('sp', lambda e: e.dma_start(out=D['KT2'][0:64, r0:r0 + 128], in_=kTs[64:128, :]), reads=[tk['kTs']], writes=[tk['out']], dma=True)
            P.op('sp', lambda e: e.dma_start(out=D['KT2'][64:128, r0:r0 + 128], in_=kTs[0:64, :]), reads=[tk['kTs']], writes=[tk['out']], dma=True)

        seq_all = [(i * 128, True) for i in range(NCB)] + [(TC + i * 128, False) for i in range(NB)]
        for n, (r0, is_ctx) in enumerate(seq_all):
            pre_block(r0, is_ctx, n)
        P.barrier()

        qkv = cxs; qkb = sb('qkb', [128, 8, 128], BF16); fT = sb('fT', [128, 8, 128], BF16)
        sm16 = small
        gs = sb('gs', [128, 8]); GB = sb('GB', [128, 4, 128]); totrow = sb('totrow', [128, 8])
        Lts = sb('Lts', [128, 4, 128]); Lst = sb('Lst', [128, 4, 128]); LstS = sb('LstS', [128, 4, 128])
        Am = sb('Am', [128, 4, 128]); AT = sb('AT', [128, 4, 128]); Qm = sb('Qm', [128, 4, 128]); QT = sb('QT', [128, 4, 128])
        Rm = sb('Rm', [128, 4, 128]); Rb = sb('Rb', [128, 4, 128], BF16)
        kbg = sb('kbg', [128, 4, 128], BF16); kd = sb('kd', [128, 4, 128], BF16); bv = sb('bv', [128, 4, 128], BF16)
        u = sb('u', [128, 4, 128]); wT = sb('wT', [128, 4, 128], BF16)
        S = sb('S1', [128, 4, 128]); Sb = [sb('Sb%d' % i, [128, 4, 128], BF16) for i in range(2)]
        vn = sb('vn', [128, 4, 128], BF16); Pst = sb('Pst', [128, 4, 128], BF16); egr = sb('egr', [128, 4, 128]); qg = sb('qg', [128, 4, 128], BF16)
        Oc = sb('Oc1', [128, DM]); ofl = sb('ofl1', [128, 512]); og = sb('og1', [128, 512]); yb = sb('yb1', [128, DM], BF16)
        yT = sb('yT1', [128, 8, 128], BF16)
        q8 = sb('q8', [128, 512]); q8b = sb('q8b', [128, 512], BF16); qT8 = sb('qT8', [128, 4, 128], BF16)
        kTl2 = [sb('kTl%d' % i, [128, 384]) for i in range(2)]; kTlb2 = [sb('kTlb%d' % i, [128, 384], BF16) for i in range(2)]; vl = sb('vl', [128, 3, 128]); vlb = sb('vlb', [128, 3, 128], BF16)
        kcT = sb('kcT', [128, max(TC, 128)]); kcTb2 = [sb('kcTb%d' % i, [128, max(TC, 128)], BF16) for i in range(2)]
        vc = sb('vc', [128, max(NCB, 1), 128]); vcb = sb('vcb', [128, max(NCB, 1), 128], BF16)
        NK = TC + 384
        ssb = sb('ssb', [128, NK]); pbf = sb('pbf', [128, NK], BF16); pT = sb('pT', [128, NK // 128, 128], BF16)
        v4 = lambda ap: ap.rearrange("p (h t) -> p h t", h=4)
        snap_ctr = [0]

        def scan_block(r0, is_ctx, d, first, bidx):
            v = 1 if is_ctx else 0
            x_t, tx = norm_hT(r0, v, bidx)
            c16 = 1536
            P.op('pe', mmgroup([(ps[2][:, 0:16], tokmm(c16, 16))]), reads=rh, writes=[tp[2]])
            P.op('act', lambda e: e.activation(out=small[:, 8:24], in_=ps[2][:, 0:16], func=AF.Identity), reads=[tp[2]], writes=[tk['small']])
            cb = 8 + d * 4; ca = 16 + d * 4; pA = d * 8; pB = d * 8 + 4
            sm = [tk['small']]
            P.op('act', lambda e: e.activation(out=small[:, 24:28], in_=small[:, cb:cb + 4], func=AF.Sigmoid), reads=sm, writes=sm)
            P.op('dve', lambda e: e.tensor_tensor(out=small[:, 28:32], in0=small[:, ca:ca + 4], in1=pvec[:, pB:pB + 4], op=ALU.add), reads=sm + c_, writes=sm)
            P.op('act', lambda e: e.activation(out=small[:, 28:32], in_=small[:, 28:32], func=AF.Exp), reads=sm, writes=sm)
            P.op('act', lambda e: e.activation(out=small[:, 28:32], in_=small[:, 28:32], func=AF.Ln, bias=onec[:, 0:1], scale=1.0), reads=sm, writes=sm)
            P.op('dve', lambda e: e.tensor_tensor(out=small[:, 28:32], in0=small[:, 28:32], in1=pvec[:, pA:pA + 4], op=ALU.mult), reads=sm + c_, writes=sm)
            tri = maskf if d == 0 else maskr
            P.op('pe', mmgroup([(ps[2][:, 32:36], [(tri[:], small[:, 28:32])]), (ps[2][:, 36:40], [(same[:], small[:, 28:32])])]),
                 reads=sm + c_, writes=[tp[2]])
            P.op('dve', lambda e: e.tensor_copy(out=small[:, 32:40], in_=ps[2][:, 32:40]), reads=[tp[2]], writes=sm)
            P.op('act', lambda e: e.activation(out=small[:, 40:44], in_=small[:, 32:36], func=AF.Exp), reads=sm, writes=sm)
            P.op('dve', lambda e: e.tensor_tensor(out=small[:, 40:44], in0=small[:, 40:44], in1=small[:, 24:28], op=ALU.mult), reads=sm, writes=sm)
            P.op('dve', lambda e: e.tensor_tensor(out=small[:, 44:48], in0=small[:, 36:40], in1=small[:, 32:36], op=ALU.subtract), reads=sm, writes=sm)
            P.op('act', lambda e: e.activation(out=small[:, 44:48], in_=small[:, 44:48], func=AF.Exp), reads=sm, writes=sm)
            P.op('dve', lambda e: e.tensor_tensor(out=GB[:], in0=self.ones[:].unsqueeze(1).to_broadcast([128, 4, 128]),
                                                  in1=small[:, 28:32].unsqueeze(2).to_broadcast([128, 4, 128]), op=ALU.mult),
                 reads=sm + [self.t_const], writes=[tk['GB']])
            P.op('dve', lambda e: e.tensor_tensor(out=gs[:].rearrange("p (h c) -> p h c", c=2), in0=small[:, 28:32].unsqueeze(2).to_broadcast([128, 4, 2]),
                                                  in1=csel[:].unsqueeze(1).to_broadcast([128, 4, 2]), op=ALU.mult), reads=sm + c_, writes=[tk['gs']])
            P.op('pe', mmgroup([(ps[3][:, h * 128:(h + 1) * 128], [(GB[:, h, :], tri[:])]) for h in range(4)]), reads=[tk['GB']] + c_, writes=[tp[3]])
            P.op('pe', mmgroup([(ps[2][:, 48:56], [(self.ones[:], gs[:])])]), reads=[tk['gs'], self.t_const], writes=[tp[2]])
            P.op('act', lambda e: e.activation(out=totrow[:], in_=ps[2][:, 48:56], func=AF.Exp), reads=[tp[2]], writes=[tk['gs']])
            gamrow = v4(ps[3][:])
            gcol = small[:, 32:36].unsqueeze(2).to_broadcast([128, 4, 128])
            mts, mst = (maskr, maskf) if d == 0 else (maskf, maskr)
            mtsS, mstS = (maskrs, maskfs) if d == 0 else (maskfs, maskrs)
            P.op('dve', lambda e: e.tensor_tensor(out=Lts[:], in0=gamrow, in1=gcol, op=ALU.subtract), reads=[tp[3]] + sm, writes=[tk['Lts']])
            P.op('pool', lambda e: e.tensor_scalar(out=Lst[:], in0=Lts[:], scalar1=0.0, scalar2=None, op0=ALU.min), reads=[tk['Lts']], writes=[tk['Lst']])
            P.op('dve', lambda e: e.tensor_scalar(out=Lts[:], in0=Lts[:], scalar1=0.0, scalar2=None, op0=ALU.max), reads=[tk['Lst']], writes=[tk['Lts']])
            P.op('act', lambda e: e.activation(out=egr[:], in_=gamrow, func=AF.Exp), reads=[tp[3], tk['Lts']], writes=[tk['egr']])
            P.op('act', lambda e: e.activation(out=Lts[:], in_=Lts[:], func=AF.Exp, scale=-1.0), writes=[tk['Lts']])
            P.op('act', lambda e: e.activation(out=Lst[:], in_=Lst[:], func=AF.Exp), writes=[tk['Lst']])
            P.op('dve', lambda e: e.tensor_tensor(out=Lts[:], in0=Lts[:], in1=mtsS[:].unsqueeze(1).to_broadcast([128, 4, 128]), op=ALU.mult),
                 reads=c_, writes=[tk['Lts']])
            P.op('pool', lambda e: e.tensor_tensor(out=LstS[:], in0=Lst[:], in1=mst[:].unsqueeze(1).to_broadcast([128, 4, 128]), op=ALU.mult),
                 reads=[tk['Lst']] + c_, writes=[tk['A']])
            cr = cxrow(r0)
            for j in range(5):
                cl = cxl[j % 2]; tcl = tk['cxl%d' % (j % 2)]
                P.op('sp', lambda e, j=j, cl=cl: e.dma_start(out=cl[:], in_=D['CX'][cr + j - 2:cr + j - 2 + 128, :]), writes=[tcl], dma=True)
                if j == 0:
                    P.op('dve', lambda e, cl=cl: e.tensor_tensor(out=qkv[:], in0=cl[:], in1=convw[:, 0, :], op=ALU.mult), reads=[tcl] + c_, writes=[tk['qkv']])
                else:
                    P.op('pool', lambda e, j=j, cl=cl: e.tensor_tensor(out=cl[:], in0=cl[:], in1=convw[:, j, :], op=ALU.mult), reads=c_, writes=[tcl])
                    P.op('dve', lambda e, cl=cl: e.tensor_tensor(out=qkv[:], in0=qkv[:], in1=cl[:], op=ALU.add), reads=[tcl], writes=[tk['qkv']])
            P.op('act', lambda e: e.activation(out=qkv[:], in_=qkv[:], func=AF.Silu), writes=[tk['qkv']])
            P.op('pool', lambda e: e.tensor_tensor(out=Oc[:], in0=qkv[:, 0:1024], in1=qkv[:, 0:1024], op=ALU.mult), reads=[tk['qkv']], writes=[tk['Oc']])
            P.op('dve', lambda e: e.tensor_reduce(out=small[:, 48:56], in_=Oc[:].rearrange("p (h v) -> p h v", h=8), axis=AX.X, op=ALU.add),
                 reads=[tk['Oc']], writes=sm)
            P.op('act', lambda e: e.activation(out=small[:, 48:56], in_=small[:, 48:56], func=AF.Sqrt, bias=self.epsc[:, 0:1], scale=1.0), reads=sm, writes=sm)
            P.op('dve', lambda e: e.reciprocal(out=small[:, 56:64], in_=small[:, 48:56]), reads=sm, writes=sm)
            P.op('dve', lambda e: e.tensor_scalar(out=small[:, 56:60], in0=small[:, 56:60], scalar1=128.0 ** -0.5, scalar2=None, op0=ALU.mult), reads=sm, writes=sm)
            P.op('dve', lambda e: e.tensor_tensor(out=qkb[:], in0=qkv[:, 0:1024].rearrange("p (h v) -> p h v", h=8),
                                                  in1=small[:, 56:64].unsqueeze(2).to_broadcast([128, 8, 128]), op=ALU.mult),
                 reads=[tk['qkv']] + sm, writes=[tk['qkb']])
            for half in range(2):
                P.op('pe', trs([(psb[4 + half][:, j * 128:(j + 1) * 128], qkb[:, half * 4 + j, :]) for j in range(4)], self.identb[:]),
                     reads=[tk['qkb'], self.t_const], writes=[tp[4 + half]])
                P.op('act', lambda e, half=half: e.activation(out=fT[:, half * 4:(half + 1) * 4, :].rearrange("p h t -> p (h t)"),
                                                              in_=psb[4 + half][:, 0:512], func=AF.Identity), reads=[tp[4 + half]], writes=[tk['fT']])
            kn = qkb[:, 4:8, :]
            P.op('dve', lambda e: e.tensor_tensor(out=kbg[:], in0=kn, in1=small[:, 40:44].unsqueeze(2).to_broadcast([128, 4, 128]), op=ALU.mult),
                 reads=[tk['qkb']] + sm, writes=[tk['kbg']])
            P.op('pool', lambda e: e.tensor_tensor(out=kd[:], in0=kn, in1=small[:, 44:48].unsqueeze(2).to_broadcast([128, 4, 128]), op=ALU.mult),
                 reads=[tk['qkb']] + sm, writes=[tk['kd']])
            P.op('dve', lambda e: e.tensor_tensor(out=bv[:], in0=qkv[:, 1024:1536].rearrange("p (h v) -> p h v", h=4),
                                                  in1=small[:, 24:28].unsqueeze(2).to_broadcast([128, 4, 128]), op=ALU.mult),
                 reads=[tk['qkv']] + sm, writes=[tk['bv']])
            P.op('dve', lambda e: e.tensor_tensor(out=qg[:], in0=fT[:, 0:4, :], in1=egr[:], op=ALU.mult), reads=[tk['fT'], tk['egr']], writes=[tk['qg']])
            P.op('pe', mmgroup([(ps[6][:, h * 128:(h + 1) * 128], [(fT[:, 4 + h, :], fT[:, 4 + h, :])]) for h in range(4)]), reads=[tk['fT']], writes=[tp[6]])
            P.op('pe', mmgroup([(ps[7][:, h * 128:(h + 1) * 128], [(fT[:, 4 + h, :], fT[:, h, :])]) for h in range(4)]), reads=[tk['fT']], writes=[tp[7]])
            P.op('dve', lambda e: e.tensor_tensor(out=Am[:], in0=v4(ps[6][:]), in1=Lts[:], op=ALU.mult), reads=[tp[6], tk['Lts']], writes=[tk['A']])
            P.op('dve', lambda e: e.tensor_tensor(out=Am[:], in0=Am[:], in1=small[:, 24:28].unsqueeze(2).to_broadcast([128, 4, 128]), op=ALU.mult),
                 reads=sm, writes=[tk['A']])
            P.op('dve', lambda e: e.tensor_tensor(out=Pst[:], in0=v4(ps[7][:]), in1=LstS[:], op=ALU.mult), reads=[tp[7], tk['A']], writes=[tk['Pst']])
            P.op('pe', trs([(ps[6][:, h * 128:(h + 1) * 128], Am[:, h, :]) for h in range(4)], self.ident[:]), reads=[tk['A'], self.t_const], writes=[tp[6]])
            P.op('act', lambda e: e.activation(out=AT[:], in_=v4(ps[6][:]), func=AF.Identity), reads=[tp[6]], writes=[tk['AT']])
            P.op('dve', lambda e: e.scalar_tensor_tensor(out=Rm[:], in0=AT[:], scalar=-1.0, in1=self.ident[:].unsqueeze(1).to_broadcast([128, 4, 128]),
                                                         op0=ALU.mult, op1=ALU.add), reads=[tk['AT'], self.t_const], writes=[tk['Rm']])
            curQ, curQT = AT, Am
            tQ, tQT = tk['AT'], tk['A']
            for step in range(5):
                last = step == 4
                if not last:
                    P.op('pe', mmgroup([(ps[6][:, h * 128:(h + 1) * 128], [(curQT[:, h, :], curQ[:, h, :])]) for h in range(4)]), reads=[tQ, tQT], writes=[tp[6]])
                P.op('pe', mmgroup([(ps[7][:, h * 128:(h + 1) * 128], [(curQ[:, h, :], curQT[:, h, :])]) for h in range(4)]), reads=[tQ, tQT], writes=[tp[7]])
                if not last:
                    P.op('act', lambda e: e.activation(out=Qm[:], in_=v4(ps[6][:]), func=AF.Identity), reads=[tp[6]], writes=[tk['Q']])
                P.op('act', lambda e: e.activation(out=QT[:], in_=v4(ps[7][:]), func=AF.Identity), reads=[tp[7]], writes=[tk['QT']])
                curQ, curQT, tQ, tQT = Qm, QT, tk['Q'], tk['QT']
                P.op('pe', mmgroup([(ps[3][:, h * 128:(h + 1) * 128], [(QT[:, h, :], Rm[:, h, :])]) for h in range(4)]), reads=[tk['QT'], tk['Rm']], writes=[tp[3]])
                P.op('dve', lambda e: e.tensor_tensor(out=Rm[:], in0=v4(ps[3][:]), in1=Rm[:], op=ALU.add), reads=[tp[3]], writes=[tk['Rm']])
            P.op('act', lambda e: e.activation(out=Rb[:], in_=Rm[:], func=AF.Identity), reads=[tk['Rm']], writes=[tk['Rb']])
            P.op('pe', mmgroup([(ps[6][:, h * 128:(h + 1) * 128], [(Rb[:, h, :], bv[:, h, :])]) for h in range(4)]), reads=[tk['Rb'], tk['bv']], writes=[tp[6]])
            P.op('pe', mmgroup([(ps[7][:, h * 128:(h + 1) * 128], [(kbg[:, h, :], Rb[:, h, :])]) for h in range(4)]), reads=[tk['Rb'], tk['kbg']], writes=[tp[7]])
            P.op('act', lambda e: e.activation(out=u[:], in_=v4(ps[6][:]), func=AF.Identity), reads=[tp[6]], writes=[tk['u']])
            P.op('act', lambda e: e.activation(out=wT[:], in_=v4(ps[7][:]), func=AF.Identity), reads=[tp[7]], writes=[tk['wT']])
            if first:
                P.op('pool', lambda e: e.memset(S[:], 0.0), writes=[tk['S']])
                P.op('pool', lambda e: e.memset(Sb[snap_ctr[0] % 2][:], 0.0), writes=[tk['Sb%d' % (snap_ctr[0] % 2)]])
            order = (0, 1) if d == 0 else (1, 0)
            for c in order:
                cs = slice(c * 64, c * 64 + 64)
                si = snap_ctr[0] % 2; snap_ctr[0] += 1; sn = (si + 1) % 2
                Sbi = Sb[si]; tSb = tk['Sb%d' % si]
                P.op('pe', mmgroup([(ps[6][cs, h * 128:(h + 1) * 128], [(wT[:, h, cs], Sbi[:, h, :])]) for h in range(4)]), reads=[tk['wT'], tSb], writes=[tp[6]])
                P.op('dve', lambda e, cs=cs: e.tensor_tensor(out=vn[cs], in0=u[cs], in1=v4(ps[6][cs, :]), op=ALU.subtract),
                     reads=[tp[6], tk['u']], writes=[tk['vn']])
                def omm(e, cs=cs, Sbi=Sbi):
                    for h in range(4):
                        e.matmul(ps[5][cs, h * 128:(h + 1) * 128], qg[:, h, cs], Sbi[:, h, :], start=True, stop=False)
                        ins = e.matmul(ps[5][cs, h * 128:(h + 1) * 128], Pst[cs, h, cs], vn[cs, h, :], start=False, stop=True)
                    return ins
                P.op('pe', omm, reads=[tk['qg'], tSb, tk['Pst'], tk['vn']], writes=[tp[5]])
                P.op('pe', mmgroup([(ps[7][:, h * 128:(h + 1) * 128], [(kd[cs, h, :], vn[cs, h, :])]) for h in range(4)]), reads=[tk['kd'], tk['vn']], writes=[tp[7]])
                P.op('dve', lambda e, c=c: e.tensor_tensor(out=S[:], in0=S[:], in1=totrow[:].rearrange("p (h c) -> p h c", c=2)[:, :, c:c + 1].to_broadcast([128, 4, 128]),
                                                           op=ALU.mult), reads=[tk['gs']], writes=[tk['S']])
                P.op('dve', lambda e: e.tensor_tensor(out=S[:], in0=S[:], in1=v4(ps[7][:]), op=ALU.add), reads=[tp[7]], writes=[tk['S']])
                P.op('act', lambda e, sn=sn: e.activation(out=Sb[sn][:], in_=S[:], func=AF.Identity), reads=[tk['S']], writes=[tk['Sb%d' % sn]])
            if d == 0:
                P.op('act', lambda e: e.activation(out=ofl[:], in_=ps[5][:], func=AF.Identity), reads=[tp[5]], writes=[tk['ofl']])
                P.op('sp', lambda e: e.dma_start(out=D['OF'][r0:r0 + 128, 0:512], in_=ofl[:]), reads=[tk['ofl']], writes=[tk['out']], dma=True)
                return
            if is_ctx:
                return
            P.op('sp', lambda e: e.dma_start(out=ofl[:], in_=D['OF'][r0:r0 + 128, 0:512]), writes=[tk['ofl']], dma=True)
            P.op('dve', lambda e: e.tensor_tensor(out=Oc[:, 0:512], in0=ps[5][:], in1=ofl[:], op=ALU.add), reads=[tp[5], tk['ofl']], writes=[tk['Oc']])
            P.op('pe', mmgroup([(ps[6][:], tokmm(1552, 512))]), reads=rh, writes=[tp[6]])
            P.op('act', lambda e: e.activation(out=og[:], in_=ps[6][:], func=AF.Silu), reads=[tp[6]], writes=[tk['og']])
            P.op('pool', lambda e: e.tensor_tensor(out=ofl[:], in0=Oc[:, 0:512], in1=Oc[:, 0:512], op=ALU.mult), reads=[tk['Oc']], writes=[tk['ofl']])
            P.op('dve', lambda e: e.tensor_reduce(out=small[:, 48:52], in_=ofl[:].rearrange("p (h v) -> p h v", h=4), axis=AX.X, op=ALU.add),
                 reads=[tk['ofl']], writes=sm)
            P.op('act', lambda e: e.activation(out=small[:, 48:52], in_=small[:, 48:52], func=AF.Sqrt, scale=1.0 / 128, bias=self.epsc[:, 0:1]), reads=sm, writes=sm)
            P.op('dve', lambda e: e.reciprocal(out=small[:, 52:56], in_=small[:, 48:52]), reads=sm, writes=sm)
            O4 = Oc[:, 0:512].rearrange("p (h v) -> p h v", h=4)
            P.op('dve', lambda e: e.tensor_tensor(out=O4, in0=O4, in1=small[:, 52:56].unsqueeze(2).to_broadcast([128, 4, 128]), op=ALU.mult), reads=sm, writes=[tk['Oc']])
            P.op('pool', lambda e: e.tensor_tensor(out=Oc[:, 0:512], in0=Oc[:, 0:512], in1=dnw[:], op=ALU.mult), reads=c_, writes=[tk['Oc']])
            P.op('dve', lambda e: e.tensor_tensor(out=yb[:, 0:512], in0=Oc[:, 0:512], in1=og[:], op=ALU.mult), reads=[tk['Oc'], tk['og']], writes=[tk['yb']])
            self.l1_attention(self._l1, r0)
            P.op('pe', trs([(psb[4][:, k * 128:(k + 1) * 128], yb[:, k * 128:(k + 1) * 128]) for k in range(8)], self.identb[:]),
                 reads=[tk['yb'], self.t_const], writes=[tp[4]])
            P.op('act', lambda e: e.activation(out=yT[:].rearrange("p k t -> p (k t)"), in_=psb[4][:, 0:1024], func=AF.Identity), reads=[tp[4]], writes=[tk['yT']])
            for half in range(2):
                P.op('pe', mmgroup([(ps[6 + half][:], [(yT[:, k, :], wout[:, k, half * 512:(half + 1) * 512]) for k in range(8)])]),
                     reads=[tk['yT'], tk['c']], writes=[tp[6 + half]])
                P.op('dve', lambda e, half=half: e.tensor_tensor(out=Oc[:, half * 512:(half + 1) * 512], in0=ps[6 + half][:],
                                                                 in1=self.gbc[0][:, half * 512:(half + 1) * 512], op=ALU.mult),
                     reads=[tp[6 + half], self.t_gbc], writes=[tk['Oc']])
            P.op('pool', lambda e: e.tensor_tensor(out=x_t[:], in0=x_t[:], in1=Oc[:], op=ALU.add), reads=[tk['Oc']], writes=[tx])
            P.op('sp', lambda e: e.dma_start(out=D[dst][r0:r0 + 128, :], in_=x_t[:]), reads=[tx], writes=[tk['out']], dma=True)

        self._l1 = locals()
        if TC:
            for s_, nm in enumerate(('KT', 'KT2')):
                P.op('sp', lambda e, nm=nm: e.dma_start(out=kcT[:, 0:TC], in_=D[nm][:, 0:TC]), writes=[tk['kcT']], dma=True)
                P.op('dve', lambda e, s_=s_: e.tensor_copy(out=kcTb2[s_][:, 0:TC], in_=kcT[:, 0:TC]), reads=[tk['kcT']], writes=[tk['kcT']])
            P.op('sp', lambda e: e.dma_start(out=vc[:], in_=D['VV'][0:TC, :].rearrange("(b p) n -> p b n", p=128)), writes=[tk['vc']], dma=True)
            P.op('dve', lambda e: e.tensor_copy(out=vcb[:], in_=vc[:]), reads=[tk['vc']], writes=[tk['vc']])
        for d in range(2):
            seq = seq_all
            if d == 1:
                seq = [(i * 128, True) for i in reversed(range(NCB))] + [(TC + i * 128, False) for i in reversed(range(NB))]
            for n, (r0, is_ctx) in enumerate(seq):
                scan_block(r0, is_ctx, d, n == 0, n)
            P.barrier()
        P.emit()


def apply_rope(self, x3, nh, rope, rt, t_x, t_rope, t_rt):
    P = self.P
    cosb = rope[:, 0:32].unsqueeze(1).to_broadcast([128, nh, 32]); sinb = rope[:, 32:64].unsqueeze(1).to_broadcast([128, nh, 32])
    x1 = x3[:, :, 0:32]; x2 = x3[:, :, 32:64]
    r = rt[:, 0:nh, :]
    rd = list(t_x) + [t_rope]
    P.op('dve', lambda e: e.tensor_tensor(out=r[:, :, 0:32], in0=x2, in1=sinb, op=ALU.mult), reads=rd, writes=[t_rt])
    P.op('dve', lambda e: e.tensor_tensor(out=r[:, :, 32:64], in0=x1, in1=sinb, op=ALU.mult), reads=rd, writes=[t_rt])
    P.op('dve', lambda e: e.tensor_tensor(out=x1, in0=x1, in1=cosb, op=ALU.mult), reads=[t_rope], writes=list(t_x))
    P.op('dve', lambda e: e.tensor_tensor(out=x2, in0=x2, in1=cosb, op=ALU.mult), reads=[t_rope], writes=list(t_x))
    P.op('dve', lambda e: e.tensor_tensor(out=x1, in0=x1, in1=r[:, :, 0:32], op=ALU.subtract), reads=[t_rt], writes=list(t_x))
    P.op('dve', lambda e: e.tensor_tensor(out=x2, in0=x2, in1=r[:, :, 32:64], op=ALU.add), reads=[t_rt], writes=list(t_x))


Builder.phase_l1mix = phase_l1mix
Builder.apply_rope = apply_rope


def l1_attention(self, L, r0):
    P, D = self.P, self.D
    TC, NB, NCB = self.TC, self.NB, self.NCB
    ps, psb, tp, tk = L['ps'], L['psb'], L['tp'], L['tk']
    mmgroup, trs, tokmm, rh = L['mmgroup'], L['trs'], L['tokmm'], L['rh']
    q8, q8b, qT8, small, yb = L['q8'], L['q8b'], L['qT8'], L['small'], L['yb']
    kTl, kTlb, vl, vlb, kcTb, vcb = L['kTl2'], L['kTlb2'], L['vl'], L['vlb'], L['kcTb2'], L['vcb']
    ssb, pbf, pT, rope, rt, sinks, mbp, mbn = L['ssb'], L['pbf'], L['pT'], L['rope'], L['rt'], L['sinks'], L['mbp'], L['mbn']
    blk = (r0 - TC) // 128
    has_prev, has_next = blk > 0, blk < NB - 1
    NK = TC + 384
    sm = [tk['small']]
    P.op('pe', mmgroup([(ps[0][:], tokmm(2064, 512))]), reads=rh, writes=[tp[0]])
    P.op('act', lambda e: e.activation(out=q8[:], in_=ps[0][:], func=AF.Identity), reads=[tp[0]], writes=[tk['q8']])
    t0 = r0 - TC
    P.op('sp', lambda e: e.dma_start(out=rope[:], in_=D['rope'][t0:t0 + 128, :]), writes=[tk['rope']], dma=True)
    self.apply_rope(q8[:].rearrange("p (h f) -> p h f", h=8), 8, rope, rt, [tk['q8']], tk['rope'], tk['rt'])
    P.op('dve', lambda e: e.tensor_scalar(out=q8b[:], in0=q8[:], scalar1=0.125, scalar2=None, op0=ALU.mult), reads=[tk['q8']], writes=[tk['q8']])
    P.op('pe', trs([(psb[1][:, j * 128:(j + 1) * 128], q8b[:, j * 128:(j + 1) * 128]) for j in range(4)], self.identb[:]),
         reads=[tk['q8'], self.t_const], writes=[tp[1]])
    P.op('act', lambda e: e.activation(out=qT8[:].rearrange("p j t -> p (j t)"), in_=psb[1][:, 0:512], func=AF.Identity), reads=[tp[1]], writes=[tk['qT8']])
    c0 = r0 - 128 if has_prev else r0
    c1 = r0 + 256 if has_next else r0 + 128
    o0 = 0 if has_prev else 128
    for s_, nm in enumerate(('KT', 'KT2')):
        P.op('sp', lambda e, s_=s_, nm=nm: e.dma_start(out=kTl[s_][:, o0:o0 + (c1 - c0)], in_=D[nm][:, c0:c1]), writes=[tk['kTl']], dma=True)
        P.op('pool', lambda e, s_=s_: e.tensor_copy(out=kTlb[s_][:, o0:o0 + (c1 - c0)], in_=kTl[s_][:, o0:o0 + (c1 - c0)]), reads=[tk['kTl']], writes=[tk['kTl']])
    nbk = (c1 - c0) // 128
    b0 = o0 // 128
    P.op('sp', lambda e: e.dma_start(out=vl[:, b0:b0 + nbk, :], in_=D['VV'][c0:c1, :].rearrange("(b p) n -> p b n", p=128)), writes=[tk['vl']], dma=True)
    P.op('pool', lambda e: e.tensor_copy(out=vlb[:, b0:b0 + nbk, :], in_=vl[:, b0:b0 + nbk, :]), reads=[tk['vl']], writes=[tk['vl']])
    for h in range(8):
        pbse = (h % 2) * 64; g = h // 4
        s_ = 0 if g == (h % 2) else 1
        rs = slice(pbse, pbse + 64)
        bA, bB = 2 + 2 * (h % 2), 3 + 2 * (h % 2)
        ql = qT8[rs, h // 2, :]
        outsA = []
        if TC:
            outsA.append((ps[bA][:, 0:TC], [(ql, kcTb[s_][rs, 0:TC])]))
        if has_prev:
            outsA.append((ps[bA][:, TC:TC + 128], [(ql, kTlb[s_][rs, 0:128])]))
        outsA.append((ps[bA][:, TC + 128:TC + 256], [(ql, kTlb[s_][rs, 128:256])]))
        P.op('pe', mmgroup(outsA), reads=[tk['qT8'], tk['kTl'], tk['kcT']], writes=[tp[bA]])
        if has_next:
            P.op('pe', mmgroup([(ps[bB][:, 0:128], [(ql, kTlb[s_][rs, 256:384])])]), reads=[tk['qT8'], tk['kTl']], writes=[tp[bB]])
        w_ = [tk['ssb']]
        if TC:
            P.op('act', lambda e, bA=bA: e.activation(out=ssb[:, 0:TC], in_=ps[bA][:, 0:TC], func=AF.Identity), reads=[tp[bA]], writes=w_)
        if has_prev:
            P.op('dve', lambda e, bA=bA: e.tensor_tensor(out=ssb[:, TC:TC + 128], in0=ps[bA][:, TC:TC + 128], in1=mbp[:], op=ALU.add), reads=[tp[bA], tk['c']], writes=w_)
        else:
            P.op('pool', lambda e: e.memset(ssb[:, TC:TC + 128], NEG), writes=w_)
        P.op('act', lambda e, bA=bA: e.activation(out=ssb[:, TC + 128:TC + 256], in_=ps[bA][:, TC + 128:TC + 256], func=AF.Identity), reads=[tp[bA]], writes=w_)
        if has_next:
            P.op('dve', lambda e, bB=bB: e.tensor_tensor(out=ssb[:, TC + 256:TC + 384], in0=ps[bB][:, 0:128], in1=mbn[:], op=ALU.add), reads=[tp[bB], tk['c']], writes=w_)
        else:
            P.op('pool', lambda e: e.memset(ssb[:, TC + 256:TC + 384], NEG), writes=w_)
        P.op('dve', lambda e: e.tensor_reduce(out=small[:, 16:17], in_=ssb[:], axis=AX.X, op=ALU.max), reads=w_, writes=sm)
        P.op('dve', lambda e, h=h: e.tensor_tensor(out=small[:, 16:17], in0=small[:, 16:17], in1=sinks[:, h:h + 1], op=ALU.max), reads=sm + [tk['c']], writes=sm)
        P.op('dve', lambda e: e.tensor_scalar(out=small[:, 17:18], in0=small[:, 16:17], scalar1=-1.0, scalar2=None, op0=ALU.mult), reads=sm, writes=sm)
        P.op('act', lambda e: e.activation(out=pbf[:], in_=ssb[:], func=AF.Exp, bias=small[:, 17:18], scale=1.0, accum_out=small[:, 18:19]),
             reads=w_ + sm, writes=[tk['pb']] + sm)
        P.op('act', lambda e, h=h: e.activation(out=small[:, 19:20], in_=sinks[:, h:h + 1], func=AF.Exp, bias=small[:, 17:18], scale=1.0), reads=sm, writes=sm)
        P.op('dve', lambda e: e.tensor_tensor(out=small[:, 19:20], in0=small[:, 19:20], in1=small[:, 18:19], op=ALU.add), reads=sm, writes=sm)
        P.op('dve', lambda e, h=h: e.reciprocal(out=small[:, 8 + h:9 + h], in_=small[:, 19:20]), reads=sm, writes=sm)
        nkb = NK // 128
        P.op('pe', trs([(psb[6][:, kb * 128:(kb + 1) * 128], pbf[:, kb * 128:(kb + 1) * 128]) for kb in range(nkb)], self.identb[:]),
             reads=[tk['pb'], self.t_const], writes=[tp[6]])
        P.op('act', lambda e: e.activation(out=pT[:].rearrange("p k t -> p (k t)"), in_=psb[6][:, 0:NK], func=AF.Identity), reads=[tp[6]], writes=[tk['pT']])
        pairs = [(pT[:, b, :], vcb[:, b, g * 64:(g + 1) * 64]) for b in range(NCB)]
        for j in range(3):
            if (j == 0 and not has_prev) or (j == 2 and not has_next):
                continue
            pairs.append((pT[:, NCB + j, :], vlb[:, j, g * 64:(g + 1) * 64]))
        P.op('pe', mmgroup([(ps[7][:, h * 64:(h + 1) * 64], pairs)]), reads=[tk['pT'], tk['vl'], tk['vc']], writes=[tp[7]])
    P.op('dve', lambda e: e.tensor_tensor(out=yb[:, 512:1024].rearrange("p (h f) -> p h f", h=8), in0=ps[7][:].rearrange("p (h f) -> p h f", h=8),
                                          in1=small[:, 8:16].unsqueeze(2).to_broadcast([128, 8, 64]), op=ALU.mult), reads=[tp[7]] + sm, writes=[tk['yb']])


Builder.l1_attention = l1_attention
```

```python
import numpy as np
from contextlib import ExitStack
import concourse.bass as bass
import concourse.mybir as mybir
from concourse.bass_utils import run_bass_kernel_spmd

dt = mybir.dt
F32 = dt.float32
BF16 = dt.bfloat16
AF = mybir.ActivationFunctionType
ALU = mybir.AluOpType
AX = mybir.AxisListType

ENGS = ['pe', 'act', 'dve', 'pool', 'sp']
NDMA = 6
EPS = 1e-6
DM = 1024
NE = 32


class Tok:
    __slots__ = ('w', 'r')

    def __init__(self):
        self.w = None
        self.r = {}


class Prog:
    def __init__(self, nc):
        self.nc = nc
        self.stack = ExitStack()
        self.sems = {}
        self.cnt = {e: 0 for e in ENGS}
        self.dma_cnt = {}
        self.dma_rr = {e: 0 for e in ENGS}
        self.seen = {e: {} for e in ENGS}
        self.q = {e: [] for e in ENGS}
        self.nops = 0
        for e in ENGS:
            self.sems[('eng', e)] = self.stack.enter_context(nc.semaphore('s_' + e))
        for e in ('sp', 'pool', 'act'):
            for k in range(NDMA):
                self.sems[('dma', e, k)] = self.stack.enter_context(nc.semaphore('d_%s%d' % (e, k)))

    def op(self, eng, fn, reads=(), writes=(), dma=False):
        deps = {}

        def add(k, v):
            if deps.get(k, 0) < v:
                deps[k] = v

        for t in reads:
            if t.w is not None:
                add(*t.w)
        for t in writes:
            if t.w is not None:
                add(*t.w)
            for k, v in t.r.items():
                add(k, v)
        if dma:
            k = self.dma_rr[eng]
            self.dma_rr[eng] = (k + 1) % NDMA
            key = ('dma', eng, k)
            prev = self.dma_cnt.get(key, 0)
            if prev:
                add(key, prev)
            val = prev + 16
            self.dma_cnt[key] = val
        else:
            key = ('eng', eng)
            self.cnt[eng] += 1
            val = self.cnt[eng]
        seen = self.seen[eng]
        waits = []
        for k, v in deps.items():
            if eng == 'pe' and k == ('eng', 'pe'):
                continue
            if seen.get(k, 0) >= v:
                continue
            seen[k] = v
            waits.append((k, v))
        self.q[eng].append((waits, fn, key, dma))
        self.nops += 1
        for t in reads:
            if t.r.get(key, 0) < val:
                t.r[key] = val
        for t in writes:
            t.w = (key, val)
            t.r = {}

    def barrier(self):
        evs = [(('eng', e), self.cnt[e]) for e in ENGS if self.cnt[e]]
        evs += list(self.dma_cnt.items())
        for e in ENGS:
            seen = self.seen[e]
            waits = []
            for k, v in evs:
                if seen.get(k, 0) >= v:
                    continue
                seen[k] = v
                waits.append((k, v))
            if waits:
                self.q[e].append((waits, None, None, False))

    def emit(self):
        nc = self.nc
        sems = self.sems
        with nc.Block() as block:
            def mk(name):
                lst = self.q[name]

                def f(e):
                    for waits, fn, key, dma in lst:
                        for k, v in waits:
                            e.wait_ge(sems[k], v)
                        if fn is None:
                            continue
                        ins = fn(e)
                        ins.then_inc(sems[key], 16 if dma else 1)
                return f
            block.tensor(mk('pe'))
            block.scalar(mk('act'))
            block.vector(mk('dve'))
            block.gpsimd(mk('pool'))
            block.sync(mk('sp'))
        self.q = {e: [] for e in ENGS}

    def close(self):
        self.stack.close()


class Builder:
    def __init__(self, T, TC, phases=('ada', 'l0mix', 'l0moe', 'l1mix', 'l1moe'), dbg=()):
        self.T, self.TC = T, TC
        self.NB, self.NCB = T // 128, TC // 128
        self.phases = phases
        self.dbg = dbg
        self.nc = bass.Bass("TRN2", target_bir_lowering=False)
        self.D = {}
        self.P = Prog(self.nc)
        self.in_names = []
        self.t_cast = [[Tok() for _ in range(NE)] for _ in range(2)]
        self.cast_done = [set(), set()]

    def din(self, name, shape, d=F32):
        self.D[name] = self.nc.dram_tensor(name, list(shape), d, kind="ExternalInput").ap()
        self.in_names.append(name)
        return self.D[name]

    def dscr(self, name, shape, d=F32):
        kind = "ExternalOutput" if name in self.dbg else "Internal"
        self.D[name] = self.nc.dram_tensor(name, list(shape), d, kind=kind).ap()
        return self.D[name]

    def declare(self):
        T, TC = self.T, self.TC
        din = self.din
        din('x', [T, DM]); din('ctx', [max(TC, 128), DM]); din('cvec', [128, 8, 2])
        din('ident', [128, 128]); din('ones', [128, 128])
        for l in range(2):
            p = 'l%d_' % l
            din(p + 'ada_w', [DM, 6 * DM]); din(p + 'ada_bT', [128, 48])
            din(p + 'nmix', [128, 8]); din(p + 'nffn', [128, 8])
            if ('l%dmoe' % l) in self.phases:
                self.D['WUB%d' % l] = self.nc.dram_tensor('WUB%d' % l, [NE, DM, 2 * DM], BF16).ap()
                self.D['WDB%d' % l] = self.nc.dram_tensor('WDB%d' % l, [NE, DM, DM], BF16).ap()
                din(p + 'router_w', [DM, NE]); din(p + 'router_b', [128, NE])
                din(p + 'w_up', [NE, DM, 2 * DM]); din(p + 'b_upT', [128, NE, 16])
                din(p + 'w_down', [NE, DM, DM]); din(p + 'b_down', [NE, DM])
        din('final_w', [128, DM])
        self.out = self.nc.dram_tensor('out', [T, DM], F32, kind="ExternalOutput").ap()
        R = TC + T
        self.dscr('X1', [R, DM]); self.dscr('X2', [R, DM]); self.dscr('X3', [R, DM]); self.dscr('OF', [R, DM])
        if 'l1mix' in self.phases:
            din('l1_w_in', [DM, 2832]); din('l1_w_out', [DM, DM]); din('l1_convw', [128, 5, 1536]); din('l1_pvec', [128, 16])
            din('l1_dnw', [128, 512]); din('l1_sinks', [128, 8]); din('rope', [T, 64])
            for nm in ('mask_fs', 'mask_rs', 'same', 'mb_prev', 'mb_next'):
                din(nm, [128, 128])
            din('csel', [128, 2])
            self.dscr('CX', [R + 8, 1536]); self.dscr('KT', [128, R]); self.dscr('KT2', [128, R]); self.dscr('VV', [R, 128])
            if 'l0mix' not in self.phases:
                din('mask_f', [128, 128]); din('mask_r', [128, 128])
        if 'l0mix' not in self.phases:
            return
        din('l0_w_in', [DM, 4128]); din('l0_w_out', [DM, DM]); din('l0_w2', [16, 2, 256]); din('l0_b2', [128, 2, 2])
        din('l0_lb', [128, 2, 4]); din('l0_normw', [128, DM]); din('rmask', [128, 768]); din('mask_f', [128, 128]); din('mask_r', [128, 128])

    def build(self):
        nc, P = self.nc, self.P
        self.declare()
        with ExitStack() as st0:
            self.st0 = st0
            sb = lambda name, shape, d=F32: st0.enter_context(nc.sbuf_tensor(name, shape, d))
            self.ident = sb('ident_t', [128, 128]); self.ones = sb('ones_t', [128, 128])
            self.identb = sb('identb_t', [128, 128], BF16)
            self.modT = [sb('modT%d' % l, [128, 48, 2]) for l in range(2)]
            self.Acol = [[sb('A%d_%d' % (l, i), [128, 8, 2]) for i in range(2)] for l in range(2)]
            self.gbc = [sb('gbc_x', [128, DM]), sb('gbc_c', [128, DM])]
            self.t_const = Tok(); self.t_mod = Tok(); self.t_gbc = Tok()
            P.op('sp', lambda e: e.dma_start(out=self.ident[:], in_=self.D['ident']), writes=[self.t_const], dma=True)
            P.op('sp', lambda e: e.dma_start(out=self.ones[:], in_=self.D['ones']), writes=[self.t_const], dma=True)
            P.op('dve', lambda e: e.tensor_copy(out=self.identb[:], in_=self.ident[:]), reads=[self.t_const], writes=[self.t_const])
            P.barrier()
            self.phase_ada()
            cur = 'x_in'
            if 'l0mix' in self.phases:
                self.phase_l0mix(cur, 'X1'); cur = 'X1'
            if 'l0moe' in self.phases:
                self.phase_moe(0, cur, 'X2', final=False); cur = 'X2'
            if 'l1mix' in self.phases:
                self.phase_l1mix(cur, 'X3'); cur = 'X3'
            if 'l1moe' in self.phases:
                self.phase_moe(1, cur, None, final=True)
            elif 'final' in self.phases:
                self.phase_final(cur)
            P.barrier()
            P.emit()
        P.close()
        return nc

    def issue_cast(self, l, ex):
        if ('l%dmoe' % l) not in self.phases or ex >= NE or ex in self.cast_done[l]:
            return
        self.cast_done[l].add(ex)
        D, P = self.D, self.P
        p = 'l%d_' % l
        tk_ = self.t_cast[l][ex]
        P.op('pool', lambda e: e.dma_start(out=D['WUB%d' % l][ex], in_=D[p + 'w_up'][ex]), writes=[tk_], dma=True)
        P.op('pool', lambda e: e.dma_start(out=D['WDB%d' % l][ex], in_=D[p + 'w_down'][ex]), writes=[tk_], dma=True)

    def rows(self, name, r0, n=128):
        if name == 'x_in':
            if r0 < self.TC:
                return self.D['ctx'][r0:r0 + n, :]
            return self.D['x'][r0 - self.TC:r0 - self.TC + n, :]
        return self.D[name][r0:r0 + n, :]

    def phase_ada(self):
        nc, P, D = self.nc, self.P, self.D
        with ExitStack() as st:
            sb = lambda name, shape, d=F32: st.enter_context(nc.sbuf_tensor(name, shape, d))
            ps = [st.enter_context(nc.psum_tensor('pa%d' % i, [128, 512], F32)) for i in range(2)]
            cv = sb('cv', [128, 8, 2]); scv = sb('scv', [128, 8, 2])
            wb = [sb('adaw%d' % i, [128, 8, 512]) for i in range(2)]
            bT = sb('adab', [128, 48]); nw = sb('nw', [128, 8])
            t_cv, t_ps, t_b = Tok(), Tok(), Tok()
            t_wb = [Tok(), Tok()]
            P.op('sp', lambda e: e.dma_start(out=cv[:], in_=D['cvec']), writes=[t_cv], dma=True)
            P.op('act', lambda e: e.activation(out=scv[:], in_=cv[:], func=AF.Silu), reads=[t_cv], writes=[t_cv])
            for l in range(2):
                p = 'l%d_' % l
                P.op('sp', lambda e, p=p: e.dma_start(out=bT[:], in_=D[p + 'ada_bT']), writes=[t_b], dma=True)
                for cb in range(12):
                    w = wb[cb % 2]; tw = t_wb[cb % 2]
                    src = D[p + 'ada_w'][:, cb * 512:(cb + 1) * 512].rearrange("(k p) n -> p k n", p=128)
                    P.op('sp', lambda e, w=w, src=src: e.dma_start(out=w[:], in_=src), writes=[tw], dma=True)

                    def mm(e, w=w, cb=cb):
                        for jj in range(4):
                            j = cb * 4 + jj
                            for kc in range(8):
                                ins = e.matmul(ps[0][:, j * 2:j * 2 + 2], w[:, kc, jj * 128:(jj + 1) * 128],
                                               scv[:, kc, :], start=(kc == 0), stop=(kc == 7))
                        return ins
                    P.op('pe', mm, reads=[tw, t_cv], writes=[t_ps])
                modT = self.modT[l]
                P.op('dve', lambda e, modT=modT: e.tensor_tensor(
                    out=modT[:], in0=ps[0][:, 0:96].rearrange("p (j v) -> p j v", v=2),
                    in1=bT[:].unsqueeze(2).to_broadcast([128, 48, 2]), op=ALU.add),
                    reads=[t_ps, t_b], writes=[self.t_mod])
                for i, (nm, c0) in enumerate((('nmix', 8), ('nffn', 32))):
                    A = self.Acol[l][i]
                    P.op('sp', lambda e, nm=nm, p=p: e.dma_start(out=nw[:], in_=D[p + nm]), writes=[t_b], dma=True)
                    P.op('dve', lambda e, A=A, modT=modT, c0=c0: e.scalar_tensor_tensor(
                        out=A[:], in0=modT[:, c0:c0 + 8, :], scalar=1.0,
                        in1=nw[:].unsqueeze(2).to_broadcast([128, 8, 2]), op0=ALU.add, op1=ALU.mult),
                        reads=[self.t_mod, t_b], writes=[self.t_mod])
            P.barrier()
            P.emit()

    def gate_bcast(self, dg, ps, l, c0, variants):
        nc, P = self.nc, self.P
        t_dg, t_p = Tok(), Tok()
        for v in variants:
            for k in range(8):
                P.op('dve', lambda e, k=k, v=v: e.tensor_scalar(
                    out=dg[:, k, :], in0=self.ident[:], scalar1=self.modT[l][:, c0 + k, v:v + 1], scalar2=None,
                    op0=ALU.mult), reads=[self.t_mod, self.t_const], writes=[t_dg])
            for h in range(2):
                def mm(e, h=h):
                    for j in range(4):
                        ins = e.matmul(ps[h][:, j * 128:(j + 1) * 128], self.ones[:], dg[:, h * 4 + j, :],
                                       start=True, stop=True)
                    return ins
                P.op('pe', mm, reads=[t_dg, self.t_const], writes=[t_p])
                P.op('act', lambda e, h=h, v=v: e.activation(out=self.gbc[v][:, h * 512:(h + 1) * 512], in_=ps[h][:],
                                                            func=AF.Identity), reads=[t_p], writes=[self.t_gbc])

    def norm_xn(self, src_rows, xt, t_x, xn, t_xn, small, t_small, junk, t_junk):
        P = self.P
        P.op('sp', lambda e: e.dma_start(out=xt[:], in_=src_rows), writes=[t_x], dma=True)
        P.op('act', lambda e: e.activation(out=junk[:], in_=xt[:], func=AF.Square, accum_out=small[:, 0:1]),
             reads=[t_x], writes=[t_junk, t_small])
        P.op('act', lambda e: e.activation(out=small[:, 1:2], in_=small[:, 0:1], func=AF.Sqrt, scale=1.0 / DM, bias=self.epsc[:, 0:1]),
             reads=[t_small], writes=[t_small])
        P.op('dve', lambda e: e.reciprocal(out=small[:, 2:3], in_=small[:, 1:2]), reads=[t_small], writes=[t_small])
        P.op('dve', lambda e: e.tensor_scalar(out=xn[:], in0=xt[:], scalar1=small[:, 2:3], scalar2=None, op0=ALU.mult),
             reads=[t_x, t_small], writes=[t_xn])

    def phase_moe(self, l, src, dst, final):
        nc, P, D = self.nc, self.P, self.D
        p = 'l%d_' % l
        TC = self.TC if l == 0 else 0
        r_begin = 0 if l == 0 else self.TC
        nblk = (TC + self.T) // 128
        NSB = 8
        with ExitStack() as st:
            sb = lambda name, shape, d=F32: st.enter_context(nc.sbuf_tensor(name + '_m%d' % l, shape, d))
            ps = [st.enter_context(nc.psum_tensor('pm%d_%d' % (l, i), [128, 512], F32)) for i in range(8)]
            self.epsc = sb('epsc', [128, 1])
            h2T = sb('h2T', [128, 8, NSB * 128], BF16)
            acc = sb('acc', [128, NSB, DM])
            gates = sb('gates', [128, NSB, NE])
            wu = [sb('wu%d' % i, [128, 8, 2 * DM], BF16) for i in range(2)]
            wd = [sb('wd%d' % i, [128, 8, DM], BF16) for i in range(2)]
            actT = [sb('actT%d' % i, [128, 8, 512], BF16) for i in range(2)]
            xt = [sb('xt0', [128, DM])] * 2
            junk = sb('junk', [128, DM], BF16)
            small = sb('small', [128, 16]); h32 = sb('h32', [128, 8, 128])
            rw = sb('rw', [128, 8, NE]); rb = sb('rb', [128, NE]); bup = sb('bup', [128, NE, 16])
            bdn = sb('bdn', [NE, DM]); lg = sb('lg', [128, NE]); top8 = sb('top8', [128, 8])
            em = sb('em', [128, NE]); gT = sb('gT', [NE, 128])
            eg = [sb('eg%d' % i, [128, 512]) for i in range(2)]
            es = [sb('es%d' % i, [128, 512]) for i in range(2)]
            el = [sb('el%d' % i, [128, 512]) for i in range(2)]
            fw = sb('fw', [128, DM]) if final else None
            T_ = lambda: Tok()
            t_c, t_h2T, t_acc, t_gates = T_(), T_(), T_(), T_()
            t_wu, t_wd = [T_(), T_()], [T_(), T_()]
            t_act = [T_(), T_()]
            t_xt, t_xn, t_junk, t_small, t_h32 = [T_()] * 2, T_(), T_(), T_(), T_()
            t_lg, t_gT = T_(), T_()
            t_ps = [T_() for _ in range(8)]
            t_eg, t_es, t_el = [T_(), T_()], [T_(), T_()], [T_(), T_()]
            t_out = T_()
            P.op('pool', lambda e: e.memset(self.epsc[:], EPS), writes=[t_c])
            P.op('sp', lambda e: e.dma_start(out=rw[:], in_=D[p + 'router_w'].rearrange("(k p) n -> p k n", p=128)), writes=[t_c], dma=True)
            P.op('sp', lambda e: e.dma_start(out=rb[:], in_=D[p + 'router_b']), writes=[t_c], dma=True)
            P.op('sp', lambda e: e.dma_start(out=bup[:], in_=D[p + 'b_upT']), writes=[t_c], dma=True)
            P.op('sp', lambda e: e.dma_start(out=bdn[:], in_=D[p + 'b_down']), writes=[t_c], dma=True)
            if final:
                P.op('sp', lambda e: e.dma_start(out=fw[:], in_=D['final_w']), writes=[t_c], dma=True)
            variants = (0, 1) if TC else (0,)
            self.gate_bcast(h32, ps[0:2], l, 40, variants)
            P.barrier()
            A2 = self.Acol[l][1]; modT = self.modT[l]
            wcount = 0
            for ex_ in range(NE):
                self.issue_cast(l, ex_)
            for sb0 in range(0, nblk, NSB):
                nb = min(NSB, nblk - sb0)
                for bi in range(nb):
                    r0 = r_begin + (sb0 + bi) * 128
                    v = 1 if (r0 < self.TC) else 0
                    x_t = xt[bi % 2]; tx = t_xt[bi % 2]
                    self.norm_xn(self.rows(src, r0), x_t, tx, x_t, tx, small, t_small, junk, t_junk)
                    xn, t_xn = x_t, tx
                    for h in range(2):
                        def tr(e, h=h, xn=xn):
                            for j in range(4):
                                k = h * 4 + j
                                ins = e.transpose(ps[h][:, j * 128:(j + 1) * 128], xn[:, k * 128:(k + 1) * 128], self.ident[:])
                            return ins
                        P.op('pe', tr, reads=[t_xn, self.t_const], writes=[t_ps[h]])
                        for j in range(4):
                            k = h * 4 + j
                            P.op('act', lambda e, h=h, j=j, k=k, v=v: e.activation(
                                out=h32[:, k, :], in_=ps[h][:, j * 128:(j + 1) * 128], func=AF.Identity,
                                scale=A2[:, k, v:v + 1], bias=modT[:, 24 + k, v:v + 1]),
                                reads=[t_ps[h], self.t_mod], writes=[t_h32])
                    P.op('dve', lambda e, bi=bi: e.tensor_copy(out=h2T[:, :, bi * 128:(bi + 1) * 128], in_=h32[:]),
                         reads=[t_h32], writes=[t_h2T])

                    def rmm(e):
                        for k in range(8):
                            ins = e.matmul(ps[2][:, 0:NE], h32[:, k, :], rw[:, k, :], start=(k == 0), stop=(k == 7))
                        return ins
                    P.op('pe', rmm, reads=[t_h32, t_c], writes=[t_ps[2]])
                    P.op('dve', lambda e: e.tensor_tensor(out=lg[:], in0=ps[2][:, 0:NE], in1=rb[:], op=ALU.add),
                         reads=[t_ps[2], t_c], writes=[t_lg])
                    P.op('dve', lambda e: e.max(out=top8[:], in_=lg[:]), reads=[t_lg], writes=[t_lg])
                    P.op('dve', lambda e: e.tensor_scalar(out=small[:, 4:5], in0=top8[:, 0:1], scalar1=-1.0, scalar2=None, op0=ALU.mult),
                         reads=[t_lg], writes=[t_small])
                    P.op('act', lambda e: e.activation(out=em[:], in_=lg[:], func=AF.Exp, bias=small[:, 4:5], scale=1.0),
                         reads=[t_lg, t_small], writes=[t_lg])
                    P.op('dve', lambda e: e.scalar_tensor_tensor(out=em[:], in0=lg[:], scalar=top8[:, 3:4], in1=em[:],
                                                                  op0=ALU.is_ge, op1=ALU.mult), reads=[t_lg], writes=[t_lg])
                    P.op('dve', lambda e: e.tensor_reduce(out=small[:, 5:6], in_=em[:], axis=AX.X, op=ALU.add),
                         reads=[t_lg], writes=[t_small])
                    P.op('dve', lambda e: e.reciprocal(out=small[:, 6:7], in_=small[:, 5:6]), reads=[t_small], writes=[t_small])
                    P.op('dve', lambda e, bi=bi: e.tensor_scalar(out=gates[:, bi, :], in0=em[:], scalar1=small[:, 6:7], scalar2=None,
                                                                 op0=ALU.mult), reads=[t_lg, t_small], writes=[t_gates])
                    P.op('pe', lambda e, bi=bi: e.transpose(ps[3][0:NE, 0:128], gates[:, bi, :], self.ident[:]),
                         reads=[t_gates, self.t_const], writes=[t_ps[3]])
                    P.op('act', lambda e: e.activation(out=gT[:], in_=ps[3][0:NE, 0:128], func=AF.Identity),
                         reads=[t_ps[3]], writes=[t_gT])
                    for h in range(2):
                        P.op('pe', lambda e, h=h: e.matmul(ps[h][:], gT[:], bdn[:, h * 512:(h + 1) * 512], start=True, stop=True),
                             reads=[t_gT, t_c], writes=[t_ps[h]])
                        P.op('act', lambda e, h=h, bi=bi: e.activation(out=acc[:, bi, h * 512:(h + 1) * 512], in_=ps[h][:], func=AF.Identity),
                             reads=[t_ps[h]], writes=[t_acc])
                ngrp = (nb + 3) // 4
                units = [(ex, g) for ex in range(NE) for g in range(ngrp)]

                def load_w(ex):
                    wi = (wbase + ex) % 2
                    P.op('sp', lambda e: e.dma_start(out=wu[wi][:], in_=D['WUB%d' % l][ex].rearrange("(k p) n -> p k n", p=128)),
                         reads=[self.t_cast[l][ex]], writes=[t_wu[wi]], dma=True)
                    P.op('sp', lambda e: e.dma_start(out=wd[wi][:], in_=D['WDB%d' % l][ex].rearrange("(k p) n -> p k n", p=128)),
                         reads=[self.t_cast[l][ex]], writes=[t_wd[wi]], dma=True)

                def up_unit(ui):
                    ex, g = units[ui]
                    wi = (wbase + ex) % 2
                    ai = ui % 2
                    gb = min(4, nb - g * 4)
                    N = gb * 128
                    t0 = g * 512
                    for fc in range(8):
                        ei = fc % 2
                        for part, pb in ((0, 4 + ei), (1, 6 + ei)):
                            def umm(e, part=part, pb=pb, fc=fc):
                                c0 = part * DM + fc * 128
                                for k in range(8):
                                    ins = e.matmul(ps[pb][:, 0:N], wu[wi][:, k, c0:c0 + 128], h2T[:, k, t0:t0 + N],
                                                   start=(k == 0), stop=(k == 7))
                                return ins
                            P.op('pe', umm, reads=[t_wu[wi], t_h2T], writes=[t_ps[pb]])
                        pg, pl = ps[4 + ei], ps[6 + ei]
                        P.op('dve', lambda e, pg=pg, ei=ei, fc=fc: e.tensor_scalar(
                            out=eg[ei][:, 0:N], in0=pg[:, 0:N], scalar1=bup[:, ex, fc:fc + 1], scalar2=7.0,
                            op0=ALU.add, op1=ALU.min), reads=[t_ps[4 + ei], t_c], writes=[t_eg[ei]])
                        P.op('act', lambda e, pl=pl, ei=ei, fc=fc: e.activation(
                            out=el[ei][:, 0:N], in_=pl[:, 0:N], func=AF.Identity, bias=bup[:, ex, 8 + fc:9 + fc], scale=1.0),
                            reads=[t_ps[6 + ei], t_c], writes=[t_el[ei]])
                        P.op('act', lambda e, ei=ei: e.activation(out=es[ei][:, 0:N], in_=eg[ei][:, 0:N], func=AF.Sigmoid, scale=1.702),
                             reads=[t_eg[ei]], writes=[t_es[ei]])
                        P.op('dve', lambda e, ei=ei: e.tensor_scalar(
                            out=el[ei][:, 0:N], in0=el[ei][:, 0:N], scalar1=7.0, scalar2=-7.0,
                            op0=ALU.min, op1=ALU.max), reads=[t_el[ei]], writes=[t_el[ei]])
                        P.op('dve' if fc % 2 else 'pool', lambda e, ei=ei: e.tensor_tensor(out=es[ei][:, 0:N], in0=es[ei][:, 0:N], in1=eg[ei][:, 0:N], op=ALU.mult),
                             reads=[t_eg[ei], t_es[ei]], writes=[t_es[ei]])
                        P.op('dve', lambda e, ei=ei, fc=fc: e.scalar_tensor_tensor(
                            out=actT[ai][:, fc, 0:N], in0=el[ei][:, 0:N], scalar=1.0, in1=es[ei][:, 0:N], op0=ALU.add, op1=ALU.mult),
                            reads=[t_es[ei], t_el[ei]], writes=[t_act[ai]])

                def down_unit(ui):
                    ex, g = units[ui]
                    wi = (wbase + ex) % 2
                    ai = ui % 2
                    gb = min(4, nb - g * 4)
                    for b4 in range(gb):
                        bi = g * 4 + b4
                        for h in range(2):
                            pb = (bi * 2 + h) % 4

                            def dmm(e, h=h, pb=pb, b4=b4):
                                for fc in range(8):
                                    ins = e.matmul(ps[pb][:], actT[ai][:, fc, b4 * 128:(b4 + 1) * 128],
                                                   wd[wi][:, fc, h * 512:(h + 1) * 512], start=(fc == 0), stop=(fc == 7))
                                return ins
                            P.op('pe', dmm, reads=[t_act[ai], t_wd[wi]], writes=[t_ps[pb]])
                            P.op('dve', lambda e, h=h, pb=pb, bi=bi: e.scalar_tensor_tensor(
                                out=acc[:, bi, h * 512:(h + 1) * 512], in0=ps[pb][:], scalar=gates[:, bi, ex:ex + 1],
                                in1=acc[:, bi, h * 512:(h + 1) * 512], op0=ALU.mult, op1=ALU.add),
                                reads=[t_ps[pb], t_gates, t_acc], writes=[t_acc])

                wbase = wcount
                wcount += NE
                load_w(0)
                load_w(1)
                up_unit(0)
                for ui in range(len(units)):
                    if ui + 1 < len(units):
                        up_unit(ui + 1)
                    down_unit(ui)
                    ex, g = units[ui]
                    if g == ngrp - 1 and ex + 2 < NE:
                        load_w(ex + 2)
                for bi in range(nb):
                    r0 = r_begin + (sb0 + bi) * 128
                    v = 1 if (r0 < self.TC) else 0
                    x_t = xt[bi % 2]; tx = t_xt[bi % 2]
                    P.op('sp', lambda e, x_t=x_t, r0=r0: e.dma_start(out=x_t[:], in_=self.rows(src, r0)), writes=[tx], dma=True)
                    P.op('pool', lambda e, bi=bi, v=v: e.tensor_tensor(out=acc[:, bi, :], in0=acc[:, bi, :], in1=self.gbc[v][:], op=ALU.mult),
                         reads=[t_acc, self.t_gbc], writes=[t_acc])
                    P.op('dve', lambda e, bi=bi, x_t=x_t: e.tensor_tensor(out=x_t[:], in0=x_t[:], in1=acc[:, bi, :], op=ALU.add),
                         reads=[t_acc, tx], writes=[tx])
                    if final:
                        P.op('act', lambda e, x_t=x_t: e.activation(out=junk[:], in_=x_t[:], func=AF.Square, accum_out=small[:, 8:9]),
                             reads=[tx], writes=[t_junk, t_small])
                        P.op('act', lambda e: e.activation(out=small[:, 9:10], in_=small[:, 8:9], func=AF.Sqrt, scale=1.0 / DM, bias=self.epsc[:, 0:1]),
                             reads=[t_small], writes=[t_small])
                        P.op('dve', lambda e: e.reciprocal(out=small[:, 10:11], in_=small[:, 9:10]), reads=[t_small], writes=[t_small])
                        P.op('dve', lambda e, x_t=x_t: e.scalar_tensor_tensor(out=x_t[:], in0=x_t[:], scalar=small[:, 10:11], in1=fw[:],
                                                                              op0=ALU.mult, op1=ALU.mult), reads=[tx, t_small, t_c], writes=[tx])
                        dst_ap = self.out[r0 - self.TC:r0 - self.TC + 128, :]
                    else:
                        dst_ap = self.D[dst][r0:r0 + 128, :]
                    P.op('sp', lambda e, x_t=x_t, dst_ap=dst_ap: e.dma_start(out=dst_ap, in_=x_t[:]), reads=[tx], writes=[t_out], dma=True)
            P.barrier()
            P.emit()


def col(v, n=128):
    return np.ascontiguousarray(np.asarray(v, np.float32).reshape(-1, n).T)


def rep(v, n=128):
    return np.ascontiguousarray(np.broadcast_to(np.asarray(v, np.float32)[None, :], (n, np.asarray(v).shape[0])))


def make_inputs(inp, b, T, TC):
    m = {}
    m['x'] = np.ascontiguousarray(inp['x'][b, :T])
    m['ctx'] = np.ascontiguousarray(inp['ctx'][b, :max(TC, 128)])
    cv = np.stack([col(inp['c'][b]), col(inp['c_ctx'])], axis=-1)
    m['cvec'] = np.ascontiguousarray(cv)
    m['ident'] = np.eye(128, dtype=np.float32)
    m['ones'] = np.ones((128, 128), np.float32)
    for l in range(2):
        p = 'l%d_' % l
        m[p + 'ada_w'] = inp[p + 'ada_w']
        m[p + 'ada_bT'] = col(inp[p + 'ada_b'])
        m[p + 'nmix'] = col(inp[p + 'norm_mix_w'])
        m[p + 'nffn'] = col(inp[p + 'norm_ffn_w'])
        m[p + 'router_w'] = inp[p + 'router_w']
        m[p + 'router_b'] = rep(inp[p + 'router_b'])
        m[p + 'w_up'] = inp[p + 'w_up']
        m[p + 'b_upT'] = np.ascontiguousarray(np.asarray(inp[p + 'b_up']).reshape(NE, 16, 128).transpose(2, 0, 1))
        m[p + 'w_down'] = inp[p + 'w_down']
        m[p + 'b_down'] = inp[p + 'b_down']
    m['final_w'] = rep(inp['final_norm_w'])
    m['l0_w_in'] = inp['l0_w_in']; m['l0_w_out'] = inp['l0_w_out']
    m['l0_w2'] = np.ascontiguousarray(np.stack([inp['l0_gla_w2_f'], inp['l0_gla_w2_b']], axis=1))
    m['l0_b2'] = np.ascontiguousarray(np.stack([col(inp['l0_gla_b_f']), col(inp['l0_gla_b_b'])], axis=1))
    m['l0_lb'] = np.ascontiguousarray(np.asarray(inp['hgrn_lb_logits'], np.float32).reshape(2, 4, 128).transpose(2, 0, 1))
    m['l0_normw'] = rep(np.concatenate([np.tile(inp['l0_gla_norm_w'], 4), np.tile(inp['l0_hgrn_norm_w'], 4)]))
    m['l1_w_in'] = inp['l1_w_in']; m['l1_w_out'] = inp['l1_w_out']
    m['l1_convw'] = np.ascontiguousarray(np.broadcast_to(np.asarray(inp['l1_conv_w'], np.float32)[None], (128, 5, 1536)))
    m['l1_pvec'] = rep(np.concatenate([inp['l1_a_log_f'], inp['l1_dt_bias_f'], inp['l1_a_log_b'], inp['l1_dt_bias_b']]))
    m['l1_dnw'] = rep(np.tile(inp['l1_dn_norm_w'], 4)); m['l1_sinks'] = rep(inp['l1_sinks'])
    m['rope'] = rope_table(T)
    t_ = np.arange(128)
    m['rmask'] = np.ascontiguousarray(np.broadcast_to(np.tile((t_ % 64 != 0).astype(np.float32), 6)[None, :], (128, 768)))
    same = (t_[:, None] // 64) == (t_[None, :] // 64)
    m['mask_f'] = (same & (t_[:, None] <= t_[None, :])).astype(np.float32)
    m['mask_r'] = (same & (t_[:, None] >= t_[None, :])).astype(np.float32)
    m['mask_fs'] = (same & (t_[:, None] < t_[None, :])).astype(np.float32)
    m['mask_rs'] = (same & (t_[:, None] > t_[None, :])).astype(np.float32)
    m['same'] = same.astype(np.float32)
    m['csel'] = np.stack([(t_ < 64), (t_ >= 64)], axis=1).astype(np.float32)
    m['mb_prev'] = np.where(t_[None, :] >= t_[:, None], 0.0, NEG).astype(np.float32)
    m['mb_next'] = np.where(t_[None, :] <= t_[:, None], 0.0, NEG).astype(np.float32)
    return m


def rope_table(T):
    rows = T // 64
    row = np.repeat(np.arange(rows, dtype=np.float32), 64)
    colp = np.tile(np.arange(64, dtype=np.float32), rows)
    inv = (10000.0 ** (-np.arange(16, dtype=np.float32) / 16)).astype(np.float32)
    ang = np.concatenate([row[:, None] * inv, colp[:, None] * inv], axis=-1).astype(np.float32)
    return np.ascontiguousarray(np.concatenate([np.cos(ang), np.sin(ang)], axis=-1).astype(np.float32))


_CACHE = {}


def kernel(**inputs):
    inp = {k: np.asarray(v) for k, v in inputs.items()}
    B, T, _ = inp['x'].shape
    TC = inp['ctx'].shape[1]
    key = (T, TC)
    import os
    ph = os.environ.get('KPHASES')
    bld = Builder(T, TC, phases=tuple(ph.split(','))) if ph else Builder(T, TC)
    nc = bld.build()
    in_maps = []
    for b in range(B):
        m = make_inputs(inp, b, T, TC)
        in_maps.append({k: m[k] for k in bld.in_names})
    res = run_bass_kernel_spmd(nc, in_maps, core_ids=list(range(B)))
    return np.stack([res.results[b]['out'] for b in range(B)], axis=0)


def phase_l0mix(self, src, dst):
    nc, P, D = self.nc, self.P, self.D
    TC, T = self.TC, self.T
    with ExitStack() as st:
        sb = lambda name, shape, d=F32: st.enter_context(nc.sbuf_tensor(name, shape, d))
        ps = [st.enter_context(nc.psum_tensor('pq%d' % i, [128, 512], F32)) for i in range(8)]
        ps4b = ps[4][:].bitcast(BF16)
        self.epsc = sb('epsc0', [128, 1])
        onec = sb('onec', [128, 1])
        win = sb('win', [128, 8, 4128], BF16)
        wout = sb('wout', [128, 8, DM], BF16)
        w2 = sb('w2', [16, 2, 256], BF16)
        b2 = sb('b2', [128, 2, 2]); lbl = sb('lbl', [128, 2, 4]); lbc = sb('lbc', [128, 4]); omlb = sb('omlb', [128, 4])
        normw = sb('normw', [128, DM]); rmask = sb('rmask_t', [128, 768])
        maskf = sb('maskf', [128, 128]); maskr = sb('maskr', [128, 128])
        xt = [sb('mxt%d' % i, [128, DM]) for i in range(2)]
        xn = sb('mxn', [128, DM]); junk = sb('mjunk', [128, DM], BF16); small = sb('msmall', [128, 32])
        hT = sb('hT', [128, 8, 128], BF16)
        arT = sb('arT', [16, 128], BF16)
        LF = sb('LF', [128, 6, 128]); bb = sb('bb', [128, 6, 128]); EA = sb('EA', [128, 6, 128]); EB = sb('EB', [128, 6, 128])
        E = sb('E', [128, 6, 2]); kH = sb('kH', [128, 4, 128]); sg = sb('sg', [128, 4, 128])
        qT = sb('qT', [128, 6, 128], BF16); kT = sb('kT', [128, 6, 128], BF16)
        ktok = sb('ktok', [128, 768], BF16); vtok = sb('vtok', [128, DM], BF16)
        SCm = sb('SCm', [128, 8, 128], BF16)
        S = sb('S', [128, 6, 128]); Tmp = sb('Tmp', [128, 6, 128])
        SB = [sb('SB%d' % i, [128, 6, 128], BF16) for i in range(4)]
        Oc = sb('Oc', [128, DM]); ofl = sb('ofl', [128, DM]); og = sb('og', [128, DM])
        yb = sb('yb', [128, DM], BF16); yT = sb('yT', [128, 8, 128], BF16)
        tk = {n: Tok() for n in ('c', 'x0', 'x1', 'xn', 'junk', 'small', 'hT', 'arT', 'LF', 'bb', 'EA', 'EB', 'E', 'kH', 'sg',
                                 'qT', 'kT', 'ktok', 'vtok', 'SCm', 'S', 'Tmp', 'SB0', 'SB1', 'SB2', 'SB3', 'Oc', 'ofl', 'og', 'yb', 'yT', 'out')}
        tp = [Tok() for _ in range(8)]
        c_ = [tk['c']]
        P.op('pool', lambda e: e.memset(self.epsc[:], EPS), writes=c_)
        P.op('pool', lambda e: e.memset(onec[:], 1.0), writes=c_)
        for (a, b_) in ((0, 2048), (2048, 4096), (4096, 4128)):
            P.op('pool', lambda e, a=a, b_=b_: e.dma_start(out=win[:, :, a:b_], in_=D['l0_w_in'][:, a:b_].rearrange("(k p) n -> p k n", p=128)),
                 writes=c_, dma=True)
        P.op('pool', lambda e: e.dma_start(out=wout[:], in_=D['l0_w_out'].rearrange("(k p) n -> p k n", p=128)), writes=c_, dma=True)
        P.op('pool', lambda e: e.dma_start(out=w2[:], in_=D['l0_w2']), writes=c_, dma=True)
        for nm, t_ in (('l0_b2', b2), ('l0_lb', lbl), ('l0_normw', normw), ('rmask', rmask), ('mask_f', maskf), ('mask_r', maskr)):
            P.op('sp', lambda e, nm=nm, t_=t_: e.dma_start(out=t_[:], in_=D[nm]), writes=c_, dma=True)
        P.op('dve', lambda e: e.tensor_scalar(out=b2[:], in0=b2[:], scalar1=-1.0, scalar2=None, op0=ALU.mult), reads=c_, writes=c_)
        P.op('dve', lambda e: e.tensor_tensor(out=lbc[:], in0=lbl[:, 0, :], in1=lbl[:, 1, :], op=ALU.subtract), reads=c_, writes=c_)
        P.op('act', lambda e: e.activation(out=omlb[:], in_=lbc[:], func=AF.Sigmoid, scale=-1.0), reads=c_, writes=c_)
        P.op('act', lambda e: e.activation(out=lbc[:], in_=lbc[:], func=AF.Sigmoid), reads=c_, writes=c_)
        self.gate_bcast(Oc[:].rearrange("p (k t) -> p k t", k=8), ps[0:2], 0, 16, (0, 1) if TC else (0,))
        P.barrier()
        A1 = self.Acol[0][0]; modT = self.modT[0]

        def mmgroup(pb, outs):
            def f(e):
                for out_ap, pairs in outs:
                    n = len(pairs)
                    for i, (l_, r_) in enumerate(pairs):
                        ins = e.matmul(out_ap, l_, r_, start=(i == 0), stop=(i == n - 1))
                return ins
            return f

        import os
        STOP = int(os.environ.get('L0STOP', '99'))

        def block(r0, is_ctx, d, first, bidx):
            v = 1 if is_ctx else 0
            x_t = xt[bidx % 2]; tx = tk['x%d' % (bidx % 2)]
            self.norm_xn(self.rows(src, r0), x_t, tx, xn, tk['xn'], small, tk['small'], junk, tk['junk'])
            for h in range(2):
                P.op('pe', mmgroup(None, []) if False else (lambda e, h=h: [e.transpose(ps[h][:, j * 128:(j + 1) * 128], xn[:, (h * 4 + j) * 128:(h * 4 + j + 1) * 128], self.ident[:]) for j in range(4)][-1]),
                     reads=[tk['xn'], self.t_const], writes=[tp[h]])
                for j in range(4):
                    k = h * 4 + j
                    P.op('act', lambda e, h=h, j=j, k=k: e.activation(out=hT[:, k, :], in_=ps[h][:, j * 128:(j + 1) * 128], func=AF.Identity,
                                                                      scale=A1[:, k, v:v + 1], bias=modT[:, k, v:v + 1]),
                         reads=[tp[h], self.t_mod], writes=[tk['hT']])
            rh = [tk['hT'], tk['c']]
            wcol = lambda c0, n=128: [(win[:, k, c0:c0 + n], hT[:, k, :]) for k in range(8)]
            c_ar = 1024 + d * 16
            P.op('pe', mmgroup(0, [(ps[0][0:16, 0:128], [(win[:, k, c_ar:c_ar + 16], hT[:, k, :]) for k in range(8)])]), reads=rh, writes=[tp[0]])
            P.op('act', lambda e: e.activation(out=arT[:], in_=ps[0][0:16, 0:128], func=AF.Identity), reads=[tp[0]], writes=[tk['arT']])
            P.op('pe', mmgroup(0, [(ps[0][:, 128 + c * 128:256 + c * 128], [(w2[:, d, c * 128:(c + 1) * 128], arT[:])]) for c in range(2)]),
                 reads=[tk['arT'], tk['c']], writes=[tp[0]])
            if STOP < 3:
                return
            c_bz = 2080 + d * 512
            P.op('pe', mmgroup(1, [(ps[1][:, c * 128:(c + 1) * 128], wcol(c_bz + c * 128)) for c in range(4)]), reads=rh, writes=[tp[1]])
            if STOP < 4:
                return
            for c in range(2):
                P.op('act', lambda e, c=c: e.activation(out=LF[:, c, :], in_=ps[0][:, 128 + c * 128:256 + c * 128], func=AF.Exp,
                                                        scale=-1.0, bias=b2[:, d, c:c + 1]), reads=[tp[0], tk['c']], writes=[tk['LF']])
            P.op('act', lambda e: e.activation(out=LF[:, 0:2, :], in_=LF[:, 0:2, :], func=AF.Ln, bias=onec[:, 0:1], scale=1.0),
                 reads=[tk['c']], writes=[tk['LF']])
            P.op('act', lambda e: e.activation(out=sg[:], in_=ps[1][:].rearrange("p (c t) -> p c t", c=4), func=AF.Sigmoid),
                 reads=[tp[1]], writes=[tk['sg']])
            P.op('dve', lambda e: e.tensor_scalar(out=LF[:, 0:2, :], in0=LF[:, 0:2, :], scalar1=-1.0 / 16.0, scalar2=None, op0=ALU.mult),
                 writes=[tk['LF']])
            P.op('dve', lambda e: e.tensor_tensor(out=sg[:], in0=sg[:], in1=omlb[:].unsqueeze(2).to_broadcast([128, 4, 128]), op=ALU.mult),
                 reads=[tk['c']], writes=[tk['sg']])
            P.op('dve', lambda e: e.tensor_tensor(out=sg[:], in0=sg[:], in1=lbc[:].unsqueeze(2).to_broadcast([128, 4, 128]), op=ALU.add),
                 reads=[tk['c']], writes=[tk['sg']])
            P.op('act', lambda e: e.activation(out=LF[:, 2:6, :], in_=sg[:], func=AF.Ln), reads=[tk['sg']], writes=[tk['LF']])
            P.op('dve', lambda e: e.tensor_scalar(out=kH[:], in0=sg[:], scalar1=-1.0, scalar2=1.0, op0=ALU.mult, op1=ALU.add),
                 reads=[tk['sg']], writes=[tk['kH']])
            if STOP < 5:
                return
            LF2 = LF[:].rearrange("p c t -> p (c t)"); bb2 = bb[:].rearrange("p c t -> p (c t)")
            P.op('dve', lambda e: e.tensor_tensor_scan(out=bb2, data0=rmask[:], data1=LF2, initial=0.0, op0=ALU.mult, op1=ALU.add),
                 reads=[tk['LF'], tk['c']], writes=[tk['bb']])
            tot = bb[:].rearrange("p c (a t) -> p c a t", a=2)[:, :, :, 63]
            P.op('act', lambda e: e.activation(out=E[:], in_=tot, func=AF.Exp), reads=[tk['bb']], writes=[tk['E']])
            if d == 1:
                P.op('dve', lambda e: e.tensor_tensor(out=bb[:], in0=bb[:], in1=LF[:], op=ALU.subtract), reads=[tk['LF']], writes=[tk['bb']])
            sa, sb_ = (1.0, -1.0) if d == 0 else (-1.0, 1.0)
            P.op('act', lambda e: e.activation(out=EA[:], in_=bb[:], func=AF.Exp, scale=sa), reads=[tk['bb']], writes=[tk['EA']])
            P.op('act', lambda e: e.activation(out=EB[:], in_=bb[:], func=AF.Exp, scale=sb_), reads=[tk['bb']], writes=[tk['EB']])
            if STOP < 6:
                return
            P.op('pe', mmgroup(2, [(ps[2][:, 0:128], wcol(0)), (ps[2][:, 128:256], wcol(128)),
                                   (ps[2][:, 256:384], wcol(1568)), (ps[2][:, 384:512], wcol(1696))]), reads=rh, writes=[tp[2]])
            P.op('pe', mmgroup(3, [(ps[3][:, 0:128], wcol(1824)), (ps[3][:, 128:256], wcol(1952)),
                                   (ps[3][:, 256:384], wcol(256)), (ps[3][:, 384:512], wcol(384))]), reads=rh, writes=[tp[3]])
            v3 = lambda ap, n: ap.rearrange("p (c t) -> p c t", c=n)
            P.op('dve', lambda e: e.scalar_tensor_tensor(out=qT[:, 0:2, :], in0=v3(ps[2][:, 0:256], 2), scalar=0.125, in1=EA[:, 0:2, :],
                                                         op0=ALU.mult, op1=ALU.mult), reads=[tp[2], tk['EA']], writes=[tk['qT']])
            P.op('dve', lambda e: e.tensor_tensor(out=qT[:, 2:4, :], in0=v3(ps[2][:, 256:512], 2), in1=EA[:, 2:4, :], op=ALU.mult),
                 reads=[tp[2], tk['EA']], writes=[tk['qT']])
            P.op('dve', lambda e: e.tensor_tensor(out=qT[:, 4:6, :], in0=v3(ps[3][:, 0:256], 2), in1=EA[:, 4:6, :], op=ALU.mult),
                 reads=[tp[3], tk['EA']], writes=[tk['qT']])
            P.op('dve', lambda e: e.tensor_tensor(out=kT[:, 0:2, :], in0=v3(ps[3][:, 256:512], 2), in1=EB[:, 0:2, :], op=ALU.mult),
                 reads=[tp[3], tk['EB']], writes=[tk['kT']])
            P.op('pool', lambda e: e.tensor_tensor(out=kT[:, 2:6, :], in0=kH[:], in1=EB[:, 2:6, :], op=ALU.mult),
                 reads=[tk['kH'], tk['EB']], writes=[tk['kT']])
            if STOP < 7:
                return
            P.op('pe', lambda e: [e.transpose(ps4b[:, c * 128:(c + 1) * 128], kT[:, c, :], self.identb[:]) for c in range(6)][-1],
                 reads=[tk['kT'], self.t_const], writes=[tp[4]])
            P.op('act', lambda e: e.activation(out=ktok[:], in_=ps4b[:, 0:768], func=AF.Identity), reads=[tp[4]], writes=[tk['ktok']])
            if STOP < 8:
                return
            for i, c0 in enumerate((512, 3104)):
                P.op('pe', mmgroup(5, [(ps[5][:], [(hT[:, k, :], win[:, k, c0:c0 + 512]) for k in range(8)])]), reads=rh, writes=[tp[5]])
                P.op('act', lambda e, i=i: e.activation(out=vtok[:, i * 512:(i + 1) * 512], in_=ps[5][:], func=AF.Identity),
                     reads=[tp[5]], writes=[tk['vtok']])
            if STOP < 9:
                return
            def rows_of(h):
                if h < 4:
                    return slice((h % 2) * 64, (h % 2) * 64 + 64), h // 2
                return slice(0, 128), h - 2
            bankheads = ((0, 2, 4, 5), (1, 3, 6, 7))
            scidx = {h: half * 4 + hh for half in range(2) for hh, h in enumerate(bankheads[half])}
            for half in range(2):
                outs = []
                for hh in range(4):
                    h = bankheads[half][hh]
                    rs, bk = rows_of(h)
                    outs.append((ps[6 + half][:, hh * 128:(hh + 1) * 128], [(kT[rs, bk, :], qT[rs, bk, :])]))
                P.op('pe', mmgroup(6 + half, outs), reads=[tk['kT'], tk['qT']], writes=[tp[6 + half]])
                mk = maskf if d == 0 else maskr
                P.op('dve', lambda e, half=half, mk=mk: e.tensor_tensor(
                    out=SCm[:, half * 4:(half + 1) * 4, :], in0=v3(ps[6 + half][:], 4), in1=mk[:].unsqueeze(1).to_broadcast([128, 4, 128]),
                    op=ALU.mult), reads=[tp[6 + half], tk['c']], writes=[tk['SCm']])
            if STOP < 10:
                return
            order = (0, 1) if d == 0 else (1, 0)
            pbank = {order[0]: (0, 1), order[1]: (2, 3)}
            for c in order:
                pa, pb_ = pbank[c]
                cs = slice(c * 64, c * 64 + 64)
                outsA, outsB = [], []
                for h in range(8):
                    rs, bk = rows_of(h)
                    if h < 4:
                        l_ = ktok[cs, bk * 128 + (h % 2) * 64: bk * 128 + (h % 2) * 64 + 64]
                    else:
                        l_ = ktok[cs, bk * 128:(bk + 1) * 128]
                    r_ = vtok[cs, h * 128:(h + 1) * 128]
                    if bk < 4:
                        outsA.append((ps[pa][rs, bk * 128:(bk + 1) * 128], [(l_, r_)]))
                    else:
                        outsB.append((ps[pb_][rs, (bk - 4) * 128:(bk - 3) * 128], [(l_, r_)]))
                P.op('pe', mmgroup(pa, outsA), reads=[tk['ktok'], tk['vtok']], writes=[tp[pa]])
                P.op('pe', mmgroup(pb_, outsB), reads=[tk['ktok'], tk['vtok']], writes=[tp[pb_]])
            if STOP < 11:
                return
            st_i = 2 * (bidx % 2); mid_i = st_i + 1; end_i = 2 * ((bidx + 1) % 2)
            if first:
                P.op('pool', lambda e: e.memset(S[:], 0.0), writes=[tk['S']])
                P.op('pool', lambda e: e.memset(SB[st_i][:], 0.0), writes=[tk['SB%d' % st_i]])
            for i, c in enumerate(order):
                pa, pb_ = pbank[c]
                Ebc = E[:, :, c:c + 1].to_broadcast([128, 6, 128])
                PA = v3(ps[pa][:], 4); PB = v3(ps[pb_][:, 0:256], 2)
                if d == 0:
                    wi_ = mid_i if i == 0 else end_i
                    P.op('dve', lambda e, PA=PA: e.tensor_tensor(out=Tmp[:, 0:4, :], in0=PA, in1=S[:, 0:4, :], op=ALU.add),
                         reads=[tp[pa], tk['S']], writes=[tk['Tmp']])
                    P.op('dve', lambda e, PB=PB: e.tensor_tensor(out=Tmp[:, 4:6, :], in0=PB, in1=S[:, 4:6, :], op=ALU.add),
                         reads=[tp[pb_], tk['S']], writes=[tk['Tmp']])
                    P.op('dve', lambda e, Ebc=Ebc: e.tensor_tensor(out=S[:], in0=Tmp[:], in1=Ebc, op=ALU.mult),
                         reads=[tk['Tmp'], tk['E']], writes=[tk['S']])
                    P.op('act', lambda e, wi_=wi_: e.activation(out=SB[wi_][:], in_=S[:], func=AF.Identity),
                         reads=[tk['S']], writes=[tk['SB%d' % wi_]])
                else:
                    wi_ = st_i if i == 0 else mid_i
                    P.op('dve', lambda e, Ebc=Ebc: e.tensor_tensor(out=S[:], in0=S[:], in1=Ebc, op=ALU.mult),
                         reads=[tk['E']], writes=[tk['S']])
                    P.op('act', lambda e, wi_=wi_: e.activation(out=SB[wi_][:], in_=S[:], func=AF.Identity),
                         reads=[tk['S']], writes=[tk['SB%d' % wi_]])
                    P.op('dve', lambda e, PA=PA: e.tensor_tensor(out=S[:, 0:4, :], in0=PA, in1=S[:, 0:4, :], op=ALU.add),
                         reads=[tp[pa]], writes=[tk['S']])
                    P.op('dve', lambda e, PB=PB: e.tensor_tensor(out=S[:, 4:6, :], in0=PB, in1=S[:, 4:6, :], op=ALU.add),
                         reads=[tp[pb_]], writes=[tk['S']])
            if STOP < 12:
                return
            for half in range(2):
                def omm(e, half=half):
                    for hh in range(4):
                        h = half * 4 + hh
                        rs, bk = rows_of(h)
                        ob = ps[4 + half]
                        e.matmul(ob[:, hh * 128:(hh + 1) * 128], SCm[:, scidx[h], :], vtok[:, h * 128:(h + 1) * 128], start=True, stop=False)
                        for i, c in enumerate(order):
                            snap = SB[st_i] if i == 0 else SB[mid_i]
                            ins = e.matmul(ob[c * 64:(c + 1) * 64, hh * 128:(hh + 1) * 128], qT[rs, bk, c * 64:(c + 1) * 64],
                                           snap[rs, bk, :], start=False, stop=True)
                    return ins
                P.op('pe', omm, reads=[tk['SCm'], tk['vtok'], tk['qT'], tk['SB%d' % st_i], tk['SB%d' % mid_i]], writes=[tp[4 + half]])
            if STOP < 13:
                return
            if d == 0:
                for half in range(2):
                    P.op('act', lambda e, half=half: e.activation(out=Oc[:, half * 512:(half + 1) * 512], in_=ps[4 + half][:], func=AF.Identity),
                         reads=[tp[4 + half]], writes=[tk['Oc']])
                P.op('sp', lambda e: e.dma_start(out=D['OF'][r0:r0 + 128, :], in_=Oc[:]), reads=[tk['Oc']], writes=[tk['out']], dma=True)
                return
            P.op('sp', lambda e: e.dma_start(out=ofl[:], in_=D['OF'][r0:r0 + 128, :]), writes=[tk['ofl']], dma=True)
            for half in range(2):
                P.op('dve', lambda e, half=half: e.tensor_tensor(out=Oc[:, half * 512:(half + 1) * 512], in0=ps[4 + half][:],
                                                                 in1=ofl[:, half * 512:(half + 1) * 512], op=ALU.add),
                     reads=[tp[4 + half], tk['ofl']], writes=[tk['Oc']])
            for half, c0 in enumerate((1056, 3616)):
                P.op('pe', mmgroup(6 + half, [(ps[6 + half][:], [(hT[:, k, :], win[:, k, c0:c0 + 512]) for k in range(8)])]),
                     reads=rh, writes=[tp[6 + half]])
                P.op('act', lambda e, half=half: e.activation(out=og[:, half * 512:(half + 1) * 512], in_=ps[6 + half][:], func=AF.Silu),
                     reads=[tp[6 + half]], writes=[tk['og']])
            O3 = Oc[:].rearrange("p (h v) -> p h v", h=8)
            P.op('pool', lambda e: e.tensor_tensor(out=ofl[:], in0=Oc[:], in1=Oc[:], op=ALU.mult), reads=[tk['Oc']], writes=[tk['ofl']])
            P.op('dve', lambda e: e.tensor_reduce(out=small[:, 8:16], in_=ofl[:].rearrange("p (h v) -> p h v", h=8), axis=AX.X, op=ALU.add),
                 reads=[tk['ofl']], writes=[tk['small']])
            P.op('act', lambda e: e.activation(out=small[:, 16:24], in_=small[:, 8:16], func=AF.Sqrt, scale=1.0 / 128, bias=self.epsc[:, 0:1]),
                 reads=[tk['small']], writes=[tk['small']])
            P.op('dve', lambda e: e.reciprocal(out=small[:, 24:32], in_=small[:, 16:24]), reads=[tk['small']], writes=[tk['small']])
            P.op('dve', lambda e: e.tensor_tensor(out=O3, in0=O3, in1=small[:, 24:32].unsqueeze(2).to_broadcast([128, 8, 128]), op=ALU.mult),
                 reads=[tk['small']], writes=[tk['Oc']])
            P.op('pool', lambda e: e.tensor_tensor(out=Oc[:], in0=Oc[:], in1=normw[:], op=ALU.mult), reads=[tk['c']], writes=[tk['Oc']])
            P.op('dve', lambda e: e.tensor_tensor(out=yb[:], in0=Oc[:], in1=og[:], op=ALU.mult), reads=[tk['Oc'], tk['og']], writes=[tk['yb']])
            P.op('pe', lambda e: [e.transpose(ps4b[:, k * 128:(k + 1) * 128], yb[:, k * 128:(k + 1) * 128], self.identb[:]) for k in range(8)][-1],
                 reads=[tk['yb'], self.t_const], writes=[tp[4]])
            P.op('act', lambda e: e.activation(out=yT[:].rearrange("p k t -> p (k t)"), in_=ps4b[:, 0:1024], func=AF.Identity),
                 reads=[tp[4]], writes=[tk['yT']])
            for half in range(2):
                P.op('pe', mmgroup(6 + half, [(ps[6 + half][:], [(yT[:, k, :], wout[:, k, half * 512:(half + 1) * 512]) for k in range(8)])]),
                     reads=[tk['yT'], tk['c']], writes=[tp[6 + half]])
                P.op('dve', lambda e, half=half: e.tensor_tensor(out=Oc[:, half * 512:(half + 1) * 512], in0=ps[6 + half][:],
                                                                 in1=self.gbc[v][:, half * 512:(half + 1) * 512], op=ALU.mult),
                     reads=[tp[6 + half], self.t_gbc], writes=[tk['Oc']])
            P.op('pool', lambda e: e.tensor_tensor(out=x_t[:], in0=x_t[:], in1=Oc[:], op=ALU.add), reads=[tk['Oc']], writes=[tx])
            P.op('sp', lambda e: e.dma_start(out=D[dst][r0:r0 + 128, :], in_=x_t[:]), reads=[tx], writes=[tk['out']], dma=True)

        NCB, NB = self.NCB, self.NB
        for d in range(2):
            seq = [(i * 128, True) for i in range(NCB)] + [(TC + i * 128, False) for i in range(NB)]
            if d == 1:
                seq = [(i * 128, True) for i in reversed(range(NCB))] + [(TC + i * 128, False) for i in reversed(range(NB))]
            for n, (r0, is_ctx) in enumerate(seq):
                block(r0, is_ctx, d, n == 0, n)
                if d == 0:
                    self.issue_cast(0, n)
            P.barrier()
        P.emit()


Builder.phase_l0mix = phase_l0mix


def phase_final(self, src):
    nc, P, D = self.nc, self.P, self.D
    with ExitStack() as st:
        sb = lambda name, shape, d=F32: st.enter_context(nc.sbuf_tensor(name, shape, d))
        self.epsc = sb('epscf', [128, 1])
        xt = [sb('fxt%d' % i, [128, DM]) for i in range(2)]
        junk = sb('fjunk', [128, DM], BF16); small = sb('fsmall', [128, 8]); fw = sb('ffw', [128, DM])
        tc_, tj, ts, to = Tok(), Tok(), Tok(), Tok()
        txs = [Tok(), Tok()]
        P.op('pool', lambda e: e.memset(self.epsc[:], EPS), writes=[tc_])
        P.op('sp', lambda e: e.dma_start(out=fw[:], in_=D['final_w']), writes=[tc_], dma=True)
        for bi in range(self.NB):
            r0 = self.TC + bi * 128
            x_t = xt[bi % 2]; tx = txs[bi % 2]
            self.norm_xn(self.rows(src, r0), x_t, tx, x_t, tx, small, ts, junk, tj)
            P.op('dve', lambda e, x_t=x_t: e.tensor_tensor(out=x_t[:], in0=x_t[:], in1=fw[:], op=ALU.mult), reads=[tc_], writes=[tx])
            P.op('sp', lambda e, x_t=x_t, r0=r0: e.dma_start(out=self.out[r0 - self.TC:r0 - self.TC + 128, :], in_=x_t[:]),
                 reads=[tx], writes=[to], dma=True)
        P.barrier()
        P.emit()


Builder.phase_final = phase_final


NEG = -30000.0


def phase_l1mix(self, src, dst):
    nc, P, D = self.nc, self.P, self.D
    TC, T, NCB, NB = self.TC, self.T, self.NCB, self.NB
    R = TC + T
    cxrow = lambda r: (2 + r) if r < TC else (r + 6)
    A1 = self.Acol[1][0]; modT = self.modT[1]

    def mmgroup(outs):
        def f(e):
            for out_ap, pairs in outs:
                n = len(pairs)
                for i, (l_, r_) in enumerate(pairs):
                    ins = e.matmul(out_ap, l_, r_, start=(i == 0), stop=(i == n - 1))
            return ins
        return f

    def trs(dsts_srcs, idt):
        def f(e):
            for o_, i_ in dsts_srcs:
                ins = e.transpose(o_, i_, idt)
            return ins
        return f

    with ExitStack() as st:
        sb = lambda name, shape, d=F32: st.enter_context(nc.sbuf_tensor(name, shape, d))
        ps = [st.enter_context(nc.psum_tensor('pr%d' % i, [128, 512], F32)) for i in range(8)]
        psb = [p_[:].bitcast(BF16) for p_ in ps]
        self.epsc = sb('epsc1', [128, 1]); onec = sb('onec1', [128, 1])
        win = sb('win1', [128, 8, 2832], BF16); wout = sb('wout1', [128, 8, DM], BF16)
        convw = sb('convw', [128, 5, 1536], BF16); pvec = sb('pvec', [128, 16]); dnw = sb('dnw', [128, 512]); sinks = sb('sinks', [128, 8])
        maskf = sb('maskf1', [128, 128]); maskr = sb('maskr1', [128, 128]); maskfs = sb('maskfs', [128, 128]); maskrs = sb('maskrs', [128, 128])
        same = sb('same_t', [128, 128]); csel = sb('csel_t', [128, 2]); mbp = sb('mbp', [128, 128]); mbn = sb('mbn', [128, 128])
        xt = [sb('lxt%d' % i, [128, DM]) for i in range(2)]
        xn = sb('lxn', [128, DM]); junk = sb('ljunk', [128, DM], BF16); small = sb('lsmall', [128, 64])
        hT = sb('lhT', [128, 8, 128], BF16)
        cxs = sb('cxs', [128, 1536]); cxl = [sb('cxl%d' % i, [128, 1536]) for i in range(2)]
        kv = sb('kv', [128, 256]); rope = sb('rope_t', [128, 64]); rt = sb('rt', [128, 8, 64]); kTs = sb('kTs', [128, 128])
        tk = {n: Tok() for n in ('c', 'x0', 'x1', 'xn', 'junk', 'small', 'hT', 'cxs', 'cxl0', 'cxl1', 'kv', 'rope', 'rt', 'kTs', 'out',
                                 'qkv', 'qkb', 'fT', 'g', 'gs', 'GB', 'Lts', 'Lst', 'A', 'AT', 'Q', 'QT', 'Rm', 'Rb', 'kbg', 'kd', 'bv',
                                 'u', 'wT', 'S', 'Sb0', 'Sb1', 'vn', 'Pst', 'egr', 'qg', 'Oc', 'ofl', 'og', 'yb', 'yT', 'q8', 'qT8',
                                 'kTl', 'vl', 'kcT', 'vc', 'ssb', 'pb', 'pT', 'att')}
        tp = [Tok() for _ in range(8)]
        c_ = [tk['c']]
        P.op('pool', lambda e: e.memset(self.epsc[:], EPS), writes=c_)
        P.op('pool', lambda e: e.memset(onec[:], 1.0), writes=c_)
        for (a, b_) in ((0, 2048), (2048, 2832)):
            P.op('pool', lambda e, a=a, b_=b_: e.dma_start(out=win[:, :, a:b_], in_=D['l1_w_in'][:, a:b_].rearrange("(k p) n -> p k n", p=128)),
                 writes=c_, dma=True)
        P.op('pool', lambda e: e.dma_start(out=wout[:], in_=D['l1_w_out'].rearrange("(k p) n -> p k n", p=128)), writes=c_, dma=True)
        P.op('pool', lambda e: e.dma_start(out=convw[:], in_=D['l1_convw']), writes=c_, dma=True)
        for nm, t_ in (('l1_pvec', pvec), ('l1_dnw', dnw), ('l1_sinks', sinks), ('mask_f', maskf), ('mask_r', maskr),
                       ('mask_fs', maskfs), ('mask_rs', maskrs), ('same', same), ('csel', csel), ('mb_prev', mbp), ('mb_next', mbn)):
            P.op('sp', lambda e, nm=nm, t_=t_: e.dma_start(out=t_[:], in_=D[nm]), writes=c_, dma=True)
        for c0 in (0, 8):
            P.op('act', lambda e, c0=c0: e.activation(out=pvec[:, c0:c0 + 4], in_=pvec[:, c0:c0 + 4], func=AF.Exp), reads=c_, writes=c_)
            P.op('dve', lambda e, c0=c0: e.tensor_scalar(out=pvec[:, c0:c0 + 4], in0=pvec[:, c0:c0 + 4], scalar1=-1.0, scalar2=None, op0=ALU.mult),
                 reads=c_, writes=c_)
        P.op('pool', lambda e: e.memset(cxs[:], 0.0), writes=[tk['cxs']])
        for r in (0, TC + 2, TC + 4, TC + T + 6):
            P.op('sp', lambda e, r=r: e.dma_start(out=D['CX'][r:r + 2, :], in_=cxs[0:2, :]), reads=[tk['cxs']], writes=[tk['out']], dma=True)
        self.gate_bcast(xn[:].rearrange("p (k t) -> p k t", k=8), ps[0:2], 1, 16, (0,))
        P.barrier()

        def norm_hT(r0, v, bidx):
            x_t = xt[bidx % 2]; tx = tk['x%d' % (bidx % 2)]
            self.norm_xn(self.rows(src, r0), x_t, tx, xn, tk['xn'], small, tk['small'], junk, tk['junk'])
            for h in range(2):
                P.op('pe', trs([(ps[h][:, j * 128:(j + 1) * 128], xn[:, (h * 4 + j) * 128:(h * 4 + j + 1) * 128]) for j in range(4)], self.ident[:]),
                     reads=[tk['xn'], self.t_const], writes=[tp[h]])
                for j in range(4):
                    k = h * 4 + j
                    P.op('act', lambda e, h=h, j=j, k=k: e.activation(out=hT[:, k, :], in_=ps[h][:, j * 128:(j + 1) * 128], func=AF.Identity,
                                                                      scale=A1[:, k, v:v + 1], bias=modT[:, k, v:v + 1]),
                         reads=[tp[h], self.t_mod], writes=[tk['hT']])
            return x_t, tx

        rh = [tk['hT'], tk['c']]
        tokmm = lambda c0, n: [(hT[:, k, :], win[:, k, c0:c0 + n]) for k in range(8)]

        def pre_block(r0, is_ctx, bidx):
            norm_hT(r0, 1 if is_ctx else 0, bidx)
            for j in range(3):
                P.op('pe', mmgroup([(ps[2 + j][:], tokmm(j * 512, 512))]), reads=rh, writes=[tp[2 + j]])
                P.op('act', lambda e, j=j: e.activation(out=cxs[:, j * 512:(j + 1) * 512], in_=ps[2 + j][:], func=AF.Identity),
                     reads=[tp[2 + j]], writes=[tk['cxs']])
            cr = cxrow(r0)
            P.op('sp', lambda e: e.dma_start(out=D['CX'][cr:cr + 128, :], in_=cxs[:]), reads=[tk['cxs']], writes=[tk['out']], dma=True)
            P.op('pe', mmgroup([(ps[5][:, 0:256], tokmm(2576, 256))]), reads=rh, writes=[tp[5]])
            P.op('act', lambda e: e.activation(out=kv[:], in_=ps[5][:, 0:256], func=AF.Identity), reads=[tp[5]], writes=[tk['kv']])
            if not is_ctx:
                t0 = r0 - TC
                P.op('sp', lambda e: e.dma_start(out=rope[:], in_=D['rope'][t0:t0 + 128, :]), writes=[tk['rope']], dma=True)
                self.apply_rope(kv[:, 0:128].rearrange("p (h f) -> p h f", h=2), 2, rope, rt, [tk['kv']], tk['rope'], tk['rt'])
            P.op('sp', lambda e: e.dma_start(out=D['VV'][r0:r0 + 128, :], in_=kv[:, 128:256]), reads=[tk['kv']], writes=[tk['out']], dma=True)
            P.op('pe', trs([(ps[6][:, 0:128], kv[:, 0:128])], self.ident[:]), reads=[tk['kv'], self.t_const], writes=[tp[6]])
            P.op('act', lambda e: e.activation(out=kTs[:], in_=ps[6][:, 0:128], func=AF.Identity), reads=[tp[6]], writes=[tk['kTs']])
            P.op('sp', lambda e: e.dma_start(out=D['KT'][:, r0:r0 + 128], in_=kTs[:]), reads=[tk['kTs']], writes=[tk['out']], dma=True)
            P.op('sp', lambda e: e.dma_start(out=D['KT2'][0:64, r0:r0 + 128], in_=kTs[64:128, :]), reads=[tk['kTs']], writes=[tk['out']], dma=True)
            P.op('sp', lambda e: e.dma_start(out=D['KT2'][64:128, r0:r0 + 128], in_=kTs[0:64, :]), reads=[tk['kTs']], writes=[tk['out']], dma=True)

        seq_all = [(i * 128, True) for i in range(NCB)] + [(TC + i * 128, False) for i in range(NB)]
        for n, (r0, is_ctx) in enumerate(seq_all):
            pre_block(r0, is_ctx, n)
        P.barrier()

        qkv = cxs; qkb = sb('qkb', [128, 8, 128], BF16); fT = sb('fT', [128, 8, 128], BF16)
        sm16 = small
        gs = sb('gs', [128, 8]); GB = sb('GB', [128, 4, 128]); totrow = sb('totrow', [128, 8])
        Lts = sb('Lts', [128, 4, 128]); Lst = sb('Lst', [128, 4, 128]); LstS = sb('LstS', [128, 4, 128])
        Am = sb('Am', [128, 4, 128]); AT = sb('AT', [128, 4, 128]); Qm = sb('Qm', [128, 4, 128]); QT = sb('QT', [128, 4, 128])
        Rm = sb('Rm', [128, 4, 128]); Rb = sb('Rb', [128, 4, 128], BF16)
        kbg = sb('kbg', [128, 4, 128], BF16); kd = sb('kd', [128, 4, 128], BF16); bv = sb('bv', [128, 4, 128], BF16)
        u = sb('u', [128, 4, 128]); wT = sb('wT', [128, 4, 128], BF16)
        S = sb('S1', [128, 4, 128]); Sb = [sb('Sb%d' % i, [128, 4, 128], BF16) for i in range(2)]
        vn = sb('vn', [128, 4, 128], BF16); Pst = sb('Pst', [128, 4, 128], BF16); egr = sb('egr', [128, 4, 128]); qg = sb('qg', [128, 4, 128], BF16)
        Oc = sb('Oc1', [128, DM]); ofl = sb('ofl1', [128, 512]); og = sb('og1', [128, 512]); yb = sb('yb1', [128, DM], BF16)
        yT = sb('yT1', [128, 8, 128], BF16)
        q8 = sb('q8', [128, 512]); q8b = sb('q8b', [128, 512], BF16); qT8 = sb('qT8', [128, 4, 128], BF16)
        kTl2 = [sb('kTl%d' % i, [128, 384]) for i in range(2)]; kTlb2 = [sb('kTlb%d' % i, [128, 384], BF16) for i in range(2)]; vl = sb('vl', [128, 3, 128]); vlb = sb('vlb', [128, 3, 128], BF16)
        kcT = sb('kcT', [128, max(TC, 128)]); kcTb2 = [sb('kcTb%d' % i, [128, max(TC, 128)], BF16) for i in range(2)]
        vc = sb('vc', [128, max(NCB, 1), 128]); vcb = sb('vcb', [128, max(NCB, 1), 128], BF16)
        NK = TC + 384
        ssb = sb('ssb', [128, NK]); pbf = sb('pbf', [128, NK], BF16); pT = sb('pT', [128, NK // 128, 128], BF16)
        v4 = lambda ap: ap.rearrange("p (h t) -> p h t", h=4)
        snap_ctr = [0]

        def scan_block(r0, is_ctx, d, first, bidx):
            v = 1 if is_ctx else 0
            x_t, tx = norm_hT(r0, v, bidx)
            c16 = 1536
            P.op('pe', mmgroup([(ps[2][:, 0:16], tokmm(c16, 16))]), reads=rh, writes=[tp[2]])
            P.op('act', lambda e: e.activation(out=small[:, 8:24], in_=ps[2][:, 0:16], func=AF.Identity), reads=[tp[2]], writes=[tk['small']])
            cb = 8 + d * 4; ca = 16 + d * 4; pA = d * 8; pB = d * 8 + 4
            sm = [tk['small']]
            P.op('act', lambda e: e.activation(out=small[:, 24:28], in_=small[:, cb:cb + 4], func=AF.Sigmoid), reads=sm, writes=sm)
            P.op('dve', lambda e: e.tensor_tensor(out=small[:, 28:32], in0=small[:, ca:ca + 4], in1=pvec[:, pB:pB + 4], op=ALU.add), reads=sm + c_, writes=sm)
            P.op('act', lambda e: e.activation(out=small[:, 28:32], in_=small[:, 28:32], func=AF.Exp), reads=sm, writes=sm)
            P.op('act', lambda e: e.activation(out=small[:, 28:32], in_=small[:, 28:32], func=AF.Ln, bias=onec[:, 0:1], scale=1.0), reads=sm, writes=sm)
            P.op('dve', lambda e: e.tensor_tensor(out=small[:, 28:32], in0=small[:, 28:32], in1=pvec[:, pA:pA + 4], op=ALU.mult), reads=sm + c_, writes=sm)
            tri = maskf if d == 0 else maskr
            P.op('pe', mmgroup([(ps[2][:, 32:36], [(tri[:], small[:, 28:32])]), (ps[2][:, 36:40], [(same[:], small[:, 28:32])])]),
                 reads=sm + c_, writes=[tp[2]])
            P.op('dve', lambda e: e.tensor_copy(out=small[:, 32:40], in_=ps[2][:, 32:40]), reads=[tp[2]], writes=sm)
            P.op('act', lambda e: e.activation(out=small[:, 40:44], in_=small[:, 32:36], func=AF.Exp), reads=sm, writes=sm)
            P.op('dve', lambda e: e.tensor_tensor(out=small[:, 40:44], in0=small[:, 40:44], in1=small[:, 24:28], op=ALU.mult), reads=sm, writes=sm)
            P.op('dve', lambda e: e.tensor_tensor(out=small[:, 44:48], in0=small[:, 36:40], in1=small[:, 32:36], op=ALU.subtract), reads=sm, writes=sm)
            P.op('act', lambda e: e.activation(out=small[:, 44:48], in_=small[:, 44:48], func=AF.Exp), reads=sm, writes=sm)
            P.op('dve', lambda e: e.tensor_tensor(out=GB[:], in0=self.ones[:].unsqueeze(1).to_broadcast([128, 4, 128]),
                                                  in1=small[:, 28:32].unsqueeze(2).to_broadcast([128, 4, 128]), op=ALU.mult),
                 reads=sm + [self.t_const], writes=[tk['GB']])
            P.op('dve', lambda e: e.tensor_tensor(out=gs[:].rearrange("p (h c) -> p h c", c=2), in0=small[:, 28:32].unsqueeze(2).to_broadcast([128, 4, 2]),
                                                  in1=csel[:].unsqueeze(1).to_broadcast([128, 4, 2]), op=ALU.mult), reads=sm + c_, writes=[tk['gs']])
            P.op('pe', mmgroup([(ps[3][:, h * 128:(h + 1) * 128], [(GB[:, h, :], tri[:])]) for h in range(4)]), reads=[tk['GB']] + c_, writes=[tp[3]])
            P.op('pe', mmgroup([(ps[2][:, 48:56], [(self.ones[:], gs[:])])]), reads=[tk['gs'], self.t_const], writes=[tp[2]])
            P.op('act', lambda e: e.activation(out=totrow[:], in_=ps[2][:, 48:56], func=AF.Exp), reads=[tp[2]], writes=[tk['gs']])
            gamrow = v4(ps[3][:])
            gcol = small[:, 32:36].unsqueeze(2).to_broadcast([128, 4, 128])
            mts, mst = (maskr, maskf) if d == 0 else (maskf, maskr)
            mtsS, mstS = (maskrs, maskfs) if d == 0 else (maskfs, maskrs)
            P.op('dve', lambda e: e.tensor_tensor(out=Lts[:], in0=gamrow, in1=gcol, op=ALU.subtract), reads=[tp[3]] + sm, writes=[tk['Lts']])
            P.op('pool', lambda e: e.tensor_scalar(out=Lst[:], in0=Lts[:], scalar1=0.0, scalar2=None, op0=ALU.min), reads=[tk['Lts']], writes=[tk['Lst']])
            P.op('dve', lambda e: e.tensor_scalar(out=Lts[:], in0=Lts[:], scalar1=0.0, scalar2=None, op0=ALU.max), reads=[tk['Lst']], writes=[tk['Lts']])
            P.op('act', lambda e: e.activation(out=egr[:], in_=gamrow, func=AF.Exp), reads=[tp[3], tk['Lts']], writes=[tk['egr']])
            P.op('act', lambda e: e.activation(out=Lts[:], in_=Lts[:], func=AF.Exp, scale=-1.0), writes=[tk['Lts']])
            P.op('act', lambda e: e.activation(out=Lst[:], in_=Lst[:], func=AF.Exp), writes=[tk['Lst']])
            P.op('dve', lambda e: e.tensor_tensor(out=Lts[:], in0=Lts[:], in1=mtsS[:].unsqueeze(1).to_broadcast([128, 4, 128]), op=ALU.mult),
                 reads=c_, writes=[tk['Lts']])
            P.op('pool', lambda e: e.tensor_tensor(out=LstS[:], in0=Lst[:], in1=mst[:].unsqueeze(1).to_broadcast([128, 4, 128]), op=ALU.mult),
                 reads=[tk['Lst']] + c_, writes=[tk['A']])
            cr = cxrow(r0)
            for j in range(5):
                cl = cxl[j % 2]; tcl = tk['cxl%d' % (j % 2)]
                P.op('sp', lambda e, j=j, cl=cl: e.dma_start(out=cl[:], in_=D['CX'][cr + j - 2:cr + j - 2 + 128, :]), writes=[tcl], dma=True)
                if j == 0:
                    P.op('dve', lambda e, cl=cl: e.tensor_tensor(out=qkv[:], in0=cl[:], in1=convw[:, 0, :], op=ALU.mult), reads=[tcl] + c_, writes=[tk['qkv']])
                else:
                    P.op('pool', lambda e, j=j, cl=cl: e.tensor_tensor(out=cl[:], in0=cl[:], in1=convw[:, j, :], op=ALU.mult), reads=c_, writes=[tcl])
                    P.op('dve', lambda e, cl=cl: e.tensor_tensor(out=qkv[:], in0=qkv[:], in1=cl[:], op=ALU.add), reads=[tcl], writes=[tk['qkv']])
            P.op('act', lambda e: e.activation(out=qkv[:], in_=qkv[:], func=AF.Silu), writes=[tk['qkv']])
            P.op('pool', lambda e: e.tensor_tensor(out=Oc[:], in0=qkv[:, 0:1024], in1=qkv[:, 0:1024], op=ALU.mult), reads=[tk['qkv']], writes=[tk['Oc']])
            P.op('dve', lambda e: e.tensor_reduce(out=small[:, 48:56], in_=Oc[:].rearrange("p (h v) -> p h v", h=8), axis=AX.X, op=ALU.add),
                 reads=[tk['Oc']], writes=sm)
            P.op('act', lambda e: e.activation(out=small[:, 48:56], in_=small[:, 48:56], func=AF.Sqrt, bias=self.epsc[:, 0:1], scale=1.0), reads=sm, writes=sm)
            P.op('dve', lambda e: e.reciprocal(out=small[:, 56:64], in_=small[:, 48:56]), reads=sm, writes=sm)
            P.op('dve', lambda e: e.tensor_scalar(out=small[:, 56:60], in0=small[:, 56:60], scalar1=128.0 ** -0.5, scalar2=None, op0=ALU.mult), reads=sm, writes=sm)
            P.op('dve', lambda e: e.tensor_tensor(out=qkb[:], in0=qkv[:, 0:1024].rearrange("p (h v) -> p h v", h=8),
                                                  in1=small[:, 56:64].unsqueeze(2).to_broadcast([128, 8, 128]), op=ALU.mult),
                 reads=[tk['qkv']] + sm, writes=[tk['qkb']])
            for half in range(2):
                P.op('pe', trs([(psb[4 + half][:, j * 128:(j + 1) * 128], qkb[:, half * 4 + j, :]) for j in range(4)], self.identb[:]),
                     reads=[tk['qkb'], self.t_const], writes=[tp[4 + half]])
                P.op('act', lambda e, half=half: e.activation(out=fT[:, half * 4:(half + 1) * 4, :].rearrange("p h t -> p (h t)"),
                                                              in_=psb[4 + half][:, 0:512], func=AF.Identity), reads=[tp[4 + half]], writes=[tk['fT']])
            kn = qkb[:, 4:8, :]
            P.op('dve', lambda e: e.tensor_tensor(out=kbg[:], in0=kn, in1=small[:, 40:44].unsqueeze(2).to_broadcast([128, 4, 128]), op=ALU.mult),
                 reads=[tk['qkb']] + sm, writes=[tk['kbg']])
            P.op('pool', lambda e: e.tensor_tensor(out=kd[:], in0=kn, in1=small[:, 44:48].unsqueeze(2).to_broadcast([128, 4, 128]), op=ALU.mult),
                 reads=[tk['qkb']] + sm, writes=[tk['kd']])
            P.op('dve', lambda e: e.tensor_tensor(out=bv[:], in0=qkv[:, 1024:1536].rearrange("p (h v) -> p h v", h=4),
                                                  in1=small[:, 24:28].unsqueeze(2).to_broadcast([128, 4, 128]), op=ALU.mult),
                 reads=[tk['qkv']] + sm, writes=[tk['bv']])
            P.op('dve', lambda e: e.tensor_tensor(out=qg[:], in0=fT[:, 0:4, :], in1=egr[:], op=ALU.mult), reads=[tk['fT'], tk['egr']], writes=[tk['qg']])
            P.op('pe', mmgroup([(ps[6][:, h * 128:(h + 1) * 128], [(fT[:, 4 + h, :], fT[:, 4 + h, :])]) for h in range(4)]), reads=[tk['fT']], writes=[tp[6]])
            P.op('pe', mmgroup([(ps[7][:, h * 128:(h + 1) * 128], [(fT[:, 4 + h, :], fT[:, h, :])]) for h in range(4)]), reads=[tk['fT']], writes=[tp[7]])
            P.op('dve', lambda e: e.tensor_tensor(out=Am[:], in0=v4(ps[6][:]), in1=Lts[:], op=ALU.mult), reads=[tp[6], tk['Lts']], writes=[tk['A']])
            P.op('dve', lambda e: e.tensor_tensor(out=Am[:], in0=Am[:], in1=small[:, 24:28].unsqueeze(2).to_broadcast([128, 4, 128]), op=ALU.mult),
                 reads=sm, writes=[tk['A']])
            P.op('dve', lambda e: e.tensor_tensor(out=Pst[:], in0=v4(ps[7][:]), in1=LstS[:], op=ALU.mult), reads=[tp[7], tk['A']], writes=[tk['Pst']])
            P.op('pe', trs([(ps[6][:, h * 128:(h + 1) * 128], Am[:, h, :]) for h in range(4)], self.ident[:]), reads=[tk['A'], self.t_const], writes=[tp[6]])
            P.op('act', lambda e: e.activation(out=AT[:], in_=v4(ps[6][:]), func=AF.Identity), reads=[tp[6]], writes=[tk['AT']])
            P.op('dve', lambda e: e.scalar_tensor_tensor(out=Rm[:], in0=AT[:], scalar=-1.0, in1=self.ident[:].unsqueeze(1).to_broadcast([128, 4, 128]),
                                                         op0=ALU.mult, op1=ALU.add), reads=[tk['AT'], self.t_const], writes=[tk['Rm']])
            curQ, curQT = AT, Am
            tQ, tQT = tk['AT'], tk['A']
            for step in range(5):
                last = step == 4
                if not last:
                    P.op('pe', mmgroup([(ps[6][:, h * 128:(h + 1) * 128], [(curQT[:, h, :], curQ[:, h, :])]) for h in range(4)]), reads=[tQ, tQT], writes=[tp[6]])
                P.op('pe', mmgroup([(ps[7][:, h * 128:(h + 1) * 128], [(curQ[:, h, :], curQT[:, h, :])]) for h in range(4)]), reads=[tQ, tQT], writes=[tp[7]])
                if not last:
                    P.op('act', lambda e: e.activation(out=Qm[:], in_=v4(ps[6][:]), func=AF.Identity), reads=[tp[6]], writes=[tk['Q']])
                P.op('act', lambda e: e.activation(out=QT[:], in_=v4(ps[7][:]), func=AF.Identity), reads=[tp[7]], writes=[tk['QT']])
                curQ, curQT, tQ, tQT = Qm, QT, tk['Q'], tk['QT']
                P.op('pe', mmgroup([(ps[3][:, h * 128:(h + 1) * 128], [(QT[:, h, :], Rm[:, h, :])]) for h in range(4)]), reads=[tk['QT'], tk['Rm']], writes=[tp[3]])
                P.op('dve', lambda e: e.tensor_tensor(out=Rm[:], in0=v4(ps[3][:]), in1=Rm[:], op=ALU.add), reads=[tp[3]], writes=[tk['Rm']])
            P.op('act', lambda e: e.activation(out=Rb[:], in_=Rm[:], func=AF.Identity), reads=[tk['Rm']], writes=[tk['Rb']])
            P.op('pe', mmgroup([(ps[6][:, h * 128:(h + 1) * 128], [(Rb[:, h, :], bv[:, h, :])]) for h in range(4)]), reads=[tk['Rb'], tk['bv']], writes=[tp[6]])
            P.op('pe', mmgroup([(ps[7][:, h * 128:(h + 1) * 128], [(kbg[:, h, :], Rb[:, h, :])]) for h in range(4)]), reads=[tk['Rb'], tk['kbg']], writes=[tp[7]])
            P.op('act', lambda e: e.activation(out=u[:], in_=v4(ps[6][:]), func=AF.Identity), reads=[tp[6]], writes=[tk['u']])
            P.op('act', lambda e: e.activation(out=wT[:], in_=v4(ps[7][:]), func=AF.Identity), reads=[tp[7]], writes=[tk['wT']])
            if first:
                P.op('pool', lambda e: e.memset(S[:], 0.0), writes=[tk['S']])
                P.op('pool', lambda e: e.memset(Sb[snap_ctr[0] % 2][:], 0.0), writes=[tk['Sb%d' % (snap_ctr[0] % 2)]])
            order = (0, 1) if d == 0 else (1, 0)
            for c in order:
                cs = slice(c * 64, c * 64 + 64)
                si = snap_ctr[0] % 2; snap_ctr[0] += 1; sn = (si + 1) % 2
                Sbi = Sb[si]; tSb = tk['Sb%d' % si]
                P.op('pe', mmgroup([(ps[6][cs, h * 128:(h + 1) * 128], [(wT[:, h, cs], Sbi[:, h, :])]) for h in range(4)]), reads=[tk['wT'], tSb], writes=[tp[6]])
                P.op('dve', lambda e, cs=cs: e.tensor_tensor(out=vn[cs], in0=u[cs], in1=v4(ps[6][cs, :]), op=ALU.subtract),
                     reads=[tp[6], tk['u']], writes=[tk['vn']])
                def omm(e, cs=cs, Sbi=Sbi):
                    for h in range(4):
                        e.matmul(ps[5][cs, h * 128:(h + 1) * 128], qg[:, h, cs], Sbi[:, h, :], start=True, stop=False)
                        ins = e.matmul(ps[5][cs, h * 128:(h + 1) * 128], Pst[cs, h, cs], vn[cs, h, :], start=False, stop=True)
                    return ins
                P.op('pe', omm, reads=[tk['qg'], tSb, tk['Pst'], tk['vn']], writes=[tp[5]])
                P.op('pe', mmgroup([(ps[7][:, h * 128:(h + 1) * 128], [(kd[cs, h, :], vn[cs, h, :])]) for h in range(4)]), reads=[tk['kd'], tk['vn']], writes=[tp[7]])
                P.op('dve', lambda e, c=c: e.tensor_tensor(out=S[:], in0=S[:], in1=totrow[:].rearrange("p (h c) -> p h c", c=2)[:, :, c:c + 1].to_broadcast([128, 4, 128]),
                                                           op=ALU.mult), reads=[tk['gs']], writes=[tk['S']])
                P.op('dve', lambda e: e.tensor_tensor(out=S[:], in0=S[:], in1=v4(ps[7][:]), op=ALU.add), reads=[tp[7]], writes=[tk['S']])
                P.op('act', lambda e, sn=sn: e.activation(out=Sb[sn][:], in_=S[:], func=AF.Identity), reads=[tk['S']], writes=[tk['Sb%d' % sn]])
            if d == 0:
                P.op('act', lambda e: e.activation(out=ofl[:], in_=ps[5][:], func=AF.Identity), reads=[tp[5]], writes=[tk['ofl']])
                P.op('sp', lambda e: e.dma_start(out=D['OF'][r0:r0 + 128, 0:512], in_=ofl[:]), reads=[tk['ofl']], writes=[tk['out']], dma=True)
                return
            if is_ctx:
                return
            P.op('sp', lambda e: e.dma_start(out=ofl[:], in_=D['OF'][r0:r0 + 128, 0:512]), writes=[tk['ofl']], dma=True)
            P.op('dve', lambda e: e.tensor_tensor(out=Oc[:, 0:512], in0=ps[5][:], in1=ofl[:], op=ALU.add), reads=[tp[5], tk['ofl']], writes=[tk['Oc']])
            P.op('pe', mmgroup([(ps[6][:], tokmm(1552, 512))]), reads=rh, writes=[tp[6]])
            P.op('act', lambda e: e.activation(out=og[:], in_=ps[6][:], func=AF.Silu), reads=[tp[6]], writes=[tk['og']])
            P.op('pool', lambda e: e.tensor_tensor(out=ofl[:], in0=Oc[:, 0:512], in1=Oc[:, 0:512], op=ALU.mult), reads=[tk['Oc']], writes=[tk['ofl']])
            P.op('dve', lambda e: e.tensor_reduce(out=small[:, 48:52], in_=ofl[:].rearrange("p (h v) -> p h v", h=4), axis=AX.X, op=ALU.add),
                 reads=[tk['ofl']], writes=sm)
            P.op('act', lambda e: e.activation(out=small[:, 48:52], in_=small[:, 48:52], func=AF.Sqrt, scale=1.0 / 128, bias=self.epsc[:, 0:1]), reads=sm, writes=sm)
            P.op('dve', lambda e: e.reciprocal(out=small[:, 52:56], in_=small[:, 48:52]), reads=sm, writes=sm)
            O4 = Oc[:, 0:512].rearrange("p (h v) -> p h v", h=4)
            P.op('dve', lambda e: e.tensor_tensor(out=O4, in0=O4, in1=small[:, 52:56].unsqueeze(2).to_broadcast([128, 4, 128]), op=ALU.mult), reads=sm, writes=[tk['Oc']])
            P.op('pool', lambda e: e.tensor_tensor(out=Oc[:, 0:512], in0=Oc[:, 0:512], in1=dnw[:], op=ALU.mult), reads=c_, writes=[tk['Oc']])
            P.op('dve', lambda e: e.tensor_tensor(out=yb[:, 0:512], in0=Oc[:, 0:512], in1=og[:], op=ALU.mult), reads=[tk['Oc'], tk['og']], writes=[tk['yb']])
            self.l1_attention(self._l1, r0)
            P.op('pe', trs([(psb[4][:, k * 128:(k + 1) * 128], yb[:, k * 128:(k + 1) * 128]) for k in range(8)], self.identb[:]),
                 reads=[tk['yb'], self.t_const], writes=[tp[4]])
            P.op('act', lambda e: e.activation(out=yT[:].rearrange("p k t -> p (k t)"), in_=psb[4][:, 0:1024], func=AF.Identity), reads=[tp[4]], writes=[tk['yT']])
            for half in range(2):
                P.op('pe', mmgroup([(ps[6 + half][:], [(yT[:, k, :], wout[:, k, half * 512:(half + 1) * 512]) for k in range(8)])]),
                     reads=[tk['yT'], tk['c']], writes=[tp[6 + half]])
                P.op('dve', lambda e, half=half: e.tensor_tensor(out=Oc[:, half * 512:(half + 1) * 512], in0=ps[6 + half][:],
                                                                 in1=self.gbc[0][:, half * 512:(half + 1) * 512], op=ALU.mult),
                     reads=[tp[6 + half], self.t_gbc], writes=[tk['Oc']])
            P.op('pool', lambda e: e.tensor_tensor(out=x_t[:], in0=x_t[:], in1=Oc[:], op=ALU.add), reads=[tk['Oc']], writes=[tx])
            P.op('sp', lambda e: e.dma_start(out=D[dst][r0:r0 + 128, :], in_=x_t[:]), reads=[tx], writes=[tk['out']], dma=True)

        self._l1 = locals()
        if TC:
            for s_, nm in enumerate(('KT', 'KT2')):
                P.op('sp', lambda e, nm=nm: e.dma_start(out=kcT[:, 0:TC], in_=D[nm][:, 0:TC]), writes=[tk['kcT']], dma=True)
                P.op('dve', lambda e, s_=s_: e.tensor_copy(out=kcTb2[s_][:, 0:TC], in_=kcT[:, 0:TC]), reads=[tk['kcT']], writes=[tk['kcT']])
            P.op('sp', lambda e: e.dma_start(out=vc[:], in_=D['VV'][0:TC, :].rearrange("(b p) n -> p b n", p=128)), writes=[tk['vc']], dma=True)
            P.op('dve', lambda e: e.tensor_copy(out=vcb[:], in_=vc[:]), reads=[tk['vc']], writes=[tk['vc']])
        for d in range(2):
            seq = seq_all
            if d == 1:
                seq = [(i * 128, True) for i in reversed(range(NCB))] + [(TC + i * 128, False) for i in reversed(range(NB))]
            for n, (r0, is_ctx) in enumerate(seq):
                scan_block(r0, is_ctx, d, n == 0, n)
                if d == 0:
                    self.issue_cast(1, n)
            P.barrier()
        P.emit()


def apply_rope(self, x3, nh, rope, rt, t_x, t_rope, t_rt):
    P = self.P
    cosb = rope[:, 0:32].unsqueeze(1).to_broadcast([128, nh, 32]); sinb = rope[:, 32:64].unsqueeze(1).to_broadcast([128, nh, 32])
    x1 = x3[:, :, 0:32]; x2 = x3[:, :, 32:64]
    r = rt[:, 0:nh, :]
    rd = list(t_x) + [t_rope]
    P.op('dve', lambda e: e.tensor_tensor(out=r[:, :, 0:32], in0=x2, in1=sinb, op=ALU.mult), reads=rd, writes=[t_rt])
    P.op('dve', lambda e: e.tensor_tensor(out=r[:, :, 32:64], in0=x1, in1=sinb, op=ALU.mult), reads=rd, writes=[t_rt])
    P.op('dve', lambda e: e.tensor_tensor(out=x1, in0=x1, in1=cosb, op=ALU.mult), reads=[t_rope], writes=list(t_x))
    P.op('dve', lambda e: e.tensor_tensor(out=x2, in0=x2, in1=cosb, op=ALU.mult), reads=[t_rope], writes=list(t_x))
    P.op('dve', lambda e: e.tensor_tensor(out=x1, in0=x1, in1=r[:, :, 0:32], op=ALU.subtract), reads=[t_rt], writes=list(t_x))
    P.op('dve', lambda e: e.tensor_tensor(out=x2, in0=x2, in1=r[:, :, 32:64], op=ALU.add), reads=[t_rt], writes=list(t_x))


Builder.phase_l1mix = phase_l1mix
Builder.apply_rope = apply_rope


def l1_attention(self, L, r0):
    P, D = self.P, self.D
    TC, NB, NCB = self.TC, self.NB, self.NCB
    ps, psb, tp, tk = L['ps'], L['psb'], L['tp'], L['tk']
    mmgroup, trs, tokmm, rh = L['mmgroup'], L['trs'], L['tokmm'], L['rh']
    q8, q8b, qT8, small, yb = L['q8'], L['q8b'], L['qT8'], L['small'], L['yb']
    kTl, kTlb, vl, vlb, kcTb, vcb = L['kTl2'], L['kTlb2'], L['vl'], L['vlb'], L['kcTb2'], L['vcb']
    ssb, pbf, pT, rope, rt, sinks, mbp, mbn = L['ssb'], L['pbf'], L['pT'], L['rope'], L['rt'], L['sinks'], L['mbp'], L['mbn']
    blk = (r0 - TC) // 128
    has_prev, has_next = blk > 0, blk < NB - 1
    NK = TC + 384
    sm = [tk['small']]
    P.op('pe', mmgroup([(ps[0][:], tokmm(2064, 512))]), reads=rh, writes=[tp[0]])
    P.op('act', lambda e: e.activation(out=q8[:], in_=ps[0][:], func=AF.Identity), reads=[tp[0]], writes=[tk['q8']])
    t0 = r0 - TC
    P.op('sp', lambda e: e.dma_start(out=rope[:], in_=D['rope'][t0:t0 + 128, :]), writes=[tk['rope']], dma=True)
    self.apply_rope(q8[:].rearrange("p (h f) -> p h f", h=8), 8, rope, rt, [tk['q8']], tk['rope'], tk['rt'])
    P.op('dve', lambda e: e.tensor_scalar(out=q8b[:], in0=q8[:], scalar1=0.125, scalar2=None, op0=ALU.mult), reads=[tk['q8']], writes=[tk['q8']])
    P.op('pe', trs([(psb[1][:, j * 128:(j + 1) * 128], q8b[:, j * 128:(j + 1) * 128]) for j in range(4)], self.identb[:]),
         reads=[tk['q8'], self.t_const], writes=[tp[1]])
    P.op('act', lambda e: e.activation(out=qT8[:].rearrange("p j t -> p (j t)"), in_=psb[1][:, 0:512], func=AF.Identity), reads=[tp[1]], writes=[tk['qT8']])
    c0 = r0 - 128 if has_prev else r0
    c1 = r0 + 256 if has_next else r0 + 128
    o0 = 0 if has_prev else 128
    for s_, nm in enumerate(('KT', 'KT2')):
        P.op('sp', lambda e, s_=s_, nm=nm: e.dma_start(out=kTl[s_][:, o0:o0 + (c1 - c0)], in_=D[nm][:, c0:c1]), writes=[tk['kTl']], dma=True)
        P.op('pool', lambda e, s_=s_: e.tensor_copy(out=kTlb[s_][:, o0:o0 + (c1 - c0)], in_=kTl[s_][:, o0:o0 + (c1 - c0)]), reads=[tk['kTl']], writes=[tk['kTl']])
    nbk = (c1 - c0) // 128
    b0 = o0 // 128
    P.op('sp', lambda e: e.dma_start(out=vl[:, b0:b0 + nbk, :], in_=D['VV'][c0:c1, :].rearrange("(b p) n -> p b n", p=128)), writes=[tk['vl']], dma=True)
    P.op('pool', lambda e: e.tensor_copy(out=vlb[:, b0:b0 + nbk, :], in_=vl[:, b0:b0 + nbk, :]), reads=[tk['vl']], writes=[tk['vl']])
    for h in range(8):
        pbse = (h % 2) * 64; g = h // 4
        s_ = 0 if g == (h % 2) else 1
        rs = slice(pbse, pbse + 64)
        bA, bB = 2 + 2 * (h % 2), 3 + 2 * (h % 2)
        ql = qT8[rs, h // 2, :]
        outsA = []
        if TC:
            outsA.append((ps[bA][:, 0:TC], [(ql, kcTb[s_][rs, 0:TC])]))
        if has_prev:
            outsA.append((ps[bA][:, TC:TC + 128], [(ql, kTlb[s_][rs, 0:128])]))
        outsA.append((ps[bA][:, TC + 128:TC + 256], [(ql, kTlb[s_][rs, 128:256])]))
        P.op('pe', mmgroup(outsA), reads=[tk['qT8'], tk['kTl'], tk['kcT']], writes=[tp[bA]])
        if has_next:
            P.op('pe', mmgroup([(ps[bB][:, 0:128], [(ql, kTlb[s_][rs, 256:384])])]), reads=[tk['qT8'], tk['kTl']], writes=[tp[bB]])
        w_ = [tk['ssb']]
        if TC:
            P.op('act', lambda e, bA=bA: e.activation(out=ssb[:, 0:TC], in_=ps[bA][:, 0:TC], func=AF.Identity), reads=[tp[bA]], writes=w_)
        if has_prev:
            P.op('dve', lambda e, bA=bA: e.tensor_tensor(out=ssb[:, TC:TC + 128], in0=ps[bA][:, TC:TC + 128], in1=mbp[:], op=ALU.add), reads=[tp[bA], tk['c']], writes=w_)
        else:
            P.op('pool', lambda e: e.memset(ssb[:, TC:TC + 128], NEG), writes=w_)
        P.op('act', lambda e, bA=bA: e.activation(out=ssb[:, TC + 128:TC + 256], in_=ps[bA][:, TC + 128:TC + 256], func=AF.Identity), reads=[tp[bA]], writes=w_)
        if has_next:
            P.op('dve', lambda e, bB=bB: e.tensor_tensor(out=ssb[:, TC + 256:TC + 384], in0=ps[bB][:, 0:128], in1=mbn[:], op=ALU.add), reads=[tp[bB], tk['c']], writes=w_)
        else:
            P.op('pool', lambda e: e.memset(ssb[:, TC + 256:TC + 384], NEG), writes=w_)
        P.op('dve', lambda e: e.tensor_reduce(out=small[:, 16:17], in_=ssb[:], axis=AX.X, op=ALU.max), reads=w_, writes=sm)
        P.op('dve', lambda e, h=h: e.tensor_tensor(out=small[:, 16:17], in0=small[:, 16:17], in1=sinks[:, h:h + 1], op=ALU.max), reads=sm + [tk['c']], writes=sm)
        P.op('dve', lambda e: e.tensor_scalar(out=small[:, 17:18], in0=small[:, 16:17], scalar1=-1.0, scalar2=None, op0=ALU.mult), reads=sm, writes=sm)
        P.op('act', lambda e: e.activation(out=pbf[:], in_=ssb[:], func=AF.Exp, bias=small[:, 17:18], scale=1.0, accum_out=small[:, 18:19]),
             reads=w_ + sm, writes=[tk['pb']] + sm)
        P.op('act', lambda e, h=h: e.activation(out=small[:, 19:20], in_=sinks[:, h:h + 1], func=AF.Exp, bias=small[:, 17:18], scale=1.0), reads=sm, writes=sm)
        P.op('dve', lambda e: e.tensor_tensor(out=small[:, 19:20], in0=small[:, 19:20], in1=small[:, 18:19], op=ALU.add), reads=sm, writes=sm)
        P.op('dve', lambda e, h=h: e.reciprocal(out=small[:, 8 + h:9 + h], in_=small[:, 19:20]), reads=sm, writes=sm)
        nkb = NK // 128
        P.op('pe', trs([(psb[6][:, kb * 128:(kb + 1) * 128], pbf[:, kb * 128:(kb + 1) * 128]) for kb in range(nkb)], self.identb[:]),
             reads=[tk['pb'], self.t_const], writes=[tp[6]])
        P.op('act', lambda e: e.activation(out=pT[:].rearrange("p k t -> p (k t)"), in_=psb[6][:, 0:NK], func=AF.Identity), reads=[tp[6]], writes=[tk['pT']])
        pairs = [(pT[:, b, :], vcb[:, b, g * 64:(g + 1) * 64]) for b in range(NCB)]
        for j in range(3):
            if (j == 0 and not has_prev) or (j == 2 and not has_next):
                continue
            pairs.append((pT[:, NCB + j, :], vlb[:, j, g * 64:(g + 1) * 64]))
        P.op('pe', mmgroup([(ps[7][:, h * 64:(h + 1) * 64], pairs)]), reads=[tk['pT'], tk['vl'], tk['vc']], writes=[tp[7]])
    P.op('dve', lambda e: e.tensor_tensor(out=yb[:, 512:1024].rearrange("p (h f) -> p h f", h=8), in0=ps[7][:].rearrange("p (h f) -> p h f", h=8),
                                          in1=small[:, 8:16].unsqueeze(2).to_broadcast([128, 8, 64]), op=ALU.mult), reads=[tp[7]] + sm, writes=[tk['yb']])


Builder.l1_attention = l1_attention
```

```python
import numpy as np
from contextlib import ExitStack
import concourse.bass as bass
import concourse.mybir as mybir
from concourse.bass_utils import run_bass_kernel_spmd

dt = mybir.dt
F32 = dt.float32
BF16 = dt.bfloat16
AF = mybir.ActivationFunctionType
ALU = mybir.AluOpType
AX = mybir.AxisListType

ENGS = ['pe', 'act', 'dve', 'pool', 'sp']
NDMA = 6
EPS = 1e-6
DM = 1024
NE = 32


class Tok:
    __slots__ = ('w', 'r')

    def __init__(self):
        self.w = None
        self.r = {}


class Prog:
    def __init__(self, nc):
        self.nc = nc
        self.stack = ExitStack()
        self.sems = {}
        self.cnt = {e: 0 for e in ENGS}
        self.dma_cnt = {}
        self.dma_rr = {e: 0 for e in ENGS}
        self.seen = {e: {} for e in ENGS}
        self.q = {e: [] for e in ENGS}
        self.nops = 0
        for e in ENGS:
            self.sems[('eng', e)] = self.stack.enter_context(nc.semaphore('s_' + e))
        for e in ('sp', 'pool', 'act'):
            for k in range(NDMA):
                self.sems[('dma', e, k)] = self.stack.enter_context(nc.semaphore('d_%s%d' % (e, k)))

    def op(self, eng, fn, reads=(), writes=(), dma=False):
        deps = {}

        def add(k, v):
            if deps.get(k, 0) < v:
                deps[k] = v

        for t in reads:
            if t.w is not None:
                add(*t.w)
        for t in writes:
            if t.w is not None:
                add(*t.w)
            for k, v in t.r.items():
                add(k, v)
        if dma:
            k = self.dma_rr[eng]
            self.dma_rr[eng] = (k + 1) % NDMA
            key = ('dma', eng, k)
            prev = self.dma_cnt.get(key, 0)
            if prev:
                add(key, prev)
            val = prev + 16
            self.dma_cnt[key] = val
        else:
            key = ('eng', eng)
            self.cnt[eng] += 1
            val = self.cnt[eng]
        seen = self.seen[eng]
        waits = []
        for k, v in deps.items():
            if eng == 'pe' and k == ('eng', 'pe'):
                continue
            if seen.get(k, 0) >= v:
                continue
            seen[k] = v
            waits.append((k, v))
        self.q[eng].append((waits, fn, key, dma))
        self.nops += 1
        for t in reads:
            if t.r.get(key, 0) < val:
                t.r[key] = val
        for t in writes:
            t.w = (key, val)
            t.r = {}

    def barrier(self):
        evs = [(('eng', e), self.cnt[e]) for e in ENGS if self.cnt[e]]
        evs += list(self.dma_cnt.items())
        for e in ENGS:
            seen = self.seen[e]
            waits = []
            for k, v in evs:
                if seen.get(k, 0) >= v:
                    continue
                seen[k] = v
                waits.append((k, v))
            if waits:
                self.q[e].append((waits, None, None, False))

    def emit(self):
        nc = self.nc
        sems = self.sems
        with nc.Block() as block:
            def mk(name):
                lst = self.q[name]

                def f(e):
                    for waits, fn, key, dma in lst:
                        for k, v in waits:
                            e.wait_ge(sems[k], v)
                        if fn is None:
                            continue
                        ins = fn(e)
                        ins.then_inc(sems[key], 16 if dma else 1)
                return f
            block.tensor(mk('pe'))
            block.scalar(mk('act'))
            block.vector(mk('dve'))
            block.gpsimd(mk('pool'))
            block.sync(mk('sp'))
        self.q = {e: [] for e in ENGS}

    def close(self):
        self.stack.close()


class Builder:
    def __init__(self, T, TC, phases=('ada', 'l0mix', 'l0moe', 'l1mix', 'l1moe'), dbg=()):
        self.T, self.TC = T, TC
        self.NB, self.NCB = T // 128, TC // 128
        self.phases = phases
        self.dbg = dbg
        self.nc = bass.Bass("TRN2", target_bir_lowering=False)
        self.D = {}
        self.P = Prog(self.nc)
        self.in_names = []
        self.t_cast = [[Tok() for _ in range(NE)] for _ in range(2)]
        self.cast_done = [set(), set()]

    def din(self, name, shape, d=F32):
        self.D[name] = self.nc.dram_tensor(name, list(shape), d, kind="ExternalInput").ap()
        self.in_names.append(name)
        return self.D[name]

    def dscr(self, name, shape, d=F32):
        kind = "ExternalOutput" if name in self.dbg else "Internal"
        self.D[name] = self.nc.dram_tensor(name, list(shape), d, kind=kind).ap()
        return self.D[name]

    def declare(self):
        T, TC = self.T, self.TC
        din = self.din
        din('x', [T, DM]); din('ctx', [max(TC, 128), DM]); din('cvec', [128, 8, 2])
        din('ident', [128, 128]); din('ones', [128, 128])
        for l in range(2):
            p = 'l%d_' % l
            din(p + 'ada_w', [DM, 6 * DM]); din(p + 'ada_bT', [128, 48])
            din(p + 'nmix', [128, 8]); din(p + 'nffn', [128, 8])
            if ('l%dmoe' % l) in self.phases:
                self.D['WUB%d' % l] = self.nc.dram_tensor('WUB%d' % l, [NE, DM, 2 * DM], BF16).ap()
                self.D['WDB%d' % l] = self.nc.dram_tensor('WDB%d' % l, [NE, DM, DM], BF16).ap()
                din(p + 'router_w', [DM, NE]); din(p + 'router_b', [128, NE])
                din(p + 'w_up', [NE, DM, 2 * DM]); din(p + 'b_upT', [128, NE, 16])
                din(p + 'w_down', [NE, DM, DM]); din(p + 'b_down', [NE, DM])
        din('final_w', [128, DM])
        self.out = self.nc.dram_tensor('out', [T, DM], F32, kind="ExternalOutput").ap()
        R = TC + T
        self.dscr('X1', [R, DM]); self.dscr('X2', [R, DM]); self.dscr('X3', [R, DM]); self.dscr('OF', [R, DM])
        if 'l1mix' in self.phases:
            din('l1_w_in', [DM, 2832]); din('l1_w_out', [DM, DM]); din('l1_convw', [128, 5, 1536]); din('l1_pvec', [128, 16])
            din('l1_dnw', [128, 512]); din('l1_sinks', [128, 8]); din('rope', [T, 64])
            for nm in ('mask_fs', 'mask_rs', 'same', 'mb_prev', 'mb_next'):
                din(nm, [128, 128])
            din('csel', [128, 2])
            self.dscr('CX', [R + 8, 1536]); self.dscr('KT', [128, R]); self.dscr('KT2', [128, R]); self.dscr('VV', [R, 128])
            if 'l0mix' not in self.phases:
                din('mask_f', [128, 128]); din('mask_r', [128, 128])
        if 'l0mix' not in self.phases:
            return
        din('l0_w_in', [DM, 4128]); din('l0_w_out', [DM, DM]); din('l0_w2', [16, 2, 256]); din('l0_b2', [128, 2, 2])
        din('l0_lb', [128, 2, 4]); din('l0_normw', [128, DM]); din('rmask', [128, 768]); din('mask_f', [128, 128]); din('mask_r', [128, 128])

    def build(self):
        nc, P = self.nc, self.P
        self.declare()
        with ExitStack() as st0:
            self.st0 = st0
            sb = lambda name, shape, d=F32: st0.enter_context(nc.sbuf_tensor(name, shape, d))
            self.ident = sb('ident_t', [128, 128]); self.ones = sb('ones_t', [128, 128])
            self.identb = sb('identb_t', [128, 128], BF16)
            self.modT = [sb('modT%d' % l, [128, 48, 2]) for l in range(2)]
            self.Acol = [[sb('A%d_%d' % (l, i), [128, 8, 2]) for i in range(2)] for l in range(2)]
            self.gbc = [sb('gbc_x', [128, DM]), sb('gbc_c', [128, DM])]
            self.t_const = Tok(); self.t_mod = Tok(); self.t_gbc = Tok()
            P.op('sp', lambda e: e.dma_start(out=self.ident[:], in_=self.D['ident']), writes=[self.t_const], dma=True)
            P.op('sp', lambda e: e.dma_start(out=self.ones[:], in_=self.D['ones']), writes=[self.t_const], dma=True)
            P.op('dve', lambda e: e.tensor_copy(out=self.identb[:], in_=self.ident[:]), reads=[self.t_const], writes=[self.t_const])
            P.barrier()
            self.phase_ada()
            cur = 'x_in'
            if 'l0mix' in self.phases:
                self.phase_l0mix(cur, 'X1'); cur = 'X1'
            if 'l0moe' in self.phases:
                self.phase_moe(0, cur, 'X2', final=False); cur = 'X2'
            if 'l1mix' in self.phases:
                self.phase_l1mix(cur, 'X3'); cur = 'X3'
            if 'l1moe' in self.phases:
                self.phase_moe(1, cur, None, final=True)
            elif 'final' in self.phases:
                self.phase_final(cur)
            P.barrier()
            P.emit()
        P.close()
        return nc

    def issue_cast(self, l, ex):
        if ('l%dmoe' % l) not in self.phases or ex >= NE or ex in self.cast_done[l]:
            return
        self.cast_done[l].add(ex)
        D, P = self.D, self.P
        p = 'l%d_' % l
        tk_ = self.t_cast[l][ex]
        P.op('pool', lambda e: e.dma_start(out=D['WUB%d' % l][ex], in_=D[p + 'w_up'][ex]), writes=[tk_], dma=True)
        P.op('pool', lambda e: e.dma_start(out=D['WDB%d' % l][ex], in_=D[p + 'w_down'][ex]), writes=[tk_], dma=True)

    def rows(self, name, r0, n=128):
        if name == 'x_in':
            if r0 < self.TC:
                return self.D['ctx'][r0:r0 + n, :]
            return self.D['x'][r0 - self.TC:r0 - self.TC + n, :]
        return self.D[name][r0:r0 + n, :]

    def phase_ada(self):
        nc, P, D = self.nc, self.P, self.D
        with ExitStack() as st:
            sb = lambda name, shape, d=F32: st.enter_context(nc.sbuf_tensor(name, shape, d))
            ps = [st.enter_context(nc.psum_tensor('pa%d' % i, [128, 512], F32)) for i in range(2)]
            cv = sb('cv', [128, 8, 2]); scv = sb('scv', [128, 8, 2])
            wb = [sb('adaw%d' % i, [128, 8, 512]) for i in range(2)]
            bT = sb('adab', [128, 48]); nw = sb('nw', [128, 8])
            t_cv, t_ps, t_b = Tok(), Tok(), Tok()
            t_wb = [Tok(), Tok()]
            P.op('sp', lambda e: e.dma_start(out=cv[:], in_=D['cvec']), writes=[t_cv], dma=True)
            P.op('act', lambda e: e.activation(out=scv[:], in_=cv[:], func=AF.Silu), reads=[t_cv], writes=[t_cv])
            for l in range(2):
                p = 'l%d_' % l
                P.op('sp', lambda e, p=p: e.dma_start(out=bT[:], in_=D[p + 'ada_bT']), writes=[t_b], dma=True)
                for cb in range(12):
                    w = wb[cb % 2]; tw = t_wb[cb % 2]
                    src = D[p + 'ada_w'][:, cb * 512:(cb + 1) * 512].rearrange("(k p) n -> p k n", p=128)
                    P.op('sp', lambda e, w=w, src=src: e.dma_start(out=w[:], in_=src), writes=[tw], dma=True)

                    def mm(e, w=w, cb=cb):
                        for jj in range(4):
                            j = cb * 4 + jj
                            for kc in range(8):
                                ins = e.matmul(ps[0][:, j * 2:j * 2 + 2], w[:, kc, jj * 128:(jj + 1) * 128],
                                               scv[:, kc, :], start=(kc == 0), stop=(kc == 7))
                        return ins
                    P.op('pe', mm, reads=[tw, t_cv], writes=[t_ps])
                modT = self.modT[l]
                P.op('dve', lambda e, modT=modT: e.tensor_tensor(
                    out=modT[:], in0=ps[0][:, 0:96].rearrange("p (j v) -> p j v", v=2),
                    in1=bT[:].unsqueeze(2).to_broadcast([128, 48, 2]), op=ALU.add),
                    reads=[t_ps, t_b], writes=[self.t_mod])
                for i, (nm, c0) in enumerate((('nmix', 8), ('nffn', 32))):
                    A = self.Acol[l][i]
                    P.op('sp', lambda e, nm=nm, p=p: e.dma_start(out=nw[:], in_=D[p + nm]), writes=[t_b], dma=True)
                    P.op('dve', lambda e, A=A, modT=modT, c0=c0: e.scalar_tensor_tensor(
                        out=A[:], in0=modT[:, c0:c0 + 8, :], scalar=1.0,
                        in1=nw[:].unsqueeze(2).to_broadcast([128, 8, 2]), op0=ALU.add, op1=ALU.mult),
                        reads=[self.t_mod, t_b], writes=[self.t_mod])
            P.barrier()
            P.emit()

    def gate_bcast(self, dg, ps, l, c0, variants):
        nc, P = self.nc, self.P
        t_dg, t_p = Tok(), Tok()
        for v in variants:
            for k in range(8):
                P.op('dve', lambda e, k=k, v=v: e.tensor_scalar(
                    out=dg[:, k, :], in0=self.ident[:], scalar1=self.modT[l][:, c0 + k, v:v + 1], scalar2=None,
                    op0=ALU.mult), reads=[self.t_mod, self.t_const], writes=[t_dg])
            for h in range(2):
                def mm(e, h=h):
                    for j in range(4):
                        ins = e.matmul(ps[h][:, j * 128:(j + 1) * 128], self.ones[:], dg[:, h * 4 + j, :],
                                       start=True, stop=True)
                    return ins
                P.op('pe', mm, reads=[t_dg, self.t_const], writes=[t_p])
                P.op('act', lambda e, h=h, v=v: e.activation(out=self.gbc[v][:, h * 512:(h + 1) * 512], in_=ps[h][:],
                                                            func=AF.Identity), reads=[t_p], writes=[self.t_gbc])

    def norm_xn(self, src_rows, xt, t_x, xn, t_xn, small, t_small, junk, t_junk):
        P = self.P
        P.op('sp', lambda e: e.dma_start(out=xt[:], in_=src_rows), writes=[t_x], dma=True)
        P.op('act', lambda e: e.activation(out=junk[:], in_=xt[:], func=AF.Square, accum_out=small[:, 0:1]),
             reads=[t_x], writes=[t_junk, t_small])
        P.op('act', lambda e: e.activation(out=small[:, 1:2], in_=small[:, 0:1], func=AF.Sqrt, scale=1.0 / DM, bias=self.epsc[:, 0:1]),
             reads=[t_small], writes=[t_small])
        P.op('dve', lambda e: e.reciprocal(out=small[:, 2:3], in_=small[:, 1:2]), reads=[t_small], writes=[t_small])
        P.op('dve', lambda e: e.tensor_scalar(out=xn[:], in0=xt[:], scalar1=small[:, 2:3], scalar2=None, op0=ALU.mult),
             reads=[t_x, t_small], writes=[t_xn])

    def phase_moe(self, l, src, dst, final):
        nc, P, D = self.nc, self.P, self.D
        p = 'l%d_' % l
        TC = self.TC if l == 0 else 0
        r_begin = 0 if l == 0 else self.TC
        nblk = (TC + self.T) // 128
        NSB = 8
        with ExitStack() as st:
            sb = lambda name, shape, d=F32: st.enter_context(nc.sbuf_tensor(name + '_m%d' % l, shape, d))
            ps = [st.enter_context(nc.psum_tensor('pm%d_%d' % (l, i), [128, 512], F32)) for i in range(8)]
            self.epsc = sb('epsc', [128, 1])
            h2T = sb('h2T', [128, 8, NSB * 128], BF16)
            acc = sb('acc', [128, NSB, DM])
            gates = sb('gates', [128, NSB, NE])
            wu = [sb('wu%d' % i, [128, 8, 2 * DM], BF16) for i in range(2)]
            wd = [sb('wd%d' % i, [128, 8, DM], BF16) for i in range(2)]
            actT = [sb('actT%d' % i, [128, 8, 512], BF16) for i in range(2)]
            xt = [sb('xt0', [128, DM])] * 2
            junk = sb('junk', [128, DM], BF16)
            small = sb('small', [128, 16]); h32 = sb('h32', [128, 8, 128])
            rw = sb('rw', [128, 8, NE]); rb = sb('rb', [128, NE]); bup = sb('bup', [128, NE, 16])
            bdn = sb('bdn', [NE, DM]); lg = sb('lg', [128, NE]); top8 = sb('top8', [128, 8])
            em = sb('em', [128, NE]); gT = sb('gT', [NE, 128])
            eg = [sb('eg%d' % i, [128, 512]) for i in range(2)]
            es = [sb('es%d' % i, [128, 512]) for i in range(2)]
            el = [sb('el%d' % i, [128, 512]) for i in range(2)]
            fw = sb('fw', [128, DM]) if final else None
            T_ = lambda: Tok()
            t_c, t_h2T, t_acc, t_gates = T_(), T_(), T_(), T_()
            t_wu, t_wd = [T_(), T_()], [T_(), T_()]
            t_act = [T_(), T_()]
            t_xt, t_xn, t_junk, t_small, t_h32 = [T_()] * 2, T_(), T_(), T_(), T_()
            t_lg, t_gT = T_(), T_()
            t_ps = [T_() for _ in range(8)]
            t_eg, t_es, t_el = [T_(), T_()], [T_(), T_()], [T_(), T_()]
            t_out = T_()
            P.op('pool', lambda e: e.memset(self.epsc[:], EPS), writes=[t_c])
            P.op('sp', lambda e: e.dma_start(out=rw[:], in_=D[p + 'router_w'].rearrange("(k p) n -> p k n", p=128)), writes=[t_c], dma=True)
            P.op('sp', lambda e: e.dma_start(out=rb[:], in_=D[p + 'router_b']), writes=[t_c], dma=True)
            P.op('sp', lambda e: e.dma_start(out=bup[:], in_=D[p + 'b_upT']), writes=[t_c], dma=True)
            P.op('sp', lambda e: e.dma_start(out=bdn[:], in_=D[p + 'b_down']), writes=[t_c], dma=True)
            if final:
                P.op('sp', lambda e: e.dma_start(out=fw[:], in_=D['final_w']), writes=[t_c], dma=True)
            variants = (0, 1) if TC else (0,)
            self.gate_bcast(h32, ps[0:2], l, 40, variants)
            P.barrier()
            A2 = self.Acol[l][1]; modT = self.modT[l]
            wcount = 0
            for ex_ in range(NE):
                self.issue_cast(l, ex_)
            for sb0 in range(0, nblk, NSB):
                nb = min(NSB, nblk - sb0)
                for bi in range(nb):
                    r0 = r_begin + (sb0 + bi) * 128
                    v = 1 if (r0 < self.TC) else 0
                    x_t = xt[bi % 2]; tx = t_xt[bi % 2]
                    self.norm_xn(self.rows(src, r0), x_t, tx, x_t, tx, small, t_small, junk, t_junk)
                    xn, t_xn = x_t, tx
                    for h in range(2):
                        def tr(e, h=h, xn=xn):
                            for j in range(4):
                                k = h * 4 + j
                                ins = e.transpose(ps[h][:, j * 128:(j + 1) * 128], xn[:, k * 128:(k + 1) * 128], self.ident[:])
                            return ins
                        P.op('pe', tr, reads=[t_xn, self.t_const], writes=[t_ps[h]])
                        for j in range(4):
                            k = h * 4 + j
                            P.op('act', lambda e, h=h, j=j, k=k, v=v: e.activation(
                                out=h32[:, k, :], in_=ps[h][:, j * 128:(j + 1) * 128], func=AF.Identity,
                                scale=A2[:, k, v:v + 1], bias=modT[:, 24 + k, v:v + 1]),
                                reads=[t_ps[h], self.t_mod], writes=[t_h32])
                    P.op('dve', lambda e, bi=bi: e.tensor_copy(out=h2T[:, :, bi * 128:(bi + 1) * 128], in_=h32[:]),
                         reads=[t_h32], writes=[t_h2T])

                    def rmm(e):
                        for k in range(8):
                            ins = e.matmul(ps[2][:, 0:NE], h32[:, k, :], rw[:, k, :], start=(k == 0), stop=(k == 7))
                        return ins
                    P.op('pe', rmm, reads=[t_h32, t_c], writes=[t_ps[2]])
                    P.op('dve', lambda e: e.tensor_tensor(out=lg[:], in0=ps[2][:, 0:NE], in1=rb[:], op=ALU.add),
                         reads=[t_ps[2], t_c], writes=[t_lg])
                    P.op('dve', lambda e: e.max(out=top8[:], in_=lg[:]), reads=[t_lg], writes=[t_lg])
                    P.op('dve', lambda e: e.tensor_scalar(out=small[:, 4:5], in0=top8[:, 0:1], scalar1=-1.0, scalar2=None, op0=ALU.mult),
                         reads=[t_lg], writes=[t_small])
                    P.op('act', lambda e: e.activation(out=em[:], in_=lg[:], func=AF.Exp, bias=small[:, 4:5], scale=1.0),
                         reads=[t_lg, t_small], writes=[t_lg])
                    P.op('dve', lambda e: e.scalar_tensor_tensor(out=em[:], in0=lg[:], scalar=top8[:, 3:4], in1=em[:],
                                                                  op0=ALU.is_ge, op1=ALU.mult), reads=[t_lg], writes=[t_lg])
                    P.op('dve', lambda e: e.tensor_reduce(out=small[:, 5:6], in_=em[:], axis=AX.X, op=ALU.add),
                         reads=[t_lg], writes=[t_small])
                    P.op('dve', lambda e: e.reciprocal(out=small[:, 6:7], in_=small[:, 5:6]), reads=[t_small], writes=[t_small])
                    P.op('dve', lambda e, bi=bi: e.tensor_scalar(out=gates[:, bi, :], in0=em[:], scalar1=small[:, 6:7], scalar2=None,
                                                                 op0=ALU.mult), reads=[t_lg, t_small], writes=[t_gates])
                    P.op('pe', lambda e, bi=bi: e.transpose(ps[3][0:NE, 0:128], gates[:, bi, :], self.ident[:]),
                         reads=[t_gates, self.t_const], writes=[t_ps[3]])
                    P.op('act', lambda e: e.activation(out=gT[:], in_=ps[3][0:NE, 0:128], func=AF.Identity),
                         reads=[t_ps[3]], writes=[t_gT])
                    for h in range(2):
                        P.op('pe', lambda e, h=h: e.matmul(ps[h][:], gT[:], bdn[:, h * 512:(h + 1) * 512], start=True, stop=True),
                             reads=[t_gT, t_c], writes=[t_ps[h]])
                        P.op('act', lambda e, h=h, bi=bi: e.activation(out=acc[:, bi, h * 512:(h + 1) * 512], in_=ps[h][:], func=AF.Identity),
                             reads=[t_ps[h]], writes=[t_acc])
                ngrp = (nb + 3) // 4
                units = [(ex, g) for ex in range(NE) for g in range(ngrp)]

                def load_w(ex):
                    wi = (wbase + ex) % 2
                    P.op('sp', lambda e: e.dma_start(out=wu[wi][:], in_=D['WUB%d' % l][ex].rearrange("(k p) n -> p k n", p=128)),
                         reads=[self.t_cast[l][ex]], writes=[t_wu[wi]], dma=True)
                    P.op('sp', lambda e: e.dma_start(out=wd[wi][:], in_=D['WDB%d' % l][ex].rearrange("(k p) n -> p k n", p=128)),
                         reads=[self.t_cast[l][ex]], writes=[t_wd[wi]], dma=True)

                def up_unit(ui):
                    ex, g = units[ui]
                    wi = (wbase + ex) % 2
                    ai = ui % 2
                    gb = min(4, nb - g * 4)
                    N = gb * 128
                    t0 = g * 512
                    for fc in range(8):
                        ei = fc % 2
                        for part, pb in ((0, 4 + ei), (1, 6 + ei)):
                            def umm(e, part=part, pb=pb, fc=fc):
                                c0 = part * DM + fc * 128
                                for k in range(8):
                                    ins = e.matmul(ps[pb][:, 0:N], wu[wi][:, k, c0:c0 + 128], h2T[:, k, t0:t0 + N],
                                                   start=(k == 0), stop=(k == 7))
                                return ins
                            P.op('pe', umm, reads=[t_wu[wi], t_h2T], writes=[t_ps[pb]])
                        pg, pl = ps[4 + ei], ps[6 + ei]
                        P.op('dve', lambda e, pg=pg, ei=ei, fc=fc: e.tensor_scalar(
                            out=eg[ei][:, 0:N], in0=pg[:, 0:N], scalar1=bup[:, ex, fc:fc + 1], scalar2=7.0,
                            op0=ALU.add, op1=ALU.min), reads=[t_ps[4 + ei], t_c], writes=[t_eg[ei]])
                        P.op('act', lambda e, pl=pl, ei=ei, fc=fc: e.activation(
                            out=el[ei][:, 0:N], in_=pl[:, 0:N], func=AF.Identity, bias=bup[:, ex, 8 + fc:9 + fc], scale=1.0),
                            reads=[t_ps[6 + ei], t_c], writes=[t_el[ei]])
                        P.op('act', lambda e, ei=ei: e.activation(out=es[ei][:, 0:N], in_=eg[ei][:, 0:N], func=AF.Sigmoid, scale=1.702),
                             reads=[t_eg[ei]], writes=[t_es[ei]])
                        P.op('dve', lambda e, ei=ei: e.tensor_scalar(
                            out=el[ei][:, 0:N], in0=el[ei][:, 0:N], scalar1=7.0, scalar2=-7.0,
                            op0=ALU.min, op1=ALU.max), reads=[t_el[ei]], writes=[t_el[ei]])
                        P.op('dve' if fc % 2 else 'pool', lambda e, ei=ei: e.tensor_tensor(out=es[ei][:, 0:N], in0=es[ei][:, 0:N], in1=eg[ei][:, 0:N], op=ALU.mult),
                             reads=[t_eg[ei], t_es[ei]], writes=[t_es[ei]])
                        P.op('dve', lambda e, ei=ei, fc=fc: e.scalar_tensor_tensor(
                            out=actT[ai][:, fc, 0:N], in0=el[ei][:, 0:N], scalar=1.0, in1=es[ei][:, 0:N], op0=ALU.add, op1=ALU.mult),
                            reads=[t_es[ei], t_el[ei]], writes=[t_act[ai]])

                def down_unit(ui):
                    ex, g = units[ui]
                    wi = (wbase + ex) % 2
                    ai = ui % 2
                    gb = min(4, nb - g * 4)
                    for b4 in range(gb):
                        bi = g * 4 + b4
                        for h in range(2):
                            pb = (bi * 2 + h) % 4

                            def dmm(e, h=h, pb=pb, b4=b4):
                                for fc in range(8):
                                    ins = e.matmul(ps[pb][:], actT[ai][:, fc, b4 * 128:(b4 + 1) * 128],
                                                   wd[wi][:, fc, h * 512:(h + 1) * 512], start=(fc == 0), stop=(fc == 7))
                                return ins
                            P.op('pe', dmm, reads=[t_act[ai], t_wd[wi]], writes=[t_ps[pb]])
                            P.op('dve', lambda e, h=h, pb=pb, bi=bi: e.scalar_tensor_tensor(
                                out=acc[:, bi, h * 512:(h + 1) * 512], in0=ps[pb][:], scalar=gates[:, bi, ex:ex + 1],
                                in1=acc[:, bi, h * 512:(h + 1) * 512], op0=ALU.mult, op1=ALU.add),
                                reads=[t_ps[pb], t_gates, t_acc], writes=[t_acc])

                wbase = wcount
                wcount += NE
                load_w(0)
                load_w(1)
                up_unit(0)
                for ui in range(len(units)):
                    if ui + 1 < len(units):
                        up_unit(ui + 1)
                    down_unit(ui)
                    ex, g = units[ui]
                    if g == ngrp - 1 and ex + 2 < NE:
                        load_w(ex + 2)
                for bi in range(nb):
                    r0 = r_begin + (sb0 + bi) * 128
                    v = 1 if (r0 < self.TC) else 0
                    x_t = xt[bi % 2]; tx = t_xt[bi % 2]
                    P.op('sp', lambda e, x_t=x_t, r0=r0: e.dma_start(out=x_t[:], in_=self.rows(src, r0)), writes=[tx], dma=True)
                    P.op('pool', lambda e, bi=bi, v=v: e.tensor_tensor(out=acc[:, bi, :], in0=acc[:, bi, :], in1=self.gbc[v][:], op=ALU.mult),
                         reads=[t_acc, self.t_gbc], writes=[t_acc])
                    P.op('dve', lambda e, bi=bi, x_t=x_t: e.tensor_tensor(out=x_t[:], in0=x_t[:], in1=acc[:, bi, :], op=ALU.add),
                         reads=[t_acc, tx], writes=[tx])
                    if final:
                        P.op('act', lambda e, x_t=x_t: e.activation(out=junk[:], in_=x_t[:], func=AF.Square, accum_out=small[:, 8:9]),
                             reads=[tx], writes=[t_junk, t_small])
                        P.op('act', lambda e: e.activation(out=small[:, 9:10], in_=small[:, 8:9], func=AF.Sqrt, scale=1.0 / DM, bias=self.epsc[:, 0:1]),
                             reads=[t_small], writes=[t_small])
                        P.op('dve', lambda e: e.reciprocal(out=small[:, 10:11], in_=small[:, 9:10]), reads=[t_small], writes=[t_small])
                        P.op('dve', lambda e, x_t=x_t: e.scalar_tensor_tensor(out=x_t[:], in0=x_t[:], scalar=small[:, 10:11], in1=fw[:],
                                                                              op0=ALU.mult, op1=ALU.mult), reads=[tx, t_small, t_c], writes=[tx])
                        dst_ap = self.out[r0 - self.TC:r0 - self.TC + 128, :]
                    else:
                        dst_ap = self.D[dst][r0:r0 + 128, :]
                    P.op('sp', lambda e, x_t=x_t, dst_ap=dst_ap: e.dma_start(out=dst_ap, in_=x_t[:]), reads=[tx], writes=[t_out], dma=True)
            P.barrier()
            P.emit()


def col(v, n=128):
    return np.ascontiguousarray(np.asarray(v, np.float32).reshape(-1, n).T)


def rep(v, n=128):
    return np.ascontiguousarray(np.broadcast_to(np.asarray(v, np.float32)[None, :], (n, np.asarray(v).shape[0])))


def make_inputs(inp, b, T, TC):
    m = {}
    m['x'] = np.ascontiguousarray(inp['x'][b, :T])
    m['ctx'] = np.ascontiguousarray(inp['ctx'][b, :max(TC, 128)])
    cv = np.stack([col(inp['c'][b]), col(inp['c_ctx'])], axis=-1)
    m['cvec'] = np.ascontiguousarray(cv)
    m['ident'] = np.eye(128, dtype=np.float32)
    m['ones'] = np.ones((128, 128), np.float32)
    for l in range(2):
        p = 'l%d_' % l
        m[p + 'ada_w'] = inp[p + 'ada_w']
        m[p + 'ada_bT'] = col(inp[p + 'ada_b'])
        m[p + 'nmix'] = col(inp[p + 'norm_mix_w'])
        m[p + 'nffn'] = col(inp[p + 'norm_ffn_w'])
        m[p + 'router_w'] = inp[p + 'router_w']
        m[p + 'router_b'] = rep(inp[p + 'router_b'])
        m[p + 'w_up'] = inp[p + 'w_up']
        m[p + 'b_upT'] = np.ascontiguousarray(np.asarray(inp[p + 'b_up']).reshape(NE, 16, 128).transpose(2, 0, 1))
        m[p + 'w_down'] = inp[p + 'w_down']
        m[p + 'b_down'] = inp[p + 'b_down']
    m['final_w'] = rep(inp['final_norm_w'])
    m['l0_w_in'] = inp['l0_w_in']; m['l0_w_out'] = inp['l0_w_out']
    m['l0_w2'] = np.ascontiguousarray(np.stack([inp['l0_gla_w2_f'], inp['l0_gla_w2_b']], axis=1))
    m['l0_b2'] = np.ascontiguousarray(np.stack([col(inp['l0_gla_b_f']), col(inp['l0_gla_b_b'])], axis=1))
    m['l0_lb'] = np.ascontiguousarray(np.asarray(inp['hgrn_lb_logits'], np.float32).reshape(2, 4, 128).transpose(2, 0, 1))
    m['l0_normw'] = rep(np.concatenate([np.tile(inp['l0_gla_norm_w'], 4), np.tile(inp['l0_hgrn_norm_w'], 4)]))
    m['l1_w_in'] = inp['l1_w_in']; m['l1_w_out'] = inp['l1_w_out']
    m['l1_convw'] = np.ascontiguousarray(np.broadcast_to(np.asarray(inp['l1_conv_w'], np.float32)[None], (128, 5, 1536)))
    m['l1_pvec'] = rep(np.concatenate([inp['l1_a_log_f'], inp['l1_dt_bias_f'], inp['l1_a_log_b'], inp['l1_dt_bias_b']]))
    m['l1_dnw'] = rep(np.tile(inp['l1_dn_norm_w'], 4)); m['l1_sinks'] = rep(inp['l1_sinks'])
    m['rope'] = rope_table(T)
    t_ = np.arange(128)
    m['rmask'] = np.ascontiguousarray(np.broadcast_to(np.tile((t_ % 64 != 0).astype(np.float32), 6)[None, :], (128, 768)))
    same = (t_[:, None] // 64) == (t_[None, :] // 64)
    m['mask_f'] = (same & (t_[:, None] <= t_[None, :])).astype(np.float32)
    m['mask_r'] = (same & (t_[:, None] >= t_[None, :])).astype(np.float32)
    m['mask_fs'] = (same & (t_[:, None] < t_[None, :])).astype(np.float32)
    m['mask_rs'] = (same & (t_[:, None] > t_[None, :])).astype(np.float32)
    m['same'] = same.astype(np.float32)
    m['csel'] = np.stack([(t_ < 64), (t_ >= 64)], axis=1).astype(np.float32)
    m['mb_prev'] = np.where(t_[None, :] >= t_[:, None], 0.0, NEG).astype(np.float32)
    m['mb_next'] = np.where(t_[None, :] <= t_[:, None], 0.0, NEG).astype(np.float32)
    return m


def rope_table(T):
    rows = T // 64
    row = np.repeat(np.arange(rows, dtype=np.float32), 64)
    colp = np.tile(np.arange(64, dtype=np.float32), rows)
    inv = (10000.0 ** (-np.arange(16, dtype=np.float32) / 16)).astype(np.float32)
    ang = np.concatenate([row[:, None] * inv, colp[:, None] * inv], axis=-1).astype(np.float32)
    return np.ascontiguousarray(np.concatenate([np.cos(ang), np.sin(ang)], axis=-1).astype(np.float32))


_CACHE = {}


def kernel(**inputs):
    inp = {k: np.asarray(v) for k, v in inputs.items()}
    B, T, _ = inp['x'].shape
    TC = inp['ctx'].shape[1]
    key = (T, TC)
    import os
    ph = os.environ.get('KPHASES')
    bld = Builder(T, TC, phases=tuple(ph.split(','))) if ph else Builder(T, TC)
    nc = bld.build()
    in_maps = []
    for b in range(B):
        m = make_inputs(inp, b, T, TC)
        in_maps.append({k: m[k] for k in bld.in_names})
    res = run_bass_kernel_spmd(nc, in_maps, core_ids=list(range(B)))
    return np.stack([res.results[b]['out'] for b in range(B)], axis=0)


def phase_l0mix(self, src, dst):
    nc, P, D = self.nc, self.P, self.D
    TC, T = self.TC, self.T
    with ExitStack() as st:
        sb = lambda name, shape, d=F32: st.enter_context(nc.sbuf_tensor(name, shape, d))
        ps = [st.enter_context(nc.psum_tensor('pq%d' % i, [128, 512], F32)) for i in range(8)]
        ps4b = ps[4][:].bitcast(BF16)
        self.epsc = sb('epsc0', [128, 1])
        onec = sb('onec', [128, 1])
        win = sb('win', [128, 8, 4128], BF16)
        wout = sb('wout', [128, 8, DM], BF16)
        w2 = sb('w2', [16, 2, 256], BF16)
        b2 = sb('b2', [128, 2, 2]); lbl = sb('lbl', [128, 2, 4]); lbc = sb('lbc', [128, 4]); omlb = sb('omlb', [128, 4])
        normw = sb('normw', [128, DM]); rmask = sb('rmask_t', [128, 768])
        maskf = sb('maskf', [128, 128]); maskr = sb('maskr', [128, 128])
        xt = [sb('mxt%d' % i, [128, DM]) for i in range(2)]
        xn = sb('mxn', [128, DM]); junk = sb('mjunk', [128, DM], BF16); small = sb('msmall', [128, 32])
        hT = sb('hT', [128, 8, 128], BF16)
        arT = sb('arT', [16, 128], BF16)
        LF = sb('LF', [128, 6, 128]); bb = sb('bb', [128, 6, 128]); EA = sb('EA', [128, 6, 128]); EB = sb('EB', [128, 6, 128])
        E = sb('E', [128, 6, 2]); kH = sb('kH', [128, 4, 128]); sg = sb('sg', [128, 4, 128])
        qT = sb('qT', [128, 6, 128], BF16); kT = sb('kT', [128, 6, 128], BF16)
        ktok = sb('ktok', [128, 768], BF16); vtok = sb('vtok', [128, DM], BF16)
        SCm = sb('SCm', [128, 8, 128], BF16)
        S = sb('S', [128, 6, 128]); Tmp = sb('Tmp', [128, 6, 128])
        SB = [sb('SB%d' % i, [128, 6, 128], BF16) for i in range(4)]
        Oc = sb('Oc', [128, DM]); ofl = sb('ofl', [128, DM]); og = sb('og', [128, DM])
        yb = sb('yb', [128, DM], BF16); yT = sb('yT', [128, 8, 128], BF16)
        tk = {n: Tok() for n in ('c', 'x0', 'x1', 'xn', 'junk', 'small', 'hT', 'arT', 'LF', 'bb', 'EA', 'EB', 'E', 'kH', 'sg',
                                 'qT', 'kT', 'ktok', 'vtok', 'SCm', 'S', 'Tmp', 'SB0', 'SB1', 'SB2', 'SB3', 'Oc', 'ofl', 'og', 'yb', 'yT', 'out')}
        tp = [Tok() for _ in range(8)]
        c_ = [tk['c']]
        P.op('pool', lambda e: e.memset(self.epsc[:], EPS), writes=c_)
        P.op('pool', lambda e: e.memset(onec[:], 1.0), writes=c_)
        for (a, b_) in ((0, 2048), (2048, 4096), (4096, 4128)):
            P.op('pool', lambda e, a=a, b_=b_: e.dma_start(out=win[:, :, a:b_], in_=D['l0_w_in'][:, a:b_].rearrange("(k p) n -> p k n", p=128)),
                 writes=c_, dma=True)
        P.op('pool', lambda e: e.dma_start(out=wout[:], in_=D['l0_w_out'].rearrange("(k p) n -> p k n", p=128)), writes=c_, dma=True)
        P.op('pool', lambda e: e.dma_start(out=w2[:], in_=D['l0_w2']), writes=c_, dma=True)
        for nm, t_ in (('l0_b2', b2), ('l0_lb', lbl), ('l0_normw', normw), ('rmask', rmask), ('mask_f', maskf), ('mask_r', maskr)):
            P.op('sp', lambda e, nm=nm, t_=t_: e.dma_start(out=t_[:], in_=D[nm]), writes=c_, dma=True)
        P.op('dve', lambda e: e.tensor_scalar(out=b2[:], in0=b2[:], scalar1=-1.0, scalar2=None, op0=ALU.mult), reads=c_, writes=c_)
        P.op('dve', lambda e: e.tensor_tensor(out=lbc[:], in0=lbl[:, 0, :], in1=lbl[:, 1, :], op=ALU.subtract), reads=c_, writes=c_)
        P.op('act', lambda e: e.activation(out=omlb[:], in_=lbc[:], func=AF.Sigmoid, scale=-1.0), reads=c_, writes=c_)
        P.op('act', lambda e: e.activation(out=lbc[:], in_=lbc[:], func=AF.Sigmoid), reads=c_, writes=c_)
        self.gate_bcast(Oc[:].rearrange("p (k t) -> p k t", k=8), ps[0:2], 0, 16, (0, 1) if TC else (0,))
        P.barrier()
        A1 = self.Acol[0][0]; modT = self.modT[0]

        def mmgroup(pb, outs):
            def f(e):
                for out_ap, pairs in outs:
                    n = len(pairs)
                    for i, (l_, r_) in enumerate(pairs):
                        ins = e.matmul(out_ap, l_, r_, start=(i == 0), stop=(i == n - 1))
                return ins
            return f

        import os
        STOP = int(os.environ.get('L0STOP', '99'))

        def block(r0, is_ctx, d, first, bidx):
            v = 1 if is_ctx else 0
            x_t = xt[bidx % 2]; tx = tk['x%d' % (bidx % 2)]
            self.norm_xn(self.rows(src, r0), x_t, tx, xn, tk['xn'], small, tk['small'], junk, tk['junk'])
            for h in range(2):
                P.op('pe', mmgroup(None, []) if False else (lambda e, h=h: [e.transpose(ps[h][:, j * 128:(j + 1) * 128], xn[:, (h * 4 + j) * 128:(h * 4 + j + 1) * 128], self.ident[:]) for j in range(4)][-1]),
                     reads=[tk['xn'], self.t_const], writes=[tp[h]])
                for j in range(4):
                    k = h * 4 + j
                    P.op('act', lambda e, h=h, j=j, k=k: e.activation(out=hT[:, k, :], in_=ps[h][:, j * 128:(j + 1) * 128], func=AF.Identity,
                                                                      scale=A1[:, k, v:v + 1], bias=modT[:, k, v:v + 1]),
                         reads=[tp[h], self.t_mod], writes=[tk['hT']])
            rh = [tk['hT'], tk['c']]
            wcol = lambda c0, n=128: [(win[:, k, c0:c0 + n], hT[:, k, :]) for k in range(8)]
            c_ar = 1024 + d * 16
            P.op('pe', mmgroup(0, [(ps[0][0:16, 0:128], [(win[:, k, c_ar:c_ar + 16], hT[:, k, :]) for k in range(8)])]), reads=rh, writes=[tp[0]])
            P.op('act', lambda e: e.activation(out=arT[:], in_=ps[0][0:16, 0:128], func=AF.Identity), reads=[tp[0]], writes=[tk['arT']])
            P.op('pe', mmgroup(0, [(ps[0][:, 128 + c * 128:256 + c * 128], [(w2[:, d, c * 128:(c + 1) * 128], arT[:])]) for c in range(2)]),
                 reads=[tk['arT'], tk['c']], writes=[tp[0]])
            if STOP < 3:
                return
            c_bz = 2080 + d * 512
            P.op('pe', mmgroup(1, [(ps[1][:, c * 128:(c + 1) * 128], wcol(c_bz + c * 128)) for c in range(4)]), reads=rh, writes=[tp[1]])
            if STOP < 4:
                return
            for c in range(2):
                P.op('act', lambda e, c=c: e.activation(out=LF[:, c, :], in_=ps[0][:, 128 + c * 128:256 + c * 128], func=AF.Exp,
                                                        scale=-1.0, bias=b2[:, d, c:c + 1]), reads=[tp[0], tk['c']], writes=[tk['LF']])
            P.op('act', lambda e: e.activation(out=LF[:, 0:2, :], in_=LF[:, 0:2, :], func=AF.Ln, bias=onec[:, 0:1], scale=1.0),
                 reads=[tk['c']], writes=[tk['LF']])
            P.op('act', lambda e: e.activation(out=sg[:], in_=ps[1][:].rearrange("p (c t) -> p c t", c=4), func=AF.Sigmoid),
                 reads=[tp[1]], writes=[tk['sg']])
            P.op('dve', lambda e: e.tensor_scalar(out=LF[:, 0:2, :], in0=LF[:, 0:2, :], scalar1=-1.0 / 16.0, scalar2=None, op0=ALU.mult),
                 writes=[tk['LF']])
            P.op('dve', lambda e: e.tensor_tensor(out=sg[:], in0=sg[:], in1=omlb[:].unsqueeze(2).to_broadcast([128, 4, 128]), op=ALU.mult),
                 reads=[tk['c']], writes=[tk['sg']])
            P.op('dve', lambda e: e.tensor_tensor(out=sg[:], in0=sg[:], in1=lbc[:].unsqueeze(2).to_broadcast([128, 4, 128]), op=ALU.add),
                 reads=[tk['c']], writes=[tk['sg']])
            P.op('act', lambda e: e.activation(out=LF[:, 2:6, :], in_=sg[:], func=AF.Ln), reads=[tk['sg']], writes=[tk['LF']])
            P.op('dve', lambda e: e.tensor_scalar(out=kH[:], in0=sg[:], scalar1=-1.0, scalar2=1.0, op0=ALU.mult, op1=ALU.add),
                 reads=[tk['sg']], writes=[tk['kH']])
            if STOP < 5:
                return
            LF2 = LF[:].rearrange("p c t -> p (c t)"); bb2 = bb[:].rearrange("p c t -> p (c t)")
            P.op('dve', lambda e: e.tensor_tensor_scan(out=bb2, data0=rmask[:], data1=LF2, initial=0.0, op0=ALU.mult, op1=ALU.add),
                 reads=[tk['LF'], tk['c']], writes=[tk['bb']])
            tot = bb[:].rearrange("p c (a t) -> p c a t", a=2)[:, :, :, 63]
            P.op('act', lambda e: e.activation(out=E[:], in_=tot, func=AF.Exp), reads=[tk['bb']], writes=[tk['E']])
            if d == 1:
                P.op('dve', lambda e: e.tensor_tensor(out=bb[:], in0=bb[:], in1=LF[:], op=ALU.subtract), reads=[tk['LF']], writes=[tk['bb']])
            sa, sb_ = (1.0, -1.0) if d == 0 else (-1.0, 1.0)
            P.op('act', lambda e: e.activation(out=EA[:], in_=bb[:], func=AF.Exp, scale=sa), reads=[tk['bb']], writes=[tk['EA']])
            P.op('act', lambda e: e.activation(out=EB[:], in_=bb[:], func=AF.Exp, scale=sb_), reads=[tk['bb']], writes=[tk['EB']])
            if STOP < 6:
                return
            P.op('pe', mmgroup(2, [(ps[2][:, 0:128], wcol(0)), (ps[2][:, 128:256], wcol(128)),
                                   (ps[2][:, 256:384], wcol(1568)), (ps[2][:, 384:512], wcol(1696))]), reads=rh, writes=[tp[2]])
            P.op('pe', mmgroup(3, [(ps[3][:, 0:128], wcol(1824)), (ps[3][:, 128:256], wcol(1952)),
                                   (ps[3][:, 256:384], wcol(256)), (ps[3][:, 384:512], wcol(384))]), reads=rh, writes=[tp[3]])
            v3 = lambda ap, n: ap.rearrange("p (c t) -> p c t", c=n)
            P.op('dve', lambda e: e.scalar_tensor_tensor(out=qT[:, 0:2, :], in0=v3(ps[2][:, 0:256], 2), scalar=0.125, in1=EA[:, 0:2, :],
                                                         op0=ALU.mult, op1=ALU.mult), reads=[tp[2], tk['EA']], writes=[tk['qT']])
            P.op('dve', lambda e: e.tensor_tensor(out=qT[:, 2:4, :], in0=v3(ps[2][:, 256:512], 2), in1=EA[:, 2:4, :], op=ALU.mult),
                 reads=[tp[2], tk['EA']], writes=[tk['qT']])
            P.op('dve', lambda e: e.tensor_tensor(out=qT[:, 4:6, :], in0=v3(ps[3][:, 0:256], 2), in1=EA[:, 4:6, :], op=ALU.mult),
                 reads=[tp[3], tk['EA']], writes=[tk['qT']])
            P.op('dve', lambda e: e.tensor_tensor(out=kT[:, 0:2, :], in0=v3(ps[3][:, 256:512], 2), in1=EB[:, 0:2, :], op=ALU.mult),
                 reads=[tp[3], tk['EB']], writes=[tk['kT']])
            P.op('pool', lambda e: e.tensor_tensor(out=kT[:, 2:6, :], in0=kH[:], in1=EB[:, 2:6, :], op=ALU.mult),
                 reads=[tk['kH'], tk['EB']], writes=[tk['kT']])
            if STOP < 7:
                return
            P.op('pe', lambda e: [e.transpose(ps4b[:, c * 128:(c + 1) * 128], kT[:, c, :], self.identb[:]) for c in range(6)][-1],
                 reads=[tk['kT'], self.t_const], writes=[tp[4]])
            P.op('act', lambda e: e.activation(out=ktok[:], in_=ps4b[:, 0:768], func=AF.Identity), reads=[tp[4]], writes=[tk['ktok']])
            if STOP < 8:
                return
            for i, c0 in enumerate((512, 3104)):
                P.op('pe', mmgroup(5, [(ps[5][:], [(hT[:, k, :], win[:, k, c0:c0 + 512]) for k in range(8)])]), reads=rh, writes=[tp[5]])
                P.op('act', lambda e, i=i: e.activation(out=vtok[:, i * 512:(i + 1) * 512], in_=ps[5][:], func=AF.Identity),
                     reads=[tp[5]], writes=[tk['vtok']])
            if STOP < 9:
                return
            def rows_of(h):
                if h < 4:
                    return slice((h % 2) * 64, (h % 2) * 64 + 64), h // 2
                return slice(0, 128), h - 2
            bankheads = ((0, 2, 4, 5), (1, 3, 6, 7))
            scidx = {h: half * 4 + hh for half in range(2) for hh, h in enumerate(bankheads[half])}
            for half in range(2):
                outs = []
                for hh in range(4):
                    h = bankheads[half][hh]
                    rs, bk = rows_of(h)
                    outs.append((ps[6 + half][:, hh * 128:(hh + 1) * 128], [(kT[rs, bk, :], qT[rs, bk, :])]))
                P.op('pe', mmgroup(6 + half, outs), reads=[tk['kT'], tk['qT']], writes=[tp[6 + half]])
                mk = maskf if d == 0 else maskr
                P.op('dve', lambda e, half=half, mk=mk: e.tensor_tensor(
                    out=SCm[:, half * 4:(half + 1) * 4, :], in0=v3(ps[6 + half][:], 4), in1=mk[:].unsqueeze(1).to_broadcast([128, 4, 128]),
                    op=ALU.mult), reads=[tp[6 + half], tk['c']], writes=[tk['SCm']])
            if STOP < 10:
                return
            order = (0, 1) if d == 0 else (1, 0)
            pbank = {order[0]: (0, 1), order[1]: (2, 3)}
            for c in order:
                pa, pb_ = pbank[c]
                cs = slice(c * 64, c * 64 + 64)
                outsA, outsB = [], []
                for h in range(8):
                    rs, bk = rows_of(h)
                    if h < 4:
                        l_ = ktok[cs, bk * 128 + (h % 2) * 64: bk * 128 + (h % 2) * 64 + 64]
                    else:
                        l_ = ktok[cs, bk * 128:(bk + 1) * 128]
                    r_ = vtok[cs, h * 128:(h + 1) * 128]
                    if bk < 4:
                        outsA.append((ps[pa][rs, bk * 128:(bk + 1) * 128], [(l_, r_)]))
                    else:
                        outsB.append((ps[pb_][rs, (bk - 4) * 128:(bk - 3) * 128], [(l_, r_)]))
                P.op('pe', mmgroup(pa, outsA), reads=[tk['ktok'], tk['vtok']], writes=[tp[pa]])
                P.op('pe', mmgroup(pb_, outsB), reads=[tk['ktok'], tk['vtok']], writes=[tp[pb_]])
            if STOP < 11:
                return
            st_i = 2 * (bidx % 2); mid_i = st_i + 1; end_i = 2 * ((bidx + 1) % 2)
            if first:
                P.op('pool', lambda e: e.memset(S[:], 0.0), writes=[tk['S']])
                P.op('pool', lambda e: e.memset(SB[st_i][:], 0.0), writes=[tk['SB%d' % st_i]])
            for i, c in enumerate(order):
                pa, pb_ = pbank[c]
                Ebc = E[:, :, c:c + 1].to_broadcast([128, 6, 128])
                PA = v3(ps[pa][:], 4); PB = v3(ps[pb_][:, 0:256], 2)
                if d == 0:
                    wi_ = mid_i if i == 0 else end_i
                    P.op('dve', lambda e, PA=PA: e.tensor_tensor(out=Tmp[:, 0:4, :], in0=PA, in1=S[:, 0:4, :], op=ALU.add),
                         reads=[tp[pa], tk['S']], writes=[tk['Tmp']])
                    P.op('dve', lambda e, PB=PB: e.tensor_tensor(out=Tmp[:, 4:6, :], in0=PB, in1=S[:, 4:6, :], op=ALU.add),
                         reads=[tp[pb_], tk['S']], writes=[tk['Tmp']])
                    P.op('dve', lambda e, Ebc=Ebc: e.tensor_tensor(out=S[:], in0=Tmp[:], in1=Ebc, op=ALU.mult),
                         reads=[tk['Tmp'], tk['E']], writes=[tk['S']])
                    P.op('act', lambda e, wi_=wi_: e.activation(out=SB[wi_][:], in_=S[:], func=AF.Identity),
                         reads=[tk['S']], writes=[tk['SB%d' % wi_]])
                else:
                    wi_ = st_i if i == 0 else mid_i
                    P.op('dve', lambda e, Ebc=Ebc: e.tensor_tensor(out=S[:], in0=S[:], in1=Ebc, op=ALU.mult),
                         reads=[tk['E']], writes=[tk['S']])
                    P.op('act', lambda e, wi_=wi_: e.activation(out=SB[wi_][:], in_=S[:], func=AF.Identity),
                         reads=[tk['S']], writes=[tk['SB%d' % wi_]])
                    P.op('dve', lambda e, PA=PA: e.tensor_tensor(out=S[:, 0:4, :], in0=PA, in1=S[:, 0:4, :], op=ALU.add),
                         reads=[tp[pa]], writes=[tk['S']])
                    P.op('dve', lambda e, PB=PB: e.tensor_tensor(out=S[:, 4:6, :], in0=PB, in1=S[:, 4:6, :], op=ALU.add),
                         reads=[tp[pb_]], writes=[tk['S']])
            if STOP < 12:
                return
            for half in range(2):
                def omm(e, half=half):
                    for hh in range(4):
                        h = half * 4 + hh
                        rs, bk = rows_of(h)
                        ob = ps[4 + half]
                        e.matmul(ob[:, hh * 128:(hh + 1) * 128], SCm[:, scidx[h], :], vtok[:, h * 128:(h + 1) * 128], start=True, stop=False)
                        for i, c in enumerate(order):
                            snap = SB[st_i] if i == 0 else SB[mid_i]
                            ins = e.matmul(ob[c * 64:(c + 1) * 64, hh * 128:(hh + 1) * 128], qT[rs, bk, c * 64:(c + 1) * 64],
                                           snap[rs, bk, :], start=False, stop=True)
                    return ins
                P.op('pe', omm, reads=[tk['SCm'], tk['vtok'], tk['qT'], tk['SB%d' % st_i], tk['SB%d' % mid_i]], writes=[tp[4 + half]])
            if STOP < 13:
                return
            if d == 0:
                for half in range(2):
                    P.op('act', lambda e, half=half: e.activation(out=Oc[:, half * 512:(half + 1) * 512], in_=ps[4 + half][:], func=AF.Identity),
                         reads=[tp[4 + half]], writes=[tk['Oc']])
                P.op('act', lambda e: e.dma_start(out=D['OF'][r0:r0 + 128, :], in_=Oc[:]), reads=[tk['Oc']], writes=[tk['out']], dma=True)
                return
            P.op('sp', lambda e: e.dma_start(out=ofl[:], in_=D['OF'][r0:r0 + 128, :]), writes=[tk['ofl']], dma=True)
            for half in range(2):
                P.op('dve', lambda e, half=half: e.tensor_tensor(out=Oc[:, half * 512:(half + 1) * 512], in0=ps[4 + half][:],
                                                                 in1=ofl[:, half * 512:(half + 1) * 512], op=ALU.add),
                     reads=[tp[4 + half], tk['ofl']], writes=[tk['Oc']])
            for half, c0 in enumerate((1056, 3616)):
                P.op('pe', mmgroup(6 + half, [(ps[6 + half][:], [(hT[:, k, :], win[:, k, c0:c0 + 512]) for k in range(8)])]),
                     reads=rh, writes=[tp[6 + half]])
                P.op('act', lambda e, half=half: e.activation(out=og[:, half * 512:(half + 1) * 512], in_=ps[6 + half][:], func=AF.Silu),
                     reads=[tp[6 + half]], writes=[tk['og']])
            O3 = Oc[:].rearrange("p (h v) -> p h v", h=8)
            P.op('pool', lambda e: e.tensor_tensor(out=ofl[:], in0=Oc[:], in1=Oc[:], op=ALU.mult), reads=[tk['Oc']], writes=[tk['ofl']])
            P.op('dve', lambda e: e.tensor_reduce(out=small[:, 8:16], in_=ofl[:].rearrange("p (h v) -> p h v", h=8), axis=AX.X, op=ALU.add),
                 reads=[tk['ofl']], writes=[tk['small']])
            P.op('act', lambda e: e.activation(out=small[:, 16:24], in_=small[:, 8:16], func=AF.Sqrt, scale=1.0 / 128, bias=self.epsc[:, 0:1]),
                 reads=[tk['small']], writes=[tk['small']])
            P.op('dve', lambda e: e.reciprocal(out=small[:, 24:32], in_=small[:, 16:24]), reads=[tk['small']], writes=[tk['small']])
            P.op('dve', lambda e: e.tensor_tensor(out=O3, in0=O3, in1=small[:, 24:32].unsqueeze(2).to_broadcast([128, 8, 128]), op=ALU.mult),
                 reads=[tk['small']], writes=[tk['Oc']])
            P.op('pool', lambda e: e.tensor_tensor(out=Oc[:], in0=Oc[:], in1=normw[:], op=ALU.mult), reads=[tk['c']], writes=[tk['Oc']])
            P.op('dve', lambda e: e.tensor_tensor(out=yb[:], in0=Oc[:], in1=og[:], op=ALU.mult), reads=[tk['Oc'], tk['og']], writes=[tk['yb']])
            P.op('pe', lambda e: [e.transpose(ps4b[:, k * 128:(k + 1) * 128], yb[:, k * 128:(k + 1) * 128], self.identb[:]) for k in range(8)][-1],
                 reads=[tk['yb'], self.t_const], writes=[tp[4]])
            P.op('act', lambda e: e.activation(out=yT[:].rearrange("p k t -> p (k t)"), in_=ps4b[:, 0:1024], func=AF.Identity),
                 reads=[tp[4]], writes=[tk['yT']])
            for half in range(2):
                P.op('pe', mmgroup(6 + half, [(ps[6 + half][:], [(yT[:, k, :], wout[:, k, half * 512:(half + 1) * 512]) for k in range(8)])]),
                     reads=[tk['yT'], tk['c']], writes=[tp[6 + half]])
                P.op('dve', lambda e, half=half: e.tensor_tensor(out=Oc[:, half * 512:(half + 1) * 512], in0=ps[6 + half][:],
                                                                 in1=self.gbc[v][:, half * 512:(half + 1) * 512], op=ALU.mult),
                     reads=[tp[6 + half], self.t_gbc], writes=[tk['Oc']])
            P.op('pool', lambda e: e.tensor_tensor(out=x_t[:], in0=x_t[:], in1=Oc[:], op=ALU.add), reads=[tk['Oc']], writes=[tx])
            P.op('act', lambda e: e.dma_start(out=D[dst][r0:r0 + 128, :], in_=x_t[:]), reads=[tx], writes=[tk['out']], dma=True)

        NCB, NB = self.NCB, self.NB
        for d in range(2):
            seq = [(i * 128, True) for i in range(NCB)] + [(TC + i * 128, False) for i in range(NB)]
            if d == 1:
                seq = [(i * 128, True) for i in reversed(range(NCB))] + [(TC + i * 128, False) for i in reversed(range(NB))]
            for n, (r0, is_ctx) in enumerate(seq):
                block(r0, is_ctx, d, n == 0, n)
                if d == 0:
                    self.issue_cast(0, n)
            P.barrier()
        P.emit()


Builder.phase_l0mix = phase_l0mix


def phase_final(self, src):
    nc, P, D = self.nc, self.P, self.D
    with ExitStack() as st:
        sb = lambda name, shape, d=F32: st.enter_context(nc.sbuf_tensor(name, shape, d))
        self.epsc = sb('epscf', [128, 1])
        xt = [sb('fxt%d' % i, [128, DM]) for i in range(2)]
        junk = sb('fjunk', [128, DM], BF16); small = sb('fsmall', [128, 8]); fw = sb('ffw', [128, DM])
        tc_, tj, ts, to = Tok(), Tok(), Tok(), Tok()
        txs = [Tok(), Tok()]
        P.op('pool', lambda e: e.memset(self.epsc[:], EPS), writes=[tc_])
        P.op('sp', lambda e: e.dma_start(out=fw[:], in_=D['final_w']), writes=[tc_], dma=True)
        for bi in range(self.NB):
            r0 = self.TC + bi * 128
            x_t = xt[bi % 2]; tx = txs[bi % 2]
            self.norm_xn(self.rows(src, r0), x_t, tx, x_t, tx, small, ts, junk, tj)
            P.op('dve', lambda e, x_t=x_t: e.tensor_tensor(out=x_t[:], in0=x_t[:], in1=fw[:], op=ALU.mult), reads=[tc_], writes=[tx])
            P.op('sp', lambda e, x_t=x_t, r0=r0: e.dma_start(out=self.out[r0 - self.TC:r0 - self.TC + 128, :], in_=x_t[:]),
                 reads=[tx], writes=[to], dma=True)
        P.barrier()
        P.emit()


Builder.phase_final = phase_final


NEG = -30000.0


def phase_l1mix(self, src, dst):
    nc, P, D = self.nc, self.P, self.D
    TC, T, NCB, NB = self.TC, self.T, self.NCB, self.NB
    R = TC + T
    cxrow = lambda r: (2 + r) if r < TC else (r + 6)
    A1 = self.Acol[1][0]; modT = self.modT[1]

    def mmgroup(outs):
        def f(e):
            for out_ap, pairs in outs:
                n = len(pairs)
                for i, (l_, r_) in enumerate(pairs):
                    ins = e.matmul(out_ap, l_, r_, start=(i == 0), stop=(i == n - 1))
            return ins
        return f

    def trs(dsts_srcs, idt):
        def f(e):
            for o_, i_ in dsts_srcs:
                ins = e.transpose(o_, i_, idt)
            return ins
        return f

    with ExitStack() as st:
        sb = lambda name, shape, d=F32: st.enter_context(nc.sbuf_tensor(name, shape, d))
        ps = [st.enter_context(nc.psum_tensor('pr%d' % i, [128, 512], F32)) for i in range(8)]
        psb = [p_[:].bitcast(BF16) for p_ in ps]
        self.epsc = sb('epsc1', [128, 1]); onec = sb('onec1', [128, 1])
        win = sb('win1', [128, 8, 2832], BF16); wout = sb('wout1', [128, 8, DM], BF16)
        convw = sb('convw', [128, 5, 1536], BF16); pvec = sb('pvec', [128, 16]); dnw = sb('dnw', [128, 512]); sinks = sb('sinks', [128, 8])
        maskf = sb('maskf1', [128, 128]); maskr = sb('maskr1', [128, 128]); maskfs = sb('maskfs', [128, 128]); maskrs = sb('maskrs', [128, 128])
        same = sb('same_t', [128, 128]); csel = sb('csel_t', [128, 2]); mbp = sb('mbp', [128, 128]); mbn = sb('mbn', [128, 128])
        xt = [sb('lxt%d' % i, [128, DM]) for i in range(2)]
        xn = sb('lxn', [128, DM]); junk = sb('ljunk', [128, DM], BF16); small = sb('lsmall', [128, 64])
        hT = sb('lhT', [128, 8, 128], BF16)
        cxs = sb('cxs', [128, 1536]); cxl = [sb('cxl%d' % i, [128, 1536]) for i in range(2)]
        kv = sb('kv', [128, 256]); rope = sb('rope_t', [128, 64]); rt = sb('rt', [128, 8, 64]); kTs = sb('kTs', [128, 128])
        tk = {n: Tok() for n in ('c', 'x0', 'x1', 'xn', 'junk', 'small', 'hT', 'cxs', 'cxl0', 'cxl1', 'kv', 'rope', 'rt', 'kTs', 'out',
                                 'qkv', 'qkb', 'fT', 'g', 'gs', 'GB', 'Lts', 'Lst', 'A', 'AT', 'Q', 'QT', 'Rm', 'Rb', 'kbg', 'kd', 'bv',
                                 'u', 'wT', 'S', 'Sb0', 'Sb1', 'vn', 'Pst', 'egr', 'qg', 'Oc', 'ofl', 'og', 'yb', 'yT', 'q8', 'qT8',
                                 'kTl', 'vl', 'kcT', 'vc', 'ssb', 'pb', 'pT', 'att')}
        tp = [Tok() for _ in range(8)]
        c_ = [tk['c']]
        P.op('pool', lambda e: e.memset(self.epsc[:], EPS), writes=c_)
        P.op('pool', lambda e: e.memset(onec[:], 1.0), writes=c_)
        for (a, b_) in ((0, 2048), (2048, 2832)):
            P.op('pool', lambda e, a=a, b_=b_: e.dma_start(out=win[:, :, a:b_], in_=D['l1_w_in'][:, a:b_].rearrange("(k p) n -> p k n", p=128)),
                 writes=c_, dma=True)
        P.op('pool', lambda e: e.dma_start(out=wout[:], in_=D['l1_w_out'].rearrange("(k p) n -> p k n", p=128)), writes=c_, dma=True)
        P.op('pool', lambda e: e.dma_start(out=convw[:], in_=D['l1_convw']), writes=c_, dma=True)
        for nm, t_ in (('l1_pvec', pvec), ('l1_dnw', dnw), ('l1_sinks', sinks), ('mask_f', maskf), ('mask_r', maskr),
                       ('mask_fs', maskfs), ('mask_rs', maskrs), ('same', same), ('csel', csel), ('mb_prev', mbp), ('mb_next', mbn)):
            P.op('sp', lambda e, nm=nm, t_=t_: e.dma_start(out=t_[:], in_=D[nm]), writes=c_, dma=True)
        for c0 in (0, 8):
            P.op('act', lambda e, c0=c0: e.activation(out=pvec[:, c0:c0 + 4], in_=pvec[:, c0:c0 + 4], func=AF.Exp), reads=c_, writes=c_)
            P.op('dve', lambda e, c0=c0: e.tensor_scalar(out=pvec[:, c0:c0 + 4], in0=pvec[:, c0:c0 + 4], scalar1=-1.0, scalar2=None, op0=ALU.mult),
                 reads=c_, writes=c_)
        P.op('pool', lambda e: e.memset(cxs[:], 0.0), writes=[tk['cxs']])
        for r in (0, TC + 2, TC + 4, TC + T + 6):
            P.op('sp', lambda e, r=r: e.dma_start(out=D['CX'][r:r + 2, :], in_=cxs[0:2, :]), reads=[tk['cxs']], writes=[tk['out']], dma=True)
        self.gate_bcast(xn[:].rearrange("p (k t) -> p k t", k=8), ps[0:2], 1, 16, (0,))
        P.barrier()

        def norm_hT(r0, v, bidx):
            x_t = xt[bidx % 2]; tx = tk['x%d' % (bidx % 2)]
            self.norm_xn(self.rows(src, r0), x_t, tx, xn, tk['xn'], small, tk['small'], junk, tk['junk'])
            for h in range(2):
                P.op('pe', trs([(ps[h][:, j * 128:(j + 1) * 128], xn[:, (h * 4 + j) * 128:(h * 4 + j + 1) * 128]) for j in range(4)], self.ident[:]),
                     reads=[tk['xn'], self.t_const], writes=[tp[h]])
                for j in range(4):
                    k = h * 4 + j
                    P.op('act', lambda e, h=h, j=j, k=k: e.activation(out=hT[:, k, :], in_=ps[h][:, j * 128:(j + 1) * 128], func=AF.Identity,
                                                                      scale=A1[:, k, v:v + 1], bias=modT[:, k, v:v + 1]),
                         reads=[tp[h], self.t_mod], writes=[tk['hT']])
            return x_t, tx

        rh = [tk['hT'], tk['c']]
        tokmm = lambda c0, n: [(hT[:, k, :], win[:, k, c0:c0 + n]) for k in range(8)]

        def pre_block(r0, is_ctx, bidx):
            norm_hT(r0, 1 if is_ctx else 0, bidx)
            for j in range(3):
                P.op('pe', mmgroup([(ps[2 + j][:], tokmm(j * 512, 512))]), reads=rh, writes=[tp[2 + j]])
                P.op('act', lambda e, j=j: e.activation(out=cxs[:, j * 512:(j + 1) * 512], in_=ps[2 + j][:], func=AF.Identity),
                     reads=[tp[2 + j]], writes=[tk['cxs']])
            cr = cxrow(r0)
            P.op('act', lambda e: e.dma_start(out=D['CX'][cr:cr + 128, :], in_=cxs[:]), reads=[tk['cxs']], writes=[tk['out']], dma=True)
            P.op('pe', mmgroup([(ps[5][:, 0:256], tokmm(2576, 256))]), reads=rh, writes=[tp[5]])
            P.op('act', lambda e: e.activation(out=kv[:], in_=ps[5][:, 0:256], func=AF.Identity), reads=[tp[5]], writes=[tk['kv']])
            if not is_ctx:
                t0 = r0 - TC
                P.op('sp', lambda e: e.dma_start(out=rope[:], in_=D['rope'][t0:t0 + 128, :]), writes=[tk['rope']], dma=True)
                self.apply_rope(kv[:, 0:128].rearrange("p (h f) -> p h f", h=2), 2, rope, rt, [tk['kv']], tk['rope'], tk['rt'])
            P.op('act', lambda e: e.dma_start(out=D['VV'][r0:r0 + 128, :], in_=kv[:, 128:256]), reads=[tk['kv']], writes=[tk['out']], dma=True)
            P.op('pe', trs([(ps[6][:, 0:128], kv[:, 0:128])], self.ident[:]), reads=[tk['kv'], self.t_const], writes=[tp[6]])
            P.op('act', lambda e: e.activation(out=kTs[:], in_=ps[6][:, 0:128], func=AF.Identity), reads=[tp[6]], writes=[tk['kTs']])
            P.op('act', lambda e: e.dma_start(out=D['KT'][:, r0:r0 + 128], in_=kTs[:]), reads=[tk['kTs']], writes=[tk['out']], dma=True)
            P.op('act', lambda e: e.dma_start(out=D['KT2'][0:64, r0:r0 + 128], in_=kTs[64:128, :]), reads=[tk['kTs']], writes=[tk['out']], dma=True)
            P.op('act', lambda e: e.dma_start(out=D['KT2'][64:128, r0:r0 + 128], in_=kTs[0:64, :]), reads=[tk['kTs']], writes=[tk['out']], dma=True)

        seq_all = [(i * 128, True) for i in range(NCB)] + [(TC + i * 128, False) for i in range(NB)]
        for n, (r0, is_ctx) in enumerate(seq_all):
            pre_block(r0, is_ctx, n)
        P.barrier()

        qkv = cxs; qkb = sb('qkb', [128, 8, 128], BF16); fT = sb('fT', [128, 8, 128], BF16)
        sm16 = small
        gs = sb('gs', [128, 8]); GB = sb('GB', [128, 4, 128]); totrow = sb('totrow', [128, 8])
        Lts = sb('Lts', [128, 4, 128]); Lst = sb('Lst', [128, 4, 128]); LstS = sb('LstS', [128, 4, 128])
        Am = sb('Am', [128, 4, 128]); AT = sb('AT', [128, 4, 128]); Qm = sb('Qm', [128, 4, 128]); QT = sb('QT', [128, 4, 128])
        Rm = sb('Rm', [128, 4, 128]); Rb = sb('Rb', [128, 4, 128], BF16)
        kbg = sb('kbg', [128, 4, 128], BF16); kd = sb('kd', [128, 4, 128], BF16); bv = sb('bv', [128, 4, 128], BF16)
        u = sb('u', [128, 4, 128]); wT = sb('wT', [128, 4, 128], BF16)
        S = sb('S1', [128, 4, 128]); Sb = [sb('Sb%d' % i, [128, 4, 128], BF16) for i in range(2)]
        vn = sb('vn', [128, 4, 128], BF16); Pst = sb('Pst', [128, 4, 128], BF16); egr = sb('egr', [128, 4, 128]); qg = sb('qg', [128, 4, 128], BF16)
        Oc = sb('Oc1', [128, DM]); ofl = sb('ofl1', [128, 512]); og = sb('og1', [128, 512]); yb = sb('yb1', [128, DM], BF16)
        yT = sb('yT1', [128, 8, 128], BF16)
        q8 = sb('q8', [128, 512]); q8b = sb('q8b', [128, 512], BF16); qT8 = sb('qT8', [128, 4, 128], BF16)
        kTl2 = [sb('kTl%d' % i, [128, 384]) for i in range(2)]; kTlb2 = [sb('kTlb%d' % i, [128, 384], BF16) for i in range(2)]; vl = sb('vl', [128, 3, 128]); vlb = sb('vlb', [128, 3, 128], BF16)
        kcT = sb('kcT', [128, max(TC, 128)]); kcTb2 = [sb('kcTb%d' % i, [128, max(TC, 128)], BF16) for i in range(2)]
        vc = sb('vc', [128, max(NCB, 1), 128]); vcb = sb('vcb', [128, max(NCB, 1), 128], BF16)
        NK = TC + 384
        ssb = sb('ssb', [128, NK]); pbf = sb('pbf', [128, NK], BF16); pT = sb('pT', [128, NK // 128, 128], BF16)
        v4 = lambda ap: ap.rearrange("p (h t) -> p h t", h=4)
        snap_ctr = [0]

        def scan_block(r0, is_ctx, d, first, bidx):
            v = 1 if is_ctx else 0
            x_t, tx = norm_hT(r0, v, bidx)
            c16 = 1536
            P.op('pe', mmgroup([(ps[2][:, 0:16], tokmm(c16, 16))]), reads=rh, writes=[tp[2]])
            P.op('act', lambda e: e.activation(out=small[:, 8:24], in_=ps[2][:, 0:16], func=AF.Identity), reads=[tp[2]], writes=[tk['small']])
            cb = 8 + d * 4; ca = 16 + d * 4; pA = d * 8; pB = d * 8 + 4
            sm = [tk['small']]
            P.op('act', lambda e: e.activation(out=small[:, 24:28], in_=small[:, cb:cb + 4], func=AF.Sigmoid), reads=sm, writes=sm)
            P.op('dve', lambda e: e.tensor_tensor(out=small[:, 28:32], in0=small[:, ca:ca + 4], in1=pvec[:, pB:pB + 4], op=ALU.add), reads=sm + c_, writes=sm)
            P.op('act', lambda e: e.activation(out=small[:, 28:32], in_=small[:, 28:32], func=AF.Exp), reads=sm, writes=sm)
            P.op('act', lambda e: e.activation(out=small[:, 28:32], in_=small[:, 28:32], func=AF.Ln, bias=onec[:, 0:1], scale=1.0), reads=sm, writes=sm)
            P.op('dve', lambda e: e.tensor_tensor(out=small[:, 28:32], in0=small[:, 28:32], in1=pvec[:, pA:pA + 4], op=ALU.mult), reads=sm + c_, writes=sm)
            tri = maskf if d == 0 else maskr
            P.op('pe', mmgroup([(ps[2][:, 32:36], [(tri[:], small[:, 28:32])]), (ps[2][:, 36:40], [(same[:], small[:, 28:32])])]),
                 reads=sm + c_, writes=[tp[2]])
            P.op('dve', lambda e: e.tensor_copy(out=small[:, 32:40], in_=ps[2][:, 32:40]), reads=[tp[2]], writes=sm)
            P.op('act', lambda e: e.activation(out=small[:, 40:44], in_=small[:, 32:36], func=AF.Exp), reads=sm, writes=sm)
            P.op('dve', lambda e: e.tensor_tensor(out=small[:, 40:44], in0=small[:, 40:44], in1=small[:, 24:28], op=ALU.mult), reads=sm, writes=sm)
            P.op('dve', lambda e: e.tensor_tensor(out=small[:, 44:48], in0=small[:, 36:40], in1=small[:, 32:36], op=ALU.subtract), reads=sm, writes=sm)
            P.op('act', lambda e: e.activation(out=small[:, 44:48], in_=small[:, 44:48], func=AF.Exp), reads=sm, writes=sm)
            P.op('dve', lambda e: e.tensor_tensor(out=GB[:], in0=self.ones[:].unsqueeze(1).to_broadcast([128, 4, 128]),
                                                  in1=small[:, 28:32].unsqueeze(2).to_broadcast([128, 4, 128]), op=ALU.mult),
                 reads=sm + [self.t_const], writes=[tk['GB']])
            P.op('dve', lambda e: e.tensor_tensor(out=gs[:].rearrange("p (h c) -> p h c", c=2), in0=small[:, 28:32].unsqueeze(2).to_broadcast([128, 4, 2]),
                                                  in1=csel[:].unsqueeze(1).to_broadcast([128, 4, 2]), op=ALU.mult), reads=sm + c_, writes=[tk['gs']])
            P.op('pe', mmgroup([(ps[3][:, h * 128:(h + 1) * 128], [(GB[:, h, :], tri[:])]) for h in range(4)]), reads=[tk['GB']] + c_, writes=[tp[3]])
            P.op('pe', mmgroup([(ps[2][:, 48:56], [(self.ones[:], gs[:])])]), reads=[tk['gs'], self.t_const], writes=[tp[2]])
            P.op('act', lambda e: e.activation(out=totrow[:], in_=ps[2][:, 48:56], func=AF.Exp), reads=[tp[2]], writes=[tk['gs']])
            gamrow = v4(ps[3][:])
            gcol = small[:, 32:36].unsqueeze(2).to_broadcast([128, 4, 128])
            mts, mst = (maskr, maskf) if d == 0 else (maskf, maskr)
            mtsS, mstS = (maskrs, maskfs) if d == 0 else (maskfs, maskrs)
            P.op('dve', lambda e: e.tensor_tensor(out=Lts[:], in0=gamrow, in1=gcol, op=ALU.subtract), reads=[tp[3]] + sm, writes=[tk['Lts']])
            P.op('dve', lambda e: e.tensor_scalar(out=Lst[:], in0=Lts[:], scalar1=0.0, scalar2=None, op0=ALU.min), reads=[tk['Lts']], writes=[tk['Lst']])
            P.op('dve', lambda e: e.tensor_scalar(out=Lts[:], in0=Lts[:], scalar1=0.0, scalar2=None, op0=ALU.max), reads=[tk['Lst']], writes=[tk['Lts']])
            P.op('act', lambda e: e.activation(out=egr[:], in_=gamrow, func=AF.Exp), reads=[tp[3], tk['Lts']], writes=[tk['egr']])
            P.op('act', lambda e: e.activation(out=Lts[:], in_=Lts[:], func=AF.Exp, scale=-1.0), writes=[tk['Lts']])
            P.op('act', lambda e: e.activation(out=Lst[:], in_=Lst[:], func=AF.Exp), writes=[tk['Lst']])
            P.op('dve', lambda e: e.tensor_tensor(out=Lts[:], in0=Lts[:], in1=mtsS[:].unsqueeze(1).to_broadcast([128, 4, 128]), op=ALU.mult),
                 reads=c_, writes=[tk['Lts']])
            P.op('pool', lambda e: e.tensor_tensor(out=LstS[:], in0=Lst[:], in1=mst[:].unsqueeze(1).to_broadcast([128, 4, 128]), op=ALU.mult),
                 reads=[tk['Lst']] + c_, writes=[tk['A']])
            cr = cxrow(r0)
            for j in range(5):
                cl = cxl[j % 2]; tcl = tk['cxl%d' % (j % 2)]
                P.op('sp', lambda e, j=j, cl=cl: e.dma_start(out=cl[:], in_=D['CX'][cr + j - 2:cr + j - 2 + 128, :]), writes=[tcl], dma=True)
                if j == 0:
                    P.op('dve', lambda e, cl=cl: e.tensor_tensor(out=qkv[:], in0=cl[:], in1=convw[:, 0, :], op=ALU.mult), reads=[tcl] + c_, writes=[tk['qkv']])
                else:
                    P.op('dve' if j == 3 else 'pool', lambda e, j=j, cl=cl: e.tensor_tensor(out=cl[:], in0=cl[:], in1=convw[:, j, :], op=ALU.mult), reads=c_, writes=[tcl])
                    P.op('dve', lambda e, cl=cl: e.tensor_tensor(out=qkv[:], in0=qkv[:], in1=cl[:], op=ALU.add), reads=[tcl], writes=[tk['qkv']])
            P.op('act', lambda e: e.activation(out=qkv[:], in_=qkv[:], func=AF.Silu), writes=[tk['qkv']])
            P.op('pool', lambda e: e.tensor_tensor(out=Oc[:], in0=qkv[:, 0:1024], in1=qkv[:, 0:1024], op=ALU.mult), reads=[tk['qkv']], writes=[tk['Oc']])
            P.op('dve', lambda e: e.tensor_reduce(out=small[:, 48:56], in_=Oc[:].rearrange("p (h v) -> p h v", h=8), axis=AX.X, op=ALU.add),
                 reads=[tk['Oc']], writes=sm)
            P.op('act', lambda e: e.activation(out=small[:, 48:56], in_=small[:, 48:56], func=AF.Sqrt, bias=self.epsc[:, 0:1], scale=1.0), reads=sm, writes=sm)
            P.op('dve', lambda e: e.reciprocal(out=small[:, 56:64], in_=small[:, 48:56]), reads=sm, writes=sm)
            P.op('dve', lambda e: e.tensor_scalar(out=small[:, 56:60], in0=small[:, 56:60], scalar1=128.0 ** -0.5, scalar2=None, op0=ALU.mult), reads=sm, writes=sm)
            P.op('dve', lambda e: e.tensor_tensor(out=qkb[:], in0=qkv[:, 0:1024].rearrange("p (h v) -> p h v", h=8),
                                                  in1=small[:, 56:64].unsqueeze(2).to_broadcast([128, 8, 128]), op=ALU.mult),
                 reads=[tk['qkv']] + sm, writes=[tk['qkb']])
            for half in range(2):
                P.op('pe', trs([(psb[4 + half][:, j * 128:(j + 1) * 128], qkb[:, half * 4 + j, :]) for j in range(4)], self.identb[:]),
                     reads=[tk['qkb'], self.t_const], writes=[tp[4 + half]])
                P.op('act', lambda e, half=half: e.activation(out=fT[:, half * 4:(half + 1) * 4, :].rearrange("p h t -> p (h t)"),
                                                              in_=psb[4 + half][:, 0:512], func=AF.Identity), reads=[tp[4 + half]], writes=[tk['fT']])
            kn = qkb[:, 4:8, :]
            P.op('dve', lambda e: e.tensor_tensor(out=kbg[:], in0=kn, in1=small[:, 40:44].unsqueeze(2).to_broadcast([128, 4, 128]), op=ALU.mult),
                 reads=[tk['qkb']] + sm, writes=[tk['kbg']])
            P.op('pool', lambda e: e.tensor_tensor(out=kd[:], in0=kn, in1=small[:, 44:48].unsqueeze(2).to_broadcast([128, 4, 128]), op=ALU.mult),
                 reads=[tk['qkb']] + sm, writes=[tk['kd']])
            P.op('dve', lambda e: e.tensor_tensor(out=bv[:], in0=qkv[:, 1024:1536].rearrange("p (h v) -> p h v", h=4),
                                                  in1=small[:, 24:28].unsqueeze(2).to_broadcast([128, 4, 128]), op=ALU.mult),
                 reads=[tk['qkv']] + sm, writes=[tk['bv']])
            P.op('dve', lambda e: e.tensor_tensor(out=qg[:], in0=fT[:, 0:4, :], in1=egr[:], op=ALU.mult), reads=[tk['fT'], tk['egr']], writes=[tk['qg']])
            P.op('pe', mmgroup([(ps[6][:, h * 128:(h + 1) * 128], [(fT[:, 4 + h, :], fT[:, 4 + h, :])]) for h in range(4)]), reads=[tk['fT']], writes=[tp[6]])
            P.op('pe', mmgroup([(ps[7][:, h * 128:(h + 1) * 128], [(fT[:, 4 + h, :], fT[:, h, :])]) for h in range(4)]), reads=[tk['fT']], writes=[tp[7]])
            P.op('dve', lambda e: e.tensor_tensor(out=Am[:], in0=v4(ps[6][:]), in1=Lts[:], op=ALU.mult), reads=[tp[6], tk['Lts']], writes=[tk['A']])
            P.op('dve', lambda e: e.tensor_tensor(out=Am[:], in0=Am[:], in1=small[:, 24:28].unsqueeze(2).to_broadcast([128, 4, 128]), op=ALU.mult),
                 reads=sm, writes=[tk['A']])
            P.op('dve', lambda e: e.tensor_tensor(out=Pst[:], in0=v4(ps[7][:]), in1=LstS[:], op=ALU.mult), reads=[tp[7], tk['A']], writes=[tk['Pst']])
            P.op('pe', trs([(ps[6][:, h * 128:(h + 1) * 128], Am[:, h, :]) for h in range(4)], self.ident[:]), reads=[tk['A'], self.t_const], writes=[tp[6]])
            P.op('act', lambda e: e.activation(out=AT[:], in_=v4(ps[6][:]), func=AF.Identity), reads=[tp[6]], writes=[tk['AT']])
            P.op('dve', lambda e: e.scalar_tensor_tensor(out=Rm[:], in0=AT[:], scalar=-1.0, in1=self.ident[:].unsqueeze(1).to_broadcast([128, 4, 128]),
                                                         op0=ALU.mult, op1=ALU.add), reads=[tk['AT'], self.t_const], writes=[tk['Rm']])
            curQ, curQT = AT, Am
            tQ, tQT = tk['AT'], tk['A']
            for step in range(5):
                last = step == 4
                if not last:
                    P.op('pe', mmgroup([(ps[6][:, h * 128:(h + 1) * 128], [(curQT[:, h, :], curQ[:, h, :])]) for h in range(4)]), reads=[tQ, tQT], writes=[tp[6]])
                P.op('pe', mmgroup([(ps[7][:, h * 128:(h + 1) * 128], [(curQ[:, h, :], curQT[:, h, :])]) for h in range(4)]), reads=[tQ, tQT], writes=[tp[7]])
                if not last:
                    P.op('act', lambda e: e.activation(out=Qm[:], in_=v4(ps[6][:]), func=AF.Identity), reads=[tp[6]], writes=[tk['Q']])
                P.op('act', lambda e: e.activation(out=QT[:], in_=v4(ps[7][:]), func=AF.Identity), reads=[tp[7]], writes=[tk['QT']])
                curQ, curQT, tQ, tQT = Qm, QT, tk['Q'], tk['QT']
                P.op('pe', mmgroup([(ps[3][:, h * 128:(h + 1) * 128], [(QT[:, h, :], Rm[:, h, :])]) for h in range(4)]), reads=[tk['QT'], tk['Rm']], writes=[tp[3]])
                P.op('dve', lambda e: e.tensor_tensor(out=Rm[:], in0=v4(ps[3][:]), in1=Rm[:], op=ALU.add), reads=[tp[3]], writes=[tk['Rm']])
            P.op('act', lambda e: e.activation(out=Rb[:], in_=Rm[:], func=AF.Identity), reads=[tk['Rm']], writes=[tk['Rb']])
            P.op('pe', mmgroup([(ps[6][:, h * 128:(h + 1) * 128], [(Rb[:, h, :], bv[:, h, :])]) for h in range(4)]), reads=[tk['Rb'], tk['bv']], writes=[tp[6]])
            P.op('pe', mmgroup([(ps[7][:, h * 128:(h + 1) * 128], [(kbg[:, h, :], Rb[:, h, :])]) for h in range(4)]), reads=[tk['Rb'], tk['kbg']], writes=[tp[7]])
            P.op('act', lambda e: e.activation(out=u[:], in_=v4(ps[6][:]), func=AF.Identity), reads=[tp[6]], writes=[tk['u']])
            P.op('act', lambda e: e.activation(out=wT[:], in_=v4(ps[7][:]), func=AF.Identity), reads=[tp[7]], writes=[tk['wT']])
            if first:
                P.op('pool', lambda e: e.memset(S[:], 0.0), writes=[tk['S']])
                P.op('pool', lambda e: e.memset(Sb[snap_ctr[0] % 2][:], 0.0), writes=[tk['Sb%d' % (snap_ctr[0] % 2)]])
            order = (0, 1) if d == 0 else (1, 0)
            for c in order:
                cs = slice(c * 64, c * 64 + 64)
                si = snap_ctr[0] % 2; snap_ctr[0] += 1; sn = (si + 1) % 2
                Sbi = Sb[si]; tSb = tk['Sb%d' % si]
                P.op('pe', mmgroup([(ps[6][cs, h * 128:(h + 1) * 128], [(wT[:, h, cs], Sbi[:, h, :])]) for h in range(4)]), reads=[tk['wT'], tSb], writes=[tp[6]])
                P.op('dve', lambda e, cs=cs: e.tensor_tensor(out=vn[cs], in0=u[cs], in1=v4(ps[6][cs, :]), op=ALU.subtract),
                     reads=[tp[6], tk['u']], writes=[tk['vn']])
                def omm(e, cs=cs, Sbi=Sbi):
                    for h in range(4):
                        e.matmul(ps[5][cs, h * 128:(h + 1) * 128], qg[:, h, cs], Sbi[:, h, :], start=True, stop=False)
                        ins = e.matmul(ps[5][cs, h * 128:(h + 1) * 128], Pst[cs, h, cs], vn[cs, h, :], start=False, stop=True)
                    return ins
                P.op('pe', omm, reads=[tk['qg'], tSb, tk['Pst'], tk['vn']], writes=[tp[5]])
                P.op('pe', mmgroup([(ps[7][:, h * 128:(h + 1) * 128], [(kd[cs, h, :], vn[cs, h, :])]) for h in range(4)]), reads=[tk['kd'], tk['vn']], writes=[tp[7]])
                P.op('dve', lambda e, c=c: e.tensor_tensor(out=S[:], in0=S[:], in1=totrow[:].rearrange("p (h c) -> p h c", c=2)[:, :, c:c + 1].to_broadcast([128, 4, 128]),
                                                           op=ALU.mult), reads=[tk['gs']], writes=[tk['S']])
                P.op('dve', lambda e: e.tensor_tensor(out=S[:], in0=S[:], in1=v4(ps[7][:]), op=ALU.add), reads=[tp[7]], writes=[tk['S']])
                P.op('act', lambda e, sn=sn: e.activation(out=Sb[sn][:], in_=S[:], func=AF.Identity), reads=[tk['S']], writes=[tk['Sb%d' % sn]])
            if d == 0:
                P.op('act', lambda e: e.activation(out=ofl[:], in_=ps[5][:], func=AF.Identity), reads=[tp[5]], writes=[tk['ofl']])
                P.op('act', lambda e: e.dma_start(out=D['OF'][r0:r0 + 128, 0:512], in_=ofl[:]), reads=[tk['ofl']], writes=[tk['out']], dma=True)
                return
            if is_ctx:
                return
            P.op('sp', lambda e: e.dma_start(out=ofl[:], in_=D['OF'][r0:r0 + 128, 0:512]), writes=[tk['ofl']], dma=True)
            P.op('dve', lambda e: e.tensor_tensor(out=Oc[:, 0:512], in0=ps[5][:], in1=ofl[:], op=ALU.add), reads=[tp[5], tk['ofl']], writes=[tk['Oc']])
            P.op('pe', mmgroup([(ps[6][:], tokmm(1552, 512))]), reads=rh, writes=[tp[6]])
            P.op('act', lambda e: e.activation(out=og[:], in_=ps[6][:], func=AF.Silu), reads=[tp[6]], writes=[tk['og']])
            P.op('pool', lambda e: e.tensor_tensor(out=ofl[:], in0=Oc[:, 0:512], in1=Oc[:, 0:512], op=ALU.mult), reads=[tk['Oc']], writes=[tk['ofl']])
            P.op('dve', lambda e: e.tensor_reduce(out=small[:, 48:52], in_=ofl[:].rearrange("p (h v) -> p h v", h=4), axis=AX.X, op=ALU.add),
                 reads=[tk['ofl']], writes=sm)
            P.op('act', lambda e: e.activation(out=small[:, 48:52], in_=small[:, 48:52], func=AF.Sqrt, scale=1.0 / 128, bias=self.epsc[:, 0:1]), reads=sm, writes=sm)
            P.op('dve', lambda e: e.reciprocal(out=small[:, 52:56], in_=small[:, 48:52]), reads=sm, writes=sm)
            O4 = Oc[:, 0:512].rearrange("p (h v) -> p h v", h=4)
            P.op('dve', lambda e: e.tensor_tensor(out=O4, in0=O4, in1=small[:, 52:56].unsqueeze(2).to_broadcast([128, 4, 128]), op=ALU.mult), reads=sm, writes=[tk['Oc']])
            P.op('pool', lambda e: e.tensor_tensor(out=Oc[:, 0:512], in0=Oc[:, 0:512], in1=dnw[:], op=ALU.mult), reads=c_, writes=[tk['Oc']])
            P.op('dve', lambda e: e.tensor_tensor(out=yb[:, 0:512], in0=Oc[:, 0:512], in1=og[:], op=ALU.mult), reads=[tk['Oc'], tk['og']], writes=[tk['yb']])
            self.l1_attention(self._l1, r0)
            P.op('pe', trs([(psb[4][:, k * 128:(k + 1) * 128], yb[:, k * 128:(k + 1) * 128]) for k in range(8)], self.identb[:]),
                 reads=[tk['yb'], self.t_const], writes=[tp[4]])
            P.op('act', lambda e: e.activation(out=yT[:].rearrange("p k t -> p (k t)"), in_=psb[4][:, 0:1024], func=AF.Identity), reads=[tp[4]], writes=[tk['yT']])
            for half in range(2):
                P.op('pe', mmgroup([(ps[6 + half][:], [(yT[:, k, :], wout[:, k, half * 512:(half + 1) * 512]) for k in range(8)])]),
                     reads=[tk['yT'], tk['c']], writes=[tp[6 + half]])
                P.op('dve', lambda e, half=half: e.tensor_tensor(out=Oc[:, half * 512:(half + 1) * 512], in0=ps[6 + half][:],
                                                                 in1=self.gbc[0][:, half * 512:(half + 1) * 512], op=ALU.mult),
                     reads=[tp[6 + half], self.t_gbc], writes=[tk['Oc']])
            P.op('pool', lambda e: e.tensor_tensor(out=x_t[:], in0=x_t[:], in1=Oc[:], op=ALU.add), reads=[tk['Oc']], writes=[tx])
            P.op('act', lambda e: e.dma_start(out=D[dst][r0:r0 + 128, :], in_=x_t[:]), reads=[tx], writes=[tk['out']], dma=True)

        self._l1 = locals()
        if TC:
            for s_, nm in enumerate(('KT', 'KT2')):
                P.op('sp', lambda e, nm=nm: e.dma_start(out=kcT[:, 0:TC], in_=D[nm][:, 0:TC]), writes=[tk['kcT']], dma=True)
                P.op('dve', lambda e, s_=s_: e.tensor_copy(out=kcTb2[s_][:, 0:TC], in_=kcT[:, 0:TC]), reads=[tk['kcT']], writes=[tk['kcT']])
            P.op('sp', lambda e: e.dma_start(out=vc[:], in_=D['VV'][0:TC, :].rearrange("(b p) n -> p b n", p=128)), writes=[tk['vc']], dma=True)
            P.op('dve', lambda e: e.tensor_copy(out=vcb[:], in_=vc[:]), reads=[tk['vc']], writes=[tk['vc']])
        for d in range(2):
            seq = seq_all
            if d == 1:
                seq = [(i * 128, True) for i in reversed(range(NCB))] + [(TC + i * 128, False) for i in reversed(range(NB))]
            for n, (r0, is_ctx) in enumerate(seq):
                scan_block(r0, is_ctx, d, n == 0, n)
                if d == 0:
                    self.issue_cast(1, n)
            P.barrier()
        P.emit()


def apply_rope(self, x3, nh, rope, rt, t_x, t_rope, t_rt):
    P = self.P
    cosb = rope[:, 0:32].unsqueeze(1).to_broadcast([128, nh, 32]); sinb = rope[:, 32:64].unsqueeze(1).to_broadcast([128, nh, 32])
    x1 = x3[:, :, 0:32]; x2 = x3[:, :, 32:64]
    r = rt[:, 0:nh, :]
    rd = list(t_x) + [t_rope]
    P.op('dve', lambda e: e.tensor_tensor(out=r[:, :, 0:32], in0=x2, in1=sinb, op=ALU.mult), reads=rd, writes=[t_rt])
    P.op('dve', lambda e: e.tensor_tensor(out=r[:, :, 32:64], in0=x1, in1=sinb, op=ALU.mult), reads=rd, writes=[t_rt])
    P.op('dve', lambda e: e.tensor_tensor(out=x1, in0=x1, in1=cosb, op=ALU.mult), reads=[t_rope], writes=list(t_x))
    P.op('dve', lambda e: e.tensor_tensor(out=x2, in0=x2, in1=cosb, op=ALU.mult), reads=[t_rope], writes=list(t_x))
    P.op('dve', lambda e: e.tensor_tensor(out=x1, in0=x1, in1=r[:, :, 0:32], op=ALU.subtract), reads=[t_rt], writes=list(t_x))
    P.op('dve', lambda e: e.tensor_tensor(out=x2, in0=x2, in1=r[:, :, 32:64], op=ALU.add), reads=[t_rt], writes=list(t_x))


Builder.phase_l1mix = phase_l1mix
Builder.apply_rope = apply_rope


def l1_attention(self, L, r0):
    P, D = self.P, self.D
    TC, NB, NCB = self.TC, self.NB, self.NCB
    ps, psb, tp, tk = L['ps'], L['psb'], L['tp'], L['tk']
    mmgroup, trs, tokmm, rh = L['mmgroup'], L['trs'], L['tokmm'], L['rh']
    q8, q8b, qT8, small, yb = L['q8'], L['q8b'], L['qT8'], L['small'], L['yb']
    kTl, kTlb, vl, vlb, kcTb, vcb = L['kTl2'], L['kTlb2'], L['vl'], L['vlb'], L['kcTb2'], L['vcb']
    ssb, pbf, pT, rope, rt, sinks, mbp, mbn = L['ssb'], L['pbf'], L['pT'], L['rope'], L['rt'], L['sinks'], L['mbp'], L['mbn']
    blk = (r0 - TC) // 128
    has_prev, has_next = blk > 0, blk < NB - 1
    NK = TC + 384
    sm = [tk['small']]
    P.op('pe', mmgroup([(ps[0][:], tokmm(2064, 512))]), reads=rh, writes=[tp[0]])
    P.op('act', lambda e: e.activation(out=q8[:], in_=ps[0][:], func=AF.Identity), reads=[tp[0]], writes=[tk['q8']])
    t0 = r0 - TC
    P.op('sp', lambda e: e.dma_start(out=rope[:], in_=D['rope'][t0:t0 + 128, :]), writes=[tk['rope']], dma=True)
    self.apply_rope(q8[:].rearrange("p (h f) -> p h f", h=8), 8, rope, rt, [tk['q8']], tk['rope'], tk['rt'])
    P.op('dve', lambda e: e.tensor_scalar(out=q8b[:], in0=q8[:], scalar1=0.125, scalar2=None, op0=ALU.mult), reads=[tk['q8']], writes=[tk['q8']])
    P.op('pe', trs([(psb[1][:, j * 128:(j + 1) * 128], q8b[:, j * 128:(j + 1) * 128]) for j in range(4)], self.identb[:]),
         reads=[tk['q8'], self.t_const], writes=[tp[1]])
    P.op('act', lambda e: e.activation(out=qT8[:].rearrange("p j t -> p (j t)"), in_=psb[1][:, 0:512], func=AF.Identity), reads=[tp[1]], writes=[tk['qT8']])
    c0 = r0 - 128 if has_prev else r0
    c1 = r0 + 256 if has_next else r0 + 128
    o0 = 0 if has_prev else 128
    for s_, nm in enumerate(('KT', 'KT2')):
        P.op('sp', lambda e, s_=s_, nm=nm: e.dma_start(out=kTl[s_][:, o0:o0 + (c1 - c0)], in_=D[nm][:, c0:c1]), writes=[tk['kTl']], dma=True)
        P.op('pool', lambda e, s_=s_: e.tensor_copy(out=kTlb[s_][:, o0:o0 + (c1 - c0)], in_=kTl[s_][:, o0:o0 + (c1 - c0)]), reads=[tk['kTl']], writes=[tk['kTl']])
    nbk = (c1 - c0) // 128
    b0 = o0 // 128
    P.op('sp', lambda e: e.dma_start(out=vl[:, b0:b0 + nbk, :], in_=D['VV'][c0:c1, :].rearrange("(b p) n -> p b n", p=128)), writes=[tk['vl']], dma=True)
    P.op('pool', lambda e: e.tensor_copy(out=vlb[:, b0:b0 + nbk, :], in_=vl[:, b0:b0 + nbk, :]), reads=[tk['vl']], writes=[tk['vl']])
    for h in range(8):
        pbse = (h % 2) * 64; g = h // 4
        s_ = 0 if g == (h % 2) else 1
        rs = slice(pbse, pbse + 64)
        bA, bB = 2 + 2 * (h % 2), 3 + 2 * (h % 2)
        ql = qT8[rs, h // 2, :]
        outsA = []
        if TC:
            outsA.append((ps[bA][:, 0:TC], [(ql, kcTb[s_][rs, 0:TC])]))
        if has_prev:
            outsA.append((ps[bA][:, TC:TC + 128], [(ql, kTlb[s_][rs, 0:128])]))
        outsA.append((ps[bA][:, TC + 128:TC + 256], [(ql, kTlb[s_][rs, 128:256])]))
        P.op('pe', mmgroup(outsA), reads=[tk['qT8'], tk['kTl'], tk['kcT']], writes=[tp[bA]])
        if has_next:
            P.op('pe', mmgroup([(ps[bB][:, 0:128], [(ql, kTlb[s_][rs, 256:384])])]), reads=[tk['qT8'], tk['kTl']], writes=[tp[bB]])
        w_ = [tk['ssb']]
        if TC:
            P.op('act', lambda e, bA=bA: e.activation(out=ssb[:, 0:TC], in_=ps[bA][:, 0:TC], func=AF.Identity), reads=[tp[bA]], writes=w_)
        if has_prev:
            P.op('dve', lambda e, bA=bA: e.tensor_tensor(out=ssb[:, TC:TC + 128], in0=ps[bA][:, TC:TC + 128], in1=mbp[:], op=ALU.add), reads=[tp[bA], tk['c']], writes=w_)
        else:
            P.op('pool', lambda e: e.memset(ssb[:, TC:TC + 128], NEG), writes=w_)
        P.op('act', lambda e, bA=bA: e.activation(out=ssb[:, TC + 128:TC + 256], in_=ps[bA][:, TC + 128:TC + 256], func=AF.Identity), reads=[tp[bA]], writes=w_)
        if has_next:
            P.op('dve', lambda e, bB=bB: e.tensor_tensor(out=ssb[:, TC + 256:TC + 384], in0=ps[bB][:, 0:128], in1=mbn[:], op=ALU.add), reads=[tp[bB], tk['c']], writes=w_)
        else:
            P.op('pool', lambda e: e.memset(ssb[:, TC + 256:TC + 384], NEG), writes=w_)
        P.op('dve', lambda e: e.tensor_reduce(out=small[:, 16:17], in_=ssb[:], axis=AX.X, op=ALU.max), reads=w_, writes=sm)
        P.op('dve', lambda e, h=h: e.tensor_tensor(out=small[:, 16:17], in0=small[:, 16:17], in1=sinks[:, h:h + 1], op=ALU.max), reads=sm + [tk['c']], writes=sm)
        P.op('dve', lambda e: e.tensor_scalar(out=small[:, 17:18], in0=small[:, 16:17], scalar1=-1.0, scalar2=None, op0=ALU.mult), reads=sm, writes=sm)
        P.op('act', lambda e: e.activation(out=pbf[:], in_=ssb[:], func=AF.Exp, bias=small[:, 17:18], scale=1.0, accum_out=small[:, 18:19]),
             reads=w_ + sm, writes=[tk['pb']] + sm)
        P.op('act', lambda e, h=h: e.activation(out=small[:, 19:20], in_=sinks[:, h:h + 1], func=AF.Exp, bias=small[:, 17:18], scale=1.0), reads=sm, writes=sm)
        P.op('dve', lambda e: e.tensor_tensor(out=small[:, 19:20], in0=small[:, 19:20], in1=small[:, 18:19], op=ALU.add), reads=sm, writes=sm)
        P.op('dve', lambda e, h=h: e.reciprocal(out=small[:, 8 + h:9 + h], in_=small[:, 19:20]), reads=sm, writes=sm)
        nkb = NK // 128
        P.op('pe', trs([(psb[6][:, kb * 128:(kb + 1) * 128], pbf[:, kb * 128:(kb + 1) * 128]) for kb in range(nkb)], self.identb[:]),
             reads=[tk['pb'], self.t_const], writes=[tp[6]])
        P.op('act', lambda e: e.activation(out=pT[:].rearrange("p k t -> p (k t)"), in_=psb[6][:, 0:NK], func=AF.Identity), reads=[tp[6]], writes=[tk['pT']])
        pairs = [(pT[:, b, :], vcb[:, b, g * 64:(g + 1) * 64]) for b in range(NCB)]
        for j in range(3):
            if (j == 0 and not has_prev) or (j == 2 and not has_next):
                continue
            pairs.append((pT[:, NCB + j, :], vlb[:, j, g * 64:(g + 1) * 64]))
        P.op('pe', mmgroup([(ps[7][:, h * 64:(h + 1) * 64], pairs)]), reads=[tk['pT'], tk['vl'], tk['vc']], writes=[tp[7]])
    P.op('dve', lambda e: e.tensor_tensor(out=yb[:, 512:1024].rearrange("p (h f) -> p h f", h=8), in0=ps[7][:].rearrange("p (h f) -> p h f", h=8),
                                          in1=small[:, 8:16].unsqueeze(2).to_broadcast([128, 8, 64]), op=ALU.mult), reads=[tp[7]] + sm, writes=[tk['yb']])


Builder.l1_attention = l1_attention
```
